# Optimizing a Trainium2 kernel written in Bass

```python
import math
import jax
import jax.numpy as jnp
from jax import lax
import numpy as np

D_MODEL = 1024
BATCH = 8
SEQ = 4096
DEPTH = 2

GRID_W = 64
CTX_LEN = 256
HEAD_DIM = 64
EPS = 1e-6
ATTN_HEADS = 8
ATTN_KV_HEADS = 2
ATTN_GROUP = ATTN_HEADS // ATTN_KV_HEADS
ATTN_WIDTH = ATTN_HEADS * HEAD_DIM
ATTN_KV_WIDTH = ATTN_KV_HEADS * HEAD_DIM
Q_BLOCK = 128
ROPE_THETA = 10000.0
HYENA_WIDTH = 256
HYENA_ORDER = 2
HYENA_SHORT = 3
HYENA_BANDS = 16
HYENA_POS_DIM = 1 + 2 * HYENA_BANDS
HYENA_FILTER_HIDDEN = 64
HYENA_DECAY_TARGET = 1e-2
HYENA_FAST_DECAY = 0.3
HYENA_SLOW_DECAY = 1.5
RWKV_HEADS = 4
RWKV_WIDTH = RWKV_HEADS * HEAD_DIM
RWKV_DECAY_RANK = 64
RWKV_ICLR_RANK = 64
RWKV_GATE_RANK = 128
RWKV_GN_EPS = 64e-5
RWKV_SPLITS = (RWKV_WIDTH, RWKV_WIDTH, RWKV_WIDTH, 2 * RWKV_DECAY_RANK, 2 * RWKV_ICLR_RANK, RWKV_GATE_RANK)
RWKV_PROJ = 3 * RWKV_WIDTH + 2 * RWKV_DECAY_RANK + 2 * RWKV_ICLR_RANK + RWKV_GATE_RANK
LRU_WIDTH = 256
LRU_BLOCKS = 4
LRU_BLOCK = LRU_WIDTH // LRU_BLOCKS
LRU_CONV = 4
LRU_C = 8.0
N_BRANCH = 4
IN_SPLITS = (ATTN_WIDTH, ATTN_KV_WIDTH, ATTN_KV_WIDTH, 3 * HYENA_WIDTH, RWKV_PROJ, 2 * LRU_WIDTH, N_BRANCH * D_MODEL)
N_IN = ATTN_WIDTH + 2 * ATTN_KV_WIDTH + 3 * HYENA_WIDTH + RWKV_PROJ + 2 * LRU_WIDTH + N_BRANCH * D_MODEL
N_GROUPS = 4
EXPERTS_PER_GROUP = 4
N_EXPERTS = N_GROUPS * EXPERTS_PER_GROUP
TOP_K_INNER = 2
EXPERT_HIDDEN = 512

kernel_name = 'hybrid_gated_mixers_hmoe_dit'


def _rms(x, eps=EPS):
    xf = x.astype(jnp.float32)
    return (xf * lax.rsqrt(jnp.mean(xf * xf, -1, keepdims=True) + eps)).astype(x.dtype)


def _modulate(x, shift, scale):
    return _rms(x) * (1 + scale) + shift


def _split_cols(p, sizes):
    idx = np.cumsum(sizes)[:-1].tolist()
    return jnp.split(p, idx, axis=-1)


def _dwconv(x, w, b, pad_left):
    k = w.shape[0]
    y = lax.conv_general_dilated(x, w[:, None, :].astype(x.dtype), window_strides=(1,),
                                 padding=[(pad_left, k - 1 - pad_left)],
                                 dimension_numbers=('NWC', 'WIO', 'NWC'),
                                 feature_group_count=x.shape[-1])
    return y + b


def _axial_rope_tables(n_tokens):
    rows = n_tokens // GRID_W
    row = jnp.repeat(jnp.arange(rows), GRID_W)
    col = jnp.tile(jnp.arange(GRID_W), rows)
    n_freq = HEAD_DIM // 4
    freqs = ROPE_THETA ** (-jnp.arange(n_freq, dtype=jnp.float32) / n_freq)
    pos = jnp.stack([row, col], -1).astype(jnp.float32)
    ang = pos[..., None] * freqs
    return jnp.cos(ang), jnp.sin(ang)


def _apply_rope(x, cos, sin):
    shp = x.shape
    xf = x.astype(jnp.float32).reshape(shp[0], shp[1], -1, 2, 2, HEAD_DIM // 4)
    x1, x2 = xf[..., 0, :], xf[..., 1, :]
    cs, sn = cos[None, :, None], sin[None, :, None]
    out = jnp.stack([x1 * cs - x2 * sn, x1 * sn + x2 * cs], axis=-2)
    return out.reshape(shp).astype(x.dtype)


def _qkv_heads(p_q, p_k, p_v, q_gain, k_gain):
    b, l, _ = p_q.shape
    q = _rms(p_q.reshape(b, l, ATTN_KV_HEADS, ATTN_GROUP, HEAD_DIM)) * q_gain
    k = _rms(p_k.reshape(b, l, ATTN_KV_HEADS, HEAD_DIM)) * k_gain
    v = p_v.reshape(b, l, ATTN_KV_HEADS, HEAD_DIM)
    return q, k, v


def _attend(q, k, v):
    s = jnp.einsum('bqhgd,bkhd->bhgqk', q, k).astype(jnp.float32) * (HEAD_DIM ** -0.5)
    p = jax.nn.softmax(s, axis=-1).astype(v.dtype)
    o = jnp.einsum('bhgqk,bkhd->bqhgd', p, v)
    return o.reshape(o.shape[0], o.shape[1], -1)


def _attention_mixer(pc, px, q_gain, k_gain, need_ctx):
    qc, kc, vc = _qkv_heads(pc[0], pc[1], pc[2], q_gain, k_gain)
    qx, kx, vx = _qkv_heads(px[0], px[1], px[2], q_gain, k_gain)
    b, l = qx.shape[:2]
    cos, sin = _axial_rope_tables(l)
    qx = _apply_rope(qx, cos, sin)
    kx = _apply_rope(kx, cos, sin)
    k_all = jnp.concatenate([kc, kx], axis=1)
    v_all = jnp.concatenate([vc, vx], axis=1)
    n_blk = l // Q_BLOCK
    qb = jnp.moveaxis(qx.reshape(b, n_blk, Q_BLOCK, ATTN_KV_HEADS, ATTN_GROUP, HEAD_DIM), 1, 0)
    yb = lax.map(lambda q_blk: _attend(q_blk, k_all, v_all), qb)
    yx = jnp.moveaxis(yb, 0, 1).reshape(b, l, ATTN_WIDTH)
    yc = _attend(qc, kc, vc) if need_ctx else None
    return yc, yx


def _hyena_filters(n, f1, fb1, f2, fb2, f3):
    t = jnp.arange(n, dtype=jnp.float32) / n
    bands = jnp.arange(1, HYENA_BANDS + 1, dtype=jnp.float32)
    ang = 2.0 * math.pi * t[:, None] * bands
    feat = jnp.concatenate([t[:, None], jnp.sin(ang), jnp.cos(ang)], axis=-1)
    h = jnp.sin(feat @ f1.astype(jnp.float32) + fb1.astype(jnp.float32))
    h = jnp.sin(h @ f2.astype(jnp.float32) + fb2.astype(jnp.float32))
    h = (h @ f3.astype(jnp.float32)).reshape(n, HYENA_ORDER, 2, HYENA_WIDTH)
    deltas = jnp.linspace(-math.log(HYENA_DECAY_TARGET) / HYENA_SLOW_DECAY,
                          -math.log(HYENA_DECAY_TARGET) / HYENA_FAST_DECAY, HYENA_WIDTH, dtype=jnp.float32)
    h = h * jnp.exp(-t[:, None] * deltas)[:, None, None, :]
    return h / jnp.sum(jnp.abs(h), axis=(0, 2), keepdims=True)


def _two_sided_longconv(z, h_fwd, h_bwd, skip):
    n = z.shape[1]
    filt2l = jnp.concatenate([h_fwd, jnp.zeros_like(h_fwd[:1]), h_bwd[:0:-1]], axis=0)
    zf = jnp.fft.rfft(z.astype(jnp.float32), n=2 * n, axis=1)
    hf = jnp.fft.rfft(filt2l, n=2 * n, axis=0)
    y = jnp.fft.irfft(zf * hf[None], n=2 * n, axis=1)[:, :n]
    return (y + z.astype(jnp.float32) * skip.astype(jnp.float32)).astype(z.dtype)


def _hyena_mixer(pc, px, conv_w, conv_b, f1, fb1, f2, fb2, f3, skip, need_ctx):
    def run(p):
        z = _dwconv(p, conv_w, conv_b, HYENA_SHORT // 2)
        v, x1, x2 = jnp.split(z, 3, axis=-1)
        h = _hyena_filters(p.shape[1], f1, fb1, f2, fb2, f3)
        y = v
        for o, gate in enumerate((x1, x2)):
            y = gate * _two_sided_longconv(y, h[:, o, 0], h[:, o, 1], skip[o])
        return y
    return (run(pc) if need_ctx else None), run(px)


def _shift_mix(p, mu):
    prev = jnp.pad(p[:, :-1], ((0, 0), (1, 0), (0, 0)))
    nxt = jnp.pad(p[:, 1:], ((0, 0), (0, 1), (0, 0)))
    return p + (prev - p) * mu[0] + (nxt - p) * mu[1]


def _rwkv_heads(t):
    return t.reshape(t.shape[:-1] + (RWKV_HEADS, HEAD_DIM))


def _rwkv_inputs(p, mu, w0, w2, a0, a2, g2, k_k, k_a):
    b, l, _ = p.shape
    r, k, v, w1, a1, g1 = _split_cols(_shift_mix(p, mu), RWKV_SPLITS)
    w1 = w1.reshape(b, l, 2, RWKV_DECAY_RANK)
    a1 = a1.reshape(b, l, 2, RWKV_ICLR_RANK)
    w = -jax.nn.softplus(-(w0 + jnp.einsum('bldr,drc->bldc', jnp.tanh(w1), w2))) - 0.5
    decay = _rwkv_heads(jnp.exp(-jnp.exp(w.astype(jnp.float32))))
    a = _rwkv_heads(jax.nn.sigmoid(a0 + jnp.einsum('bldr,drc->bldc', a1, a2)))
    g = jax.nn.sigmoid(g1) @ g2
    kk = _rwkv_heads(k * k_k).astype(jnp.float32)
    kk = kk * lax.rsqrt(jnp.sum(kk * kk, -1, keepdims=True) + 1e-12)
    k_dir = _rwkv_heads(k)[:, :, None] * (1 + (a - 1) * _rwkv_heads(k_a))
    return _rwkv_heads(r), _rwkv_heads(v), kk, g, decay, a, k_dir


def _rwkv_scan(s0, r, decay, k, v, kk, a, reverse, emit):
    def step(s, inp):
        r_t, w_t, k_t, v_t, kk_t, a_t = inp
        s = (s * w_t[:, :, None, :]
             - jnp.einsum('bhvk,bhk->bhv', s, kk_t)[..., None] * (kk_t * a_t)[:, :, None, :]
             + v_t[..., None] * k_t[:, :, None, :])
        return s, (jnp.einsum('bhvk,bhk->bhv', s, r_t) if emit else None)
    xs = tuple(jnp.moveaxis(t.astype(jnp.float32), 1, 0) for t in (r, decay, k, v, kk, a))
    s_fin, ys = lax.scan(step, s0, xs, reverse=reverse)
    return s_fin, (jnp.moveaxis(ys, 0, 1) if emit else None)


def _rwkv_readout(y, r, k_dir, v, g, r_k, ln_w, ln_b):
    b, l = y.shape[:2]
    mu = jnp.mean(y, -1, keepdims=True)
    var = jnp.mean(jnp.square(y - mu), -1, keepdims=True)
    yn = ((y - mu) * lax.rsqrt(var + RWKV_GN_EPS)).reshape(b, l, RWKV_WIDTH) * ln_w + ln_b
    bonus = jnp.sum(jnp.sum(r[:, :, None] * k_dir * r_k, -1, keepdims=True) * v[:, :, None], axis=2)
    return ((yn + bonus.reshape(b, l, RWKV_WIDTH)) * g).astype(g.dtype)


def _rwkv_mixer(pc, px, mu, w0, w2, a0, a2, g2, k_k, k_a, r_k, ln_w, ln_b, need_ctx):
    rc, vc, kkc, gc, dc, ac, kdc = _rwkv_inputs(pc, mu, w0, w2, a0, a2, g2, k_k, k_a)
    rx, vx, kkx, gx, dx, ax, kdx = _rwkv_inputs(px, mu, w0, w2, a0, a2, g2, k_k, k_a)
    s0 = jnp.zeros((px.shape[0], RWKV_HEADS, HEAD_DIM, HEAD_DIM), jnp.float32)
    yc = 0.0
    yx = 0.0
    for d, rev in enumerate((False, True)):
        s_ctx, yc_d = _rwkv_scan(s0, rc, dc[:, :, d], kdc[:, :, d], vc, kkc, ac[:, :, d], rev, need_ctx)
        _, yx_d = _rwkv_scan(s_ctx, rx, dx[:, :, d], kdx[:, :, d], vx, kkx, ax[:, :, d], rev, True)
        yx = yx + yx_d
        if need_ctx:
            yc = yc + yc_d
    out_x = _rwkv_readout(yx, rx, kdx, vx, gx, r_k, ln_w, ln_b)
    out_c = _rwkv_readout(yc, rc, kdc, vc, gc, r_k, ln_w, ln_b) if need_ctx else None
    return out_c, out_x


def _blockdiag(x, w, b):
    bsz, l, _ = x.shape
    y = jnp.einsum('blnc,ncd->blnd', x.reshape(bsz, l, LRU_BLOCKS, LRU_BLOCK), w)
    return y.reshape(bsz, l, LRU_WIDTH) + b


def _lru_coeffs(xc, wa, ba, wx, bx, lam):
    r = jax.nn.sigmoid(_blockdiag(xc, wa, ba)).astype(jnp.float32)
    i = jax.nn.sigmoid(_blockdiag(xc, wx, bx)).astype(jnp.float32)
    log_a = -LRU_C * r * jax.nn.softplus(-lam.astype(jnp.float32))
    a = jnp.exp(log_a)
    b = jnp.sqrt(-jnp.expm1(2.0 * log_a)) * (i * xc.astype(jnp.float32))
    return a, b


def _linear_scan(a, b, h0, reverse):
    def combine(e1, e2):
        a1, b1 = e1
        a2, b2 = e2
        return a1 * a2, a2 * b1 + b2
    a_cum, b_cum = lax.associative_scan(combine, (a, b), axis=1, reverse=reverse)
    return a_cum * h0[:, None] + b_cum


def _lru_mixer(pc, px, conv_w, conv_b, wa, ba, wx, bx, lam, need_ctx):
    gate_c, xin_c = jnp.split(pc, 2, axis=-1)
    gate_x, xin_x = jnp.split(px, 2, axis=-1)
    pad = (LRU_CONV - 1) // 2
    xc_c = _dwconv(xin_c, conv_w, conv_b, pad)
    xc_x = _dwconv(xin_x, conv_w, conv_b, pad)
    h_init = jnp.zeros((px.shape[0], LRU_WIDTH), jnp.float32)
    yc = 0.0
    yx = 0.0
    for d, rev in enumerate((False, True)):
        a_c, b_c = _lru_coeffs(xc_c, wa[d], ba[d], wx[d], bx[d], lam[d])
        h_c = _linear_scan(a_c, b_c, h_init, rev)
        a_x, b_x = _lru_coeffs(xc_x, wa[d], ba[d], wx[d], bx[d], lam[d])
        h_x = _linear_scan(a_x, b_x, h_c[:, 0] if rev else h_c[:, -1], rev)
        yx = yx + h_x
        if need_ctx:
            yc = yc + h_c
    out_x = (yx * jax.nn.gelu(gate_x.astype(jnp.float32))).astype(px.dtype)
    out_c = (yc * jax.nn.gelu(gate_c.astype(jnp.float32))).astype(pc.dtype) if need_ctx else None
    return out_c, out_x


def _merge(gates, ys, w_brs, w_out):
    b, l, _ = gates.shape
    g = jax.nn.sigmoid(gates.reshape(b, l, N_BRANCH, D_MODEL))
    m = g[:, :, 0] * (ys[0] @ w_brs[0])
    for i in range(1, N_BRANCH):
        m = m + g[:, :, i] * (ys[i] @ w_brs[i])
    return m @ w_out


def _hier_moe(h, w_grp, b_grp, w_rt, b_rt, w1, w3, w2):
    shp = h.shape
    t = h.reshape(-1, D_MODEL)
    gp = jax.nn.softmax((t @ w_grp + b_grp).astype(jnp.float32), axis=-1)
    g_val, g_idx = lax.top_k(gp, 1)
    el = (t @ w_rt + b_rt).astype(jnp.float32).reshape(-1, N_GROUPS, EXPERTS_PER_GROUP)
    el = jnp.take_along_axis(el, g_idx[:, :, None], axis=1)[:, 0]
    e_val, e_idx = lax.top_k(jax.nn.softmax(el, axis=-1), TOP_K_INNER)
    wts = g_val * e_val / jnp.sum(e_val, -1, keepdims=True)
    e_glob = g_idx * EXPERTS_PER_GROUP + e_idx
    combine = jnp.sum(jax.nn.one_hot(e_glob, N_EXPERTS, dtype=jnp.float32) * wts[..., None], axis=1)
    combine = combine.astype(t.dtype)
    out = jnp.zeros_like(t)
    for e in range(N_EXPERTS):
        y = (jax.nn.silu(t @ w1[e]) * (t @ w3[e])) @ w2[e]
        out = out + combine[:, e:e + 1] * y
    return out.reshape(shp)


def setup_inputs(seed: int = 0) -> dict:
    key = jax.random.key(seed)
    keys = iter(jax.random.split(key, 64))
    f32 = jnp.float32

    def nrm(shape, scale):
        return jax.random.normal(next(keys), shape, f32) * scale

    d = D_MODEL
    fh = HYENA_FILTER_HIDDEN
    x = nrm((BATCH, SEQ, d), 1.0)
    c = nrm((BATCH, d), 1.0)
    ctx = nrm((BATCH, CTX_LEN, d), 1.0)
    c_ctx = nrm((d,), 1.0)
    ada_w = nrm((DEPTH, d, 6 * d), d ** -0.5)
    ada_b = nrm((DEPTH, 6 * d), 0.01)
    w_in = nrm((DEPTH, d, N_IN), d ** -0.5)
    q_norm = 1.0 + nrm((DEPTH, HEAD_DIM), 0.02)
    k_norm = 1.0 + nrm((DEPTH, HEAD_DIM), 0.02)
    hy_conv_w = nrm((DEPTH, HYENA_SHORT, 3 * HYENA_WIDTH), HYENA_SHORT ** -0.5)
    hy_conv_b = nrm((DEPTH, 3 * HYENA_WIDTH), 0.01)
    hy_f1 = nrm((DEPTH, HYENA_POS_DIM, fh), HYENA_POS_DIM ** -0.5)
    hy_fb1 = nrm((DEPTH, fh), 0.01)
    hy_f2 = nrm((DEPTH, fh, fh), fh ** -0.5)
    hy_fb2 = nrm((DEPTH, fh), 0.01)
    hy_f3 = nrm((DEPTH, fh, HYENA_ORDER * 2 * HYENA_WIDTH), fh ** -0.5)
    hy_skip = nrm((DEPTH, HYENA_ORDER, HYENA_WIDTH), 1.0)
    rw_mu = jax.random.uniform(next(keys), (DEPTH, 2, RWKV_PROJ), f32, 0.0, 0.5)
    n_pos = jnp.arange(RWKV_WIDTH, dtype=f32) / (RWKV_WIDTH - 1)
    ratio = jnp.arange(DEPTH, dtype=f32) / max(DEPTH - 1, 1)
    w0_sched = -7.0 + 5.0 * n_pos[None] ** (0.85 + ratio[:, None] ** 0.5) + 0.5
    rw_w0 = w0_sched[:, None, :] + nrm((DEPTH, 2, RWKV_WIDTH), 0.1)
    rw_w2 = nrm((DEPTH, 2, RWKV_DECAY_RANK, RWKV_WIDTH), 0.1 * RWKV_DECAY_RANK ** -0.5)
    rw_a0 = nrm((DEPTH, 2, RWKV_WIDTH), 0.1)
    rw_a2 = nrm((DEPTH, 2, RWKV_ICLR_RANK, RWKV_WIDTH), 0.1 * RWKV_ICLR_RANK ** -0.5)
    rw_g2 = nrm((DEPTH, RWKV_GATE_RANK, RWKV_WIDTH), RWKV_GATE_RANK ** -0.5)
    rw_k_k = 0.85 + nrm((DEPTH, RWKV_WIDTH), 0.02)
    rw_k_a = 1.0 + nrm((DEPTH, RWKV_WIDTH), 0.02)
    rw_r_k = nrm((DEPTH, RWKV_HEADS, HEAD_DIM), 0.1)
    rw_ln_w = 1.0 + nrm((DEPTH, RWKV_WIDTH), 0.02)
    rw_ln_b = nrm((DEPTH, RWKV_WIDTH), 0.01)
    lru_conv_w = nrm((DEPTH, LRU_CONV, LRU_WIDTH), LRU_CONV ** -0.5)
    lru_conv_b = nrm((DEPTH, LRU_WIDTH), 0.01)
    lru_wa = nrm((DEPTH, 2, LRU_BLOCKS, LRU_BLOCK, LRU_BLOCK), LRU_BLOCK ** -0.5)
    lru_ba = nrm((DEPTH, 2, LRU_WIDTH), 0.01)
    lru_wx = nrm((DEPTH, 2, LRU_BLOCKS, LRU_BLOCK, LRU_BLOCK), LRU_BLOCK ** -0.5)
    lru_bx = nrm((DEPTH, 2, LRU_WIDTH), 0.01)
    u = jax.random.uniform(next(keys), (DEPTH, 2, LRU_WIDTH), f32, 0.9, 0.999)
    sig = u ** (1.0 / LRU_C)
    lru_lambda = jnp.log(sig) - jnp.log1p(-sig)
    w_br_attn = nrm((DEPTH, ATTN_WIDTH, d), ATTN_WIDTH ** -0.5)
    w_br_hyena = nrm((DEPTH, HYENA_WIDTH, d), HYENA_WIDTH ** -0.5)
    w_br_rwkv = nrm((DEPTH, RWKV_WIDTH, d), RWKV_WIDTH ** -0.5)
    w_br_lru = nrm((DEPTH, LRU_WIDTH, d), LRU_WIDTH ** -0.5)
    w_out = nrm((DEPTH, d, d), d ** -0.5)
    moe_w_grp = nrm((DEPTH, d, N_GROUPS), d ** -0.5)
    moe_b_grp = nrm((DEPTH, N_GROUPS), 0.01)
    moe_w_rt = nrm((DEPTH, d, N_EXPERTS), d ** -0.5)
    moe_b_rt = nrm((DEPTH, N_EXPERTS), 0.01)
    moe_w1 = nrm((DEPTH, N_EXPERTS, d, EXPERT_HIDDEN), d ** -0.5)
    moe_w3 = nrm((DEPTH, N_EXPERTS, d, EXPERT_HIDDEN), d ** -0.5)
    moe_w2 = nrm((DEPTH, N_EXPERTS, EXPERT_HIDDEN, d), EXPERT_HIDDEN ** -0.5)
    return {'x': x, 'c': c, 'ctx': ctx, 'c_ctx': c_ctx, 'ada_w': ada_w, 'ada_b': ada_b, 'w_in': w_in,
            'q_norm': q_norm, 'k_norm': k_norm, 'hy_conv_w': hy_conv_w, 'hy_conv_b': hy_conv_b,
            'hy_f1': hy_f1, 'hy_fb1': hy_fb1, 'hy_f2': hy_f2, 'hy_fb2': hy_fb2, 'hy_f3': hy_f3,
            'hy_skip': hy_skip, 'rw_mu': rw_mu, 'rw_w0': rw_w0, 'rw_w2': rw_w2, 'rw_a0': rw_a0,
            'rw_a2': rw_a2, 'rw_g2': rw_g2, 'rw_k_k': rw_k_k, 'rw_k_a': rw_k_a, 'rw_r_k': rw_r_k,
            'rw_ln_w': rw_ln_w, 'rw_ln_b': rw_ln_b, 'lru_conv_w': lru_conv_w, 'lru_conv_b': lru_conv_b,
            'lru_wa': lru_wa, 'lru_ba': lru_ba, 'lru_wx': lru_wx, 'lru_bx': lru_bx, 'lru_lambda': lru_lambda,
            'w_br_attn': w_br_attn, 'w_br_hyena': w_br_hyena, 'w_br_rwkv': w_br_rwkv, 'w_br_lru': w_br_lru,
            'w_out': w_out, 'moe_w_grp': moe_w_grp, 'moe_b_grp': moe_b_grp, 'moe_w_rt': moe_w_rt,
            'moe_b_rt': moe_b_rt, 'moe_w1': moe_w1, 'moe_w3': moe_w3, 'moe_w2': moe_w2}


def reference(x, c, ctx, c_ctx, ada_w, ada_b, w_in, q_norm, k_norm, hy_conv_w, hy_conv_b,
              hy_f1, hy_fb1, hy_f2, hy_fb2, hy_f3, hy_skip, rw_mu, rw_w0, rw_w2, rw_a0, rw_a2,
              rw_g2, rw_k_k, rw_k_a, rw_r_k, rw_ln_w, rw_ln_b, lru_conv_w, lru_conv_b, lru_wa,
              lru_ba, lru_wx, lru_bx, lru_lambda, w_br_attn, w_br_hyena, w_br_rwkv, w_br_lru,
              w_out, moe_w_grp, moe_b_grp, moe_w_rt, moe_b_rt, moe_w1, moe_w3, moe_w2):
    for i in range(DEPTH):
        need_ctx = i < DEPTH - 1
        mod_x = (jax.nn.silu(c) @ ada_w[i] + ada_b[i])[:, None, :]
        mod_c = (jax.nn.silu(c_ctx) @ ada_w[i] + ada_b[i])[None, None, :]
        sh1x, sc1x, g1x, sh2x, sc2x, g2x = jnp.split(mod_x, 6, axis=-1)
        sh1c, sc1c, g1c, sh2c, sc2c, g2c = jnp.split(mod_c, 6, axis=-1)

        hx = _modulate(x, sh1x, sc1x)
        hc = _modulate(ctx, sh1c, sc1c)
        qx, kx, vx, hyx, rwx, lrx, gtx = _split_cols(hx @ w_in[i], IN_SPLITS)
        qc, kc, vc, hyc, rwc, lrc, gtc = _split_cols(hc @ w_in[i], IN_SPLITS)

        att_c, att_x = _attention_mixer((qc, kc, vc), (qx, kx, vx), q_norm[i], k_norm[i], need_ctx)
        hy_c, hy_x = _hyena_mixer(hyc, hyx, hy_conv_w[i], hy_conv_b[i], hy_f1[i], hy_fb1[i],
                                  hy_f2[i], hy_fb2[i], hy_f3[i], hy_skip[i], need_ctx)
        rw_c, rw_x = _rwkv_mixer(rwc, rwx, rw_mu[i], rw_w0[i], rw_w2[i], rw_a0[i], rw_a2[i], rw_g2[i],
                                 rw_k_k[i], rw_k_a[i], rw_r_k[i], rw_ln_w[i], rw_ln_b[i], need_ctx)
        lr_c, lr_x = _lru_mixer(lrc, lrx, lru_conv_w[i], lru_conv_b[i], lru_wa[i], lru_ba[i],
                                lru_wx[i], lru_bx[i], lru_lambda[i], need_ctx)
        w_brs = (w_br_attn[i], w_br_hyena[i], w_br_rwkv[i], w_br_lru[i])
        x = x + g1x * _merge(gtx, (att_x, hy_x, rw_x, lr_x), w_brs, w_out[i])

        moe_args = (moe_w_grp[i], moe_b_grp[i], moe_w_rt[i], moe_b_rt[i], moe_w1[i], moe_w3[i], moe_w2[i])
        if need_ctx:
            ctx = ctx + g1c * _merge(gtc, (att_c, hy_c, rw_c, lr_c), w_brs, w_out[i])
            n_ctx = ctx.shape[1]
            h2 = jnp.concatenate([_modulate(ctx, sh2c, sc2c), _modulate(x, sh2x, sc2x)], axis=1)
            f2 = _hier_moe(h2, *moe_args)
            ctx = ctx + g2c * f2[:, :n_ctx]
            x = x + g2x * f2[:, n_ctx:]
        else:
            x = x + g2x * _hier_moe(_modulate(x, sh2x, sc2x), *moe_args)
    return x
```

```python
import contextlib
import math
import numpy as np
import concourse.bass as bass
import concourse.mybir as mybir
from concourse.bass_utils import run_bass_kernel_spmd

F32 = mybir.dt.float32
BF16 = mybir.dt.bfloat16
ALU = mybir.AluOpType
AF = mybir.ActivationFunctionType
AX = mybir.AxisListType

ENGS = ('pe', 'dve', 'act', 'pool', 'sp')
SEM_ROLL = 30000
NDMA = 12

D = 1024
TC = 256
TX = 4096
T = TC + TX
N_IN = 7296
DEPTH = 2
EPS = 1e-6
BLOCKS = [(0, 256)] + [(256 + 512 * j, 512) for j in range(8)]
O_Q, O_K, O_V, O_HY, O_RW, O_LR, O_GT = 0, 512, 640, 768, 1536, 2688, 3200


class Buf:
    __slots__ = ('t', 'w', 'r', 'name')

    def __init__(self, t, name=''):
        self.t = t
        self.w = None
        self.r = {}
        self.name = name

    def __getitem__(self, idx):
        return self.t[idx]


class Sched:
    def __init__(self, nc):
        self.nc = nc
        self.emap = {'pe': nc.tensor, 'dve': nc.vector, 'act': nc.scalar, 'pool': nc.gpsimd, 'sp': nc.sync}
        self.perm = contextlib.ExitStack()
        self.stack = contextlib.ExitStack()
        self.cur_sem = {}
        self.cnt = {}
        self.nsem = 0
        for e in ('pe', 'dve', 'act', 'pool'):
            self._new_sem(e)
        self.dma_sems = {}
        self.dma_k = {}
        for q in ('sp', 'pool', 'act'):
            self.dma_sems[q] = [self._alloc_sem(f'dma_{q}_{i}') for i in range(NDMA)]
            self.dma_k[q] = 0
        self.seen = {e: {} for e in ENGS}
        self.out_tokens = []
        self.ninstr = 0
        self.uid = 0

    def _alloc_sem(self, name):
        self.nsem += 1
        return self.perm.enter_context(self.nc.semaphore(f'{name}_{self.nsem}'))

    def _new_sem(self, e):
        self.cur_sem[e] = self._alloc_sem(f'c_{e}')
        self.cnt[e] = 0

    def sb(self, name, shape, dt=F32):
        self.uid += 1
        t = self.stack.enter_context(self.nc.sbuf_tensor(f'{name}_{self.uid}', list(shape), dt))
        return Buf(t, name)

    def ps(self, name, shape, dt=F32):
        self.uid += 1
        t = self.stack.enter_context(self.nc.psum_tensor(f'{name}_{self.uid}', list(shape), dt))
        return Buf(t, name)

    @contextlib.contextmanager
    def stage(self):
        old = self.stack
        self.stack = contextlib.ExitStack()
        try:
            yield
        finally:
            self.barrier()
            self.stack.close()
            self.stack = old

    def barrier(self):
        toks = []
        for f in ('pe', 'dve', 'act', 'pool'):
            if self.cnt[f] > 0:
                toks.append((self.cur_sem[f], self.cnt[f]))
        for q in ('sp', 'pool', 'act'):
            k = self.dma_k[q]
            for j in range(min(k, NDMA)):
                last = ((k - 1 - j) // NDMA) * NDMA + j
                toks.append((self.dma_sems[q][j], 16 * (last // NDMA + 1)))
        for e in ENGS:
            eng = self.emap[e]
            for s, v in toks:
                if self.seen[e].get(id(s), -1) >= v:
                    continue
                self.seen[e][id(s)] = v
                eng.wait_ge(s, v)
                self.ninstr += 1

    def op(self, eng, fn, reads=(), writes=(), dmaq=False, is_out=False):
        need = {}

        def add(tok):
            if tok is None:
                return
            sem, val, teng = tok
            if teng == eng and eng == 'pe' and not dmaq:
                return
            k = id(sem)
            if self.seen[eng].get(k, -1) >= val:
                return
            if k not in need or need[k][1] < val:
                need[k] = (sem, val)

        for b in reads:
            add(b.w)
        for b in writes:
            add(b.w)
            for t in b.r.values():
                add(t)
        if dmaq:
            k = self.dma_k[eng]
            self.dma_k[eng] = k + 1
            sem = self.dma_sems[eng][k % NDMA]
            prev = 16 * (k // NDMA)
            if prev > 0:
                add((sem, prev, 'dma'))
            tok = (sem, prev + 16, 'dma')
            inc = 16
            rkey = ('dma', eng, k % (4 * NDMA))
        else:
            if self.cnt[eng] >= SEM_ROLL:
                self._new_sem(eng)
            self.cnt[eng] += 1
            sem = self.cur_sem[eng]
            tok = (sem, self.cnt[eng], eng)
            inc = 1
            rkey = eng
        e = self.emap[eng]
        for s_, v_ in need.values():
            self.seen[eng][id(s_)] = v_
            e.wait_ge(s_, v_)
        fn(e).then_inc(sem, inc)
        self.ninstr += 1 + len(need)
        for b in reads:
            b.r[rkey] = tok
        for b in writes:
            b.w = tok
            b.r = {}
        if is_out:
            self.out_tokens.append(tok)
        return tok

    def dma(self, out_ap, in_ap, reads=(), writes=(), q='sp', is_out=False, **kw):
        return self.op(q, lambda e: e.dma_start(out=out_ap, in_=in_ap, **kw),
                       reads=reads, writes=writes, dmaq=True, is_out=is_out)

    def finish(self):
        need = {}
        for sem, val, _ in self.out_tokens:
            k = id(sem)
            if k not in need or need[k][1] < val:
                need[k] = (sem, val)
        for s_, v_ in need.values():
            self.nc.sync.wait_ge(s_, v_)
        self.barrier()
        self.stack.close()
        self.perm.close()


class Rot:
    def __init__(self, bufs):
        self.bufs = bufs
        self.i = 0

    def next(self):
        b = self.bufs[self.i % len(self.bufs)]
        self.i += 1
        return b


class Prog:
    def __init__(self, ext_in=(), ext_out=()):
        self.nc = bass.Bass("TRN2", target_bir_lowering=False)
        self.S = Sched(self.nc)
        self.ext_in = set(ext_in)
        self.ext_out = set(ext_out)
        self.dr = {}
        self.in_names = []
        self.out_names = []

    def inp(self, name, shape, dt=F32):
        t = self.nc.dram_tensor(name, list(shape), dt, kind="ExternalInput")
        self.dr[name] = t.ap()
        self.in_names.append(name)
        return self.dr[name]

    def out(self, name, shape, dt=F32):
        t = self.nc.dram_tensor(name, list(shape), dt, kind="ExternalOutput")
        self.dr[name] = t.ap()
        self.out_names.append(name)
        return self.dr[name]

    def tmp(self, name, shape, dt=F32):
        if name in self.ext_in:
            return self.inp(name, shape, dt)
        if name in self.ext_out:
            return self.out(name, shape, dt)
        t = self.nc.dram_tensor(name, list(shape), dt, kind="Internal")
        self.dr[name] = t.ap()
        return self.dr[name]


def stage_mod(P, li):
    S = P.S
    ada_w = P.dr['ada_w']
    ada_b = P.dr['ada_b']
    cvec = P.dr['cvec']
    modT_d = P.dr[f'modT{li}']
    modrow_d = P.dr[f'modrow{li}']
    with S.stage():
        cT = S.sb('cT', [128, 8, 2])
        scT = S.sb('scT', [128, 8, 2])
        abT = S.sb('abT', [128, 48])
        modT = S.sb('modT', [128, 48, 2])
        sig = S.sb('sig', [128, 8, 2])
        for s in range(2):
            S.dma(cT[:, :, s], cvec[s, :].rearrange("(k p) -> p k", p=128), writes=[cT],
                  allow_slow_non_contiguous=True)
        S.dma(abT[:, :], ada_b[li, :].rearrange("(o p) -> p o", p=128), writes=[abT],
              allow_slow_non_contiguous=True)
        S.op('act', lambda e: e.activation(out=sig[:], in_=cT[:], func=AF.Sigmoid), reads=[cT], writes=[sig])
        S.op('dve', lambda e: e.tensor_tensor(out=scT[:], in0=cT[:], in1=sig[:], op=ALU.mult), reads=[cT, sig], writes=[scT])
        wts = Rot([S.sb(f'adaw{j}', [128, 8, 512]) for j in range(2)])
        pss = Rot([S.ps(f'modps{j}', [128, 8]) for j in range(2)])
        aw = ada_w[li].rearrange("(k p) n -> p k n", p=128)
        for g in range(12):
            wt = wts.next()
            S.dma(wt[:], aw[:, :, g * 512:(g + 1) * 512], writes=[wt], q=('sp' if g % 2 == 0 else 'pool'))
            ps = pss.next()
            for j in range(4):
                oc = g * 4 + j
                for k in range(8):
                    S.op('pe', lambda e, j=j, k=k: e.matmul(ps[:, 2 * j:2 * j + 2], lhsT=wt[:, k, j * 128:(j + 1) * 128], rhs=scT[:, k, :],
                                                           start=(k == 0), stop=(k == 7)), reads=[wt, scT], writes=[ps])
            for j in range(4):
                oc = g * 4 + j
                S.op('dve', lambda e, j=j, oc=oc: e.tensor_scalar(out=modT[:, oc, :], in0=ps[:, 2 * j:2 * j + 2], scalar1=abT[:, oc:oc + 1], scalar2=None,
                                                                 op0=ALU.add), reads=[ps, abT], writes=[modT])
        S.dma(modT_d[:, :], modT[:].rearrange("p o s -> p (o s)"), reads=[modT])
        for s in range(2):
            S.dma(modrow_d[s, :].rearrange("(o p) -> p o", p=128), modT[:, :, s], reads=[modT], q='pool',
                  allow_slow_non_contiguous=True)


def load_modT(P, li):
    S = P.S
    m = S.sb('modTl', [128, 48, 2])
    S.dma(m[:].rearrange("p o s -> p (o s)"), P.dr[f'modT{li}'][:, :], writes=[m])
    return m


def norm_ctx(S, npt=2):
    C = {}
    C['sc1p'] = S.sb('sc1p', [128, 8, 2])
    C['xts'] = Rot([S.sb(f'xt{j}', [128, 1024]) for j in range(3)])
    C['sqs'] = Rot([S.sb(f'sq{j}', [128, 1024]) for j in range(2)])
    C['xns'] = Rot([S.sb(f'xn{j}', [128, 1024], BF16) for j in range(2)])
    C['sss'] = Rot([S.sb(f'ss{j}', [128, 2]) for j in range(4)])
    C['pts'] = Rot([S.ps(f'ptT{j}', [128, 4, 128], BF16) for j in range(npt)])
    return C


def norm_transpose(P, src_d, modT, sh_c, sc_c, hT, ident, tiles=None, dst0=0, C=None):
    S = P.S
    if C is None:
        C = norm_ctx(S)
    sc1p = C['sc1p']
    S.op('dve', lambda e: e.tensor_scalar(out=sc1p[:], in0=modT[:, sc_c:sc_c + 8, :], scalar1=1.0, scalar2=None, op0=ALU.add),
         reads=[modT], writes=[sc1p])
    xts, sqs, xns, sss, pts = C['xts'], C['sqs'], C['xns'], C['sss'], C['pts']
    if tiles is None:
        tiles = range(T // 128)
    for ti in tiles:
        s = 0 if ti < 2 else 1
        xt = xts.next()
        S.dma(xt[:], src_d[ti * 128:(ti + 1) * 128, :], writes=[xt], q=('sp' if ti % 2 == 0 else 'pool'))
        sq = sqs.next()
        ss = sss.next()
        S.op('act', lambda e: e.activation(out=sq[:], in_=xt[:], func=AF.Square), reads=[xt], writes=[sq])
        S.op('dve', lambda e: e.reduce_sum(out=ss[:, 0:1], in_=sq[:], axis=AX.X), reads=[sq], writes=[ss])
        S.op('dve', lambda e: e.tensor_scalar(out=ss[:, 1:2], in0=ss[:, 0:1], scalar1=1.0 / D, scalar2=EPS, op0=ALU.mult, op1=ALU.add),
             reads=[ss], writes=[ss])
        S.op('act', lambda e: e.activation(out=ss[:, 1:2], in_=ss[:, 1:2], func=AF.Sqrt), reads=[ss], writes=[ss])
        S.op('dve', lambda e: e.reciprocal(out=ss[:, 0:1], in_=ss[:, 1:2]), reads=[ss], writes=[ss])
        xn = xns.next()
        S.op('act', lambda e: e.activation(out=xn[:], in_=xt[:], func=AF.Copy, scale=ss[:, 0:1]), reads=[xt, ss], writes=[xn])
        for half in range(2):
            pt = pts.next()
            for j in range(4):
                k = half * 4 + j
                S.op('pe', lambda e, j=j, k=k: e.transpose(out=pt[:, j, :], in_=xn[:, k * 128:(k + 1) * 128], identity=ident[:]),
                     reads=[xn, ident], writes=[pt])
            for j in range(4):
                k = half * 4 + j
                S.op('dve', lambda e, j=j, k=k: e.tensor_scalar(out=hT[:, k, ti * 128 - dst0:(ti + 1) * 128 - dst0], in0=pt[:, j, :],
                                                               scalar1=sc1p[:, k, s:s + 1], scalar2=modT[:, sh_c + k, s:s + 1],
                                                               op0=ALU.mult, op1=ALU.add), reads=[pt, sc1p, modT], writes=[hT])


def stage_inproj(P, li, src_name):
    S = P.S
    src_d = P.dr[src_name]
    w_in = P.dr['w_in']
    projT = P.dr['projT']
    vtm = P.dr['vtm']
    with S.stage():
        ident = S.sb('ident', [128, 128], BF16)
        S.dma(ident[:], P.dr['ident_bf'][:, :], writes=[ident])
        modT = load_modT(P, li)
        hT = S.sb('hT', [128, 8, T], BF16)
        norm_transpose(P, src_d, modT, 0, 8, hT, ident)
        wfs = Rot([S.sb(f'wf{j}', [128, 8, 384]) for j in range(2)])
        wbs = Rot([S.sb(f'wb{j}', [128, 8, 384], BF16) for j in range(2)])
        pss = Rot([S.ps(f'ps{j}', [128, 512]) for j in range(4)])
        sts = Rot([S.sb(f'st{j}', [128, 512]) for j in range(4)])
        wv = w_in[li].rearrange("(k p) n -> p k n", p=128)
        cnt = 0
        for g in range(N_IN // 384):
            wf = wfs.next()
            S.dma(wf[:], wv[:, :, g * 384:(g + 1) * 384], writes=[wf], q=('sp' if g % 2 == 0 else 'pool'))
            wb = wbs.next()
            S.op('pool', lambda e: e.tensor_copy(out=wb[:], in_=wf[:]), reads=[wf], writes=[wb])
            for j in range(3):
                oc = g * 3 + j
                if oc == O_V // 128:
                    for ti in range(T // 128):
                        ps = pss.next()
                        for k in range(8):
                            S.op('pe', lambda e, k=k: e.matmul(ps[:, 0:128], lhsT=hT[:, k, ti * 128:(ti + 1) * 128], rhs=wb[:, k, j * 128:(j + 1) * 128],
                                                               start=(k == 0), stop=(k == 7)), reads=[hT, wb], writes=[ps])
                        st = sts.next()
                        S.op('act', lambda e: e.copy(out=st[:, 0:128], in_=ps[:, 0:128]), reads=[ps], writes=[st])
                        S.dma(vtm[ti * 128:(ti + 1) * 128, :], st[:, 0:128], reads=[st], q='act')
                    continue
                for (t0, n) in BLOCKS:
                    ps = pss.next()
                    for k in range(8):
                        S.op('pe', lambda e, k=k: e.matmul(ps[:, 0:n], lhsT=wb[:, k, j * 128:(j + 1) * 128], rhs=hT[:, k, t0:t0 + n],
                                                           start=(k == 0), stop=(k == 7)), reads=[hT, wb], writes=[ps])
                    st = sts.next()
                    if cnt % 2 == 0:
                        S.op('act', lambda e: e.copy(out=st[:, 0:n], in_=ps[:, 0:n]), reads=[ps], writes=[st])
                    else:
                        S.op('dve', lambda e: e.tensor_copy(out=st[:, 0:n], in_=ps[:, 0:n]), reads=[ps], writes=[st])
                    S.dma(projT[oc * 128:(oc + 1) * 128, t0:t0 + n], st[:, 0:n], reads=[st], q=('sp' if cnt % 2 == 0 else 'act'))
                    cnt += 1


def declare_common(P):
    P.inp('xin', [T, D])
    P.inp('cvec', [2, D])
    P.inp('ada_w', [DEPTH, D, 6 * D])
    P.inp('ada_b', [DEPTH, 6 * D])
    P.inp('w_in', [DEPTH, D, N_IN])
    P.inp('ident_bf', [128, 128], BF16)
    for li in range(DEPTH):
        P.tmp(f'modT{li}', [128, 96])
        P.tmp(f'modrow{li}', [2, 6 * D])
    P.tmp('projT', [N_IN, T])
    P.tmp('vtm', [T, 128])


def host_inputs(inputs, b):
    import ml_dtypes
    m = {}
    m['xin'] = np.ascontiguousarray(np.concatenate([inputs['ctx'][b], inputs['x'][b]], axis=0), dtype=np.float32)
    m['cvec'] = np.ascontiguousarray(np.stack([inputs['c_ctx'], inputs['c'][b]], axis=0), dtype=np.float32)
    for k in ('ada_w', 'ada_b', 'w_in'):
        m[k] = np.ascontiguousarray(inputs[k], dtype=np.float32)
    m['ident_bf'] = np.eye(128, dtype=np.float32).astype(ml_dtypes.bfloat16)
    return m


def rope_tables():
    t = np.arange(TX)
    pos = np.stack([t // 64, t % 64], 0).astype(np.float64)
    freqs = 10000.0 ** (-np.arange(16, dtype=np.float64) / 16)
    cos = np.zeros((64, TX)); sin = np.zeros((64, TX))
    for d in range(64):
        ax, half, f = d // 32, (d % 32) // 16, d % 16
        ang = pos[ax] * freqs[f]
        cos[d] = np.cos(ang)
        sin[d] = np.sin(ang) * (-1.0 if half == 0 else 1.0)
    psw = np.zeros((128, 128), np.float32)
    for m in range(128):
        d = m % 64
        src = m + 16 if (d % 32) < 16 else m - 16
        psw[src, m] = 1.0
    blk = np.zeros((128, 128), np.float32)
    blk[:64, :64] = 1.0 / 64
    blk[64:, 64:] = 1.0 / 64
    return (np.tile(cos, (2, 1)).astype(np.float32), np.tile(sin, (2, 1)).astype(np.float32), psw, blk)


def qk_prep(P, S, rows_list, gain, dst, dst_sl, scale, tok_ranges, C):
    projT = P.dr['projT']
    for (t0, n) in tok_ranges:
        raw = C['raw'].next()
        for i, (r0, nr, p0) in enumerate(rows_list):
            S.dma(raw[p0:p0 + nr, 0:n], projT[r0:r0 + nr, t0:t0 + n], writes=[raw], q=('sp' if i % 2 == 0 else 'pool'))
        sq = C['sq'].next()
        S.op('act', lambda e: e.activation(out=sq[:, 0:n], in_=raw[:, 0:n], func=AF.Square), reads=[raw], writes=[sq])
        ps = C['ps'].next()
        S.op('pe', lambda e: e.matmul(ps[:, 0:n], lhsT=C['blk'][:], rhs=sq[:, 0:n], start=True, stop=True), reads=[sq, C['blk']], writes=[ps])
        rs = C['rs'].next()
        S.op('dve', lambda e: e.tensor_scalar(out=rs[:, 0:n], in0=ps[:, 0:n], scalar1=EPS, scalar2=None, op0=ALU.add), reads=[ps], writes=[rs])
        S.op('act', lambda e: e.activation(out=rs[:, 0:n], in_=rs[:, 0:n], func=AF.Sqrt), reads=[rs], writes=[rs])
        S.op('dve', lambda e: e.reciprocal(out=rs[:, 0:n], in_=rs[:, 0:n]), reads=[rs], writes=[rs])
        kh = C['kh'].next()
        S.op('dve', lambda e: e.scalar_tensor_tensor(out=kh[:, 0:n], in0=raw[:, 0:n], scalar=gain[:, 0:1], in1=rs[:, 0:n], op0=ALU.mult, op1=ALU.mult),
             reads=[raw, gain, rs], writes=[kh])
        if t0 < TC:
            S.op('act', lambda e: e.activation(out=dst_sl(t0, n), in_=kh[:, 0:n], func=AF.Copy, scale=scale), reads=[kh], writes=[dst])
            continue
        khb = C['khb'].next()
        S.op('act', lambda e: e.copy(out=khb[:, 0:n], in_=kh[:, 0:n]), reads=[kh], writes=[khb])
        ps2 = C['ps'].next()
        S.op('pe', lambda e: e.matmul(ps2[:, 0:n], lhsT=C['psw'][:], rhs=khb[:, 0:n], start=True, stop=True), reads=[khb, C['psw']], writes=[ps2])
        x0 = t0 - TC
        t1 = C['t1'].next()
        S.op('pool', lambda e: e.tensor_tensor(out=t1[:, 0:n], in0=kh[:, 0:n], in1=C['cos'][:, x0:x0 + n], op=ALU.mult), reads=[kh, C['cos']], writes=[t1])
        t2 = C['t2'].next()
        S.op('dve', lambda e: e.tensor_tensor(out=t2[:, 0:n], in0=ps2[:, 0:n], in1=C['sin'][:, x0:x0 + n], op=ALU.mult), reads=[ps2, C['sin']], writes=[t2])
        S.op('dve', lambda e: e.scalar_tensor_tensor(out=dst_sl(t0, n), in0=t1[:, 0:n], scalar=scale, in1=t2[:, 0:n], op0=ALU.mult, op1=ALU.add),
             reads=[t1, t2], writes=[dst])
        if scale != 1.0:
            raise NotImplementedError


def stage_attn(P, li, need_ctx, dbg=0):
    S = P.S
    projT = P.dr['projT']
    vtm = P.dr['vtm']
    attT = P.dr['attT']
    with S.stage():
        C = {}
        C['cos'] = S.sb('cos', [128, TX]); C['sin'] = S.sb('sin', [128, TX])
        C['psw'] = S.sb('psw', [128, 128], BF16); C['blk'] = S.sb('blk', [128, 128])
        pswf = S.sb('pswf', [128, 128])
        S.dma(C['cos'][:], P.dr['rope_cos'][:, :], writes=[C['cos']])
        S.dma(C['sin'][:], P.dr['rope_sin'][:, :], writes=[C['sin']], q='pool')
        S.dma(pswf[:], P.dr['rope_psw'][:, :], writes=[pswf])
        S.dma(C['blk'][:], P.dr['blk64'][:, :], writes=[C['blk']])
        S.op('dve', lambda e: e.tensor_copy(out=C['psw'][:], in_=pswf[:]), reads=[pswf], writes=[C['psw']])
        qg = S.sb('qg', [128, 1]); kg = S.sb('kg', [128, 1])
        for h in range(2):
            S.dma(qg[h * 64:(h + 1) * 64, :], P.dr['q_norm'][li, :].rearrange("(d o) -> d o", o=1), writes=[qg])
            S.dma(kg[h * 64:(h + 1) * 64, :], P.dr['k_norm'][li, :].rearrange("(d o) -> d o", o=1), writes=[kg])
        S.op('dve', lambda e: e.tensor_scalar(out=qg[:], in0=qg[:], scalar1=0.125, scalar2=None, op0=ALU.mult), reads=[qg], writes=[qg])
        for nm in ('raw', 'sq', 'rs', 'kh', 't1', 't2'):
            C[nm] = Rot([S.sb(f'{nm}{j}', [128, 512]) for j in range(2)])
        C['khb'] = Rot([S.sb(f'khb{j}', [128, 512], BF16) for j in range(2)])
        C['ps'] = Rot([S.ps(f'pps{j}', [128, 512]) for j in range(2)])
        kT = S.sb('kT', [128, T], BF16)
        qT = S.sb('qT', [128, 4, T], BF16)
        qk_prep(P, S, [(O_K, 128, 0)], kg, kT, lambda t0, n: kT[:, t0:t0 + n], 1.0, BLOCKS, C)
        qblocks = BLOCKS if need_ctx else BLOCKS[1:]
        for g in range(4):
            qk_prep(P, S, [(O_Q + g * 64, 64, 0), (O_Q + 256 + g * 64, 64, 64)], qg, qT,
                    lambda t0, n, g=g: qT[:, g, t0:t0 + n], 1.0, qblocks, C)
        if dbg:
            S.dma(P.dr['dbg_k'][:, :], kT[:], reads=[kT])
            S.dma(P.dr['dbg_q'][:, :], qT[:, 0, :], reads=[qT])
        if dbg == 1:
            return
        va = S.sb('va', [128, T // 128, 2, 128], BF16)
        vf = S.sb('vf', [128, T // 128, 128])
        S.op('pool', lambda e: e.memset(va[:], 1.0), writes=[va])
        S.dma(vf[:], vtm.rearrange("(a p) c -> p a c", p=128), writes=[vf])
        S.op('dve', lambda e: e.tensor_copy(out=va[:, :, :, 0:64], in_=vf[:].rearrange("p a (h d) -> p a h d", h=2)), reads=[vf], writes=[va])
        ones_r = S.sb('ones_r', [128, 64])
        S.op('pool', lambda e: e.memset(ones_r[:], 1.0), writes=[ones_r])
        if dbg == 2:
            return
        sps = Rot([S.ps(f'sps{j}', [128, 512]) for j in range(3)])
        ops = Rot([S.ps(f'ops{j}', [128, 512]) for j in range(2)])
        bps = Rot([S.ps(f'bps{j}', [64, 512]) for j in range(1)])
        pts = Rot([S.sb(f'pT{j}', [128, 512], BF16) for j in range(3)])
        osb = Rot([S.sb(f'osb{j}', [128, 512]) for j in range(2)])
        outs = Rot([S.sb(f'aout{j}', [64, 512]) for j in range(2)])
        for kvh in range(2):
            p0 = kvh * 64
            for g in range(4):
                head = kvh * 4 + g
                for (t0, n) in qblocks:
                    nkt = (TC // 128) if t0 < TC else (T // 128)
                    op_ = ops.next()
                    for kt in range(nkt):
                        sp_ = sps.next()
                        S.op('pe', lambda e, kt=kt: e.matmul(sp_[:, 0:n], lhsT=kT[p0:p0 + 64, kt * 128:(kt + 1) * 128], rhs=qT[p0:p0 + 64, g, t0:t0 + n],
                                                             start=True, stop=True), reads=[kT, qT], writes=[sp_])
                        pt = pts.next()
                        S.op('act', lambda e: e.activation(out=pt[:, 0:n], in_=sp_[:, 0:n], func=AF.Exp), reads=[sp_], writes=[pt])
                        S.op('pe', lambda e, kt=kt: e.matmul(op_[0:65, 0:n], lhsT=va[:, kt, kvh, 0:65], rhs=pt[:, 0:n], start=(kt == 0), stop=(kt == nkt - 1)),
                             reads=[va, pt], writes=[op_])
                    ob = osb.next()
                    S.op('dve', lambda e: e.tensor_copy(out=ob[0:65, 0:n], in_=op_[0:65, 0:n]), reads=[op_], writes=[ob])
                    S.op('dve', lambda e: e.reciprocal(out=ob[64:65, 0:n], in_=ob[64:65, 0:n]), reads=[ob], writes=[ob])
                    bp = bps.next()
                    S.op('pe', lambda e: e.matmul(bp[:, 0:n], lhsT=ones_r[64:65, :], rhs=ob[64:65, 0:n], start=True, stop=True), reads=[ob, ones_r], writes=[bp])
                    ao = outs.next()
                    S.op('dve', lambda e: e.tensor_tensor(out=ao[:, 0:n], in0=ob[0:64, 0:n], in1=bp[:, 0:n], op=ALU.mult), reads=[ob, bp], writes=[ao])
                    S.dma(attT[head * 64:(head + 1) * 64, t0:t0 + n], ao[:, 0:n], reads=[ao], q='pool')


def declare_attn(P):
    P.inp('q_norm', [DEPTH, 64])
    P.inp('k_norm', [DEPTH, 64])
    P.inp('rope_cos', [128, TX])
    P.inp('rope_sin', [128, TX])
    P.inp('rope_psw', [128, 128])
    P.inp('blk64', [128, 128])
    P.tmp('attT', [512, T])


def host_attn(m, inputs):
    cos, sin, psw, blk = rope_tables()
    m['rope_cos'] = cos; m['rope_sin'] = sin; m['rope_psw'] = psw; m['blk64'] = blk
    m['q_norm'] = np.ascontiguousarray(inputs['q_norm'], np.float32)
    m['k_norm'] = np.ascontiguousarray(inputs['k_norm'], np.float32)


def dwconv_fm(S, out, x, wcol, bcol, taps, pad_left, segs, eng='dve', wbuf=None):
    for (t0, n) in segs:
        j0 = pad_left
        if bcol is not None:
            S.op(eng, lambda e: e.tensor_scalar(out=out[:, t0:t0 + n], in0=x[:, t0:t0 + n], scalar1=wcol[:, j0:j0 + 1], scalar2=bcol,
                                                op0=ALU.mult, op1=ALU.add), reads=[x, wbuf], writes=[out])
        else:
            S.op(eng, lambda e: e.tensor_scalar(out=out[:, t0:t0 + n], in0=x[:, t0:t0 + n], scalar1=wcol[:, j0:j0 + 1], scalar2=None,
                                                op0=ALU.mult), reads=[x, wbuf], writes=[out])
        for j in range(taps):
            sh = j - pad_left
            if sh == 0:
                continue
            if sh > 0:
                o_sl = slice(t0, t0 + n - sh); i_sl = slice(t0 + sh, t0 + n)
            else:
                o_sl = slice(t0 - sh, t0 + n); i_sl = slice(t0, t0 + n + sh)
            S.op(eng, lambda e, j=j, o_sl=o_sl, i_sl=i_sl: e.scalar_tensor_tensor(out=out[:, o_sl], in0=x[:, i_sl], scalar=wcol[:, j:j + 1], in1=out[:, o_sl],
                                                                                 op0=ALU.mult, op1=ALU.add), reads=[x, wbuf, out], writes=[out])


def stage_lru(P, li, need_ctx):
    S = P.S
    projT = P.dr['projT']
    lruT = P.dr['lruT']
    SEGS = [(0, TC), (TC, TX)]
    with S.stage():
        big = lambda nm: S.sb(nm, [128, T])
        gate, xin, xc, A, Bv, tmp, h0, h1 = [big(n) for n in ('gate', 'xin', 'xc', 'A', 'Bv', 'tmp', 'h0', 'h1')]
        cw = S.sb('cw', [128, 4]); cb = S.sb('cb', [128, 1])
        wbd = [S.sb(f'wbd{j}', [128, 128]) for j in range(4)]
        bias = S.sb('bias', [128, 4]); lam = S.sb('lam', [128, 2]); c8 = S.sb('c8', [128, 2])
        pss = Rot([S.ps(f'lps{j}', [128, 512]) for j in range(4)])
        for ct in range(2):
            c0 = ct * 128
            S.dma(gate[:], projT[O_LR + c0:O_LR + c0 + 128, :], writes=[gate])
            S.dma(xin[:], projT[O_LR + 256 + c0:O_LR + 256 + c0 + 128, :], writes=[xin], q='pool')
            S.dma(cw[:], P.dr['lru_conv_w'][li, :, c0:c0 + 128].rearrange("j c -> c j"), writes=[cw], allow_slow_non_contiguous=True)
            S.dma(cb[:], P.dr['lru_conv_b'][li, c0:c0 + 128].rearrange("(c o) -> c o", o=1), writes=[cb])
            for d in range(2):
                for gi, (wn, bn) in enumerate((('lru_wa', 'lru_ba'), ('lru_wx', 'lru_bx'))):
                    w = wbd[d * 2 + gi]
                    S.op('pool', lambda e, w=w: e.memset(w[:], 0.0), writes=[w])
                    for nb in range(2):
                        S.dma(w[nb * 64:(nb + 1) * 64, nb * 64:(nb + 1) * 64], P.dr[wn][li, d, ct * 2 + nb, :, :], writes=[w])
                    S.dma(bias[:, d * 2 + gi:d * 2 + gi + 1], P.dr[bn][li, d, c0:c0 + 128].rearrange("(c o) -> c o", o=1), writes=[bias])
                S.dma(lam[:, d:d + 1], P.dr['lru_lambda'][li, d, c0:c0 + 128].rearrange("(c o) -> c o", o=1), writes=[lam])
            S.op('act', lambda e: e.activation(out=c8[:], in_=lam[:], func=AF.Exp, scale=-1.0), reads=[lam], writes=[c8])
            S.op('dve', lambda e: e.tensor_scalar(out=c8[:], in0=c8[:], scalar1=1.0, scalar2=None, op0=ALU.add), reads=[c8], writes=[c8])
            S.op('act', lambda e: e.activation(out=c8[:], in_=c8[:], func=AF.Ln), reads=[c8], writes=[c8])
            S.op('dve', lambda e: e.tensor_scalar(out=c8[:], in0=c8[:], scalar1=-8.0, scalar2=None, op0=ALU.mult), reads=[c8], writes=[c8])
            dwconv_fm(S, xc, xin, cw[:, :], cb[:, 0:1], 4, 1, SEGS, wbuf=cw)
            hs = [h0, h1]
            for d in range(2):
                for (t0, n) in BLOCKS:
                    pa = pss.next(); px = pss.next()
                    S.op('pe', lambda e: e.matmul(pa[:, 0:n], lhsT=wbd[d * 2][:], rhs=xc[:, t0:t0 + n], start=True, stop=True), reads=[wbd[d * 2], xc], writes=[pa])
                    S.op('pe', lambda e: e.matmul(px[:, 0:n], lhsT=wbd[d * 2 + 1][:], rhs=xc[:, t0:t0 + n], start=True, stop=True), reads=[wbd[d * 2 + 1], xc], writes=[px])
                    S.op('act', lambda e: e.activation(out=A[:, t0:t0 + n], in_=pa[:, 0:n], func=AF.Sigmoid, bias=bias[:, d * 2:d * 2 + 1]), reads=[pa, bias], writes=[A])
                    S.op('act', lambda e: e.activation(out=Bv[:, t0:t0 + n], in_=px[:, 0:n], func=AF.Sigmoid, bias=bias[:, d * 2 + 1:d * 2 + 2]), reads=[px, bias], writes=[Bv])
                S.op('act', lambda e: e.activation(out=A[:], in_=A[:], func=AF.Exp, scale=c8[:, d:d + 1]), reads=[A, c8], writes=[A])
                S.op('dve', lambda e: e.tensor_tensor(out=tmp[:], in0=A[:], in1=A[:], op=ALU.mult), reads=[A], writes=[tmp])
                S.op('dve', lambda e: e.tensor_scalar(out=tmp[:], in0=tmp[:], scalar1=-1.0, scalar2=1.0, op0=ALU.mult, op1=ALU.add), reads=[tmp], writes=[tmp])
                S.op('dve', lambda e: e.tensor_scalar(out=tmp[:], in0=tmp[:], scalar1=0.0, scalar2=None, op0=ALU.max), reads=[tmp], writes=[tmp])
                S.op('act', lambda e: e.activation(out=tmp[:], in_=tmp[:], func=AF.Sqrt), reads=[tmp], writes=[tmp])
                S.op('pool', lambda e: e.tensor_tensor(out=Bv[:], in0=Bv[:], in1=xc[:], op=ALU.mult), reads=[Bv, xc], writes=[Bv])
                S.op('dve', lambda e: e.tensor_tensor(out=Bv[:], in0=Bv[:], in1=tmp[:], op=ALU.mult), reads=[Bv, tmp], writes=[Bv])
                h = hs[d]
                if d == 0:
                    S.op('dve', lambda e: e.tensor_tensor_scan(out=h[:, :], data0=A[:, :], data1=Bv[:, :], initial=0.0, op0=ALU.mult, op1=ALU.add),
                         reads=[A, Bv], writes=[h])
                else:
                    S.op('dve', lambda e: e.tensor_tensor_scan(out=h[:, 0:TC][:, ::-1], data0=A[:, 0:TC][:, ::-1], data1=Bv[:, 0:TC][:, ::-1], initial=0.0,
                                                               op0=ALU.mult, op1=ALU.add), reads=[A, Bv], writes=[h])
                    S.op('dve', lambda e: e.tensor_tensor_scan(out=h[:, TC:T][:, ::-1], data0=A[:, TC:T][:, ::-1], data1=Bv[:, TC:T][:, ::-1], initial=h[:, 0:1],
                                                               op0=ALU.mult, op1=ALU.add), reads=[A, Bv, h], writes=[h])
            S.op('pool', lambda e: e.tensor_tensor(out=h0[:], in0=h0[:], in1=h1[:], op=ALU.add), reads=[h0, h1], writes=[h0])
            S.op('dve', lambda e: e.tensor_tensor(out=tmp[:], in0=gate[:], in1=gate[:], op=ALU.mult), reads=[gate], writes=[tmp])
            S.op('dve', lambda e: e.tensor_scalar(out=tmp[:], in0=tmp[:], scalar1=0.044715, scalar2=1.0, op0=ALU.mult, op1=ALU.add), reads=[tmp], writes=[tmp])
            S.op('dve', lambda e: e.tensor_tensor(out=tmp[:], in0=tmp[:], in1=gate[:], op=ALU.mult), reads=[tmp, gate], writes=[tmp])
            S.op('act', lambda e: e.activation(out=tmp[:], in_=tmp[:], func=AF.Sigmoid, scale=1.5957691216), reads=[tmp], writes=[tmp])
            S.op('pool', lambda e: e.tensor_tensor(out=tmp[:], in0=tmp[:], in1=gate[:], op=ALU.mult), reads=[tmp, gate], writes=[tmp])
            S.op('dve', lambda e: e.tensor_tensor(out=h0[:], in0=h0[:], in1=tmp[:], op=ALU.mult), reads=[h0, tmp], writes=[h0])
            S.dma(lruT[c0:c0 + 128, :], h0[:], reads=[h0])


def declare_lru(P):
    P.inp('lru_conv_w', [DEPTH, 4, 256]); P.inp('lru_conv_b', [DEPTH, 256])
    P.inp('lru_wa', [DEPTH, 2, 4, 64, 64]); P.inp('lru_ba', [DEPTH, 2, 256])
    P.inp('lru_wx', [DEPTH, 2, 4, 64, 64]); P.inp('lru_bx', [DEPTH, 2, 256])
    P.inp('lru_lambda', [DEPTH, 2, 256])
    P.tmp('lruT', [256, T])


def host_lru(m, inputs):
    for k in ('lru_conv_w', 'lru_conv_b', 'lru_wa', 'lru_ba', 'lru_wx', 'lru_bx', 'lru_lambda'):
        m[k] = np.ascontiguousarray(inputs[k], np.float32)


BR = [('attT', 'w_br_attn', 4), ('hyT', 'w_br_hyena', 2), ('rwT', 'w_br_rwkv', 2), ('lruT', 'w_br_lru', 2)]


def stage_merge(P, li, need_ctx, src_name, dst_name):
    S = P.S
    projT = P.dr['projT']
    src = P.dr[src_name]
    dst = P.dr[dst_name]
    with S.stage():
        wbr = S.sb('wbr', [128, 10, D], BF16)
        wout = S.sb('wout', [128, 8, D], BF16)
        stg = Rot([S.sb(f'wstg{j}', [128, D]) for j in range(2)])
        ci = 0
        for (_, wn, nch) in BR:
            for c in range(nch):
                st = stg.next()
                S.dma(st[:], P.dr[wn][li, c * 128:(c + 1) * 128, :], writes=[st], q=('sp' if ci % 2 == 0 else 'pool'))
                S.op('pool', lambda e, ci=ci, st=st: e.tensor_copy(out=wbr[:, ci, :], in_=st[:]), reads=[st], writes=[wbr])
                ci += 1
        for c in range(8):
            st = stg.next()
            S.dma(st[:], P.dr['w_out'][li, c * 128:(c + 1) * 128, :], writes=[st], q=('sp' if c % 2 == 0 else 'pool'))
            S.op('pool', lambda e, c=c, st=st: e.tensor_copy(out=wout[:, c, :], in_=st[:]), reads=[st], writes=[wout])
        g1 = S.sb('g1', [128, 2, D])
        for s_ in range(2):
            S.dma(g1[:, s_, :], P.dr[f'modrow{li}'][s_:s_ + 1, 2 * D:3 * D].partition_broadcast(128), writes=[g1])
        yf = Rot([S.sb(f'yf{j}', [128, 10, 512]) for j in range(2)])
        yb = Rot([S.sb(f'yb{j}', [128, 10, 512], BF16) for j in range(2)])
        gts = Rot([S.sb(f'gt{j}', [128, 512]) for j in range(4)])
        sgs = Rot([S.sb(f'sg{j}', [128, 512]) for j in range(3)])
        tms = Rot([S.sb(f'tm{j}', [128, 512]) for j in range(3)])
        macc = Rot([S.sb(f'macc{j}', [128, 512]) for j in range(2)])
        mTs = Rot([S.sb(f'mT{j}', [128, 8, 512], BF16) for j in range(2)])
        pss = Rot([S.ps(f'mps{j}', [128, 512]) for j in range(4)])
        ops_ = Rot([S.ps(f'mops{j}', [128, 512]) for j in range(2)])
        xts = Rot([S.sb(f'mx{j}', [128, D]) for j in range(2)])
        blocks = BLOCKS if need_ctx else BLOCKS[1:]
        for (t0, n) in blocks:
            yfl = yf.next(); ybl = yb.next()
            ci = 0
            for (yn, _, nch) in BR:
                S.dma(yfl[:, ci:ci + nch, 0:n], P.dr[yn][:, t0:t0 + n].rearrange("(c p) t -> p c t", p=128), writes=[yfl], q=('sp' if ci % 4 == 0 else 'pool'))
                ci += nch
            S.op('pool', lambda e: e.tensor_copy(out=ybl[:, :, 0:n], in_=yfl[:, :, 0:n]), reads=[yfl], writes=[ybl])
            mT = mTs.next()
            for fc in range(8):
                ci = 0
                ma = macc.next()
                for bi, (_, _, nch) in enumerate(BR):
                    ps = pss.next()
                    for c in range(nch):
                        S.op('pe', lambda e, c=c, ci=ci: e.matmul(ps[:, 0:n], lhsT=wbr[:, ci + c, fc * 128:(fc + 1) * 128], rhs=ybl[:, ci + c, 0:n],
                                                                 start=(c == 0), stop=(c == nch - 1)), reads=[wbr, ybl], writes=[ps])
                    ci += nch
                    gt = gts.next()
                    r0 = O_GT + bi * D + fc * 128
                    S.dma(gt[:, 0:n], projT[r0:r0 + 128, t0:t0 + n], writes=[gt], q=('sp' if bi % 2 == 0 else 'act'))
                    sg = sgs.next()
                    S.op('act', lambda e: e.activation(out=sg[:, 0:n], in_=gt[:, 0:n], func=AF.Sigmoid), reads=[gt], writes=[sg])
                    if bi == 0:
                        S.op('dve', lambda e: e.tensor_tensor(out=ma[:, 0:n], in0=ps[:, 0:n], in1=sg[:, 0:n], op=ALU.mult), reads=[ps, sg], writes=[ma])
                    else:
                        tm = tms.next()
                        S.op('dve', lambda e: e.tensor_tensor(out=tm[:, 0:n], in0=ps[:, 0:n], in1=sg[:, 0:n], op=ALU.mult), reads=[ps, sg], writes=[tm])
                        if bi < 3:
                            S.op('pool', lambda e: e.tensor_tensor(out=ma[:, 0:n], in0=ma[:, 0:n], in1=tm[:, 0:n], op=ALU.add), reads=[ma, tm], writes=[ma])
                        else:
                            S.op('pool', lambda e: e.tensor_tensor(out=mT[:, fc, 0:n], in0=ma[:, 0:n], in1=tm[:, 0:n], op=ALU.add), reads=[ma, tm], writes=[mT])
            s_ = 0 if t0 < TC else 1
            for st_ in range(n // 128):
                xt = xts.next()
                S.dma(xt[:], src[t0 + st_ * 128:t0 + (st_ + 1) * 128, :], writes=[xt])
                for half in range(2):
                    po = ops_.next()
                    for fc in range(8):
                        S.op('pe', lambda e, fc=fc: e.matmul(po[:, :], lhsT=mT[:, fc, st_ * 128:(st_ + 1) * 128], rhs=wout[:, fc, half * 512:(half + 1) * 512],
                                                             start=(fc == 0), stop=(fc == 7)), reads=[mT, wout], writes=[po])
                    tm = tms.next()
                    S.op('dve', lambda e: e.tensor_tensor(out=tm[:, :], in0=po[:, :], in1=g1[:, s_, half * 512:(half + 1) * 512], op=ALU.mult), reads=[po, g1], writes=[tm])
                    S.op('pool', lambda e: e.tensor_tensor(out=xt[:, half * 512:(half + 1) * 512], in0=xt[:, half * 512:(half + 1) * 512], in1=tm[:, :], op=ALU.add),
                         reads=[xt, tm], writes=[xt])
                S.dma(dst[t0 + st_ * 128:t0 + (st_ + 1) * 128, :], xt[:], reads=[xt], q='act')


def declare_merge(P):
    P.inp('w_br_attn', [DEPTH, 512, D]); P.inp('w_br_hyena', [DEPTH, 256, D])
    P.inp('w_br_rwkv', [DEPTH, 256, D]); P.inp('w_br_lru', [DEPTH, 256, D])
    P.inp('w_out', [DEPTH, D, D])
    P.tmp('hyT', [256, T]); P.tmp('rwT', [256, T])
    P.tmp('x_mid', [T, D])


def host_merge(m, inputs):
    for k in ('w_br_attn', 'w_br_hyena', 'w_br_rwkv', 'w_br_lru', 'w_out'):
        m[k] = np.ascontiguousarray(inputs[k], np.float32)


def stage_moe(P, li, need_ctx, src_name, dst_name, dst_is_out=False):
    S = P.S
    src = P.dr[src_name]
    dst = P.dr[dst_name]
    if need_ctx:
        groups = [(0, 10), (10, 22), (22, 34)]
    else:
        groups = [(2, 12), (12, 23), (23, 34)]
    GMAX = 12
    with S.stage():
        ident = S.sb('ident', [128, 128], BF16)
        S.dma(ident[:], P.dr['ident_bf'][:, :], writes=[ident])
        modT = load_modT(P, li)
        g2 = S.sb('g2', [128, 2, D])
        for s_ in range(2):
            S.dma(g2[:, s_, :], P.dr[f'modrow{li}'][s_:s_ + 1, 5 * D:6 * D].partition_broadcast(128), writes=[g2])
        wrf = S.sb('wrf', [128, 8, 20]); wrb = S.sb('wrb', [128, 8, 20], BF16)
        S.dma(wrf[:, :, 0:4], P.dr['moe_w_grp'][li].rearrange("(k p) g -> p k g", p=128), writes=[wrf])
        S.dma(wrf[:, :, 4:20], P.dr['moe_w_rt'][li].rearrange("(k p) g -> p k g", p=128), writes=[wrf])
        S.op('dve', lambda e: e.tensor_copy(out=wrb[:], in_=wrf[:]), reads=[wrf], writes=[wrb])
        rb = S.sb('rb', [128, 20])
        S.dma(rb[:, 0:4], P.dr['moe_b_grp'][li:li + 1, :].partition_broadcast(128), writes=[rb])
        S.dma(rb[:, 4:20], P.dr['moe_b_rt'][li:li + 1, :].partition_broadcast(128), writes=[rb])
        h2T = S.sb('h2T', [128, 8, GMAX * 128], BF16)
        acc = S.sb('acc', [128, GMAX, D])
        comb = S.sb('comb', [128, GMAX, 16])
        rt = {nm: S.sb(f'rt_{nm}', shp) for nm, shp in (('lg', [128, 20]), ('mx', [128, 4]), ('ge', [128, 4]), ('gm', [128, 4]), ('m16', [128, 16]),
                                                         ('ml', [128, 16]), ('eq', [128, 16]), ('ml2', [128, 16]), ('ex', [128, 16]))}
        wst = Rot([S.sb(f'wst{j}', [128, 4, 512]) for j in range(2)])
        w1b = Rot([S.sb(f'w1b{j}', [128, 8, 512], BF16) for j in range(2)])
        w3b = Rot([S.sb(f'w3b{j}', [128, 8, 512], BF16) for j in range(2)])
        w2b = Rot([S.sb(f'w2b{j}', [128, 4, D], BF16) for j in range(2)])
        sil = Rot([S.sb(f'sil{j}', [128, 512]) for j in range(3)])
        actb = Rot([S.sb(f'actb{j}', [128, 4, 512], BF16) for j in range(2)])
        pss = Rot([S.ps(f'eps{j}', [128, 512]) for j in range(4)])
        pys = Rot([S.ps(f'yps{j}', [128, 512]) for j in range(2)])
        prs = Rot([S.ps(f'rps{j}', [128, 32]) for j in range(1)])
        xts = Rot([S.sb(f'ox{j}', [128, D]) for j in range(2)])
        dq = [0]

        def load_cast(dst_tile, dram_view, nk):
            cols = dram_view.shape[2]
            for k0 in range(0, nk, 4):
                for c0 in range(0, cols, 512):
                    st = wst.next()
                    S.dma(st[:, :, :], dram_view[:, k0:k0 + 4, c0:c0 + 512], writes=[st], q=('sp' if dq[0] % 2 == 0 else 'pool'))
                    dq[0] += 1
                    S.op('pool', lambda e, st=st, k0=k0, c0=c0: e.tensor_copy(out=dst_tile[:, k0:k0 + 4, c0:c0 + 512], in_=st[:, :, :]), reads=[st], writes=[dst_tile])

        NC_ = norm_ctx(S, npt=1)
        for (ga, gb) in groups:
            ng = gb - ga
            norm_transpose(P, src, modT, 24, 32, h2T, ident, tiles=range(ga, gb), dst0=ga * 128, C=NC_)
            for ti in range(ng):
                pr = prs.next()
                for k in range(8):
                    S.op('pe', lambda e, k=k: e.matmul(pr[:, 0:20], lhsT=h2T[:, k, ti * 128:(ti + 1) * 128], rhs=wrb[:, k, :], start=(k == 0), stop=(k == 7)),
                         reads=[h2T, wrb], writes=[pr])
                lg, mx, ge, gm, m16, ml, eq, ml2, ex = (rt[n] for n in ('lg', 'mx', 'ge', 'gm', 'm16', 'ml', 'eq', 'ml2', 'ex'))
                V = lambda fn, rd, wr: S.op('dve', fn, reads=rd, writes=wr)
                V(lambda e: e.tensor_tensor(out=lg[:], in0=pr[:, 0:20], in1=rb[:], op=ALU.add), [pr, rb], [lg])
                V(lambda e: e.reduce_max(out=mx[:, 0:1], in_=lg[:, 0:4], axis=AX.X), [lg], [mx])
                V(lambda e: e.tensor_scalar(out=gm[:], in0=lg[:, 0:4], scalar1=mx[:, 0:1], scalar2=None, op0=ALU.is_equal), [lg, mx], [gm])
                V(lambda e: e.tensor_scalar(out=ge[:], in0=lg[:, 0:4], scalar1=mx[:, 0:1], scalar2=None, op0=ALU.subtract), [lg, mx], [ge])
                S.op('act', lambda e: e.activation(out=ge[:], in_=ge[:], func=AF.Exp), reads=[ge], writes=[ge])
                V(lambda e: e.reduce_sum(out=mx[:, 1:2], in_=ge[:], axis=AX.X), [ge], [mx])
                V(lambda e: e.tensor_copy(out=m16[:].rearrange("p (g e) -> p g e", e=4), in_=gm[:].unsqueeze(2).to_broadcast([128, 4, 4])), [gm], [m16])
                V(lambda e: e.tensor_scalar(out=ml[:], in0=m16[:], scalar1=-1.0, scalar2=1e30, op0=ALU.add, op1=ALU.mult), [m16], [ml])
                V(lambda e: e.tensor_tensor(out=ml[:], in0=ml[:], in1=lg[:, 4:20], op=ALU.add), [ml, lg], [ml])
                V(lambda e: e.reduce_max(out=mx[:, 2:3], in_=ml[:], axis=AX.X), [ml], [mx])
                V(lambda e: e.tensor_scalar(out=eq[:], in0=ml[:], scalar1=mx[:, 2:3], scalar2=None, op0=ALU.is_equal), [ml, mx], [eq])
                V(lambda e: e.scalar_tensor_tensor(out=ml2[:], in0=eq[:], scalar=-1e30, in1=ml[:], op0=ALU.mult, op1=ALU.add), [eq, ml], [ml2])
                V(lambda e: e.reduce_max(out=mx[:, 3:4], in_=ml2[:], axis=AX.X), [ml2], [mx])
                V(lambda e: e.scalar_tensor_tensor(out=eq[:], in0=ml2[:], scalar=mx[:, 3:4], in1=eq[:], op0=ALU.is_equal, op1=ALU.add), [ml2, mx, eq], [eq])
                V(lambda e: e.tensor_scalar(out=ex[:], in0=ml[:], scalar1=mx[:, 2:3], scalar2=-80.0, op0=ALU.subtract, op1=ALU.max), [ml, mx], [ex])
                S.op('act', lambda e: e.activation(out=ex[:], in_=ex[:], func=AF.Exp), reads=[ex], writes=[ex])
                V(lambda e: e.tensor_tensor(out=ex[:], in0=ex[:], in1=eq[:], op=ALU.mult), [ex, eq], [ex])
                V(lambda e: e.reduce_sum(out=mx[:, 2:3], in_=ex[:], axis=AX.X), [ex], [mx])
                V(lambda e: e.tensor_tensor(out=mx[:, 2:3], in0=mx[:, 2:3], in1=mx[:, 1:2], op=ALU.mult), [mx], [mx])
                V(lambda e: e.reciprocal(out=mx[:, 2:3], in_=mx[:, 2:3]), [mx], [mx])
                V(lambda e, ti=ti: e.tensor_scalar(out=comb[:, ti, :], in0=ex[:], scalar1=mx[:, 2:3], scalar2=None, op0=ALU.mult), [ex, mx], [comb])
            nt = ng * 128
            tblocks = [(b0, min(512, nt - b0)) for b0 in range(0, nt, 512)]
            for ex_i in range(16):
                w1 = w1b.next(); w3 = w3b.next(); w2 = w2b.next()
                load_cast(w1, P.dr['moe_w1'][li, ex_i].rearrange("(k p) h -> p k h", p=128), 8)
                load_cast(w3, P.dr['moe_w3'][li, ex_i].rearrange("(k p) h -> p k h", p=128), 8)
                load_cast(w2, P.dr['moe_w2'][li, ex_i].rearrange("(k p) f -> p k f", p=128), 4)
                for (b0, n) in tblocks:
                    ab = actb.next()
                    for hc in range(4):
                        p1 = pss.next(); p3 = pss.next()
                        for k in range(8):
                            S.op('pe', lambda e, k=k: e.matmul(p1[:, 0:n], lhsT=w1[:, k, hc * 128:(hc + 1) * 128], rhs=h2T[:, k, b0:b0 + n], start=(k == 0), stop=(k == 7)),
                                 reads=[w1, h2T], writes=[p1])
                        for k in range(8):
                            S.op('pe', lambda e, k=k: e.matmul(p3[:, 0:n], lhsT=w3[:, k, hc * 128:(hc + 1) * 128], rhs=h2T[:, k, b0:b0 + n], start=(k == 0), stop=(k == 7)),
                                 reads=[w3, h2T], writes=[p3])
                        sl = sil.next()
                        S.op('act', lambda e: e.activation(out=sl[:, 0:n], in_=p1[:, 0:n], func=AF.Silu), reads=[p1], writes=[sl])
                        S.op('dve', lambda e, hc=hc: e.tensor_tensor(out=ab[:, hc, 0:n], in0=sl[:, 0:n], in1=p3[:, 0:n], op=ALU.mult), reads=[sl, p3], writes=[ab])
                    for st_ in range(n // 128):
                        ti = b0 // 128 + st_
                        for half in range(2):
                            py = pys.next()
                            for hc in range(4):
                                S.op('pe', lambda e, hc=hc: e.matmul(py[:, :], lhsT=ab[:, hc, st_ * 128:(st_ + 1) * 128], rhs=w2[:, hc, half * 512:(half + 1) * 512],
                                                                     start=(hc == 0), stop=(hc == 3)), reads=[ab, w2], writes=[py])
                            a_sl = acc[:, ti, half * 512:(half + 1) * 512]
                            if ex_i == 0:
                                S.op('dve', lambda e: e.tensor_scalar(out=a_sl, in0=py[:, :], scalar1=comb[:, ti, ex_i:ex_i + 1], scalar2=None, op0=ALU.mult),
                                     reads=[py, comb], writes=[acc])
                            else:
                                S.op('dve', lambda e: e.scalar_tensor_tensor(out=a_sl, in0=py[:, :], scalar=comb[:, ti, ex_i:ex_i + 1], in1=a_sl, op0=ALU.mult, op1=ALU.add),
                                     reads=[py, comb, acc], writes=[acc])
            for ti in range(ng):
                gt = ga + ti
                s_ = 0 if gt < 2 else 1
                xt = xts.next()
                S.dma(xt[:], src[gt * 128:(gt + 1) * 128, :], writes=[xt])
                S.op('pool', lambda e: e.tensor_tensor(out=acc[:, ti, :], in0=acc[:, ti, :], in1=g2[:, s_, :], op=ALU.mult), reads=[acc, g2], writes=[acc])
                S.op('dve', lambda e: e.tensor_tensor(out=xt[:], in0=xt[:], in1=acc[:, ti, :], op=ALU.add), reads=[xt, acc], writes=[xt])
                if dst_is_out:
                    if gt >= 2:
                        S.dma(dst[(gt - 2) * 128:(gt - 1) * 128, :], xt[:], reads=[xt], q='act', is_out=True)
                else:
                    S.dma(dst[gt * 128:(gt + 1) * 128, :], xt[:], reads=[xt], q='act')


def declare_moe(P):
    P.inp('moe_w_grp', [DEPTH, D, 4]); P.inp('moe_b_grp', [DEPTH, 4])
    P.inp('moe_w_rt', [DEPTH, D, 16]); P.inp('moe_b_rt', [DEPTH, 16])
    P.inp('moe_w1', [DEPTH, 16, D, 512]); P.inp('moe_w3', [DEPTH, 16, D, 512]); P.inp('moe_w2', [DEPTH, 16, 512, D])
    P.tmp('x_l0', [T, D])


def host_moe(m, inputs):
    for k in ('moe_w_grp', 'moe_b_grp', 'moe_w_rt', 'moe_b_rt', 'moe_w1', 'moe_w3', 'moe_w2'):
        m[k] = np.ascontiguousarray(inputs[k], np.float32)


def hyena_consts(n):
    import ml_dtypes
    t = np.arange(n, dtype=np.float32) / np.float32(n)
    bands = np.arange(1, 17, dtype=np.float32)
    ang = (np.float32(2.0 * math.pi) * t[:, None] * bands).astype(np.float32)
    feat = np.concatenate([t[:, None], np.sin(ang), np.cos(ang)], -1).astype(np.float32)
    deltas = np.linspace(-math.log(1e-2) / 1.5, -math.log(1e-2) / 0.3, 256, dtype=np.float32)
    dec = np.exp(-t[:, None] * deltas).astype(np.float32)
    nk = n // 128 + 1
    NP = nk * 128
    idx = np.arange(NP, dtype=np.int64)
    prod = (idx[:, None] * idx[None, :]) % (2 * n)
    angw = 2.0 * math.pi * prod.astype(np.float64) / (2 * n)
    valid = (idx <= n)
    m = (valid[:, None] & valid[None, :])
    wc = np.where(m, np.cos(angw), 0.0); ws = np.where(m, np.sin(angw), 0.0)
    def tile(w):
        return np.ascontiguousarray(w.reshape(nk, 128, nk, 128).transpose(2, 1, 0, 3)).astype(ml_dtypes.bfloat16)
    wk = np.full(NP, 1.0 / n, np.float32); wk[0] = 0.5 / n; wk[n] = 0.5 / n; wk[n + 1:] = 0.0
    wkT = np.ascontiguousarray(wk.reshape(nk, 128).T)
    return dict(featT=np.ascontiguousarray(feat.T), dec=dec, wc=tile(wc), ws=tile(ws), wk=wkT)


def hyena_seq(P, li, n, t_off, sfx):
    S = P.S
    projT = P.dr['projT']
    hyT = P.dr['hyT']
    hyz = P.dr['hyz']
    hspec = P.dr['hspec']
    NT = n // 128
    NK = NT + 1
    wc_d = P.dr['hy_wc' + sfx]; ws_d = P.dr['hy_ws' + sfx]
    kparts = [128] * NT + [1]
    TWO_PI = 2.0 * math.pi
    with S.stage():
        xin = Rot([S.sb(f'hxin{j}', [128, n]) for j in range(2)])
        zo = Rot([S.sb(f'hzo{j}', [128, n]) for j in range(2)])
        cw = S.sb('hcw', [128, 6, 3]); cb = S.sb('hcb', [128, 6])
        for j in range(3):
            S.dma(cw[:, :, j], P.dr['hy_conv_w'][li, j].rearrange("(c p) -> p c", p=128), writes=[cw], allow_slow_non_contiguous=True)
        S.dma(cb[:], P.dr['hy_conv_b'][li].rearrange("(c p) -> p c", p=128), writes=[cb], allow_slow_non_contiguous=True)
        for c in range(6):
            xi = xin.next(); z = zo.next()
            S.dma(xi[:], projT[O_HY + c * 128:O_HY + (c + 1) * 128, t_off:t_off + n], writes=[xi], q=('sp' if c % 2 == 0 else 'pool'))
            dwconv_fm(S, z, xi, cw[:, c, :], cb[:, c:c + 1], 3, 1, [(0, n)], eng='dve', wbuf=cw)
            S.dma(hyz[c * 128:(c + 1) * 128, t_off:t_off + n], z[:], reads=[z], q='act')
    with S.stage():
        featT = S.sb('featT', [33, n]); f1 = S.sb('f1', [33, 64]); f2 = S.sb('f2', [64, 64]); f3 = S.sb('f3', [64, 1024])
        fb = S.sb('fb', [64, 2]); h1T = S.sb('h1T', [64, n]); h2T = S.sb('h2T', [64, n])
        S.dma(featT[:], P.dr['hy_featT' + sfx][:, :], writes=[featT])
        S.dma(f1[:], P.dr['hy_f1'][li], writes=[f1]); S.dma(f2[:], P.dr['hy_f2'][li], writes=[f2]); S.dma(f3[:], P.dr['hy_f3'][li], writes=[f3])
        S.dma(fb[:, 0:1], P.dr['hy_fb1'][li].rearrange("(c o) -> c o", o=1), writes=[fb])
        S.dma(fb[:, 1:2], P.dr['hy_fb2'][li].rearrange("(c o) -> c o", o=1), writes=[fb])
        pss = Rot([S.ps(f'hps{j}', [128, 512]) for j in range(4)])
        tmpf = Rot([S.sb(f'htmp{j}', [64, 512]) for j in range(2)])
        tmpq = Rot([S.sb(f'htmq{j}', [64, 512]) for j in range(2)])
        tmpi = Rot([S.sb(f'htmi{j}', [64, 512], mybir.dt.int32) for j in range(2)])
        for (src, wt, dstT, bi) in ((featT, f1, h1T, 0), (h1T, f2, h2T, 1)):
            for b0 in range(0, n, 512):
                nb = min(512, n - b0)
                ps = pss.next()
                S.op('pe', lambda e: e.matmul(ps[0:64, 0:nb], lhsT=wt[:], rhs=src[:, b0:b0 + nb], start=True, stop=True), reads=[wt, src], writes=[ps])
                tm = tmpf.next()
                qi = tmpi.next(); qf = tmpq.next()
                S.op('dve', lambda e: e.tensor_scalar(out=tm[:, 0:nb], in0=ps[0:64, 0:nb], scalar1=fb[:, bi:bi + 1], scalar2=None, op0=ALU.add), reads=[ps, fb], writes=[tm])
                S.op('dve', lambda e: e.tensor_scalar(out=qf[:, 0:nb], in0=tm[:, 0:nb], scalar1=1.0 / TWO_PI, scalar2=None, op0=ALU.mult), reads=[tm], writes=[qf])
                S.op('dve', lambda e: e.tensor_copy(out=qi[:, 0:nb], in_=qf[:, 0:nb]), reads=[qf], writes=[qi])
                S.op('dve', lambda e: e.tensor_copy(out=qf[:, 0:nb], in_=qi[:, 0:nb]), reads=[qi], writes=[qf])
                S.op('dve', lambda e: e.scalar_tensor_tensor(out=tm[:, 0:nb], in0=qf[:, 0:nb], scalar=-TWO_PI, in1=tm[:, 0:nb], op0=ALU.mult, op1=ALU.add), reads=[qf, tm], writes=[tm])
                S.op('dve', lambda e: e.tensor_scalar(out=qf[:, 0:nb], in0=tm[:, 0:nb], scalar1=math.pi, scalar2=-TWO_PI, op0=ALU.is_gt, op1=ALU.mult), reads=[tm], writes=[qf])
                S.op('dve', lambda e: e.tensor_tensor(out=tm[:, 0:nb], in0=tm[:, 0:nb], in1=qf[:, 0:nb], op=ALU.add), reads=[tm, qf], writes=[tm])
                S.op('dve', lambda e: e.tensor_scalar(out=qf[:, 0:nb], in0=tm[:, 0:nb], scalar1=-math.pi, scalar2=TWO_PI, op0=ALU.is_lt, op1=ALU.mult), reads=[tm], writes=[qf])
                S.op('dve', lambda e: e.tensor_tensor(out=tm[:, 0:nb], in0=tm[:, 0:nb], in1=qf[:, 0:nb], op=ALU.add), reads=[tm, qf], writes=[tm])
                S.op('act', lambda e: e.activation(out=dstT[:, b0:b0 + nb], in_=tm[:, 0:nb], func=AF.Sin), reads=[tm], writes=[dstT])
        hbf = S.sb('hbf', [128, NT, 1024], BF16)
        ones = S.sb('hones', [128, 128])
        S.op('pool', lambda e: e.memset(ones[:], 1.0), writes=[ones])
        decs = Rot([S.sb(f'hdec{j}', [128, 256]) for j in range(2)])
        hraw = Rot([S.sb(f'hraw{j}', [128, 1024]) for j in range(2)])
        habs = Rot([S.sb(f'habs{j}', [128, 1024]) for j in range(2)])
        l1ps = [S.ps(f'l1ps{j}', [128, 512]) for j in range(2)]
        for tt in range(NT):
            dc = decs.next()
            S.dma(dc[:], P.dr['hy_dec' + sfx][tt * 128:(tt + 1) * 128, :], writes=[dc])
            hr = hraw.next(); ha = habs.next()
            for half in range(2):
                ps = pss.next()
                S.op('pe', lambda e: e.matmul(ps[:, :], lhsT=h2T[:, tt * 128:(tt + 1) * 128], rhs=f3[:, half * 512:(half + 1) * 512], start=True, stop=True),
                     reads=[h2T, f3], writes=[ps])
                S.op('dve', lambda e: e.tensor_tensor(out=hr[:, half * 512:(half + 1) * 512].rearrange("p (a c) -> p a c", a=2),
                                                      in0=ps[:, :].rearrange("p (a c) -> p a c", a=2),
                                                      in1=dc[:].unsqueeze(1).to_broadcast([128, 2, 256]), op=ALU.mult), reads=[ps, dc], writes=[hr])
            S.op('act', lambda e: e.activation(out=ha[:], in_=hr[:], func=AF.Abs), reads=[hr], writes=[ha])
            for half in range(2):
                S.op('pe', lambda e: e.matmul(l1ps[half][:, :], lhsT=ones[:], rhs=ha[:, half * 512:(half + 1) * 512], start=(tt == 0), stop=(tt == NT - 1)),
                     reads=[ones, ha], writes=[l1ps[half]])
            if tt == 0:
                for o in range(2):
                    S.op('dve', lambda e, o=o: e.memset(hr[0:1, o * 512 + 256:o * 512 + 512], 0.0), reads=[ha], writes=[hr])
            S.op('act', lambda e: e.copy(out=hbf[:, tt, :], in_=hr[:]), reads=[hr], writes=[hbf])
        rl1 = S.sb('rl1', [128, 2, 256])
        l1sb = S.sb('l1sb', [128, 2, 256])
        for o in range(2):
            S.op('act', lambda e, o=o: e.copy(out=l1sb[:, o, :], in_=l1ps[o][:, 0:256]), reads=[l1ps[o]], writes=[l1sb])
            S.op('dve', lambda e, o=o: e.tensor_tensor(out=rl1[:, o, :], in0=l1sb[:, o, :], in1=l1ps[o][:, 256:512], op=ALU.add), reads=[l1ps[o], l1sb], writes=[rl1])
        S.op('dve', lambda e: e.reciprocal(out=rl1[:], in_=rl1[:]), reads=[rl1], writes=[rl1])
        wk = S.sb('hwk', [128, NK])
        S.dma(wk[:], P.dr['hy_wk' + sfx][:, :], writes=[wk])
        wcs = Rot([S.sb(f'hwc{j}', [128, NK, 128], BF16) for j in range(2)])
        wss = Rot([S.sb(f'hws{j}', [128, NK, 128], BF16) for j in range(2)])
        spo = Rot([S.sb(f'hspo{j}', [128, 2, 2, 256]) for j in range(2)])
        for kt in range(NK):
            kp = kparts[kt]
            wct = wcs.next(); wst = wss.next()
            S.dma(wct[:], wc_d[kt], writes=[wct]); S.dma(wst[:], ws_d[kt], writes=[wst], q='pool')
            so = spo.next()
            pa = [pss.next(), pss.next()]
            for half in range(2):
                for tc in range(NT):
                    S.op('pe', lambda e, tc=tc: e.matmul(pa[half][0:kp, :], lhsT=wct[:, tc, 0:kp], rhs=hbf[:, tc, half * 512:(half + 1) * 512],
                                                         start=(tc == 0), stop=(tc == NT - 1)), reads=[wct, hbf], writes=[pa[half]])
            for o in range(2):
                S.op('act', lambda e, o=o: e.copy(out=so[0:kp, 0, o, :], in_=pa[o][0:kp, 0:256]), reads=[pa[o]], writes=[so])
                S.op('dve', lambda e, o=o: e.tensor_tensor(out=so[0:kp, 0, o, :], in0=so[0:kp, 0, o, :], in1=pa[o][0:kp, 256:512], op=ALU.add), reads=[pa[o], so], writes=[so])
            pb = [pss.next(), pss.next()]
            for half in range(2):
                for tc in range(NT):
                    S.op('pe', lambda e, tc=tc: e.matmul(pb[half][0:kp, :], lhsT=wst[:, tc, 0:kp], rhs=hbf[:, tc, half * 512:(half + 1) * 512],
                                                         start=(tc == 0), stop=(tc == NT - 1)), reads=[wst, hbf], writes=[pb[half]])
            for o in range(2):
                S.op('act', lambda e, o=o: e.copy(out=so[0:kp, 1, o, :], in_=pb[o][0:kp, 256:512]), reads=[pb[o]], writes=[so])
                S.op('dve', lambda e, o=o: e.tensor_tensor(out=so[0:kp, 1, o, :], in0=so[0:kp, 1, o, :], in1=pb[o][0:kp, 0:256], op=ALU.subtract), reads=[pb[o], so], writes=[so])
            for ri in range(2):
                S.op('dve', lambda e, ri=ri: e.scalar_tensor_tensor(out=so[0:kp, ri, :, :], in0=so[0:kp, ri, :, :], scalar=wk[0:kp, kt:kt + 1], in1=rl1[0:kp, :, :],
                                                                    op0=ALU.mult, op1=ALU.mult), reads=[so, wk, rl1], writes=[so])
            S.dma(hspec[kt, 0:kp].rearrange("p a o c -> p (a o c)"), so[0:kp].rearrange("p a o c -> p (a o c)"), reads=[so], q='act')
    with S.stage():
        identf = S.sb('identf', [128, 128])
        S.dma(identf[:], P.dr['ident_f'][:, :], writes=[identf])
        skip = S.sb('hskip', [128, 2, 256])
        for o in range(2):
            S.dma(skip[:, o, :], P.dr['hy_skip'][li, o:o + 1, :].partition_broadcast(128), writes=[skip])
        zf = S.sb('zf', [128, NT, 256]); zb = S.sb('zb', [128, NT, 256], BF16)
        y1 = S.sb('y1', [128, NT, 256])
        Pq = S.sb('Pq', [128, NK, 256], BF16); Qq = S.sb('Qq', [128, NK, 256], BF16)
        fms = Rot([S.sb(f'hfm{j}', [128, 128]) for j in range(4)])
        tps = Rot([S.ps(f'htp{j}', [128, 256]) for j in range(2)])
        pss = Rot([S.ps(f'hsp{j}', [128, 256]) for j in range(4)])
        wcs = Rot([S.sb(f'hwc{j}', [128, NK, 128], BF16) for j in range(2)])
        wss = Rot([S.sb(f'hws{j}', [128, NK, 128], BF16) for j in range(2)])
        hsp = Rot([S.sb(f'hsp_{j}', [128, 2, 2, 256]) for j in range(2)])
        tA = Rot([S.sb(f'htA{j}', [128, 256]) for j in range(2)]); tB = Rot([S.sb(f'htB{j}', [128, 256]) for j in range(2)])
        gtm = Rot([S.sb(f'hgt{j}', [128, 256]) for j in range(2)])
        yo = Rot([S.sb(f'hyo{j}', [128, 256]) for j in range(2)])
        ofm = Rot([S.sb(f'hofm{j}', [128, 128]) for j in range(2)])

        def to_tm(row0, tt, dst_ap, dstbuf):
            tp = tps.next()
            for c in range(2):
                fm = fms.next()
                S.dma(fm[:], hyz[row0 + c * 128:row0 + (c + 1) * 128, t_off + tt * 128:t_off + (tt + 1) * 128], writes=[fm], q=('sp' if c == 0 else 'pool'))
                S.op('pe', lambda e, c=c: e.transpose(out=tp[:, c * 128:(c + 1) * 128], in_=fm[:], identity=identf[:]), reads=[fm, identf], writes=[tp])
            S.op('act', lambda e: e.copy(out=dst_ap, in_=tp[:, :]), reads=[tp], writes=[dstbuf])

        for tt in range(NT):
            to_tm(0, tt, zf[:, tt, :], zf)
        S.op('pool', lambda e: e.tensor_copy(out=zb[:], in_=zf[:]), reads=[zf], writes=[zb])
        for o in range(2):
            zin_f = zf if o == 0 else y1
            for kt in range(NK):
                kp = kparts[kt]
                wct = wcs.next(); wst = wss.next()
                S.dma(wct[:], wc_d[kt], writes=[wct]); S.dma(wst[:], ws_d[kt], writes=[wst], q='pool')
                hs = hsp.next()
                S.dma(hs[0:kp].rearrange("p a o c -> p (a o c)"), hspec[kt, 0:kp].rearrange("p a o c -> p (a o c)"), writes=[hs], q='act')
                pa = pss.next(); pb = pss.next()
                for tc in range(NT):
                    S.op('pe', lambda e, tc=tc: e.matmul(pa[0:kp, :], lhsT=wct[:, tc, 0:kp], rhs=zb[:, tc, :], start=(tc == 0), stop=(tc == NT - 1)), reads=[wct, zb], writes=[pa])
                for tc in range(NT):
                    S.op('pe', lambda e, tc=tc: e.matmul(pb[0:kp, :], lhsT=wst[:, tc, 0:kp], rhs=zb[:, tc, :], start=(tc == 0), stop=(tc == NT - 1)), reads=[wst, zb], writes=[pb])
                a_ = tA.next(); b_ = tB.next()
                S.op('dve', lambda e: e.tensor_tensor(out=a_[0:kp], in0=pa[0:kp, :], in1=hs[0:kp, 0, o, :], op=ALU.mult), reads=[pa, hs], writes=[a_])
                S.op('dve', lambda e: e.tensor_tensor(out=b_[0:kp], in0=pb[0:kp, :], in1=hs[0:kp, 1, o, :], op=ALU.mult), reads=[pb, hs], writes=[b_])
                S.op('pool', lambda e: e.tensor_tensor(out=Pq[0:kp, kt, :], in0=a_[0:kp], in1=b_[0:kp], op=ALU.add), reads=[a_, b_], writes=[Pq])
                a2 = tA.next(); b2 = tB.next()
                S.op('dve', lambda e: e.tensor_tensor(out=b2[0:kp], in0=pb[0:kp, :], in1=hs[0:kp, 0, o, :], op=ALU.mult), reads=[pb, hs], writes=[b2])
                S.op('dve', lambda e: e.tensor_tensor(out=a2[0:kp], in0=pa[0:kp, :], in1=hs[0:kp, 1, o, :], op=ALU.mult), reads=[pa, hs], writes=[a2])
                S.op('pool', lambda e: e.tensor_tensor(out=Qq[0:kp, kt, :], in0=b2[0:kp], in1=a2[0:kp], op=ALU.subtract), reads=[a2, b2], writes=[Qq])
            for tt in range(NT):
                wct = wcs.next(); wst = wss.next()
                S.dma(wct[:], wc_d[tt], writes=[wct]); S.dma(wst[:], ws_d[tt], writes=[wst], q='pool')
                py = pss.next()
                for kc in range(NK):
                    kp = kparts[kc]
                    S.op('pe', lambda e, kc=kc, kp=kp: e.matmul(py[:, :], lhsT=wct[0:kp, kc, :], rhs=Pq[0:kp, kc, :], start=(kc == 0), stop=False), reads=[wct, Pq], writes=[py])
                    S.op('pe', lambda e, kc=kc, kp=kp: e.matmul(py[:, :], lhsT=wst[0:kp, kc, :], rhs=Qq[0:kp, kc, :], start=False, stop=(kc == NK - 1)), reads=[wst, Qq], writes=[py])
                g = gtm.next()
                to_tm(256 * (o + 1), tt, g[:, :], g)
                yy = yo.next()
                S.op('dve', lambda e: e.tensor_tensor(out=yy[:], in0=zin_f[:, tt, :], in1=skip[:, o, :], op=ALU.mult), reads=[zin_f, skip], writes=[yy])
                S.op('dve', lambda e: e.tensor_tensor(out=yy[:], in0=yy[:], in1=py[:, :], op=ALU.add), reads=[yy, py], writes=[yy])
                if o == 0:
                    S.op('pool', lambda e: e.tensor_tensor(out=y1[:, tt, :], in0=yy[:], in1=g[:], op=ALU.mult), reads=[yy, g], writes=[y1])
                else:
                    S.op('pool', lambda e: e.tensor_tensor(out=yy[:], in0=yy[:], in1=g[:], op=ALU.mult), reads=[yy, g], writes=[yy])
                    for c in range(2):
                        tp = tps.next()
                        S.op('pe', lambda e, c=c: e.transpose(out=tp[:, 0:128], in_=yy[:, c * 128:(c + 1) * 128], identity=identf[:]), reads=[yy, identf], writes=[tp])
                        of = ofm.next()
                        S.op('act', lambda e: e.copy(out=of[:], in_=tp[:, 0:128]), reads=[tp], writes=[of])
                        S.dma(hyT[c * 128:(c + 1) * 128, t_off + tt * 128:t_off + (tt + 1) * 128], of[:], reads=[of], q='act')
            if o == 0:
                S.op('pool', lambda e: e.tensor_copy(out=zb[:], in_=y1[:]), reads=[y1], writes=[zb])


def stage_hyena(P, li, need_ctx):
    hyena_seq(P, li, TX, TC, '')
    if need_ctx:
        hyena_seq(P, li, TC, 0, '_c')


def declare_hyena(P):
    P.inp('hy_conv_w', [DEPTH, 3, 768]); P.inp('hy_conv_b', [DEPTH, 768])
    P.inp('hy_f1', [DEPTH, 33, 64]); P.inp('hy_fb1', [DEPTH, 64]); P.inp('hy_f2', [DEPTH, 64, 64]); P.inp('hy_fb2', [DEPTH, 64])
    P.inp('hy_f3', [DEPTH, 64, 1024]); P.inp('hy_skip', [DEPTH, 2, 256])
    P.inp('ident_f', [128, 128])
    for sfx, n in (('', TX), ('_c', TC)):
        nk = n // 128 + 1
        P.inp('hy_featT' + sfx, [33, n]); P.inp('hy_dec' + sfx, [n, 256])
        P.inp('hy_wc' + sfx, [nk, 128, nk, 128], BF16); P.inp('hy_ws' + sfx, [nk, 128, nk, 128], BF16)
        P.inp('hy_wk' + sfx, [128, nk])
    P.tmp('hyz', [768, T])
    P.tmp('hspec', [33, 128, 2, 2, 256])


_HC = {}


def host_hyena(m, inputs):
    for k in ('hy_conv_w', 'hy_conv_b', 'hy_f1', 'hy_fb1', 'hy_f2', 'hy_fb2', 'hy_f3', 'hy_skip'):
        m[k] = np.ascontiguousarray(inputs[k], np.float32)
    m['ident_f'] = np.eye(128, dtype=np.float32)
    for sfx, n in (('', TX), ('_c', TC)):
        if n not in _HC:
            _HC[n] = hyena_consts(n)
        c = _HC[n]
        m['hy_featT' + sfx] = c['featT']; m['hy_dec' + sfx] = c['dec']
        m['hy_wc' + sfx] = c['wc']; m['hy_ws' + sfx] = c['ws']; m['hy_wk' + sfx] = c['wk']


Q_R, Q_V, Q_KK, Q_KD0, Q_KD1, Q_A0, Q_A1, Q_LD0, Q_LD1, Q_G, Q_BON = range(11)
SEGS2 = [(0, TC), (TC, TX)]


def stage_rwkv_prep(P, li):
    S = P.S
    projT = P.dr['projT']
    rwq = P.dr['rwq']
    with S.stage():
        mu = S.sb('mu', [128, 9, 3])
        for j in range(2):
            S.dma(mu[:, :, 2 * j], P.dr['rw_mu'][li, j].rearrange("(c p) -> p c", p=128), writes=[mu], allow_slow_non_contiguous=True)
        S.op('dve', lambda e: e.tensor_tensor(out=mu[:, :, 1], in0=mu[:, :, 0], in1=mu[:, :, 2], op=ALU.add), reads=[mu], writes=[mu])
        S.op('dve', lambda e: e.tensor_scalar(out=mu[:, :, 1], in0=mu[:, :, 1], scalar1=-1.0, scalar2=1.0, op0=ALU.mult, op1=ALU.add), reads=[mu], writes=[mu])
        raw = Rot([S.sb(f'rraw{j}', [128, T]) for j in range(2)])
        blk = S.sb('rblk', [128, 128])
        S.dma(blk[:], P.dr['blk64'][:, :], writes=[blk])
        pss = Rot([S.ps(f'rps{j}', [128, 512]) for j in range(4)])

        def shiftmix(c, dst):
            rw_ = raw.next()
            S.dma(rw_[:], projT[O_RW + c * 128:O_RW + (c + 1) * 128, :], writes=[rw_], q=('sp' if c % 2 == 0 else 'pool'))
            dwconv_fm(S, dst, rw_, mu[:, c, :], None, 3, 1, SEGS2, wbuf=mu)

        with S.stage():
            w1s = S.sb('w1s', [128, T]); a1s = S.sb('a1s', [128, T]); g1s = S.sb('g1s', [128, T])
            shiftmix(6, w1s); shiftmix(7, a1s); shiftmix(8, g1s)
            S.op('act', lambda e: e.activation(out=w1s[:], in_=w1s[:], func=AF.Tanh), reads=[w1s], writes=[w1s])
            S.op('act', lambda e: e.activation(out=g1s[:], in_=g1s[:], func=AF.Sigmoid), reads=[g1s], writes=[g1s])
            w2t = S.sb('w2t', [128, 256]); a2t = S.sb('a2t', [128, 256]); g2t = S.sb('g2t', [128, 256])
            S.dma(w2t[:], P.dr['rw_w2'][li].rearrange("d r c -> (d r) c"), writes=[w2t])
            S.dma(a2t[:], P.dr['rw_a2'][li].rearrange("d r c -> (d r) c"), writes=[a2t])
            S.dma(g2t[:], P.dr['rw_g2'][li], writes=[g2t])
            w0 = S.sb('w0', [128, 2, 2]); a0 = S.sb('a0', [128, 2, 2])
            for d in range(2):
                S.dma(w0[:, d, :], P.dr['rw_w0'][li, d].rearrange("(h p) -> p h", p=128), writes=[w0], allow_slow_non_contiguous=True)
                S.dma(a0[:, d, :], P.dr['rw_a0'][li, d].rearrange("(h p) -> p h", p=128), writes=[a0], allow_slow_non_contiguous=True)
            outs = Rot([S.sb(f'rout{j}', [128, 512]) for j in range(4)])
            for hp in range(2):
                cs = slice(hp * 128, (hp + 1) * 128)
                for (t0, n) in BLOCKS:
                    for d in range(2):
                        ps = pss.next()
                        S.op('pe', lambda e, d=d: e.matmul(ps[:, 0:n], lhsT=w2t[64 * d:64 * d + 64, cs], rhs=w1s[64 * d:64 * d + 64, t0:t0 + n], start=True, stop=True),
                             reads=[w2t, w1s], writes=[ps])
                        o = outs.next()
                        S.op('act', lambda e, d=d: e.activation(out=o[:, 0:n], in_=ps[:, 0:n], func=AF.Sigmoid, bias=w0[:, d, hp:hp + 1]), reads=[ps, w0], writes=[o])
                        S.op('pool', lambda e: e.tensor_scalar(out=o[:, 0:n], in0=o[:, 0:n], scalar1=-0.6065306597126334, scalar2=None, op0=ALU.mult), reads=[o], writes=[o])
                        S.dma(rwq[Q_LD0 + d, cs, t0:t0 + n], o[:, 0:n], reads=[o], q='act')
                        ps = pss.next()
                        S.op('pe', lambda e, d=d: e.matmul(ps[:, 0:n], lhsT=a2t[64 * d:64 * d + 64, cs], rhs=a1s[64 * d:64 * d + 64, t0:t0 + n], start=True, stop=True),
                             reads=[a2t, a1s], writes=[ps])
                        o = outs.next()
                        S.op('act', lambda e, d=d: e.activation(out=o[:, 0:n], in_=ps[:, 0:n], func=AF.Sigmoid, bias=a0[:, d, hp:hp + 1]), reads=[ps, a0], writes=[o])
                        S.dma(rwq[Q_A0 + d, cs, t0:t0 + n], o[:, 0:n], reads=[o], q='act')
                    ps = pss.next()
                    S.op('pe', lambda e: e.matmul(ps[:, 0:n], lhsT=g2t[:, cs], rhs=g1s[:, t0:t0 + n], start=True, stop=True), reads=[g2t, g1s], writes=[ps])
                    o = outs.next()
                    S.op('dve', lambda e: e.tensor_copy(out=o[:, 0:n], in_=ps[:, 0:n]), reads=[ps], writes=[o])
                    S.dma(rwq[Q_G, cs, t0:t0 + n], o[:, 0:n], reads=[o], q='act')
        with S.stage():
            cols = S.sb('rcols', [128, 2, 4])
            for i, nm in enumerate(('rw_k_k', 'rw_k_a')):
                S.dma(cols[:, :, i], P.dr[nm][li].rearrange("(h p) -> p h", p=128), writes=[cols], allow_slow_non_contiguous=True)
            S.dma(cols[:, :, 3], P.dr['rw_r_k'][li].rearrange("h n -> (h n)").rearrange("(h p) -> p h", p=128), writes=[cols], allow_slow_non_contiguous=True)
            S.op('dve', lambda e: e.tensor_scalar(out=cols[:, :, 2], in0=cols[:, :, 1], scalar1=-1.0, scalar2=1.0, op0=ALU.mult, op1=ALU.add), reads=[cols], writes=[cols])
            rs_ = S.sb('r_s', [128, T]); ks_ = S.sb('k_s', [128, T]); vs_ = S.sb('v_s', [128, T])
            kk = S.sb('kk', [128, T]); ad = S.sb('ad', [128, T]); kd = S.sb('kd', [128, T]); bon = S.sb('bon', [128, T])
            tmp = Rot([S.sb(f'rtmp{j}', [128, 512]) for j in range(3)])
            for hp in range(2):
                cs = slice(hp * 128, (hp + 1) * 128)
                shiftmix(0 + hp, rs_); shiftmix(2 + hp, ks_); shiftmix(4 + hp, vs_)
                S.dma(rwq[Q_R, cs, :], rs_[:], reads=[rs_], q='act')
                S.dma(rwq[Q_V, cs, :], vs_[:], reads=[vs_], q='act')
                S.op('dve', lambda e: e.tensor_scalar(out=kk[:], in0=ks_[:], scalar1=cols[:, hp, 0:1], scalar2=None, op0=ALU.mult), reads=[ks_, cols], writes=[kk])
                for (t0, n) in BLOCKS:
                    sq = tmp.next()
                    S.op('act', lambda e: e.activation(out=sq[:, 0:n], in_=kk[:, t0:t0 + n], func=AF.Square), reads=[kk], writes=[sq])
                    ps = pss.next()
                    S.op('pe', lambda e: e.matmul(ps[:, 0:n], lhsT=blk[:], rhs=sq[:, 0:n], start=True, stop=True), reads=[blk, sq], writes=[ps])
                    rs2 = tmp.next()
                    S.op('dve', lambda e: e.tensor_scalar(out=rs2[:, 0:n], in0=ps[:, 0:n], scalar1=64.0, scalar2=1e-12, op0=ALU.mult, op1=ALU.add), reads=[ps], writes=[rs2])
                    S.op('act', lambda e: e.activation(out=rs2[:, 0:n], in_=rs2[:, 0:n], func=AF.Sqrt), reads=[rs2], writes=[rs2])
                    S.op('dve', lambda e: e.reciprocal(out=rs2[:, 0:n], in_=rs2[:, 0:n]), reads=[rs2], writes=[rs2])
                    S.op('dve', lambda e: e.tensor_tensor(out=kk[:, t0:t0 + n], in0=kk[:, t0:t0 + n], in1=rs2[:, 0:n], op=ALU.mult), reads=[kk, rs2], writes=[kk])
                S.dma(rwq[Q_KK, cs, :], kk[:], reads=[kk], q='act')
                for d in range(2):
                    S.dma(ad[:], rwq[Q_A0 + d, cs, :], writes=[ad])
                    S.op('dve', lambda e: e.tensor_scalar(out=kd[:], in0=ad[:], scalar1=cols[:, hp, 1:2], scalar2=cols[:, hp, 2:3], op0=ALU.mult, op1=ALU.add),
                         reads=[ad, cols], writes=[kd])
                    S.op('pool', lambda e: e.tensor_tensor(out=kd[:], in0=kd[:], in1=ks_[:], op=ALU.mult), reads=[kd, ks_], writes=[kd])
                    S.dma(rwq[Q_KD0 + d, cs, :], kd[:], reads=[kd], q='act')
                    for (t0, n) in BLOCKS:
                        rk = tmp.next()
                        S.op('dve', lambda e: e.scalar_tensor_tensor(out=rk[:, 0:n], in0=rs_[:, t0:t0 + n], scalar=cols[:, hp, 3:4], in1=kd[:, t0:t0 + n], op0=ALU.mult, op1=ALU.mult),
                             reads=[rs_, cols, kd], writes=[rk])
                        ps = pss.next()
                        S.op('pe', lambda e: e.matmul(ps[:, 0:n], lhsT=blk[:], rhs=rk[:, 0:n], start=True, stop=True), reads=[blk, rk], writes=[ps])
                        if d == 0:
                            S.op('dve', lambda e: e.scalar_tensor_tensor(out=bon[:, t0:t0 + n], in0=ps[:, 0:n], scalar=64.0, in1=vs_[:, t0:t0 + n], op0=ALU.mult, op1=ALU.mult),
                                 reads=[ps, vs_], writes=[bon])
                        else:
                            b2 = tmp.next()
                            S.op('dve', lambda e: e.scalar_tensor_tensor(out=b2[:, 0:n], in0=ps[:, 0:n], scalar=64.0, in1=vs_[:, t0:t0 + n], op0=ALU.mult, op1=ALU.mult),
                                 reads=[ps, vs_], writes=[b2])
                            S.op('pool', lambda e: e.tensor_tensor(out=bon[:, t0:t0 + n], in0=bon[:, t0:t0 + n], in1=b2[:, 0:n], op=ALU.add), reads=[bon, b2], writes=[bon])
                S.dma(rwq[Q_BON, cs, :], bon[:], reads=[bon], q='act')


def stage_rwkv_scan(P, li, need_ctx, heads=range(4), dbg_chunks=None, dbg_level=9, dirs=(0, 1)):
    S = P.S
    rwq = P.dr['rwq']
    rwT = P.dr['rwT']
    NCH = T // 128
    with S.stage():
        identf = S.sb('identf', [128, 128]); SU = S.sb('mSU', [128, 128]); SL = S.sb('mSL', [128, 128]); UI = S.sb('mUI', [128, 128])
        blk = S.sb('rblk', [128, 128])
        S.dma(identf[:], P.dr['ident_f'][:, :], writes=[identf]); S.dma(SU[:], P.dr['mask_su'][:, :], writes=[SU])
        S.dma(SL[:], P.dr['mask_sl'][:, :], writes=[SL]); S.dma(UI[:], P.dr['mask_ui'][:, :], writes=[UI])
        S.dma(blk[:], P.dr['blk64'][:, :], writes=[blk])
        big = lambda nm: S.sb(nm, [64, T])
        t_ld, t_cum, t_kk, t_a, t_kd, t_r, t_v, stg, yacc = [big(n) for n in ('t_ld', 't_cum', 't_kk', 't_a', 't_kd', 't_r', 't_v', 'stg', 'yacc')]
        ps_tr = S.ps('ps_tr', [128, 192])
        ps_g1 = S.ps('ps_g1', [128, 4, 128]); ps_g2 = S.ps('ps_g2', [128, 128])
        ps_inv = [S.ps(f'ps_inv{j}', [128, 512]) for j in range(4)]
        ps_seq = S.ps('ps_seq', [128, 512])
        ps_ro = Rot([ps_inv[0], ps_inv[1]])

        class View:
            def __init__(self, buf, ap):
                self.buf = buf; self.ap = ap
            def __getitem__(self, idx):
                return self.ap[idx]
            w = property(lambda self: self.buf.w, lambda self, v: setattr(self.buf, 'w', v))
            r = property(lambda self: self.buf.r, lambda self, v: setattr(self.buf, 'r', v))
        B_tr = View(ps_tr, ps_tr.t[:, :])
        B_g = [View(ps_g1, ps_g1.t[:, j, :]) for j in range(4)] + [View(ps_g2, ps_g2.t[:, :])]
        B_inv = [[View(ps_inv[j], ps_inv[j].t[:, 0:128]) for j in range(4)] for i in range(2)]
        B_b1 = View(ps_seq, ps_seq.t[:, 0:64]); B_u = View(ps_seq, ps_seq.t[:, 64:128])
        B_zn = View(ps_seq, ps_seq.t[0:64, 128:192]); B_y = View(ps_seq, ps_seq.t[0:64, 256:384])
        m128 = lambda nm: S.sb(nm, [128, 128])
        Xp = [m128('Xp0'), m128('Xp1')]; XpT = [m128('XpT0'), m128('XpT1')]
        Dm = [m128('Dm0'), m128('Dm1')]; DT = [m128('DT0'), m128('DT1')]
        Wsb = [m128('Wsb0'), m128('Wsb1')]; Gsb = [m128('Gsb0'), m128('Gsb1')]
        MK = S.sb('MK', [128, 14, 128])
        S.dma(MK[:], P.dr['rw_masks'][:, :, :], writes=[MK])
        LKT = Rot([m128('LKT0'), m128('LKT1')]); GAT = Rot([m128('GAT0'), m128('GAT1')]); GKT = Rot([m128('GKT0'), m128('GKT1')])
        tokT = Rot([S.sb(f'tokT{j}', [128, 3, 64]) for j in range(2)])
        B1s = Rot([S.sb(f'B1s{j}', [128, 64]) for j in range(2)]); nU = Rot([S.sb(f'nU{j}', [128, 64]) for j in range(2)])
        Zs = [S.sb(f'Z{j}', [64, 64]) for j in range(2)]
        Zt = S.sb('Zt', [64, 64])
        ysb = Rot([S.sb(f'ysb{j}', [64, 128]) for j in range(2)])
        rot = Rot([S.sb(f'rot{j}', [64, 512]) for j in range(6)])
        lnc = S.sb('lnc', [64, 4, 2])
        S.dma(lnc[:, :, 0], P.dr['rw_ln_w'][li].rearrange("(h p) -> p h", p=64), writes=[lnc], allow_slow_non_contiguous=True)
        S.dma(lnc[:, :, 1], P.dr['rw_ln_b'][li].rearrange("(h p) -> p h", p=64), writes=[lnc], allow_slow_non_contiguous=True)

        def load(dst, qi, h, d):
            src = rwq[qi, h * 64:(h + 1) * 64, :]
            if d == 0:
                S.dma(dst[:], src, writes=[dst])
            else:
                S.dma(stg[:], src, writes=[stg])
                for (t0, n) in SEGS2:
                    S.op('pool', lambda e, t0=t0, n=n: e.tensor_copy(out=dst[:, t0:t0 + n], in_=stg[:, t0:t0 + n][:, ::-1]), reads=[stg], writes=[dst])

        for h in heads:
            for d in dirs:
                load(t_ld, Q_LD0 + d, h, d)
                ones_t = t_kk
                S.op('pool', lambda e: e.memset(ones_t[:], 1.0), writes=[ones_t])
                for c in range(NCH):
                    S.op('dve', lambda e, c=c: e.tensor_tensor_scan(out=t_cum[:, c * 128:(c + 1) * 128], data0=ones_t[:, c * 128:(c + 1) * 128],
                                                                   data1=t_ld[:, c * 128:(c + 1) * 128], initial=0.0, op0=ALU.mult, op1=ALU.add),
                         reads=[ones_t, t_ld], writes=[t_cum])
                S.op('dve', lambda e: e.tensor_tensor(out=t_ld[:], in0=t_cum[:], in1=t_ld[:], op=ALU.subtract), reads=[t_cum, t_ld], writes=[t_ld])
                S.op('act', lambda e: e.activation(out=t_ld[:], in_=t_ld[:], func=AF.Exp), reads=[t_ld], writes=[t_ld])
                load(t_kk, Q_KK, h, d); load(t_a, Q_A0 + d, h, d)
                S.op('dve', lambda e: e.tensor_tensor(out=t_a[:], in0=t_a[:], in1=t_kk[:], op=ALU.mult), reads=[t_a, t_kk], writes=[t_a])
                S.op('dve', lambda e: e.tensor_tensor(out=t_kk[:], in0=t_kk[:], in1=t_ld[:], op=ALU.mult), reads=[t_kk, t_ld], writes=[t_kk])
                S.op('act', lambda e: e.activation(out=t_ld[:], in_=t_cum[:], func=AF.Exp, scale=-1.0), reads=[t_cum], writes=[t_ld])
                load(t_kd, Q_KD0 + d, h, d)
                S.op('dve', lambda e: e.tensor_tensor(out=t_a[:], in0=t_a[:], in1=t_ld[:], op=ALU.mult), reads=[t_a, t_ld], writes=[t_a])
                S.op('pool', lambda e: e.tensor_tensor(out=t_kd[:], in0=t_kd[:], in1=t_ld[:], op=ALU.mult), reads=[t_kd, t_ld], writes=[t_kd])
                S.op('act', lambda e: e.activation(out=t_cum[:], in_=t_cum[:], func=AF.Exp), reads=[t_cum], writes=[t_cum])
                load(t_r, Q_R, h, d)
                S.op('dve', lambda e: e.tensor_tensor(out=t_r[:], in0=t_r[:], in1=t_cum[:], op=ALU.mult), reads=[t_r, t_cum], writes=[t_r])
                load(t_v, Q_V, h, d)
                zi = 0
                S.op('pool', lambda e: e.memset(Zs[0][:], 0.0), writes=[Zs[0]])
                chunks = range(NCH) if dbg_chunks is None else range(dbg_chunks)
                for c in chunks:
                    cl = slice(c * 128, (c + 1) * 128)
                    for j, src in enumerate((t_a, t_kd, t_v)):
                        S.op('pe', lambda e, j=j, src=src: e.transpose(out=B_tr[:, j * 64:(j + 1) * 64], in_=src[:, cl], identity=identf[0:64, 0:64]),
                             reads=[src, identf], writes=[B_tr])
                    tk = tokT.next()
                    S.op('act', lambda e: e.copy(out=tk[:].rearrange("p a c -> p (a c)"), in_=B_tr[:, :]), reads=[B_tr], writes=[tk])
                    if dbg_level < 2:
                        continue
                    for j, (l_, r_) in enumerate(((t_a, t_kk), (t_kk, t_a), (t_kd, t_kk), (t_a, t_r), (t_kd, t_r))):
                        S.op('pe', lambda e, j=j, l_=l_, r_=r_: e.matmul(B_g[j][:, :], lhsT=l_[:, cl], rhs=r_[:, cl], start=True, stop=True), reads=[l_, r_], writes=[B_g[j]])
                    x0, xt0 = Xp[0], XpT[0]
                    S.op('dve', lambda e: e.scalar_tensor_tensor(out=x0[:], in0=B_g[0][:, :], scalar=-1.0, in1=SU[:], op0=ALU.mult, op1=ALU.mult), reads=[B_g[0], SU], writes=[x0])
                    S.op('dve', lambda e: e.scalar_tensor_tensor(out=xt0[:], in0=B_g[1][:, :], scalar=-1.0, in1=SL[:], op0=ALU.mult, op1=ALU.mult), reads=[B_g[1], SL], writes=[xt0])
                    lkt = LKT.next(); gat = GAT.next(); gkt = GKT.next()
                    S.op('dve', lambda e: e.tensor_tensor(out=lkt[:], in0=B_g[2][:, :], in1=SU[:], op=ALU.mult), reads=[B_g[2], SU], writes=[lkt])
                    S.op('dve', lambda e: e.tensor_tensor(out=gat[:], in0=B_g[3][:, :], in1=UI[:], op=ALU.mult), reads=[B_g[3], UI], writes=[gat])
                    S.op('dve', lambda e: e.tensor_tensor(out=gkt[:], in0=B_g[4][:, :], in1=UI[:], op=ALU.mult), reads=[B_g[4], UI], writes=[gkt])
                    if dbg_level < 3:
                        continue
                    S.op('pool', lambda e: e.tensor_tensor(out=Dm[0][:], in0=xt0[:], in1=MK[:, 0, :], op=ALU.mult), reads=[xt0, MK], writes=[Dm[0]])
                    S.op('pool', lambda e: e.tensor_tensor(out=Dm[0][:], in0=Dm[0][:], in1=identf[:], op=ALU.add), reads=[Dm[0], identf], writes=[Dm[0]])
                    S.op('pool', lambda e: e.tensor_tensor(out=DT[0][:], in0=x0[:], in1=MK[:, 7, :], op=ALU.mult), reads=[x0, MK], writes=[DT[0]])
                    S.op('pool', lambda e: e.tensor_tensor(out=DT[0][:], in0=DT[0][:], in1=identf[:], op=ALU.add), reads=[DT[0], identf], writes=[DT[0]])
                    cur = 0
                    bi = B_inv[0]
                    for k in range(1, 7):
                        nxt = 1 - cur
                        if k < 6:
                            S.op('pe', lambda e, cur=cur: e.matmul(bi[0][:, :], lhsT=x0[:], rhs=Dm[cur][:], start=True, stop=True), reads=[x0, Dm[cur]], writes=[bi[0]])
                            S.op('act', lambda e: e.copy(out=Wsb[0][:], in_=bi[0][:, :]), reads=[bi[0]], writes=[Wsb[0]])
                            S.op('pe', lambda e, cur=cur: e.matmul(bi[1][:, :], lhsT=DT[cur][:], rhs=Wsb[0][:], start=True, stop=True), reads=[DT[cur], Wsb[0]], writes=[bi[1]])
                            S.op('dve', lambda e, k=k: e.tensor_tensor(out=Gsb[0][:], in0=bi[1][:, :], in1=MK[:, k, :], op=ALU.mult), reads=[bi[1], MK], writes=[Gsb[0]])
                            S.op('pool', lambda e, cur=cur, nxt=nxt: e.tensor_tensor(out=Dm[nxt][:], in0=Dm[cur][:], in1=Gsb[0][:], op=ALU.add), reads=[Dm[cur], Gsb[0]], writes=[Dm[nxt]])
                        S.op('pe', lambda e, cur=cur: e.matmul(bi[2][:, :], lhsT=xt0[:], rhs=DT[cur][:], start=True, stop=True), reads=[xt0, DT[cur]], writes=[bi[2]])
                        S.op('act', lambda e: e.copy(out=Wsb[1][:], in_=bi[2][:, :]), reads=[bi[2]], writes=[Wsb[1]])
                        S.op('pe', lambda e, cur=cur: e.matmul(bi[3][:, :], lhsT=Dm[cur][:], rhs=Wsb[1][:], start=True, stop=True), reads=[Dm[cur], Wsb[1]], writes=[bi[3]])
                        S.op('dve', lambda e, k=k: e.tensor_tensor(out=Gsb[1][:], in0=bi[3][:, :], in1=MK[:, 7 + k, :], op=ALU.mult), reads=[bi[3], MK], writes=[Gsb[1]])
                        S.op('pool', lambda e, cur=cur, nxt=nxt: e.tensor_tensor(out=DT[nxt][:], in0=DT[cur][:], in1=Gsb[1][:], op=ALU.add), reads=[DT[cur], Gsb[1]], writes=[DT[nxt]])
                        cur = nxt
                    TT = DT[cur]
                    if dbg_level < 4:
                        continue
                    Z = Zs[zi]; Zn = Zs[1 - zi]
                    S.op('pe', lambda e: e.matmul(B_b1[:, :], lhsT=t_kk[:, cl], rhs=Z[:], start=True, stop=False), reads=[t_kk, Z], writes=[B_b1])
                    S.op('pe', lambda e: e.matmul(B_b1[:, :], lhsT=lkt[:], rhs=tk[:, 2, :], start=False, stop=True), reads=[lkt, tk], writes=[B_b1])
                    b1 = B1s.next()
                    S.op('act', lambda e: e.copy(out=b1[:], in_=B_b1[:, :]), reads=[B_b1], writes=[b1])
                    S.op('pe', lambda e: e.matmul(B_u[:, :], lhsT=TT[:], rhs=b1[:], start=True, stop=True), reads=[TT, b1], writes=[B_u])
                    nu = nU.next()
                    S.op('act', lambda e: e.mul(out=nu[:], in_=B_u[:, :], mul=-1.0), reads=[B_u], writes=[nu])
                    S.op('pe', lambda e: e.matmul(B_y[:, :], lhsT=Z[:], rhs=t_r[:, cl], start=True, stop=False), reads=[Z, t_r], writes=[B_y])
                    S.op('pe', lambda e: e.matmul(B_y[:, :], lhsT=nu[:], rhs=gat[:], start=False, stop=False), reads=[nu, gat], writes=[B_y])
                    S.op('pe', lambda e: e.matmul(B_y[:, :], lhsT=tk[:, 2, :], rhs=gkt[:], start=False, stop=True), reads=[tk, gkt], writes=[B_y])
                    S.op('pe', lambda e: e.matmul(B_zn[:, :], lhsT=tk[:, 0, :], rhs=nu[:], start=True, stop=False), reads=[tk, nu], writes=[B_zn])
                    S.op('pe', lambda e: e.matmul(B_zn[:, :], lhsT=tk[:, 1, :], rhs=tk[:, 2, :], start=False, stop=True), reads=[tk], writes=[B_zn])
                    S.op('dve', lambda e: e.tensor_tensor(out=Zt[:], in0=B_zn[:, :], in1=Z[:], op=ALU.add), reads=[B_zn, Z], writes=[Zt])
                    S.op('act', lambda e, c=c: e.activation(out=Zn[:], in_=Zt[:], func=AF.Copy, scale=t_cum[:, c * 128 + 127:c * 128 + 128]), reads=[Zt, t_cum], writes=[Zn])
                    zi = 1 - zi
                    if d == 0:
                        S.op('dve', lambda e: e.tensor_copy(out=yacc[:, cl], in_=B_y[:, :]), reads=[B_y], writes=[yacc])
                    else:
                        seg0, segn = (0, TC) if c < 2 else (TC, TX)
                        j0 = c * 128 - seg0
                        lo = seg0 + segn - j0 - 128
                        yv = yacc[:, lo:lo + 128][:, ::-1]
                        S.op('dve', lambda e, yv=yv: e.tensor_tensor(out=yv, in0=B_y[:, :], in1=yv, op=ALU.add), reads=[B_y, yacc], writes=[yacc])
            if 'dbg_y' in P.dr:
                S.dma(P.dr['dbg_y'][:, :], yacc[:], reads=[yacc])
            load(t_kd, Q_BON, h, 0); load(t_a, Q_G, h, 0)
            for (t0, n) in (BLOCKS if need_ctx else BLOCKS[1:]):
                pm = ps_ro.next()
                S.op('pe', lambda e: e.matmul(pm[0:64, 0:n], lhsT=blk[0:64, 0:64], rhs=yacc[:, t0:t0 + n], start=True, stop=True), reads=[blk, yacc], writes=[pm])
                dd = rot.next()
                S.op('dve', lambda e: e.tensor_tensor(out=dd[:, 0:n], in0=yacc[:, t0:t0 + n], in1=pm[0:64, 0:n], op=ALU.subtract), reads=[yacc, pm], writes=[dd])
                sq = rot.next()
                S.op('act', lambda e: e.activation(out=sq[:, 0:n], in_=dd[:, 0:n], func=AF.Square), reads=[dd], writes=[sq])
                pv = ps_ro.next()
                S.op('pe', lambda e: e.matmul(pv[0:64, 0:n], lhsT=blk[0:64, 0:64], rhs=sq[:, 0:n], start=True, stop=True), reads=[blk, sq], writes=[pv])
                rs2 = rot.next()
                S.op('dve', lambda e: e.tensor_scalar(out=rs2[:, 0:n], in0=pv[0:64, 0:n], scalar1=64e-5, scalar2=None, op0=ALU.add), reads=[pv], writes=[rs2])
                S.op('act', lambda e: e.activation(out=rs2[:, 0:n], in_=rs2[:, 0:n], func=AF.Sqrt), reads=[rs2], writes=[rs2])
                S.op('dve', lambda e: e.reciprocal(out=rs2[:, 0:n], in_=rs2[:, 0:n]), reads=[rs2], writes=[rs2])
                S.op('dve', lambda e: e.tensor_tensor(out=dd[:, 0:n], in0=dd[:, 0:n], in1=rs2[:, 0:n], op=ALU.mult), reads=[dd, rs2], writes=[dd])
                S.op('dve', lambda e: e.tensor_scalar(out=dd[:, 0:n], in0=dd[:, 0:n], scalar1=lnc[:, h, 0:1], scalar2=lnc[:, h, 1:2], op0=ALU.mult, op1=ALU.add),
                     reads=[dd, lnc], writes=[dd])
                S.op('pool', lambda e: e.tensor_tensor(out=dd[:, 0:n], in0=dd[:, 0:n], in1=t_kd[:, t0:t0 + n], op=ALU.add), reads=[dd, t_kd], writes=[dd])
                oo = rot.next()
                S.op('pool', lambda e: e.tensor_tensor(out=oo[:, 0:n], in0=dd[:, 0:n], in1=t_a[:, t0:t0 + n], op=ALU.mult), reads=[dd, t_a], writes=[oo])
                S.dma(rwT[h * 64:(h + 1) * 64, t0:t0 + n], oo[:, 0:n], reads=[oo], q='act')


def declare_rwkv(P):
    P.inp('rw_mu', [DEPTH, 2, 1152]); P.inp('rw_w0', [DEPTH, 2, 256]); P.inp('rw_w2', [DEPTH, 2, 64, 256])
    P.inp('rw_a0', [DEPTH, 2, 256]); P.inp('rw_a2', [DEPTH, 2, 64, 256]); P.inp('rw_g2', [DEPTH, 128, 256])
    P.inp('rw_k_k', [DEPTH, 256]); P.inp('rw_k_a', [DEPTH, 256]); P.inp('rw_r_k', [DEPTH, 4, 64])
    P.inp('rw_ln_w', [DEPTH, 256]); P.inp('rw_ln_b', [DEPTH, 256])
    P.inp('mask_su', [128, 128]); P.inp('mask_sl', [128, 128]); P.inp('mask_ui', [128, 128])
    P.inp('rw_masks', [128, 14, 128])
    P.tmp('rwq', [11, 256, T])


def host_rwkv(m, inputs):
    for k in ('rw_mu', 'rw_w0', 'rw_w2', 'rw_a0', 'rw_a2', 'rw_g2', 'rw_k_k', 'rw_k_a', 'rw_r_k', 'rw_ln_w', 'rw_ln_b'):
        m[k] = np.ascontiguousarray(inputs[k], np.float32)
    su = np.triu(np.ones((128, 128), np.float32), 1)
    m['mask_su'] = su; m['mask_sl'] = np.ascontiguousarray(su.T); m['mask_ui'] = np.triu(np.ones((128, 128), np.float32), 0)
    idx = np.arange(128)
    mk = np.zeros((128, 14, 128), np.float32)
    for k in range(7):
        b = 2 ** k
        M = ((idx[:, None] // (2 * b)) == (idx[None, :] // (2 * b))) & ((idx[:, None] % (2 * b)) >= b) & ((idx[None, :] % (2 * b)) < b)
        mk[:, k, :] = M
        mk[:, 7 + k, :] = M.T
    m['rw_masks'] = mk


def build_full():
    P = Prog()
    declare_common(P); declare_attn(P); declare_lru(P); declare_merge(P); declare_hyena(P); declare_rwkv(P); declare_moe(P)
    P.out('y_out', [TX, D])
    for li in range(DEPTH):
        need_ctx = li < DEPTH - 1
        src = 'xin' if li == 0 else 'x_l0'
        stage_mod(P, li)
        stage_inproj(P, li, src)
        stage_attn(P, li, need_ctx)
        stage_hyena(P, li, need_ctx)
        stage_rwkv_prep(P, li)
        stage_rwkv_scan(P, li, need_ctx)
        stage_lru(P, li, need_ctx)
        stage_merge(P, li, need_ctx, src, 'x_mid')
        if need_ctx:
            stage_moe(P, li, True, 'x_mid', 'x_l0')
        else:
            stage_moe(P, li, False, 'x_mid', 'y_out', dst_is_out=True)
    P.S.finish()
    return P


def full_host_inputs(inputs, b, shared=None):
    if shared is None:
        shared = {}
        m = host_inputs(inputs, b)
        host_attn(m, inputs); host_lru(m, inputs); host_merge(m, inputs); host_hyena(m, inputs); host_rwkv(m, inputs); host_moe(m, inputs)
        for k, v in m.items():
            if k not in ('xin', 'cvec'):
                shared[k] = v
        return m, shared
    m = dict(shared)
    m['xin'] = np.ascontiguousarray(np.concatenate([inputs['ctx'][b], inputs['x'][b]], axis=0), dtype=np.float32)
    m['cvec'] = np.ascontiguousarray(np.stack([inputs['c_ctx'], inputs['c'][b]], axis=0), dtype=np.float32)
    return m, shared


def kernel(**inputs):
    inputs = {k: np.asarray(v) for k, v in inputs.items()}
    P = build_full()
    n = 8
    maps = []
    shared = None
    for b in range(n):
        m, shared = full_host_inputs(inputs, b, shared)
        maps.append({k: m[k] for k in P.in_names})
    res = run_bass_kernel_spmd(P.nc, maps, core_ids=list(range(n)))
    out = np.stack([np.asarray(res.results[b]['y_out'], dtype=np.float32) for b in range(n)], axis=0)
    return out
```

```python
import contextlib
import math
import numpy as np
import concourse.bass as bass
import concourse.mybir as mybir
from concourse.bass_utils import run_bass_kernel_spmd

F32 = mybir.dt.float32
BF16 = mybir.dt.bfloat16
ALU = mybir.AluOpType
AF = mybir.ActivationFunctionType
AX = mybir.AxisListType

ENGS = ('pe', 'dve', 'act', 'pool', 'sp')
SEM_ROLL = 30000
NDMA = 12

D = 1024
TC = 256
TX = 4096
T = TC + TX
N_IN = 7296
DEPTH = 2
EPS = 1e-6
BLOCKS = [(0, 256)] + [(256 + 512 * j, 512) for j in range(8)]
O_Q, O_K, O_V, O_HY, O_RW, O_LR, O_GT = 0, 512, 640, 768, 1536, 2688, 3200


class Buf:
    __slots__ = ('t', 'w', 'r', 'name')

    def __init__(self, t, name=''):
        self.t = t
        self.w = None
        self.r = {}
        self.name = name

    def __getitem__(self, idx):
        return self.t[idx]


class Sched:
    def __init__(self, nc):
        self.nc = nc
        self.emap = {'pe': nc.tensor, 'dve': nc.vector, 'act': nc.scalar, 'pool': nc.gpsimd, 'sp': nc.sync}
        self.perm = contextlib.ExitStack()
        self.stack = contextlib.ExitStack()
        self.cur_sem = {}
        self.cnt = {}
        self.nsem = 0
        for e in ('pe', 'dve', 'act', 'pool'):
            self._new_sem(e)
        self.dma_sems = {}
        self.dma_k = {}
        for q in ('sp', 'pool', 'act'):
            self.dma_sems[q] = [self._alloc_sem(f'dma_{q}_{i}') for i in range(NDMA)]
            self.dma_k[q] = 0
        self.seen = {e: {} for e in ENGS}
        self.out_tokens = []
        self.ninstr = 0
        self.uid = 0

    def _alloc_sem(self, name):
        self.nsem += 1
        return self.perm.enter_context(self.nc.semaphore(f'{name}_{self.nsem}'))

    def _new_sem(self, e):
        self.cur_sem[e] = self._alloc_sem(f'c_{e}')
        self.cnt[e] = 0

    def sb(self, name, shape, dt=F32):
        self.uid += 1
        t = self.stack.enter_context(self.nc.sbuf_tensor(f'{name}_{self.uid}', list(shape), dt))
        return Buf(t, name)

    def ps(self, name, shape, dt=F32):
        self.uid += 1
        t = self.stack.enter_context(self.nc.psum_tensor(f'{name}_{self.uid}', list(shape), dt))
        return Buf(t, name)

    @contextlib.contextmanager
    def stage(self):
        old = self.stack
        self.stack = contextlib.ExitStack()
        try:
            yield
        finally:
            self.barrier()
            self.stack.close()
            self.stack = old

    def barrier(self):
        toks = []
        for f in ('pe', 'dve', 'act', 'pool'):
            if self.cnt[f] > 0:
                toks.append((self.cur_sem[f], self.cnt[f]))
        for q in ('sp', 'pool', 'act'):
            k = self.dma_k[q]
            for j in range(min(k, NDMA)):
                last = ((k - 1 - j) // NDMA) * NDMA + j
                toks.append((self.dma_sems[q][j], 16 * (last // NDMA + 1)))
        for e in ENGS:
            eng = self.emap[e]
            for s, v in toks:
                if self.seen[e].get(id(s), -1) >= v:
                    continue
                self.seen[e][id(s)] = v
                eng.wait_ge(s, v)
                self.ninstr += 1

    def op(self, eng, fn, reads=(), writes=(), dmaq=False, is_out=False):
        need = {}

        def add(tok):
            if tok is None:
                return
            sem, val, teng = tok
            if teng == eng and eng == 'pe' and not dmaq:
                return
            k = id(sem)
            if self.seen[eng].get(k, -1) >= val:
                return
            if k not in need or need[k][1] < val:
                need[k] = (sem, val)

        for b in reads:
            add(b.w)
        for b in writes:
            add(b.w)
            for t in b.r.values():
                add(t)
        if dmaq:
            k = self.dma_k[eng]
            self.dma_k[eng] = k + 1
            sem = self.dma_sems[eng][k % NDMA]
            prev = 16 * (k // NDMA)
            if prev > 0:
                add((sem, prev, 'dma'))
            tok = (sem, prev + 16, 'dma')
            inc = 16
            rkey = ('dma', eng, k % (4 * NDMA))
        else:
            if self.cnt[eng] >= SEM_ROLL:
                self._new_sem(eng)
            self.cnt[eng] += 1
            sem = self.cur_sem[eng]
            tok = (sem, self.cnt[eng], eng)
            inc = 1
            rkey = eng
        e = self.emap[eng]
        for s_, v_ in need.values():
            self.seen[eng][id(s_)] = v_
            e.wait_ge(s_, v_)
        fn(e).then_inc(sem, inc)
        self.ninstr += 1 + len(need)
        for b in reads:
            b.r[rkey] = tok
        for b in writes:
            b.w = tok
            b.r = {}
        if is_out:
            self.out_tokens.append(tok)
        return tok

    def dma(self, out_ap, in_ap, reads=(), writes=(), q='sp', is_out=False, **kw):
        return self.op(q, lambda e: e.dma_start(out=out_ap, in_=in_ap, **kw),
                       reads=reads, writes=writes, dmaq=True, is_out=is_out)

    def finish(self):
        need = {}
        for sem, val, _ in self.out_tokens:
            k = id(sem)
            if k not in need or need[k][1] < val:
                need[k] = (sem, val)
        for s_, v_ in need.values():
            self.nc.sync.wait_ge(s_, v_)
        self.barrier()
        self.stack.close()
        self.perm.close()


class Rot:
    def __init__(self, bufs):
        self.bufs = bufs
        self.i = 0

    def next(self):
        b = self.bufs[self.i % len(self.bufs)]
        self.i += 1
        return b


class Prog:
    def __init__(self, ext_in=(), ext_out=()):
        self.nc = bass.Bass("TRN2", target_bir_lowering=False)
        self.S = Sched(self.nc)
        self.ext_in = set(ext_in)
        self.ext_out = set(ext_out)
        self.dr = {}
        self.in_names = []
        self.out_names = []

    def inp(self, name, shape, dt=F32):
        t = self.nc.dram_tensor(name, list(shape), dt, kind="ExternalInput")
        self.dr[name] = t.ap()
        self.in_names.append(name)
        return self.dr[name]

    def out(self, name, shape, dt=F32):
        t = self.nc.dram_tensor(name, list(shape), dt, kind="ExternalOutput")
        self.dr[name] = t.ap()
        self.out_names.append(name)
        return self.dr[name]

    def tmp(self, name, shape, dt=F32):
        if name in self.ext_in:
            return self.inp(name, shape, dt)
        if name in self.ext_out:
            return self.out(name, shape, dt)
        t = self.nc.dram_tensor(name, list(shape), dt, kind="Internal")
        self.dr[name] = t.ap()
        return self.dr[name]


def stage_mod(P, li):
    S = P.S
    ada_w = P.dr['ada_w']
    ada_b = P.dr['ada_b']
    cvec = P.dr['cvec']
    modT_d = P.dr[f'modT{li}']
    modrow_d = P.dr[f'modrow{li}']
    with S.stage():
        cT = S.sb('cT', [128, 8, 2])
        scT = S.sb('scT', [128, 8, 2])
        abT = S.sb('abT', [128, 48])
        modT = S.sb('modT', [128, 48, 2])
        sig = S.sb('sig', [128, 8, 2])
        for s in range(2):
            S.dma(cT[:, :, s], cvec[s, :].rearrange("(k p) -> p k", p=128), writes=[cT],
                  allow_slow_non_contiguous=True)
        S.dma(abT[:, :], ada_b[li, :].rearrange("(o p) -> p o", p=128), writes=[abT],
              allow_slow_non_contiguous=True)
        S.op('act', lambda e: e.activation(out=sig[:], in_=cT[:], func=AF.Sigmoid), reads=[cT], writes=[sig])
        S.op('dve', lambda e: e.tensor_tensor(out=scT[:], in0=cT[:], in1=sig[:], op=ALU.mult), reads=[cT, sig], writes=[scT])
        wts = Rot([S.sb(f'adaw{j}', [128, 8, 512]) for j in range(2)])
        pss = Rot([S.ps(f'modps{j}', [128, 8]) for j in range(2)])
        aw = ada_w[li].rearrange("(k p) n -> p k n", p=128)
        for g in range(12):
            wt = wts.next()
            S.dma(wt[:], aw[:, :, g * 512:(g + 1) * 512], writes=[wt], q=('sp' if g % 2 == 0 else 'pool'))
            ps = pss.next()
            for j in range(4):
                oc = g * 4 + j
                for k in range(8):
                    S.op('pe', lambda e, j=j, k=k: e.matmul(ps[:, 2 * j:2 * j + 2], lhsT=wt[:, k, j * 128:(j + 1) * 128], rhs=scT[:, k, :],
                                                           start=(k == 0), stop=(k == 7)), reads=[wt, scT], writes=[ps])
            for j in range(4):
                oc = g * 4 + j
                S.op('dve', lambda e, j=j, oc=oc: e.tensor_scalar(out=modT[:, oc, :], in0=ps[:, 2 * j:2 * j + 2], scalar1=abT[:, oc:oc + 1], scalar2=None,
                                                                 op0=ALU.add), reads=[ps, abT], writes=[modT])
        S.dma(modT_d[:, :], modT[:].rearrange("p o s -> p (o s)"), reads=[modT])
        for s in range(2):
            S.dma(modrow_d[s, :].rearrange("(o p) -> p o", p=128), modT[:, :, s], reads=[modT], q='pool',
                  allow_slow_non_contiguous=True)


def load_modT(P, li):
    S = P.S
    m = S.sb('modTl', [128, 48, 2])
    S.dma(m[:].rearrange("p o s -> p (o s)"), P.dr[f'modT{li}'][:, :], writes=[m])
    return m


def norm_ctx(S, npt=2):
    C = {}
    C['sc1p'] = S.sb('sc1p', [128, 8, 2])
    C['xts'] = Rot([S.sb(f'xt{j}', [128, 1024]) for j in range(3)])
    C['sqs'] = Rot([S.sb(f'sq{j}', [128, 1024]) for j in range(2)])
    C['xns'] = Rot([S.sb(f'xn{j}', [128, 1024], BF16) for j in range(2)])
    C['sss'] = Rot([S.sb(f'ss{j}', [128, 2]) for j in range(4)])
    C['pts'] = Rot([S.ps(f'ptT{j}', [128, 4, 128], BF16) for j in range(npt)])
    return C


def norm_transpose(P, src_d, modT, sh_c, sc_c, hT, ident, tiles=None, dst0=0, C=None):
    S = P.S
    if C is None:
        C = norm_ctx(S)
    sc1p = C['sc1p']
    S.op('dve', lambda e: e.tensor_scalar(out=sc1p[:], in0=modT[:, sc_c:sc_c + 8, :], scalar1=1.0, scalar2=None, op0=ALU.add),
         reads=[modT], writes=[sc1p])
    xts, sqs, xns, sss, pts = C['xts'], C['sqs'], C['xns'], C['sss'], C['pts']
    if tiles is None:
        tiles = range(T // 128)
    for ti in tiles:
        s = 0 if ti < 2 else 1
        xt = xts.next()
        S.dma(xt[:], src_d[ti * 128:(ti + 1) * 128, :], writes=[xt], q=('sp' if ti % 2 == 0 else 'pool'))
        sq = sqs.next()
        ss = sss.next()
        S.op('act', lambda e: e.activation(out=sq[:], in_=xt[:], func=AF.Square), reads=[xt], writes=[sq])
        S.op('dve', lambda e: e.reduce_sum(out=ss[:, 0:1], in_=sq[:], axis=AX.X), reads=[sq], writes=[ss])
        S.op('dve', lambda e: e.tensor_scalar(out=ss[:, 1:2], in0=ss[:, 0:1], scalar1=1.0 / D, scalar2=EPS, op0=ALU.mult, op1=ALU.add),
             reads=[ss], writes=[ss])
        S.op('act', lambda e: e.activation(out=ss[:, 1:2], in_=ss[:, 1:2], func=AF.Sqrt), reads=[ss], writes=[ss])
        S.op('dve', lambda e: e.reciprocal(out=ss[:, 0:1], in_=ss[:, 1:2]), reads=[ss], writes=[ss])
        xn = xns.next()
        S.op('act', lambda e: e.activation(out=xn[:], in_=xt[:], func=AF.Copy, scale=ss[:, 0:1]), reads=[xt, ss], writes=[xn])
        for half in range(2):
            pt = pts.next()
            for j in range(4):
                k = half * 4 + j
                S.op('pe', lambda e, j=j, k=k: e.transpose(out=pt[:, j, :], in_=xn[:, k * 128:(k + 1) * 128], identity=ident[:]),
                     reads=[xn, ident], writes=[pt])
            for j in range(4):
                k = half * 4 + j
                S.op('dve', lambda e, j=j, k=k: e.tensor_scalar(out=hT[:, k, ti * 128 - dst0:(ti + 1) * 128 - dst0], in0=pt[:, j, :],
                                                               scalar1=sc1p[:, k, s:s + 1], scalar2=modT[:, sh_c + k, s:s + 1],
                                                               op0=ALU.mult, op1=ALU.add), reads=[pt, sc1p, modT], writes=[hT])


def stage_inproj(P, li, src_name):
    S = P.S
    src_d = P.dr[src_name]
    w_in = P.dr['w_in']
    projT = P.dr['projT']
    vtm = P.dr['vtm']
    with S.stage():
        ident = S.sb('ident', [128, 128], BF16)
        S.dma(ident[:], P.dr['ident_bf'][:, :], writes=[ident])
        modT = load_modT(P, li)
        hT = S.sb('hT', [128, 8, T], BF16)
        norm_transpose(P, src_d, modT, 0, 8, hT, ident)
        wfs = Rot([S.sb(f'wf{j}', [128, 8, 384]) for j in range(2)])
        wbs = Rot([S.sb(f'wb{j}', [128, 8, 384], BF16) for j in range(2)])
        pss = Rot([S.ps(f'ps{j}', [128, 512]) for j in range(4)])
        sts = Rot([S.sb(f'st{j}', [128, 512]) for j in range(4)])
        wv = w_in[li].rearrange("(k p) n -> p k n", p=128)
        cnt = 0
        for g in range(N_IN // 384):
            wf = wfs.next()
            S.dma(wf[:], wv[:, :, g * 384:(g + 1) * 384], writes=[wf], q=('sp' if g % 2 == 0 else 'pool'))
            wb = wbs.next()
            S.op('pool', lambda e: e.tensor_copy(out=wb[:], in_=wf[:]), reads=[wf], writes=[wb])
            for j in range(3):
                oc = g * 3 + j
                if oc == O_V // 128:
                    for ti in range(T // 128):
                        ps = pss.next()
                        for k in range(8):
                            S.op('pe', lambda e, k=k: e.matmul(ps[:, 0:128], lhsT=hT[:, k, ti * 128:(ti + 1) * 128], rhs=wb[:, k, j * 128:(j + 1) * 128],
                                                               start=(k == 0), stop=(k == 7)), reads=[hT, wb], writes=[ps])
                        st = sts.next()
                        S.op('act', lambda e: e.copy(out=st[:, 0:128], in_=ps[:, 0:128]), reads=[ps], writes=[st])
                        S.dma(vtm[ti * 128:(ti + 1) * 128, :], st[:, 0:128], reads=[st], q='act')
                    continue
                for (t0, n) in BLOCKS:
                    ps = pss.next()
                    for k in range(8):
                        S.op('pe', lambda e, k=k: e.matmul(ps[:, 0:n], lhsT=wb[:, k, j * 128:(j + 1) * 128], rhs=hT[:, k, t0:t0 + n],
                                                           start=(k == 0), stop=(k == 7)), reads=[hT, wb], writes=[ps])
                    st = sts.next()
                    if cnt % 2 == 0:
                        S.op('act', lambda e: e.copy(out=st[:, 0:n], in_=ps[:, 0:n]), reads=[ps], writes=[st])
                    else:
                        S.op('dve', lambda e: e.tensor_copy(out=st[:, 0:n], in_=ps[:, 0:n]), reads=[ps], writes=[st])
                    S.dma(projT[oc * 128:(oc + 1) * 128, t0:t0 + n], st[:, 0:n], reads=[st], q=('sp' if cnt % 2 == 0 else 'act'))
                    cnt += 1


def declare_common(P):
    P.inp('xin', [T, D])
    P.inp('cvec', [2, D])
    P.inp('ada_w', [DEPTH, D, 6 * D])
    P.inp('ada_b', [DEPTH, 6 * D])
    P.inp('w_in', [DEPTH, D, N_IN])
    P.inp('ident_bf', [128, 128], BF16)
    for li in range(DEPTH):
        P.tmp(f'modT{li}', [128, 96])
        P.tmp(f'modrow{li}', [2, 6 * D])
    P.tmp('projT', [N_IN, T])
    P.tmp('vtm', [T, 128])


def host_inputs(inputs, b):
    import ml_dtypes
    m = {}
    m['xin'] = np.ascontiguousarray(np.concatenate([inputs['ctx'][b], inputs['x'][b]], axis=0), dtype=np.float32)
    m['cvec'] = np.ascontiguousarray(np.stack([inputs['c_ctx'], inputs['c'][b]], axis=0), dtype=np.float32)
    for k in ('ada_w', 'ada_b', 'w_in'):
        m[k] = np.ascontiguousarray(inputs[k], dtype=np.float32)
    m['ident_bf'] = np.eye(128, dtype=np.float32).astype(ml_dtypes.bfloat16)
    return m


def rope_tables():
    t = np.arange(TX)
    pos = np.stack([t // 64, t % 64], 0).astype(np.float64)
    freqs = 10000.0 ** (-np.arange(16, dtype=np.float64) / 16)
    cos = np.zeros((64, TX)); sin = np.zeros((64, TX))
    for d in range(64):
        ax, half, f = d // 32, (d % 32) // 16, d % 16
        ang = pos[ax] * freqs[f]
        cos[d] = np.cos(ang)
        sin[d] = np.sin(ang) * (-1.0 if half == 0 else 1.0)
    psw = np.zeros((128, 128), np.float32)
    for m in range(128):
        d = m % 64
        src = m + 16 if (d % 32) < 16 else m - 16
        psw[src, m] = 1.0
    blk = np.zeros((128, 128), np.float32)
    blk[:64, :64] = 1.0 / 64
    blk[64:, 64:] = 1.0 / 64
    return (np.tile(cos, (2, 1)).astype(np.float32), np.tile(sin, (2, 1)).astype(np.float32), psw, blk)


def qk_prep(P, S, rows_list, gain, dst, dst_sl, scale, tok_ranges, C):
    projT = P.dr['projT']
    for (t0, n) in tok_ranges:
        raw = C['raw'].next()
        for i, (r0, nr, p0) in enumerate(rows_list):
            S.dma(raw[p0:p0 + nr, 0:n], projT[r0:r0 + nr, t0:t0 + n], writes=[raw], q=('sp' if i % 2 == 0 else 'pool'))
        sq = C['sq'].next()
        S.op('act', lambda e: e.activation(out=sq[:, 0:n], in_=raw[:, 0:n], func=AF.Square), reads=[raw], writes=[sq])
        ps = C['ps'].next()
        S.op('pe', lambda e: e.matmul(ps[:, 0:n], lhsT=C['blk'][:], rhs=sq[:, 0:n], start=True, stop=True), reads=[sq, C['blk']], writes=[ps])
        rs = C['rs'].next()
        S.op('dve', lambda e: e.tensor_scalar(out=rs[:, 0:n], in0=ps[:, 0:n], scalar1=EPS, scalar2=None, op0=ALU.add), reads=[ps], writes=[rs])
        S.op('act', lambda e: e.activation(out=rs[:, 0:n], in_=rs[:, 0:n], func=AF.Sqrt), reads=[rs], writes=[rs])
        S.op('dve', lambda e: e.reciprocal(out=rs[:, 0:n], in_=rs[:, 0:n]), reads=[rs], writes=[rs])
        kh = C['kh'].next()
        S.op('dve', lambda e: e.scalar_tensor_tensor(out=kh[:, 0:n], in0=raw[:, 0:n], scalar=gain[:, 0:1], in1=rs[:, 0:n], op0=ALU.mult, op1=ALU.mult),
             reads=[raw, gain, rs], writes=[kh])
        if t0 < TC:
            S.op('act', lambda e: e.activation(out=dst_sl(t0, n), in_=kh[:, 0:n], func=AF.Copy, scale=scale), reads=[kh], writes=[dst])
            continue
        khb = C['khb'].next()
        S.op('act', lambda e: e.copy(out=khb[:, 0:n], in_=kh[:, 0:n]), reads=[kh], writes=[khb])
        ps2 = C['ps'].next()
        S.op('pe', lambda e: e.matmul(ps2[:, 0:n], lhsT=C['psw'][:], rhs=khb[:, 0:n], start=True, stop=True), reads=[khb, C['psw']], writes=[ps2])
        x0 = t0 - TC
        t1 = C['t1'].next()
        S.op('pool', lambda e: e.tensor_tensor(out=t1[:, 0:n], in0=kh[:, 0:n], in1=C['cos'][:, x0:x0 + n], op=ALU.mult), reads=[kh, C['cos']], writes=[t1])
        t2 = C['t2'].next()
        S.op('dve', lambda e: e.tensor_tensor(out=t2[:, 0:n], in0=ps2[:, 0:n], in1=C['sin'][:, x0:x0 + n], op=ALU.mult), reads=[ps2, C['sin']], writes=[t2])
        S.op('dve', lambda e: e.scalar_tensor_tensor(out=dst_sl(t0, n), in0=t1[:, 0:n], scalar=scale, in1=t2[:, 0:n], op0=ALU.mult, op1=ALU.add),
             reads=[t1, t2], writes=[dst])
        if scale != 1.0:
            raise NotImplementedError


def stage_attn(P, li, need_ctx, dbg=0):
    S = P.S
    projT = P.dr['projT']
    vtm = P.dr['vtm']
    attT = P.dr['attT']
    with S.stage():
        C = {}
        C['cos'] = S.sb('cos', [128, TX]); C['sin'] = S.sb('sin', [128, TX])
        C['psw'] = S.sb('psw', [128, 128], BF16); C['blk'] = S.sb('blk', [128, 128])
        pswf = S.sb('pswf', [128, 128])
        S.dma(C['cos'][:], P.dr['rope_cos'][:, :], writes=[C['cos']])
        S.dma(C['sin'][:], P.dr['rope_sin'][:, :], writes=[C['sin']], q='pool')
        S.dma(pswf[:], P.dr['rope_psw'][:, :], writes=[pswf])
        S.dma(C['blk'][:], P.dr['blk64'][:, :], writes=[C['blk']])
        S.op('dve', lambda e: e.tensor_copy(out=C['psw'][:], in_=pswf[:]), reads=[pswf], writes=[C['psw']])
        qg = S.sb('qg', [128, 1]); kg = S.sb('kg', [128, 1])
        for h in range(2):
            S.dma(qg[h * 64:(h + 1) * 64, :], P.dr['q_norm'][li, :].rearrange("(d o) -> d o", o=1), writes=[qg])
            S.dma(kg[h * 64:(h + 1) * 64, :], P.dr['k_norm'][li, :].rearrange("(d o) -> d o", o=1), writes=[kg])
        S.op('dve', lambda e: e.tensor_scalar(out=qg[:], in0=qg[:], scalar1=0.125, scalar2=None, op0=ALU.mult), reads=[qg], writes=[qg])
        for nm in ('raw', 'sq', 'rs', 'kh', 't1', 't2'):
            C[nm] = Rot([S.sb(f'{nm}{j}', [128, 512]) for j in range(2)])
        C['khb'] = Rot([S.sb(f'khb{j}', [128, 512], BF16) for j in range(2)])
        C['ps'] = Rot([S.ps(f'pps{j}', [128, 512]) for j in range(1)])
        kT = S.sb('kT', [128, T], BF16)
        qT = S.sb('qT', [128, 4, T], BF16)
        qk_prep(P, S, [(O_K, 128, 0)], kg, kT, lambda t0, n: kT[:, t0:t0 + n], 1.0, BLOCKS, C)
        qblocks = BLOCKS if need_ctx else BLOCKS[1:]
        for g in range(4):
            qk_prep(P, S, [(O_Q + g * 64, 64, 0), (O_Q + 256 + g * 64, 64, 64)], qg, qT,
                    lambda t0, n, g=g: qT[:, g, t0:t0 + n], 1.0, qblocks, C)
        if dbg:
            S.dma(P.dr['dbg_k'][:, :], kT[:], reads=[kT])
            S.dma(P.dr['dbg_q'][:, :], qT[:, 0, :], reads=[qT])
        if dbg == 1:
            return
        va = S.sb('va', [128, T // 128, 2, 128], BF16)
        vf = S.sb('vf', [128, T // 128, 128])
        S.op('pool', lambda e: e.memset(va[:], 1.0), writes=[va])
        S.dma(vf[:], vtm.rearrange("(a p) c -> p a c", p=128), writes=[vf])
        S.op('dve', lambda e: e.tensor_copy(out=va[:, :, :, 0:64], in_=vf[:].rearrange("p a (h d) -> p a h d", h=2)), reads=[vf], writes=[va])
        ones_r = S.sb('ones_r', [128, 64])
        S.op('pool', lambda e: e.memset(ones_r[:], 1.0), writes=[ones_r])
        if dbg == 2:
            return
        sps = Rot([S.ps(f'sps{j}', [128, 512]) for j in range(4)])
        ops = Rot([S.ps(f'ops{j}', [128, 512]) for j in range(2)])
        bps = Rot([S.ps(f'bps{j}', [64, 512]) for j in range(1)])
        pts = Rot([S.sb(f'pT{j}', [128, 512], BF16) for j in range(4)])
        osb = Rot([S.sb(f'osb{j}', [128, 512]) for j in range(2)])
        outs = Rot([S.sb(f'aout{j}', [64, 512]) for j in range(2)])
        for kvh in range(2):
            p0 = kvh * 64
            for g in range(4):
                head = kvh * 4 + g
                for (t0, n) in qblocks:
                    nkt = (TC // 128) if t0 < TC else (T // 128)
                    op_ = ops.next()
                    LOOK = 2
                    spq = []

                    def issue_qk(kt):
                        sp_ = sps.next()
                        S.op('pe', lambda e: e.matmul(sp_[:, 0:n], lhsT=kT[p0:p0 + 64, kt * 128:(kt + 1) * 128], rhs=qT[p0:p0 + 64, g, t0:t0 + n],
                                                      start=True, stop=True), reads=[kT, qT], writes=[sp_])
                        spq.append(sp_)

                    for kt in range(min(LOOK, nkt)):
                        issue_qk(kt)
                    for kt in range(nkt):
                        if kt + LOOK < nkt:
                            issue_qk(kt + LOOK)
                        sp_ = spq.pop(0)
                        pt = pts.next()
                        S.op('act', lambda e: e.activation(out=pt[:, 0:n], in_=sp_[:, 0:n], func=AF.Exp), reads=[sp_], writes=[pt])
                        S.op('pe', lambda e, kt=kt: e.matmul(op_[0:65, 0:n], lhsT=va[:, kt, kvh, 0:65], rhs=pt[:, 0:n], start=(kt == 0), stop=(kt == nkt - 1)),
                             reads=[va, pt], writes=[op_])
                    ob = osb.next()
                    S.op('dve', lambda e: e.tensor_copy(out=ob[0:65, 0:n], in_=op_[0:65, 0:n]), reads=[op_], writes=[ob])
                    S.op('dve', lambda e: e.reciprocal(out=ob[64:65, 0:n], in_=ob[64:65, 0:n]), reads=[ob], writes=[ob])
                    bp = bps.next()
                    S.op('pe', lambda e: e.matmul(bp[:, 0:n], lhsT=ones_r[64:65, :], rhs=ob[64:65, 0:n], start=True, stop=True), reads=[ob, ones_r], writes=[bp])
                    ao = outs.next()
                    S.op('dve', lambda e: e.tensor_tensor(out=ao[:, 0:n], in0=ob[0:64, 0:n], in1=bp[:, 0:n], op=ALU.mult), reads=[ob, bp], writes=[ao])
                    S.dma(attT[head * 64:(head + 1) * 64, t0:t0 + n], ao[:, 0:n], reads=[ao], q='pool')


def declare_attn(P):
    P.inp('q_norm', [DEPTH, 64])
    P.inp('k_norm', [DEPTH, 64])
    P.inp('rope_cos', [128, TX])
    P.inp('rope_sin', [128, TX])
    P.inp('rope_psw', [128, 128])
    P.inp('blk64', [128, 128])
    P.tmp('attT', [512, T])


def host_attn(m, inputs):
    cos, sin, psw, blk = rope_tables()
    m['rope_cos'] = cos; m['rope_sin'] = sin; m['rope_psw'] = psw; m['blk64'] = blk
    m['q_norm'] = np.ascontiguousarray(inputs['q_norm'], np.float32)
    m['k_norm'] = np.ascontiguousarray(inputs['k_norm'], np.float32)


def dwconv_fm(S, out, x, wcol, bcol, taps, pad_left, segs, eng='dve', wbuf=None):
    for (t0, n) in segs:
        j0 = pad_left
        if bcol is not None:
            S.op(eng, lambda e: e.tensor_scalar(out=out[:, t0:t0 + n], in0=x[:, t0:t0 + n], scalar1=wcol[:, j0:j0 + 1], scalar2=bcol,
                                                op0=ALU.mult, op1=ALU.add), reads=[x, wbuf], writes=[out])
        else:
            S.op(eng, lambda e: e.tensor_scalar(out=out[:, t0:t0 + n], in0=x[:, t0:t0 + n], scalar1=wcol[:, j0:j0 + 1], scalar2=None,
                                                op0=ALU.mult), reads=[x, wbuf], writes=[out])
        for j in range(taps):
            sh = j - pad_left
            if sh == 0:
                continue
            if sh > 0:
                o_sl = slice(t0, t0 + n - sh); i_sl = slice(t0 + sh, t0 + n)
            else:
                o_sl = slice(t0 - sh, t0 + n); i_sl = slice(t0, t0 + n + sh)
            S.op(eng, lambda e, j=j, o_sl=o_sl, i_sl=i_sl: e.scalar_tensor_tensor(out=out[:, o_sl], in0=x[:, i_sl], scalar=wcol[:, j:j + 1], in1=out[:, o_sl],
                                                                                 op0=ALU.mult, op1=ALU.add), reads=[x, wbuf, out], writes=[out])


def stage_lru(P, li, need_ctx):
    S = P.S
    projT = P.dr['projT']
    lruT = P.dr['lruT']
    SEGS = [(0, TC), (TC, TX)]
    with S.stage():
        big = lambda nm: S.sb(nm, [128, T])
        gate, xin, xc, A, Bv, tmp, h0, h1 = [big(n) for n in ('gate', 'xin', 'xc', 'A', 'Bv', 'tmp', 'h0', 'h1')]
        cw = S.sb('cw', [128, 4]); cb = S.sb('cb', [128, 1])
        wbd = [S.sb(f'wbd{j}', [128, 128]) for j in range(4)]
        bias = S.sb('bias', [128, 4]); lam = S.sb('lam', [128, 2]); c8 = S.sb('c8', [128, 2])
        pss = Rot([S.ps(f'lps{j}', [128, 512]) for j in range(4)])
        for ct in range(2):
            c0 = ct * 128
            S.dma(gate[:], projT[O_LR + c0:O_LR + c0 + 128, :], writes=[gate])
            S.dma(xin[:], projT[O_LR + 256 + c0:O_LR + 256 + c0 + 128, :], writes=[xin], q='pool')
            S.dma(cw[:], P.dr['lru_conv_w'][li, :, c0:c0 + 128].rearrange("j c -> c j"), writes=[cw], allow_slow_non_contiguous=True)
            S.dma(cb[:], P.dr['lru_conv_b'][li, c0:c0 + 128].rearrange("(c o) -> c o", o=1), writes=[cb])
            for d in range(2):
                for gi, (wn, bn) in enumerate((('lru_wa', 'lru_ba'), ('lru_wx', 'lru_bx'))):
                    w = wbd[d * 2 + gi]
                    S.op('pool', lambda e, w=w: e.memset(w[:], 0.0), writes=[w])
                    for nb in range(2):
                        S.dma(w[nb * 64:(nb + 1) * 64, nb * 64:(nb + 1) * 64], P.dr[wn][li, d, ct * 2 + nb, :, :], writes=[w])
                    S.dma(bias[:, d * 2 + gi:d * 2 + gi + 1], P.dr[bn][li, d, c0:c0 + 128].rearrange("(c o) -> c o", o=1), writes=[bias])
                S.dma(lam[:, d:d + 1], P.dr['lru_lambda'][li, d, c0:c0 + 128].rearrange("(c o) -> c o", o=1), writes=[lam])
            S.op('act', lambda e: e.activation(out=c8[:], in_=lam[:], func=AF.Exp, scale=-1.0), reads=[lam], writes=[c8])
            S.op('dve', lambda e: e.tensor_scalar(out=c8[:], in0=c8[:], scalar1=1.0, scalar2=None, op0=ALU.add), reads=[c8], writes=[c8])
            S.op('act', lambda e: e.activation(out=c8[:], in_=c8[:], func=AF.Ln), reads=[c8], writes=[c8])
            S.op('dve', lambda e: e.tensor_scalar(out=c8[:], in0=c8[:], scalar1=-8.0, scalar2=None, op0=ALU.mult), reads=[c8], writes=[c8])
            dwconv_fm(S, xc, xin, cw[:, :], cb[:, 0:1], 4, 1, SEGS, wbuf=cw)
            hs = [h0, h1]
            for d in range(2):
                for (t0, n) in BLOCKS:
                    pa = pss.next(); px = pss.next()
                    S.op('pe', lambda e: e.matmul(pa[:, 0:n], lhsT=wbd[d * 2][:], rhs=xc[:, t0:t0 + n], start=True, stop=True), reads=[wbd[d * 2], xc], writes=[pa])
                    S.op('pe', lambda e: e.matmul(px[:, 0:n], lhsT=wbd[d * 2 + 1][:], rhs=xc[:, t0:t0 + n], start=True, stop=True), reads=[wbd[d * 2 + 1], xc], writes=[px])
                    S.op('act', lambda e: e.activation(out=A[:, t0:t0 + n], in_=pa[:, 0:n], func=AF.Sigmoid, bias=bias[:, d * 2:d * 2 + 1]), reads=[pa, bias], writes=[A])
                    S.op('act', lambda e: e.activation(out=Bv[:, t0:t0 + n], in_=px[:, 0:n], func=AF.Sigmoid, bias=bias[:, d * 2 + 1:d * 2 + 2]), reads=[px, bias], writes=[Bv])
                S.op('act', lambda e: e.activation(out=A[:], in_=A[:], func=AF.Exp, scale=c8[:, d:d + 1]), reads=[A, c8], writes=[A])
                S.op('dve', lambda e: e.tensor_tensor(out=tmp[:], in0=A[:], in1=A[:], op=ALU.mult), reads=[A], writes=[tmp])
                S.op('dve', lambda e: e.tensor_scalar(out=tmp[:], in0=tmp[:], scalar1=-1.0, scalar2=1.0, op0=ALU.mult, op1=ALU.add), reads=[tmp], writes=[tmp])
                S.op('dve', lambda e: e.tensor_scalar(out=tmp[:], in0=tmp[:], scalar1=0.0, scalar2=None, op0=ALU.max), reads=[tmp], writes=[tmp])
                S.op('act', lambda e: e.activation(out=tmp[:], in_=tmp[:], func=AF.Sqrt), reads=[tmp], writes=[tmp])
                S.op('pool', lambda e: e.tensor_tensor(out=Bv[:], in0=Bv[:], in1=xc[:], op=ALU.mult), reads=[Bv, xc], writes=[Bv])
                S.op('dve', lambda e: e.tensor_tensor(out=Bv[:], in0=Bv[:], in1=tmp[:], op=ALU.mult), reads=[Bv, tmp], writes=[Bv])
                h = hs[d]
                if d == 0:
                    S.op('dve', lambda e: e.tensor_tensor_scan(out=h[:, :], data0=A[:, :], data1=Bv[:, :], initial=0.0, op0=ALU.mult, op1=ALU.add),
                         reads=[A, Bv], writes=[h])
                else:
                    S.op('dve', lambda e: e.tensor_tensor_scan(out=h[:, 0:TC][:, ::-1], data0=A[:, 0:TC][:, ::-1], data1=Bv[:, 0:TC][:, ::-1], initial=0.0,
                                                               op0=ALU.mult, op1=ALU.add), reads=[A, Bv], writes=[h])
                    S.op('dve', lambda e: e.tensor_tensor_scan(out=h[:, TC:T][:, ::-1], data0=A[:, TC:T][:, ::-1], data1=Bv[:, TC:T][:, ::-1], initial=h[:, 0:1],
                                                               op0=ALU.mult, op1=ALU.add), reads=[A, Bv, h], writes=[h])
            S.op('pool', lambda e: e.tensor_tensor(out=h0[:], in0=h0[:], in1=h1[:], op=ALU.add), reads=[h0, h1], writes=[h0])
            S.op('dve', lambda e: e.tensor_tensor(out=tmp[:], in0=gate[:], in1=gate[:], op=ALU.mult), reads=[gate], writes=[tmp])
            S.op('dve', lambda e: e.tensor_scalar(out=tmp[:], in0=tmp[:], scalar1=0.044715, scalar2=1.0, op0=ALU.mult, op1=ALU.add), reads=[tmp], writes=[tmp])
            S.op('dve', lambda e: e.tensor_tensor(out=tmp[:], in0=tmp[:], in1=gate[:], op=ALU.mult), reads=[tmp, gate], writes=[tmp])
            S.op('act', lambda e: e.activation(out=tmp[:], in_=tmp[:], func=AF.Sigmoid, scale=1.5957691216), reads=[tmp], writes=[tmp])
            S.op('pool', lambda e: e.tensor_tensor(out=tmp[:], in0=tmp[:], in1=gate[:], op=ALU.mult), reads=[tmp, gate], writes=[tmp])
            S.op('dve', lambda e: e.tensor_tensor(out=h0[:], in0=h0[:], in1=tmp[:], op=ALU.mult), reads=[h0, tmp], writes=[h0])
            S.dma(lruT[c0:c0 + 128, :], h0[:], reads=[h0])


def declare_lru(P):
    P.inp('lru_conv_w', [DEPTH, 4, 256]); P.inp('lru_conv_b', [DEPTH, 256])
    P.inp('lru_wa', [DEPTH, 2, 4, 64, 64]); P.inp('lru_ba', [DEPTH, 2, 256])
    P.inp('lru_wx', [DEPTH, 2, 4, 64, 64]); P.inp('lru_bx', [DEPTH, 2, 256])
    P.inp('lru_lambda', [DEPTH, 2, 256])
    P.tmp('lruT', [256, T])


def host_lru(m, inputs):
    for k in ('lru_conv_w', 'lru_conv_b', 'lru_wa', 'lru_ba', 'lru_wx', 'lru_bx', 'lru_lambda'):
        m[k] = np.ascontiguousarray(inputs[k], np.float32)


BR = [('attT', 'w_br_attn', 4), ('hyT', 'w_br_hyena', 2), ('rwT', 'w_br_rwkv', 2), ('lruT', 'w_br_lru', 2)]


def stage_merge(P, li, need_ctx, src_name, dst_name):
    S = P.S
    projT = P.dr['projT']
    src = P.dr[src_name]
    dst = P.dr[dst_name]
    with S.stage():
        wbr = S.sb('wbr', [128, 10, D], BF16)
        wout = S.sb('wout', [128, 8, D], BF16)
        stg = Rot([S.sb(f'wstg{j}', [128, D]) for j in range(2)])
        ci = 0
        for (_, wn, nch) in BR:
            for c in range(nch):
                st = stg.next()
                S.dma(st[:], P.dr[wn][li, c * 128:(c + 1) * 128, :], writes=[st], q=('sp' if ci % 2 == 0 else 'pool'))
                S.op('pool', lambda e, ci=ci, st=st: e.tensor_copy(out=wbr[:, ci, :], in_=st[:]), reads=[st], writes=[wbr])
                ci += 1
        for c in range(8):
            st = stg.next()
            S.dma(st[:], P.dr['w_out'][li, c * 128:(c + 1) * 128, :], writes=[st], q=('sp' if c % 2 == 0 else 'pool'))
            S.op('pool', lambda e, c=c, st=st: e.tensor_copy(out=wout[:, c, :], in_=st[:]), reads=[st], writes=[wout])
        g1 = S.sb('g1', [128, 2, D])
        for s_ in range(2):
            S.dma(g1[:, s_, :], P.dr[f'modrow{li}'][s_:s_ + 1, 2 * D:3 * D].partition_broadcast(128), writes=[g1])
        yf = Rot([S.sb(f'yf{j}', [128, 10, 512]) for j in range(2)])
        yb = Rot([S.sb(f'yb{j}', [128, 10, 512], BF16) for j in range(2)])
        gts = Rot([S.sb(f'gt{j}', [128, 512]) for j in range(4)])
        sgs = Rot([S.sb(f'sg{j}', [128, 512]) for j in range(3)])
        tms = Rot([S.sb(f'tm{j}', [128, 512]) for j in range(3)])
        macc = Rot([S.sb(f'macc{j}', [128, 512]) for j in range(2)])
        mTs = Rot([S.sb(f'mT{j}', [128, 8, 512], BF16) for j in range(2)])
        pss = Rot([S.ps(f'mps{j}', [128, 512]) for j in range(4)])
        ops_ = Rot([S.ps(f'mops{j}', [128, 512]) for j in range(2)])
        xts = Rot([S.sb(f'mx{j}', [128, D]) for j in range(2)])
        blocks = BLOCKS if need_ctx else BLOCKS[1:]
        for (t0, n) in blocks:
            yfl = yf.next(); ybl = yb.next()
            ci = 0
            for (yn, _, nch) in BR:
                S.dma(yfl[:, ci:ci + nch, 0:n], P.dr[yn][:, t0:t0 + n].rearrange("(c p) t -> p c t", p=128), writes=[yfl], q=('sp' if ci % 4 == 0 else 'pool'))
                ci += nch
            S.op('pool', lambda e: e.tensor_copy(out=ybl[:, :, 0:n], in_=yfl[:, :, 0:n]), reads=[yfl], writes=[ybl])
            mT = mTs.next()
            for fc in range(8):
                ci = 0
                ma = macc.next()
                for bi, (_, _, nch) in enumerate(BR):
                    ps = pss.next()
                    for c in range(nch):
                        S.op('pe', lambda e, c=c, ci=ci: e.matmul(ps[:, 0:n], lhsT=wbr[:, ci + c, fc * 128:(fc + 1) * 128], rhs=ybl[:, ci + c, 0:n],
                                                                 start=(c == 0), stop=(c == nch - 1)), reads=[wbr, ybl], writes=[ps])
                    ci += nch
                    gt = gts.next()
                    r0 = O_GT + bi * D + fc * 128
                    S.dma(gt[:, 0:n], projT[r0:r0 + 128, t0:t0 + n], writes=[gt], q=('sp' if bi % 2 == 0 else 'act'))
                    sg = sgs.next()
                    S.op('act', lambda e: e.activation(out=sg[:, 0:n], in_=gt[:, 0:n], func=AF.Sigmoid), reads=[gt], writes=[sg])
                    if bi == 0:
                        S.op('dve', lambda e: e.tensor_tensor(out=ma[:, 0:n], in0=ps[:, 0:n], in1=sg[:, 0:n], op=ALU.mult), reads=[ps, sg], writes=[ma])
                    else:
                        tm = tms.next()
                        S.op('dve', lambda e: e.tensor_tensor(out=tm[:, 0:n], in0=ps[:, 0:n], in1=sg[:, 0:n], op=ALU.mult), reads=[ps, sg], writes=[tm])
                        if bi < 3:
                            S.op('pool', lambda e: e.tensor_tensor(out=ma[:, 0:n], in0=ma[:, 0:n], in1=tm[:, 0:n], op=ALU.add), reads=[ma, tm], writes=[ma])
                        else:
                            S.op('pool', lambda e: e.tensor_tensor(out=mT[:, fc, 0:n], in0=ma[:, 0:n], in1=tm[:, 0:n], op=ALU.add), reads=[ma, tm], writes=[mT])
            s_ = 0 if t0 < TC else 1
            for st_ in range(n // 128):
                xt = xts.next()
                S.dma(xt[:], src[t0 + st_ * 128:t0 + (st_ + 1) * 128, :], writes=[xt])
                for half in range(2):
                    po = ops_.next()
                    for fc in range(8):
                        S.op('pe', lambda e, fc=fc: e.matmul(po[:, :], lhsT=mT[:, fc, st_ * 128:(st_ + 1) * 128], rhs=wout[:, fc, half * 512:(half + 1) * 512],
                                                             start=(fc == 0), stop=(fc == 7)), reads=[mT, wout], writes=[po])
                    tm = tms.next()
                    S.op('dve', lambda e: e.tensor_tensor(out=tm[:, :], in0=po[:, :], in1=g1[:, s_, half * 512:(half + 1) * 512], op=ALU.mult), reads=[po, g1], writes=[tm])
                    S.op('pool', lambda e: e.tensor_tensor(out=xt[:, half * 512:(half + 1) * 512], in0=xt[:, half * 512:(half + 1) * 512], in1=tm[:, :], op=ALU.add),
                         reads=[xt, tm], writes=[xt])
                S.dma(dst[t0 + st_ * 128:t0 + (st_ + 1) * 128, :], xt[:], reads=[xt], q='act')


def declare_merge(P):
    P.inp('w_br_attn', [DEPTH, 512, D]); P.inp('w_br_hyena', [DEPTH, 256, D])
    P.inp('w_br_rwkv', [DEPTH, 256, D]); P.inp('w_br_lru', [DEPTH, 256, D])
    P.inp('w_out', [DEPTH, D, D])
    P.tmp('hyT', [256, T]); P.tmp('rwT', [256, T])
    P.tmp('x_mid', [T, D])


def host_merge(m, inputs):
    for k in ('w_br_attn', 'w_br_hyena', 'w_br_rwkv', 'w_br_lru', 'w_out'):
        m[k] = np.ascontiguousarray(inputs[k], np.float32)


def stage_moe(P, li, need_ctx, src_name, dst_name, dst_is_out=False):
    S = P.S
    src = P.dr[src_name]
    dst = P.dr[dst_name]
    if need_ctx:
        groups = [(0, 10), (10, 22), (22, 34)]
    else:
        groups = [(2, 12), (12, 23), (23, 34)]
    GMAX = 12
    with S.stage():
        ident = S.sb('ident', [128, 128], BF16)
        S.dma(ident[:], P.dr['ident_bf'][:, :], writes=[ident])
        modT = load_modT(P, li)
        g2 = S.sb('g2', [128, 2, D])
        for s_ in range(2):
            S.dma(g2[:, s_, :], P.dr[f'modrow{li}'][s_:s_ + 1, 5 * D:6 * D].partition_broadcast(128), writes=[g2])
        wrf = S.sb('wrf', [128, 8, 20]); wrb = S.sb('wrb', [128, 8, 20], BF16)
        S.dma(wrf[:, :, 0:4], P.dr['moe_w_grp'][li].rearrange("(k p) g -> p k g", p=128), writes=[wrf])
        S.dma(wrf[:, :, 4:20], P.dr['moe_w_rt'][li].rearrange("(k p) g -> p k g", p=128), writes=[wrf])
        S.op('dve', lambda e: e.tensor_copy(out=wrb[:], in_=wrf[:]), reads=[wrf], writes=[wrb])
        rb = S.sb('rb', [128, 20])
        S.dma(rb[:, 0:4], P.dr['moe_b_grp'][li:li + 1, :].partition_broadcast(128), writes=[rb])
        S.dma(rb[:, 4:20], P.dr['moe_b_rt'][li:li + 1, :].partition_broadcast(128), writes=[rb])
        h2T = S.sb('h2T', [128, 8, GMAX * 128], BF16)
        acc = S.sb('acc', [128, GMAX, D])
        comb = S.sb('comb', [128, GMAX, 16])
        rt = {nm: S.sb(f'rt_{nm}', shp) for nm, shp in (('lg', [128, 20]), ('mx', [128, 4]), ('ge', [128, 4]), ('gm', [128, 4]), ('m16', [128, 16]),
                                                         ('ml', [128, 16]), ('eq', [128, 16]), ('ml2', [128, 16]), ('ex', [128, 16]))}
        wst = Rot([S.sb(f'wst{j}', [128, 4, 512]) for j in range(2)])
        w1b = Rot([S.sb(f'w1b{j}', [128, 8, 512], BF16) for j in range(2)])
        w3b = Rot([S.sb(f'w3b{j}', [128, 8, 512], BF16) for j in range(2)])
        w2b = Rot([S.sb(f'w2b{j}', [128, 4, D], BF16) for j in range(2)])
        sil = Rot([S.sb(f'sil{j}', [128, 512]) for j in range(3)])
        actb = Rot([S.sb(f'actb{j}', [128, 4, 512], BF16) for j in range(2)])
        pss = Rot([S.ps(f'eps{j}', [128, 512]) for j in range(4)])
        pys = Rot([S.ps(f'yps{j}', [128, 512]) for j in range(2)])
        prs = Rot([S.ps(f'rps{j}', [128, 32]) for j in range(1)])
        xts = Rot([S.sb(f'ox{j}', [128, D]) for j in range(2)])
        dq = [0]

        def load_cast(dst_tile, dram_view, nk):
            cols = dram_view.shape[2]
            for k0 in range(0, nk, 4):
                for c0 in range(0, cols, 512):
                    st = wst.next()
                    S.dma(st[:, :, :], dram_view[:, k0:k0 + 4, c0:c0 + 512], writes=[st], q=('sp' if dq[0] % 2 == 0 else 'pool'))
                    dq[0] += 1
                    S.op('pool', lambda e, st=st, k0=k0, c0=c0: e.tensor_copy(out=dst_tile[:, k0:k0 + 4, c0:c0 + 512], in_=st[:, :, :]), reads=[st], writes=[dst_tile])

        NC_ = norm_ctx(S, npt=1)
        for (ga, gb) in groups:
            ng = gb - ga
            norm_transpose(P, src, modT, 24, 32, h2T, ident, tiles=range(ga, gb), dst0=ga * 128, C=NC_)
            for ti in range(ng):
                pr = prs.next()
                for k in range(8):
                    S.op('pe', lambda e, k=k: e.matmul(pr[:, 0:20], lhsT=h2T[:, k, ti * 128:(ti + 1) * 128], rhs=wrb[:, k, :], start=(k == 0), stop=(k == 7)),
                         reads=[h2T, wrb], writes=[pr])
                lg, mx, ge, gm, m16, ml, eq, ml2, ex = (rt[n] for n in ('lg', 'mx', 'ge', 'gm', 'm16', 'ml', 'eq', 'ml2', 'ex'))
                V = lambda fn, rd, wr: S.op('dve', fn, reads=rd, writes=wr)
                V(lambda e: e.tensor_tensor(out=lg[:], in0=pr[:, 0:20], in1=rb[:], op=ALU.add), [pr, rb], [lg])
                V(lambda e: e.reduce_max(out=mx[:, 0:1], in_=lg[:, 0:4], axis=AX.X), [lg], [mx])
                V(lambda e: e.tensor_scalar(out=gm[:], in0=lg[:, 0:4], scalar1=mx[:, 0:1], scalar2=None, op0=ALU.is_equal), [lg, mx], [gm])
                V(lambda e: e.tensor_scalar(out=ge[:], in0=lg[:, 0:4], scalar1=mx[:, 0:1], scalar2=None, op0=ALU.subtract), [lg, mx], [ge])
                S.op('act', lambda e: e.activation(out=ge[:], in_=ge[:], func=AF.Exp), reads=[ge], writes=[ge])
                V(lambda e: e.reduce_sum(out=mx[:, 1:2], in_=ge[:], axis=AX.X), [ge], [mx])
                V(lambda e: e.tensor_copy(out=m16[:].rearrange("p (g e) -> p g e", e=4), in_=gm[:].unsqueeze(2).to_broadcast([128, 4, 4])), [gm], [m16])
                V(lambda e: e.tensor_scalar(out=ml[:], in0=m16[:], scalar1=-1.0, scalar2=1e30, op0=ALU.add, op1=ALU.mult), [m16], [ml])
                V(lambda e: e.tensor_tensor(out=ml[:], in0=ml[:], in1=lg[:, 4:20], op=ALU.add), [ml, lg], [ml])
                V(lambda e: e.reduce_max(out=mx[:, 2:3], in_=ml[:], axis=AX.X), [ml], [mx])
                V(lambda e: e.tensor_scalar(out=eq[:], in0=ml[:], scalar1=mx[:, 2:3], scalar2=None, op0=ALU.is_equal), [ml, mx], [eq])
                V(lambda e: e.scalar_tensor_tensor(out=ml2[:], in0=eq[:], scalar=-1e30, in1=ml[:], op0=ALU.mult, op1=ALU.add), [eq, ml], [ml2])
                V(lambda e: e.reduce_max(out=mx[:, 3:4], in_=ml2[:], axis=AX.X), [ml2], [mx])
                V(lambda e: e.scalar_tensor_tensor(out=eq[:], in0=ml2[:], scalar=mx[:, 3:4], in1=eq[:], op0=ALU.is_equal, op1=ALU.add), [ml2, mx, eq], [eq])
                V(lambda e: e.tensor_scalar(out=ex[:], in0=ml[:], scalar1=mx[:, 2:3], scalar2=-80.0, op0=ALU.subtract, op1=ALU.max), [ml, mx], [ex])
                S.op('act', lambda e: e.activation(out=ex[:], in_=ex[:], func=AF.Exp), reads=[ex], writes=[ex])
                V(lambda e: e.tensor_tensor(out=ex[:], in0=ex[:], in1=eq[:], op=ALU.mult), [ex, eq], [ex])
                V(lambda e: e.reduce_sum(out=mx[:, 2:3], in_=ex[:], axis=AX.X), [ex], [mx])
                V(lambda e: e.tensor_tensor(out=mx[:, 2:3], in0=mx[:, 2:3], in1=mx[:, 1:2], op=ALU.mult), [mx], [mx])
                V(lambda e: e.reciprocal(out=mx[:, 2:3], in_=mx[:, 2:3]), [mx], [mx])
                V(lambda e, ti=ti: e.tensor_scalar(out=comb[:, ti, :], in0=ex[:], scalar1=mx[:, 2:3], scalar2=None, op0=ALU.mult), [ex, mx], [comb])
            nt = ng * 128
            tblocks = [(b0, min(512, nt - b0)) for b0 in range(0, nt, 512)]
            for ex_i in range(16):
                w1 = w1b.next(); w3 = w3b.next(); w2 = w2b.next()
                load_cast(w1, P.dr['moe_w1'][li, ex_i].rearrange("(k p) h -> p k h", p=128), 8)
                load_cast(w3, P.dr['moe_w3'][li, ex_i].rearrange("(k p) h -> p k h", p=128), 8)
                load_cast(w2, P.dr['moe_w2'][li, ex_i].rearrange("(k p) f -> p k f", p=128), 4)
                for (b0, n) in tblocks:
                    ab = actb.next()
                    for hc in range(4):
                        p1 = pss.next(); p3 = pss.next()
                        for k in range(8):
                            S.op('pe', lambda e, k=k: e.matmul(p1[:, 0:n], lhsT=w1[:, k, hc * 128:(hc + 1) * 128], rhs=h2T[:, k, b0:b0 + n], start=(k == 0), stop=(k == 7)),
                                 reads=[w1, h2T], writes=[p1])
                        for k in range(8):
                            S.op('pe', lambda e, k=k: e.matmul(p3[:, 0:n], lhsT=w3[:, k, hc * 128:(hc + 1) * 128], rhs=h2T[:, k, b0:b0 + n], start=(k == 0), stop=(k == 7)),
                                 reads=[w3, h2T], writes=[p3])
                        sl = sil.next()
                        S.op('act', lambda e: e.activation(out=sl[:, 0:n], in_=p1[:, 0:n], func=AF.Silu), reads=[p1], writes=[sl])
                        S.op('dve', lambda e, hc=hc: e.tensor_tensor(out=ab[:, hc, 0:n], in0=sl[:, 0:n], in1=p3[:, 0:n], op=ALU.mult), reads=[sl, p3], writes=[ab])
                    for st_ in range(n // 128):
                        ti = b0 // 128 + st_
                        for half in range(2):
                            py = pys.next()
                            for hc in range(4):
                                S.op('pe', lambda e, hc=hc: e.matmul(py[:, :], lhsT=ab[:, hc, st_ * 128:(st_ + 1) * 128], rhs=w2[:, hc, half * 512:(half + 1) * 512],
                                                                     start=(hc == 0), stop=(hc == 3)), reads=[ab, w2], writes=[py])
                            a_sl = acc[:, ti, half * 512:(half + 1) * 512]
                            if ex_i == 0:
                                S.op('dve', lambda e: e.tensor_scalar(out=a_sl, in0=py[:, :], scalar1=comb[:, ti, ex_i:ex_i + 1], scalar2=None, op0=ALU.mult),
                                     reads=[py, comb], writes=[acc])
                            else:
                                S.op('dve', lambda e: e.scalar_tensor_tensor(out=a_sl, in0=py[:, :], scalar=comb[:, ti, ex_i:ex_i + 1], in1=a_sl, op0=ALU.mult, op1=ALU.add),
                                     reads=[py, comb, acc], writes=[acc])
            for ti in range(ng):
                gt = ga + ti
                s_ = 0 if gt < 2 else 1
                xt = xts.next()
                S.dma(xt[:], src[gt * 128:(gt + 1) * 128, :], writes=[xt])
                S.op('pool', lambda e: e.tensor_tensor(out=acc[:, ti, :], in0=acc[:, ti, :], in1=g2[:, s_, :], op=ALU.mult), reads=[acc, g2], writes=[acc])
                S.op('dve', lambda e: e.tensor_tensor(out=xt[:], in0=xt[:], in1=acc[:, ti, :], op=ALU.add), reads=[xt, acc], writes=[xt])
                if dst_is_out:
                    if gt >= 2:
                        S.dma(dst[(gt - 2) * 128:(gt - 1) * 128, :], xt[:], reads=[xt], q='act', is_out=True)
                else:
                    S.dma(dst[gt * 128:(gt + 1) * 128, :], xt[:], reads=[xt], q='act')


def declare_moe(P):
    P.inp('moe_w_grp', [DEPTH, D, 4]); P.inp('moe_b_grp', [DEPTH, 4])
    P.inp('moe_w_rt', [DEPTH, D, 16]); P.inp('moe_b_rt', [DEPTH, 16])
    P.inp('moe_w1', [DEPTH, 16, D, 512]); P.inp('moe_w3', [DEPTH, 16, D, 512]); P.inp('moe_w2', [DEPTH, 16, 512, D])
    P.tmp('x_l0', [T, D])


def host_moe(m, inputs):
    for k in ('moe_w_grp', 'moe_b_grp', 'moe_w_rt', 'moe_b_rt', 'moe_w1', 'moe_w3', 'moe_w2'):
        m[k] = np.ascontiguousarray(inputs[k], np.float32)


def hyena_consts(n):
    import ml_dtypes
    t = np.arange(n, dtype=np.float32) / np.float32(n)
    bands = np.arange(1, 17, dtype=np.float32)
    ang = (np.float32(2.0 * math.pi) * t[:, None] * bands).astype(np.float32)
    feat = np.concatenate([t[:, None], np.sin(ang), np.cos(ang)], -1).astype(np.float32)
    deltas = np.linspace(-math.log(1e-2) / 1.5, -math.log(1e-2) / 0.3, 256, dtype=np.float32)
    dec = np.exp(-t[:, None] * deltas).astype(np.float32)
    nk = n // 128 + 1
    NP = nk * 128
    idx = np.arange(NP, dtype=np.int64)
    prod = (idx[:, None] * idx[None, :]) % (2 * n)
    angw = 2.0 * math.pi * prod.astype(np.float64) / (2 * n)
    valid = (idx <= n)
    m = (valid[:, None] & valid[None, :])
    wc = np.where(m, np.cos(angw), 0.0); ws = np.where(m, np.sin(angw), 0.0)
    def tile(w):
        return np.ascontiguousarray(w.reshape(nk, 128, nk, 128).transpose(2, 1, 0, 3)).astype(ml_dtypes.bfloat16)
    wk = np.full(NP, 1.0 / n, np.float32); wk[0] = 0.5 / n; wk[n] = 0.5 / n; wk[n + 1:] = 0.0
    wkT = np.ascontiguousarray(wk.reshape(nk, 128).T)
    return dict(featT=np.ascontiguousarray(feat.T), dec=dec, wc=tile(wc), ws=tile(ws), wk=wkT)


def hyena_seq(P, li, n, t_off, sfx):
    S = P.S
    projT = P.dr['projT']
    hyT = P.dr['hyT']
    hyz = P.dr['hyz']
    hspec = P.dr['hspec']
    NT = n // 128
    NK = NT + 1
    wc_d = P.dr['hy_wc' + sfx]; ws_d = P.dr['hy_ws' + sfx]
    kparts = [128] * NT + [1]
    TWO_PI = 2.0 * math.pi
    with S.stage():
        xin = Rot([S.sb(f'hxin{j}', [128, n]) for j in range(2)])
        zo = Rot([S.sb(f'hzo{j}', [128, n]) for j in range(2)])
        cw = S.sb('hcw', [128, 6, 3]); cb = S.sb('hcb', [128, 6])
        for j in range(3):
            S.dma(cw[:, :, j], P.dr['hy_conv_w'][li, j].rearrange("(c p) -> p c", p=128), writes=[cw], allow_slow_non_contiguous=True)
        S.dma(cb[:], P.dr['hy_conv_b'][li].rearrange("(c p) -> p c", p=128), writes=[cb], allow_slow_non_contiguous=True)
        for c in range(6):
            xi = xin.next(); z = zo.next()
            S.dma(xi[:], projT[O_HY + c * 128:O_HY + (c + 1) * 128, t_off:t_off + n], writes=[xi], q=('sp' if c % 2 == 0 else 'pool'))
            dwconv_fm(S, z, xi, cw[:, c, :], cb[:, c:c + 1], 3, 1, [(0, n)], eng='dve', wbuf=cw)
            S.dma(hyz[c * 128:(c + 1) * 128, t_off:t_off + n], z[:], reads=[z], q='act')
    with S.stage():
        featT = S.sb('featT', [33, n]); f1 = S.sb('f1', [33, 64]); f2 = S.sb('f2', [64, 64]); f3 = S.sb('f3', [64, 1024])
        fb = S.sb('fb', [64, 2]); h1T = S.sb('h1T', [64, n]); h2T = S.sb('h2T', [64, n])
        S.dma(featT[:], P.dr['hy_featT' + sfx][:, :], writes=[featT])
        S.dma(f1[:], P.dr['hy_f1'][li], writes=[f1]); S.dma(f2[:], P.dr['hy_f2'][li], writes=[f2]); S.dma(f3[:], P.dr['hy_f3'][li], writes=[f3])
        S.dma(fb[:, 0:1], P.dr['hy_fb1'][li].rearrange("(c o) -> c o", o=1), writes=[fb])
        S.dma(fb[:, 1:2], P.dr['hy_fb2'][li].rearrange("(c o) -> c o", o=1), writes=[fb])
        pss = Rot([S.ps(f'hps{j}', [128, 512]) for j in range(4)])
        tmpf = Rot([S.sb(f'htmp{j}', [64, 512]) for j in range(2)])
        tmpq = Rot([S.sb(f'htmq{j}', [64, 512]) for j in range(2)])
        tmpi = Rot([S.sb(f'htmi{j}', [64, 512], mybir.dt.int32) for j in range(2)])
        for (src, wt, dstT, bi) in ((featT, f1, h1T, 0), (h1T, f2, h2T, 1)):
            for b0 in range(0, n, 512):
                nb = min(512, n - b0)
                ps = pss.next()
                S.op('pe', lambda e: e.matmul(ps[0:64, 0:nb], lhsT=wt[:], rhs=src[:, b0:b0 + nb], start=True, stop=True), reads=[wt, src], writes=[ps])
                tm = tmpf.next()
                qi = tmpi.next(); qf = tmpq.next()
                S.op('dve', lambda e: e.tensor_scalar(out=tm[:, 0:nb], in0=ps[0:64, 0:nb], scalar1=fb[:, bi:bi + 1], scalar2=None, op0=ALU.add), reads=[ps, fb], writes=[tm])
                S.op('dve', lambda e: e.tensor_scalar(out=qf[:, 0:nb], in0=tm[:, 0:nb], scalar1=1.0 / TWO_PI, scalar2=None, op0=ALU.mult), reads=[tm], writes=[qf])
                S.op('dve', lambda e: e.tensor_copy(out=qi[:, 0:nb], in_=qf[:, 0:nb]), reads=[qf], writes=[qi])
                S.op('dve', lambda e: e.tensor_copy(out=qf[:, 0:nb], in_=qi[:, 0:nb]), reads=[qi], writes=[qf])
                S.op('dve', lambda e: e.scalar_tensor_tensor(out=tm[:, 0:nb], in0=qf[:, 0:nb], scalar=-TWO_PI, in1=tm[:, 0:nb], op0=ALU.mult, op1=ALU.add), reads=[qf, tm], writes=[tm])
                S.op('dve', lambda e: e.tensor_scalar(out=qf[:, 0:nb], in0=tm[:, 0:nb], scalar1=math.pi, scalar2=-TWO_PI, op0=ALU.is_gt, op1=ALU.mult), reads=[tm], writes=[qf])
                S.op('dve', lambda e: e.tensor_tensor(out=tm[:, 0:nb], in0=tm[:, 0:nb], in1=qf[:, 0:nb], op=ALU.add), reads=[tm, qf], writes=[tm])
                S.op('dve', lambda e: e.tensor_scalar(out=qf[:, 0:nb], in0=tm[:, 0:nb], scalar1=-math.pi, scalar2=TWO_PI, op0=ALU.is_lt, op1=ALU.mult), reads=[tm], writes=[qf])
                S.op('dve', lambda e: e.tensor_tensor(out=tm[:, 0:nb], in0=tm[:, 0:nb], in1=qf[:, 0:nb], op=ALU.add), reads=[tm, qf], writes=[tm])
                S.op('act', lambda e: e.activation(out=dstT[:, b0:b0 + nb], in_=tm[:, 0:nb], func=AF.Sin), reads=[tm], writes=[dstT])
        hbf = S.sb('hbf', [128, NT, 1024], BF16)
        ones = S.sb('hones', [128, 128])
        S.op('pool', lambda e: e.memset(ones[:], 1.0), writes=[ones])
        decs = Rot([S.sb(f'hdec{j}', [128, 256]) for j in range(2)])
        hraw = Rot([S.sb(f'hraw{j}', [128, 1024]) for j in range(2)])
        habs = Rot([S.sb(f'habs{j}', [128, 1024]) for j in range(2)])
        l1ps = [S.ps(f'l1ps{j}', [128, 512]) for j in range(2)]
        for tt in range(NT):
            dc = decs.next()
            S.dma(dc[:], P.dr['hy_dec' + sfx][tt * 128:(tt + 1) * 128, :], writes=[dc])
            hr = hraw.next(); ha = habs.next()
            for half in range(2):
                ps = pss.next()
                S.op('pe', lambda e: e.matmul(ps[:, :], lhsT=h2T[:, tt * 128:(tt + 1) * 128], rhs=f3[:, half * 512:(half + 1) * 512], start=True, stop=True),
                     reads=[h2T, f3], writes=[ps])
                S.op('dve', lambda e: e.tensor_tensor(out=hr[:, half * 512:(half + 1) * 512].rearrange("p (a c) -> p a c", a=2),
                                                      in0=ps[:, :].rearrange("p (a c) -> p a c", a=2),
                                                      in1=dc[:].unsqueeze(1).to_broadcast([128, 2, 256]), op=ALU.mult), reads=[ps, dc], writes=[hr])
            S.op('act', lambda e: e.activation(out=ha[:], in_=hr[:], func=AF.Abs), reads=[hr], writes=[ha])
            for half in range(2):
                S.op('pe', lambda e: e.matmul(l1ps[half][:, :], lhsT=ones[:], rhs=ha[:, half * 512:(half + 1) * 512], start=(tt == 0), stop=(tt == NT - 1)),
                     reads=[ones, ha], writes=[l1ps[half]])
            if tt == 0:
                for o in range(2):
                    S.op('dve', lambda e, o=o: e.memset(hr[0:1, o * 512 + 256:o * 512 + 512], 0.0), reads=[ha], writes=[hr])
            S.op('act', lambda e: e.copy(out=hbf[:, tt, :], in_=hr[:]), reads=[hr], writes=[hbf])
        rl1 = S.sb('rl1', [128, 2, 256])
        l1sb = S.sb('l1sb', [128, 2, 256])
        for o in range(2):
            S.op('act', lambda e, o=o: e.copy(out=l1sb[:, o, :], in_=l1ps[o][:, 0:256]), reads=[l1ps[o]], writes=[l1sb])
            S.op('dve', lambda e, o=o: e.tensor_tensor(out=rl1[:, o, :], in0=l1sb[:, o, :], in1=l1ps[o][:, 256:512], op=ALU.add), reads=[l1ps[o], l1sb], writes=[rl1])
        S.op('dve', lambda e: e.reciprocal(out=rl1[:], in_=rl1[:]), reads=[rl1], writes=[rl1])
        wk = S.sb('hwk', [128, NK])
        S.dma(wk[:], P.dr['hy_wk' + sfx][:, :], writes=[wk])
        wcs = Rot([S.sb(f'hwc{j}', [128, NK, 128], BF16) for j in range(2)])
        wss = Rot([S.sb(f'hws{j}', [128, NK, 128], BF16) for j in range(2)])
        spo = Rot([S.sb(f'hspo{j}', [128, 2, 2, 256]) for j in range(2)])
        for kt in range(NK):
            kp = kparts[kt]
            wct = wcs.next(); wst = wss.next()
            S.dma(wct[:], wc_d[kt], writes=[wct]); S.dma(wst[:], ws_d[kt], writes=[wst], q='pool')
            so = spo.next()
            pa = [pss.next(), pss.next()]
            for half in range(2):
                for tc in range(NT):
                    S.op('pe', lambda e, tc=tc: e.matmul(pa[half][0:kp, :], lhsT=wct[:, tc, 0:kp], rhs=hbf[:, tc, half * 512:(half + 1) * 512],
                                                         start=(tc == 0), stop=(tc == NT - 1)), reads=[wct, hbf], writes=[pa[half]])
            for o in range(2):
                S.op('act', lambda e, o=o: e.copy(out=so[0:kp, 0, o, :], in_=pa[o][0:kp, 0:256]), reads=[pa[o]], writes=[so])
                S.op('dve', lambda e, o=o: e.tensor_tensor(out=so[0:kp, 0, o, :], in0=so[0:kp, 0, o, :], in1=pa[o][0:kp, 256:512], op=ALU.add), reads=[pa[o], so], writes=[so])
            pb = [pss.next(), pss.next()]
            for half in range(2):
                for tc in range(NT):
                    S.op('pe', lambda e, tc=tc: e.matmul(pb[half][0:kp, :], lhsT=wst[:, tc, 0:kp], rhs=hbf[:, tc, half * 512:(half + 1) * 512],
                                                         start=(tc == 0), stop=(tc == NT - 1)), reads=[wst, hbf], writes=[pb[half]])
            for o in range(2):
                S.op('act', lambda e, o=o: e.copy(out=so[0:kp, 1, o, :], in_=pb[o][0:kp, 256:512]), reads=[pb[o]], writes=[so])
                S.op('dve', lambda e, o=o: e.tensor_tensor(out=so[0:kp, 1, o, :], in0=so[0:kp, 1, o, :], in1=pb[o][0:kp, 0:256], op=ALU.subtract), reads=[pb[o], so], writes=[so])
            for ri in range(2):
                S.op('dve', lambda e, ri=ri: e.scalar_tensor_tensor(out=so[0:kp, ri, :, :], in0=so[0:kp, ri, :, :], scalar=wk[0:kp, kt:kt + 1], in1=rl1[0:kp, :, :],
                                                                    op0=ALU.mult, op1=ALU.mult), reads=[so, wk, rl1], writes=[so])
            S.dma(hspec[kt, 0:kp].rearrange("p a o c -> p (a o c)"), so[0:kp].rearrange("p a o c -> p (a o c)"), reads=[so], q='act')
    with S.stage():
        identf = S.sb('identf', [128, 128])
        S.dma(identf[:], P.dr['ident_f'][:, :], writes=[identf])
        skip = S.sb('hskip', [128, 2, 256])
        for o in range(2):
            S.dma(skip[:, o, :], P.dr['hy_skip'][li, o:o + 1, :].partition_broadcast(128), writes=[skip])
        zf = S.sb('zf', [128, NT, 256]); zb = S.sb('zb', [128, NT, 256], BF16)
        y1 = S.sb('y1', [128, NT, 256])
        Pq = S.sb('Pq', [128, NK, 256], BF16); Qq = S.sb('Qq', [128, NK, 256], BF16)
        fms = Rot([S.sb(f'hfm{j}', [128, 128]) for j in range(4)])
        tps = Rot([S.ps(f'htp{j}', [128, 256]) for j in range(2)])
        pss = Rot([S.ps(f'hsp{j}', [128, 256]) for j in range(4)])
        wcs = Rot([S.sb(f'hwc{j}', [128, NK, 128], BF16) for j in range(2)])
        wss = Rot([S.sb(f'hws{j}', [128, NK, 128], BF16) for j in range(2)])
        hsp = Rot([S.sb(f'hsp_{j}', [128, 2, 2, 256]) for j in range(2)])
        tA = Rot([S.sb(f'htA{j}', [128, 256]) for j in range(2)]); tB = Rot([S.sb(f'htB{j}', [128, 256]) for j in range(2)])
        gtm = Rot([S.sb(f'hgt{j}', [128, 256]) for j in range(2)])
        yo = Rot([S.sb(f'hyo{j}', [128, 256]) for j in range(2)])
        ofm = Rot([S.sb(f'hofm{j}', [128, 128]) for j in range(2)])

        def to_tm(row0, tt, dst_ap, dstbuf):
            tp = tps.next()
            for c in range(2):
                fm = fms.next()
                S.dma(fm[:], hyz[row0 + c * 128:row0 + (c + 1) * 128, t_off + tt * 128:t_off + (tt + 1) * 128], writes=[fm], q=('sp' if c == 0 else 'pool'))
                S.op('pe', lambda e, c=c: e.transpose(out=tp[:, c * 128:(c + 1) * 128], in_=fm[:], identity=identf[:]), reads=[fm, identf], writes=[tp])
            S.op('act', lambda e: e.copy(out=dst_ap, in_=tp[:, :]), reads=[tp], writes=[dstbuf])

        for tt in range(NT):
            to_tm(0, tt, zf[:, tt, :], zf)
        S.op('pool', lambda e: e.tensor_copy(out=zb[:], in_=zf[:]), reads=[zf], writes=[zb])
        for o in range(2):
            zin_f = zf if o == 0 else y1
            for kt in range(NK):
                kp = kparts[kt]
                wct = wcs.next(); wst = wss.next()
                S.dma(wct[:], wc_d[kt], writes=[wct]); S.dma(wst[:], ws_d[kt], writes=[wst], q='pool')
                hs = hsp.next()
                S.dma(hs[0:kp].rearrange("p a o c -> p (a o c)"), hspec[kt, 0:kp].rearrange("p a o c -> p (a o c)"), writes=[hs], q='act')
                pa = pss.next(); pb = pss.next()
                for tc in range(NT):
                    S.op('pe', lambda e, tc=tc: e.matmul(pa[0:kp, :], lhsT=wct[:, tc, 0:kp], rhs=zb[:, tc, :], start=(tc == 0), stop=(tc == NT - 1)), reads=[wct, zb], writes=[pa])
                for tc in range(NT):
                    S.op('pe', lambda e, tc=tc: e.matmul(pb[0:kp, :], lhsT=wst[:, tc, 0:kp], rhs=zb[:, tc, :], start=(tc == 0), stop=(tc == NT - 1)), reads=[wst, zb], writes=[pb])
                a_ = tA.next(); b_ = tB.next()
                S.op('dve', lambda e: e.tensor_tensor(out=a_[0:kp], in0=pa[0:kp, :], in1=hs[0:kp, 0, o, :], op=ALU.mult), reads=[pa, hs], writes=[a_])
                S.op('dve', lambda e: e.tensor_tensor(out=b_[0:kp], in0=pb[0:kp, :], in1=hs[0:kp, 1, o, :], op=ALU.mult), reads=[pb, hs], writes=[b_])
                S.op('pool', lambda e: e.tensor_tensor(out=Pq[0:kp, kt, :], in0=a_[0:kp], in1=b_[0:kp], op=ALU.add), reads=[a_, b_], writes=[Pq])
                a2 = tA.next(); b2 = tB.next()
                S.op('dve', lambda e: e.tensor_tensor(out=b2[0:kp], in0=pb[0:kp, :], in1=hs[0:kp, 0, o, :], op=ALU.mult), reads=[pb, hs], writes=[b2])
                S.op('dve', lambda e: e.tensor_tensor(out=a2[0:kp], in0=pa[0:kp, :], in1=hs[0:kp, 1, o, :], op=ALU.mult), reads=[pa, hs], writes=[a2])
                S.op('pool', lambda e: e.tensor_tensor(out=Qq[0:kp, kt, :], in0=b2[0:kp], in1=a2[0:kp], op=ALU.subtract), reads=[a2, b2], writes=[Qq])
            for tt in range(NT):
                wct = wcs.next(); wst = wss.next()
                S.dma(wct[:], wc_d[tt], writes=[wct]); S.dma(wst[:], ws_d[tt], writes=[wst], q='pool')
                py = pss.next()
                for kc in range(NK):
                    kp = kparts[kc]
                    S.op('pe', lambda e, kc=kc, kp=kp: e.matmul(py[:, :], lhsT=wct[0:kp, kc, :], rhs=Pq[0:kp, kc, :], start=(kc == 0), stop=False), reads=[wct, Pq], writes=[py])
                    S.op('pe', lambda e, kc=kc, kp=kp: e.matmul(py[:, :], lhsT=wst[0:kp, kc, :], rhs=Qq[0:kp, kc, :], start=False, stop=(kc == NK - 1)), reads=[wst, Qq], writes=[py])
                g = gtm.next()
                to_tm(256 * (o + 1), tt, g[:, :], g)
                yy = yo.next()
                S.op('dve', lambda e: e.tensor_tensor(out=yy[:], in0=zin_f[:, tt, :], in1=skip[:, o, :], op=ALU.mult), reads=[zin_f, skip], writes=[yy])
                S.op('dve', lambda e: e.tensor_tensor(out=yy[:], in0=yy[:], in1=py[:, :], op=ALU.add), reads=[yy, py], writes=[yy])
                if o == 0:
                    S.op('pool', lambda e: e.tensor_tensor(out=y1[:, tt, :], in0=yy[:], in1=g[:], op=ALU.mult), reads=[yy, g], writes=[y1])
                else:
                    S.op('pool', lambda e: e.tensor_tensor(out=yy[:], in0=yy[:], in1=g[:], op=ALU.mult), reads=[yy, g], writes=[yy])
                    for c in range(2):
                        tp = tps.next()
                        S.op('pe', lambda e, c=c: e.transpose(out=tp[:, 0:128], in_=yy[:, c * 128:(c + 1) * 128], identity=identf[:]), reads=[yy, identf], writes=[tp])
                        of = ofm.next()
                        S.op('act', lambda e: e.copy(out=of[:], in_=tp[:, 0:128]), reads=[tp], writes=[of])
                        S.dma(hyT[c * 128:(c + 1) * 128, t_off + tt * 128:t_off + (tt + 1) * 128], of[:], reads=[of], q='act')
            if o == 0:
                S.op('pool', lambda e: e.tensor_copy(out=zb[:], in_=y1[:]), reads=[y1], writes=[zb])


def stage_hyena(P, li, need_ctx):
    hyena_seq(P, li, TX, TC, '')
    if need_ctx:
        hyena_seq(P, li, TC, 0, '_c')


def declare_hyena(P):
    P.inp('hy_conv_w', [DEPTH, 3, 768]); P.inp('hy_conv_b', [DEPTH, 768])
    P.inp('hy_f1', [DEPTH, 33, 64]); P.inp('hy_fb1', [DEPTH, 64]); P.inp('hy_f2', [DEPTH, 64, 64]); P.inp('hy_fb2', [DEPTH, 64])
    P.inp('hy_f3', [DEPTH, 64, 1024]); P.inp('hy_skip', [DEPTH, 2, 256])
    P.inp('ident_f', [128, 128])
    for sfx, n in (('', TX), ('_c', TC)):
        nk = n // 128 + 1
        P.inp('hy_featT' + sfx, [33, n]); P.inp('hy_dec' + sfx, [n, 256])
        P.inp('hy_wc' + sfx, [nk, 128, nk, 128], BF16); P.inp('hy_ws' + sfx, [nk, 128, nk, 128], BF16)
        P.inp('hy_wk' + sfx, [128, nk])
    P.tmp('hyz', [768, T])
    P.tmp('hspec', [33, 128, 2, 2, 256])


_HC = {}


def host_hyena(m, inputs):
    for k in ('hy_conv_w', 'hy_conv_b', 'hy_f1', 'hy_fb1', 'hy_f2', 'hy_fb2', 'hy_f3', 'hy_skip'):
        m[k] = np.ascontiguousarray(inputs[k], np.float32)
    m['ident_f'] = np.eye(128, dtype=np.float32)
    for sfx, n in (('', TX), ('_c', TC)):
        if n not in _HC:
            _HC[n] = hyena_consts(n)
        c = _HC[n]
        m['hy_featT' + sfx] = c['featT']; m['hy_dec' + sfx] = c['dec']
        m['hy_wc' + sfx] = c['wc']; m['hy_ws' + sfx] = c['ws']; m['hy_wk' + sfx] = c['wk']


Q_R, Q_V, Q_KK, Q_KD0, Q_KD1, Q_A0, Q_A1, Q_LD0, Q_LD1, Q_G, Q_BON = range(11)
SEGS2 = [(0, TC), (TC, TX)]


def stage_rwkv_prep(P, li):
    S = P.S
    projT = P.dr['projT']
    rwq = P.dr['rwq']
    with S.stage():
        mu = S.sb('mu', [128, 9, 3])
        for j in range(2):
            S.dma(mu[:, :, 2 * j], P.dr['rw_mu'][li, j].rearrange("(c p) -> p c", p=128), writes=[mu], allow_slow_non_contiguous=True)
        S.op('dve', lambda e: e.tensor_tensor(out=mu[:, :, 1], in0=mu[:, :, 0], in1=mu[:, :, 2], op=ALU.add), reads=[mu], writes=[mu])
        S.op('dve', lambda e: e.tensor_scalar(out=mu[:, :, 1], in0=mu[:, :, 1], scalar1=-1.0, scalar2=1.0, op0=ALU.mult, op1=ALU.add), reads=[mu], writes=[mu])
        raw = Rot([S.sb(f'rraw{j}', [128, T]) for j in range(2)])
        blk = S.sb('rblk', [128, 128])
        S.dma(blk[:], P.dr['blk64'][:, :], writes=[blk])
        pss = Rot([S.ps(f'rps{j}', [128, 512]) for j in range(4)])

        def shiftmix(c, dst):
            rw_ = raw.next()
            S.dma(rw_[:], projT[O_RW + c * 128:O_RW + (c + 1) * 128, :], writes=[rw_], q=('sp' if c % 2 == 0 else 'pool'))
            dwconv_fm(S, dst, rw_, mu[:, c, :], None, 3, 1, SEGS2, wbuf=mu)

        with S.stage():
            w1s = S.sb('w1s', [128, T]); a1s = S.sb('a1s', [128, T]); g1s = S.sb('g1s', [128, T])
            shiftmix(6, w1s); shiftmix(7, a1s); shiftmix(8, g1s)
            S.op('act', lambda e: e.activation(out=w1s[:], in_=w1s[:], func=AF.Tanh), reads=[w1s], writes=[w1s])
            S.op('act', lambda e: e.activation(out=g1s[:], in_=g1s[:], func=AF.Sigmoid), reads=[g1s], writes=[g1s])
            w2t = S.sb('w2t', [128, 256]); a2t = S.sb('a2t', [128, 256]); g2t = S.sb('g2t', [128, 256])
            S.dma(w2t[:], P.dr['rw_w2'][li].rearrange("d r c -> (d r) c"), writes=[w2t])
            S.dma(a2t[:], P.dr['rw_a2'][li].rearrange("d r c -> (d r) c"), writes=[a2t])
            S.dma(g2t[:], P.dr['rw_g2'][li], writes=[g2t])
            w0 = S.sb('w0', [128, 2, 2]); a0 = S.sb('a0', [128, 2, 2])
            for d in range(2):
                S.dma(w0[:, d, :], P.dr['rw_w0'][li, d].rearrange("(h p) -> p h", p=128), writes=[w0], allow_slow_non_contiguous=True)
                S.dma(a0[:, d, :], P.dr['rw_a0'][li, d].rearrange("(h p) -> p h", p=128), writes=[a0], allow_slow_non_contiguous=True)
            outs = Rot([S.sb(f'rout{j}', [128, 512]) for j in range(4)])
            for hp in range(2):
                cs = slice(hp * 128, (hp + 1) * 128)
                for (t0, n) in BLOCKS:
                    for d in range(2):
                        ps = pss.next()
                        S.op('pe', lambda e, d=d: e.matmul(ps[:, 0:n], lhsT=w2t[64 * d:64 * d + 64, cs], rhs=w1s[64 * d:64 * d + 64, t0:t0 + n], start=True, stop=True),
                             reads=[w2t, w1s], writes=[ps])
                        o = outs.next()
                        S.op('act', lambda e, d=d: e.activation(out=o[:, 0:n], in_=ps[:, 0:n], func=AF.Sigmoid, bias=w0[:, d, hp:hp + 1]), reads=[ps, w0], writes=[o])
                        S.op('pool', lambda e: e.tensor_scalar(out=o[:, 0:n], in0=o[:, 0:n], scalar1=-0.6065306597126334, scalar2=None, op0=ALU.mult), reads=[o], writes=[o])
                        S.dma(rwq[Q_LD0 + d, cs, t0:t0 + n], o[:, 0:n], reads=[o], q='act')
                        ps = pss.next()
                        S.op('pe', lambda e, d=d: e.matmul(ps[:, 0:n], lhsT=a2t[64 * d:64 * d + 64, cs], rhs=a1s[64 * d:64 * d + 64, t0:t0 + n], start=True, stop=True),
                             reads=[a2t, a1s], writes=[ps])
                        o = outs.next()
                        S.op('act', lambda e, d=d: e.activation(out=o[:, 0:n], in_=ps[:, 0:n], func=AF.Sigmoid, bias=a0[:, d, hp:hp + 1]), reads=[ps, a0], writes=[o])
                        S.dma(rwq[Q_A0 + d, cs, t0:t0 + n], o[:, 0:n], reads=[o], q='act')
                    ps = pss.next()
                    S.op('pe', lambda e: e.matmul(ps[:, 0:n], lhsT=g2t[:, cs], rhs=g1s[:, t0:t0 + n], start=True, stop=True), reads=[g2t, g1s], writes=[ps])
                    o = outs.next()
                    S.op('dve', lambda e: e.tensor_copy(out=o[:, 0:n], in_=ps[:, 0:n]), reads=[ps], writes=[o])
                    S.dma(rwq[Q_G, cs, t0:t0 + n], o[:, 0:n], reads=[o], q='act')
        with S.stage():
            cols = S.sb('rcols', [128, 2, 4])
            for i, nm in enumerate(('rw_k_k', 'rw_k_a')):
                S.dma(cols[:, :, i], P.dr[nm][li].rearrange("(h p) -> p h", p=128), writes=[cols], allow_slow_non_contiguous=True)
            S.dma(cols[:, :, 3], P.dr['rw_r_k'][li].rearrange("h n -> (h n)").rearrange("(h p) -> p h", p=128), writes=[cols], allow_slow_non_contiguous=True)
            S.op('dve', lambda e: e.tensor_scalar(out=cols[:, :, 2], in0=cols[:, :, 1], scalar1=-1.0, scalar2=1.0, op0=ALU.mult, op1=ALU.add), reads=[cols], writes=[cols])
            rs_ = S.sb('r_s', [128, T]); ks_ = S.sb('k_s', [128, T]); vs_ = S.sb('v_s', [128, T])
            kk = S.sb('kk', [128, T]); ad = S.sb('ad', [128, T]); kd = S.sb('kd', [128, T]); bon = S.sb('bon', [128, T])
            tmp = Rot([S.sb(f'rtmp{j}', [128, 512]) for j in range(3)])
            for hp in range(2):
                cs = slice(hp * 128, (hp + 1) * 128)
                shiftmix(0 + hp, rs_); shiftmix(2 + hp, ks_); shiftmix(4 + hp, vs_)
                S.dma(rwq[Q_R, cs, :], rs_[:], reads=[rs_], q='act')
                S.dma(rwq[Q_V, cs, :], vs_[:], reads=[vs_], q='act')
                S.op('dve', lambda e: e.tensor_scalar(out=kk[:], in0=ks_[:], scalar1=cols[:, hp, 0:1], scalar2=None, op0=ALU.mult), reads=[ks_, cols], writes=[kk])
                for (t0, n) in BLOCKS:
                    sq = tmp.next()
                    S.op('act', lambda e: e.activation(out=sq[:, 0:n], in_=kk[:, t0:t0 + n], func=AF.Square), reads=[kk], writes=[sq])
                    ps = pss.next()
                    S.op('pe', lambda e: e.matmul(ps[:, 0:n], lhsT=blk[:], rhs=sq[:, 0:n], start=True, stop=True), reads=[blk, sq], writes=[ps])
                    rs2 = tmp.next()
                    S.op('dve', lambda e: e.tensor_scalar(out=rs2[:, 0:n], in0=ps[:, 0:n], scalar1=64.0, scalar2=1e-12, op0=ALU.mult, op1=ALU.add), reads=[ps], writes=[rs2])
                    S.op('act', lambda e: e.activation(out=rs2[:, 0:n], in_=rs2[:, 0:n], func=AF.Sqrt), reads=[rs2], writes=[rs2])
                    S.op('dve', lambda e: e.reciprocal(out=rs2[:, 0:n], in_=rs2[:, 0:n]), reads=[rs2], writes=[rs2])
                    S.op('dve', lambda e: e.tensor_tensor(out=kk[:, t0:t0 + n], in0=kk[:, t0:t0 + n], in1=rs2[:, 0:n], op=ALU.mult), reads=[kk, rs2], writes=[kk])
                S.dma(rwq[Q_KK, cs, :], kk[:], reads=[kk], q='act')
                for d in range(2):
                    S.dma(ad[:], rwq[Q_A0 + d, cs, :], writes=[ad])
                    S.op('dve', lambda e: e.tensor_scalar(out=kd[:], in0=ad[:], scalar1=cols[:, hp, 1:2], scalar2=cols[:, hp, 2:3], op0=ALU.mult, op1=ALU.add),
                         reads=[ad, cols], writes=[kd])
                    S.op('pool', lambda e: e.tensor_tensor(out=kd[:], in0=kd[:], in1=ks_[:], op=ALU.mult), reads=[kd, ks_], writes=[kd])
                    S.dma(rwq[Q_KD0 + d, cs, :], kd[:], reads=[kd], q='act')
                    for (t0, n) in BLOCKS:
                        rk = tmp.next()
                        S.op('dve', lambda e: e.scalar_tensor_tensor(out=rk[:, 0:n], in0=rs_[:, t0:t0 + n], scalar=cols[:, hp, 3:4], in1=kd[:, t0:t0 + n], op0=ALU.mult, op1=ALU.mult),
                             reads=[rs_, cols, kd], writes=[rk])
                        ps = pss.next()
                        S.op('pe', lambda e: e.matmul(ps[:, 0:n], lhsT=blk[:], rhs=rk[:, 0:n], start=True, stop=True), reads=[blk, rk], writes=[ps])
                        if d == 0:
                            S.op('dve', lambda e: e.scalar_tensor_tensor(out=bon[:, t0:t0 + n], in0=ps[:, 0:n], scalar=64.0, in1=vs_[:, t0:t0 + n], op0=ALU.mult, op1=ALU.mult),
                                 reads=[ps, vs_], writes=[bon])
                        else:
                            b2 = tmp.next()
                            S.op('dve', lambda e: e.scalar_tensor_tensor(out=b2[:, 0:n], in0=ps[:, 0:n], scalar=64.0, in1=vs_[:, t0:t0 + n], op0=ALU.mult, op1=ALU.mult),
                                 reads=[ps, vs_], writes=[b2])
                            S.op('pool', lambda e: e.tensor_tensor(out=bon[:, t0:t0 + n], in0=bon[:, t0:t0 + n], in1=b2[:, 0:n], op=ALU.add), reads=[bon, b2], writes=[bon])
                S.dma(rwq[Q_BON, cs, :], bon[:], reads=[bon], q='act')


class View:
    def __init__(self, buf, ap):
        self.buf = buf
        self.ap = ap

    def __getitem__(self, idx):
        return self.ap[idx]

    w = property(lambda self: self.buf.w, lambda self, v: setattr(self.buf, 'w', v))
    r = property(lambda self: self.buf.r, lambda self, v: setattr(self.buf, 'r', v))


def stage_rwkv_scan(P, li, need_ctx, heads=range(4), dbg_chunks=None, dirs=(0, 1)):
    S = P.S
    rwq = P.dr['rwq']
    rwT = P.dr['rwT']
    NCH = T // 128
    J = 3
    with S.stage():
        identf = S.sb('identf', [128, 128]); SU = S.sb('mSU', [128, 128]); SL = S.sb('mSL', [128, 128]); UI = S.sb('mUI', [128, 128])
        blk = S.sb('rblk', [128, 128])
        S.dma(identf[:], P.dr['ident_f'][:, :], writes=[identf]); S.dma(SU[:], P.dr['mask_su'][:, :], writes=[SU])
        S.dma(SL[:], P.dr['mask_sl'][:, :], writes=[SL]); S.dma(UI[:], P.dr['mask_ui'][:, :], writes=[UI])
        S.dma(blk[:], P.dr['blk64'][:, :], writes=[blk])
        MK = S.sb('MK', [128, 14, 128])
        S.dma(MK[:], P.dr['rw_masks'][:, :, :], writes=[MK])
        big = lambda nm: S.sb(nm, [64, T])
        t_kk, t_a, t_kd, t_r, t_v, yacc = [big(n) for n in ('t_kk', 't_a', 't_kd', 't_r', 't_v', 'yacc')]
        scr = [S.sb(f'scr{j}', [128, T]) for j in range(3)]
        t_ld = Buf(scr[0].t[0:64, :], 't_ld'); t_cum = Buf(scr[1].t[0:64, :], 't_cum'); stg = Buf(scr[2].t[0:64, :], 'stg')
        PC = S.sb('PC', [64, NCH])
        slots = [(scr[i // 34], (i % 34) * 128) for i in range(102)]
        si = [0]

        def slot(ncols=128):
            n = (ncols + 127) // 128
            sc, c0 = slots[si[0]]
            assert c0 + n * 128 <= T and slots[si[0] + n - 1][0] is sc
            si[0] += n
            return Buf(sc.t[:, c0:c0 + ncols], 'slot')

        sets = []
        for p in range(2):
            row = []
            for j in range(J):
                if si[0] % 34 > 34 - 15:
                    si[0] = (si[0] // 34 + 1) * 34
                d_ = {nm: slot() for nm in ('x0', 'xt0', 'Dm0', 'Dm1', 'DT0', 'DT1', 'W0', 'W1', 'G0', 'G1', 'lkt', 'gat', 'gkt')}
                d_['tk'] = slot(192)
                row.append(d_)
            sets.append(row)
        bankA = [S.ps(f'bkA{j}', [128, 512]) for j in range(J)]
        bankB = [S.ps(f'bkB{j}', [128, 512]) for j in range(J)]
        ps_seq = S.ps('ps_seq', [128, 512]); ps_ro2 = S.ps('ps_ro2', [128, 512])
        ps_ro = Rot([ps_seq, ps_ro2])
        B_b1 = View(ps_seq, ps_seq.t[:, 0:64]); B_u = View(ps_seq, ps_seq.t[:, 64:128])
        B_zn = View(ps_seq, ps_seq.t[0:64, 128:192]); B_y = View(ps_seq, ps_seq.t[0:64, 256:384])
        B1s = Rot([S.sb(f'B1s{j}', [128, 64]) for j in range(2)]); nU = Rot([S.sb(f'nU{j}', [128, 64]) for j in range(2)])
        Zs = [S.sb(f'Z{j}', [64, 64]) for j in range(2)]
        Zt = S.sb('Zt', [64, 64])
        rot = Rot([S.sb(f'rot{j}', [64, 512]) for j in range(4)])
        lnc = S.sb('lnc', [64, 4, 2])
        S.dma(lnc[:, :, 0], P.dr['rw_ln_w'][li].rearrange("(h p) -> p h", p=64), writes=[lnc], allow_slow_non_contiguous=True)
        S.dma(lnc[:, :, 1], P.dr['rw_ln_b'][li].rearrange("(h p) -> p h", p=64), writes=[lnc], allow_slow_non_contiguous=True)

        def load(dst, qi, h, d):
            src = rwq[qi, h * 64:(h + 1) * 64, :]
            if d == 0:
                S.dma(dst[:], src, writes=[dst])
            else:
                S.dma(stg[:], src, writes=[stg])
                for (t0, n) in SEGS2:
                    S.op('pool', lambda e, t0=t0, n=n: e.tensor_copy(out=dst[:, t0:t0 + n], in_=stg[:, t0:t0 + n][:, ::-1]), reads=[stg], writes=[dst])

        def indep(c, B, j):
            cl = slice(c * 128, (c + 1) * 128)
            A_ = bankA[j]; Bk = bankB[j]
            trv = View(A_, A_.t[:, 0:192])
            gv = [View(Bk, Bk.t[:, q * 128:(q + 1) * 128]) for q in range(4)] + [View(A_, A_.t[:, 256:384])]
            x0, xt0, tk, lkt, gat, gkt = B['x0'], B['xt0'], B['tk'], B['lkt'], B['gat'], B['gkt']
            Dm = [B['Dm0'], B['Dm1']]; DT = [B['DT0'], B['DT1']]; W = [B['W0'], B['W1']]; G = [B['G0'], B['G1']]
            for q, src in enumerate((t_a, t_kd, t_v)):
                S.op('pe', lambda e, q=q, src=src: e.transpose(out=trv[:, q * 64:(q + 1) * 64], in_=src[:, cl], identity=identf[0:64, 0:64]),
                     reads=[src, identf], writes=[trv])
            yield
            S.op('act', lambda e: e.copy(out=tk[:, :], in_=trv[:, :]), reads=[trv], writes=[tk])
            yield
            for q, (l_, r_) in enumerate(((t_a, t_kk), (t_kk, t_a), (t_kd, t_kk), (t_a, t_r), (t_kd, t_r))):
                S.op('pe', lambda e, q=q, l_=l_, r_=r_: e.matmul(gv[q][:, :], lhsT=l_[:, cl], rhs=r_[:, cl], start=True, stop=True), reads=[l_, r_], writes=[gv[q]])
            yield
            S.op('dve', lambda e: e.scalar_tensor_tensor(out=x0[:, :], in0=gv[0][:, :], scalar=-1.0, in1=SU[:], op0=ALU.mult, op1=ALU.mult), reads=[gv[0], SU], writes=[x0])
            S.op('dve', lambda e: e.scalar_tensor_tensor(out=xt0[:, :], in0=gv[1][:, :], scalar=-1.0, in1=SL[:], op0=ALU.mult, op1=ALU.mult), reads=[gv[1], SL], writes=[xt0])
            S.op('dve', lambda e: e.tensor_tensor(out=lkt[:, :], in0=gv[2][:, :], in1=SU[:], op=ALU.mult), reads=[gv[2], SU], writes=[lkt])
            S.op('dve', lambda e: e.tensor_tensor(out=gat[:, :], in0=gv[3][:, :], in1=UI[:], op=ALU.mult), reads=[gv[3], UI], writes=[gat])
            S.op('dve', lambda e: e.tensor_tensor(out=gkt[:, :], in0=gv[4][:, :], in1=UI[:], op=ALU.mult), reads=[gv[4], UI], writes=[gkt])
            yield
            S.op('pool', lambda e: e.tensor_tensor(out=Dm[0][:, :], in0=xt0[:, :], in1=MK[:, 0, :], op=ALU.mult), reads=[xt0, MK], writes=[Dm[0]])
            S.op('pool', lambda e: e.tensor_tensor(out=Dm[0][:, :], in0=Dm[0][:, :], in1=identf[:], op=ALU.add), reads=[Dm[0], identf], writes=[Dm[0]])
            S.op('pool', lambda e: e.tensor_tensor(out=DT[0][:, :], in0=x0[:, :], in1=MK[:, 7, :], op=ALU.mult), reads=[x0, MK], writes=[DT[0]])
            S.op('pool', lambda e: e.tensor_tensor(out=DT[0][:, :], in0=DT[0][:, :], in1=identf[:], op=ALU.add), reads=[DT[0], identf], writes=[DT[0]])
            yield
            wv = View(A_, A_.t[:, 0:128]); gv1 = View(A_, A_.t[:, 128:256])
            w2v = View(Bk, Bk.t[:, 0:128]); g2v = View(Bk, Bk.t[:, 128:256])
            cur = 0
            for k in range(1, 7):
                nxt = 1 - cur
                if k < 6:
                    S.op('pe', lambda e, cur=cur: e.matmul(wv[:, :], lhsT=x0[:, :], rhs=Dm[cur][:, :], start=True, stop=True), reads=[x0, Dm[cur]], writes=[wv])
                S.op('pe', lambda e, cur=cur: e.matmul(w2v[:, :], lhsT=xt0[:, :], rhs=DT[cur][:, :], start=True, stop=True), reads=[xt0, DT[cur]], writes=[w2v])
                yield
                if k < 6:
                    S.op('act', lambda e: e.copy(out=W[0][:, :], in_=wv[:, :]), reads=[wv], writes=[W[0]])
                S.op('act', lambda e: e.copy(out=W[1][:, :], in_=w2v[:, :]), reads=[w2v], writes=[W[1]])
                yield
                if k < 6:
                    S.op('pe', lambda e, cur=cur: e.matmul(gv1[:, :], lhsT=DT[cur][:, :], rhs=W[0][:, :], start=True, stop=True), reads=[DT[cur], W[0]], writes=[gv1])
                S.op('pe', lambda e, cur=cur: e.matmul(g2v[:, :], lhsT=Dm[cur][:, :], rhs=W[1][:, :], start=True, stop=True), reads=[Dm[cur], W[1]], writes=[g2v])
                yield
                if k < 6:
                    S.op('dve', lambda e, k=k: e.tensor_tensor(out=G[0][:, :], in0=gv1[:, :], in1=MK[:, k, :], op=ALU.mult), reads=[gv1, MK], writes=[G[0]])
                S.op('dve', lambda e, k=k: e.tensor_tensor(out=G[1][:, :], in0=g2v[:, :], in1=MK[:, 7 + k, :], op=ALU.mult), reads=[g2v, MK], writes=[G[1]])
                yield
                if k < 6:
                    S.op('pool', lambda e, cur=cur, nxt=nxt: e.tensor_tensor(out=Dm[nxt][:, :], in0=Dm[cur][:, :], in1=G[0][:, :], op=ALU.add), reads=[Dm[cur], G[0]], writes=[Dm[nxt]])
                S.op('pool', lambda e, cur=cur, nxt=nxt: e.tensor_tensor(out=DT[nxt][:, :], in0=DT[cur][:, :], in1=G[1][:, :], op=ALU.add), reads=[DT[cur], G[1]], writes=[DT[nxt]])
                yield
                cur = nxt
            B['TT'] = DT[cur]

        def seqpart(c, B, d, zi):
            cl = slice(c * 128, (c + 1) * 128)
            tk, lkt, gat, gkt, TT = B['tk'], B['lkt'], B['gat'], B['gkt'], B['TT']
            tkv = lambda q: tk[:, q * 64:(q + 1) * 64]
            Z = Zs[zi]; Zn = Zs[1 - zi]
            S.op('pe', lambda e: e.matmul(B_b1[:, :], lhsT=t_kk[:, cl], rhs=Z[:], start=True, stop=False), reads=[t_kk, Z], writes=[B_b1])
            S.op('pe', lambda e: e.matmul(B_b1[:, :], lhsT=lkt[:, :], rhs=tkv(2), start=False, stop=True), reads=[lkt, tk], writes=[B_b1])
            yield
            b1 = B1s.next()
            S.op('act', lambda e: e.copy(out=b1[:], in_=B_b1[:, :]), reads=[B_b1], writes=[b1])
            yield
            S.op('pe', lambda e: e.matmul(B_u[:, :], lhsT=TT[:, :], rhs=b1[:], start=True, stop=True), reads=[TT, b1], writes=[B_u])
            yield
            nu = nU.next()
            S.op('act', lambda e: e.mul(out=nu[:], in_=B_u[:, :], mul=-1.0), reads=[B_u], writes=[nu])
            yield
            S.op('pe', lambda e: e.matmul(B_y[:, :], lhsT=Z[:], rhs=t_r[:, cl], start=True, stop=False), reads=[Z, t_r], writes=[B_y])
            S.op('pe', lambda e: e.matmul(B_y[:, :], lhsT=nu[:], rhs=gat[:, :], start=False, stop=False), reads=[nu, gat], writes=[B_y])
            S.op('pe', lambda e: e.matmul(B_y[:, :], lhsT=tkv(2), rhs=gkt[:, :], start=False, stop=True), reads=[tk, gkt], writes=[B_y])
            S.op('pe', lambda e: e.matmul(B_zn[:, :], lhsT=tkv(0), rhs=nu[:], start=True, stop=False), reads=[tk, nu], writes=[B_zn])
            S.op('pe', lambda e: e.matmul(B_zn[:, :], lhsT=tkv(1), rhs=tkv(2), start=False, stop=True), reads=[tk], writes=[B_zn])
            yield
            S.op('dve', lambda e: e.tensor_tensor(out=Zt[:], in0=B_zn[:, :], in1=Z[:], op=ALU.add), reads=[B_zn, Z], writes=[Zt])
            if d == 0:
                S.op('dve', lambda e: e.tensor_copy(out=yacc[:, cl], in_=B_y[:, :]), reads=[B_y], writes=[yacc])
            else:
                seg0, segn = (0, TC) if c < 2 else (TC, TX)
                j0 = c * 128 - seg0
                lo = seg0 + segn - j0 - 128
                yv = yacc[:, lo:lo + 128][:, ::-1]
                S.op('dve', lambda e, yv=yv: e.tensor_tensor(out=yv, in0=B_y[:, :], in1=yv, op=ALU.add), reads=[B_y, yacc], writes=[yacc])
            yield
            S.op('act', lambda e: e.activation(out=Zn[:], in_=Zt[:], func=AF.Copy, scale=PC[:, c:c + 1]), reads=[Zt, PC], writes=[Zn])
            yield

        def seqgroup(chs, bufs, d, zi0):
            zi = zi0
            for c, B in zip(chs, bufs):
                yield from seqpart(c, B, d, zi)
                zi = 1 - zi

        def roundrobin(gens):
            gens = list(gens)
            while gens:
                for g in list(gens):
                    try:
                        next(g)
                    except StopIteration:
                        gens.remove(g)

        for h in heads:
            for d in dirs:
                load(t_ld, Q_LD0 + d, h, d)
                ones_t = t_kk
                S.op('pool', lambda e: e.memset(ones_t[:], 1.0), writes=[ones_t])
                for c in range(NCH):
                    S.op('dve', lambda e, c=c: e.tensor_tensor_scan(out=t_cum[:, c * 128:(c + 1) * 128], data0=ones_t[:, c * 128:(c + 1) * 128],
                                                                   data1=t_ld[:, c * 128:(c + 1) * 128], initial=0.0, op0=ALU.mult, op1=ALU.add),
                         reads=[ones_t, t_ld], writes=[t_cum])
                S.op('dve', lambda e: e.tensor_tensor(out=t_ld[:], in0=t_cum[:], in1=t_ld[:], op=ALU.subtract), reads=[t_cum, t_ld], writes=[t_ld])
                S.op('act', lambda e: e.activation(out=t_ld[:], in_=t_ld[:], func=AF.Exp), reads=[t_ld], writes=[t_ld])
                load(t_kk, Q_KK, h, d); load(t_a, Q_A0 + d, h, d)
                S.op('dve', lambda e: e.tensor_tensor(out=t_a[:], in0=t_a[:], in1=t_kk[:], op=ALU.mult), reads=[t_a, t_kk], writes=[t_a])
                S.op('dve', lambda e: e.tensor_tensor(out=t_kk[:], in0=t_kk[:], in1=t_ld[:], op=ALU.mult), reads=[t_kk, t_ld], writes=[t_kk])
                S.op('act', lambda e: e.activation(out=t_ld[:], in_=t_cum[:], func=AF.Exp, scale=-1.0), reads=[t_cum], writes=[t_ld])
                load(t_kd, Q_KD0 + d, h, d)
                S.op('dve', lambda e: e.tensor_tensor(out=t_a[:], in0=t_a[:], in1=t_ld[:], op=ALU.mult), reads=[t_a, t_ld], writes=[t_a])
                S.op('pool', lambda e: e.tensor_tensor(out=t_kd[:], in0=t_kd[:], in1=t_ld[:], op=ALU.mult), reads=[t_kd, t_ld], writes=[t_kd])
                S.op('act', lambda e: e.activation(out=t_cum[:], in_=t_cum[:], func=AF.Exp), reads=[t_cum], writes=[t_cum])
                load(t_r, Q_R, h, d)
                S.op('dve', lambda e: e.tensor_tensor(out=t_r[:], in0=t_r[:], in1=t_cum[:], op=ALU.mult), reads=[t_r, t_cum], writes=[t_r])
                S.op('pool', lambda e: e.tensor_copy(out=PC[:, :], in_=t_cum[:, 127:T:128]), reads=[t_cum], writes=[PC])
                load(t_v, Q_V, h, d)
                S.op('pool', lambda e: e.memset(Zs[0][:], 0.0), writes=[Zs[0]])
                S.barrier()
                nch = NCH if dbg_chunks is None else dbg_chunks
                groups = [list(range(g0, min(g0 + J, nch))) for g0 in range(0, nch, J)]
                zi = 0
                prev = None
                for gi, chs in enumerate(groups):
                    bufs = sets[gi % 2][:len(chs)]
                    gens = [indep(c, B, j) for j, (c, B) in enumerate(zip(chs, bufs))]
                    if prev is not None:
                        gens.append(seqgroup(prev[0], prev[1], d, prev[2]))
                    roundrobin(gens)
                    prev = (chs, bufs, zi)
                    zi = (zi + len(chs)) % 2
                if prev is not None:
                    roundrobin([seqgroup(prev[0], prev[1], d, prev[2])])
                S.barrier()
            if 'dbg_y' in P.dr:
                S.dma(P.dr['dbg_y'][:, :], yacc[:], reads=[yacc])
            load(t_kd, Q_BON, h, 0); load(t_a, Q_G, h, 0)
            for (t0, n) in (BLOCKS if need_ctx else BLOCKS[1:]):
                pm = ps_ro.next()
                S.op('pe', lambda e: e.matmul(pm[0:64, 0:n], lhsT=blk[0:64, 0:64], rhs=yacc[:, t0:t0 + n], start=True, stop=True), reads=[blk, yacc], writes=[pm])
                dd = rot.next()
                S.op('dve', lambda e: e.tensor_tensor(out=dd[:, 0:n], in0=yacc[:, t0:t0 + n], in1=pm[0:64, 0:n], op=ALU.subtract), reads=[yacc, pm], writes=[dd])
                sq = rot.next()
                S.op('act', lambda e: e.activation(out=sq[:, 0:n], in_=dd[:, 0:n], func=AF.Square), reads=[dd], writes=[sq])
                pv = ps_ro.next()
                S.op('pe', lambda e: e.matmul(pv[0:64, 0:n], lhsT=blk[0:64, 0:64], rhs=sq[:, 0:n], start=True, stop=True), reads=[blk, sq], writes=[pv])
                rs2 = rot.next()
                S.op('dve', lambda e: e.tensor_scalar(out=rs2[:, 0:n], in0=pv[0:64, 0:n], scalar1=64e-5, scalar2=None, op0=ALU.add), reads=[pv], writes=[rs2])
                S.op('act', lambda e: e.activation(out=rs2[:, 0:n], in_=rs2[:, 0:n], func=AF.Sqrt), reads=[rs2], writes=[rs2])
                S.op('dve', lambda e: e.reciprocal(out=rs2[:, 0:n], in_=rs2[:, 0:n]), reads=[rs2], writes=[rs2])
                S.op('dve', lambda e: e.tensor_tensor(out=dd[:, 0:n], in0=dd[:, 0:n], in1=rs2[:, 0:n], op=ALU.mult), reads=[dd, rs2], writes=[dd])
                S.op('dve', lambda e: e.tensor_scalar(out=dd[:, 0:n], in0=dd[:, 0:n], scalar1=lnc[:, h, 0:1], scalar2=lnc[:, h, 1:2], op0=ALU.mult, op1=ALU.add),
                     reads=[dd, lnc], writes=[dd])
                S.op('pool', lambda e: e.tensor_tensor(out=dd[:, 0:n], in0=dd[:, 0:n], in1=t_kd[:, t0:t0 + n], op=ALU.add), reads=[dd, t_kd], writes=[dd])
                oo = rot.next()
                S.op('pool', lambda e: e.tensor_tensor(out=oo[:, 0:n], in0=dd[:, 0:n], in1=t_a[:, t0:t0 + n], op=ALU.mult), reads=[dd, t_a], writes=[oo])
                S.dma(rwT[h * 64:(h + 1) * 64, t0:t0 + n], oo[:, 0:n], reads=[oo], q='act')


def declare_rwkv(P):
    P.inp('rw_mu', [DEPTH, 2, 1152]); P.inp('rw_w0', [DEPTH, 2, 256]); P.inp('rw_w2', [DEPTH, 2, 64, 256])
    P.inp('rw_a0', [DEPTH, 2, 256]); P.inp('rw_a2', [DEPTH, 2, 64, 256]); P.inp('rw_g2', [DEPTH, 128, 256])
    P.inp('rw_k_k', [DEPTH, 256]); P.inp('rw_k_a', [DEPTH, 256]); P.inp('rw_r_k', [DEPTH, 4, 64])
    P.inp('rw_ln_w', [DEPTH, 256]); P.inp('rw_ln_b', [DEPTH, 256])
    P.inp('mask_su', [128, 128]); P.inp('mask_sl', [128, 128]); P.inp('mask_ui', [128, 128])
    P.inp('rw_masks', [128, 14, 128])
    P.tmp('rwq', [11, 256, T])


def host_rwkv(m, inputs):
    for k in ('rw_mu', 'rw_w0', 'rw_w2', 'rw_a0', 'rw_a2', 'rw_g2', 'rw_k_k', 'rw_k_a', 'rw_r_k', 'rw_ln_w', 'rw_ln_b'):
        m[k] = np.ascontiguousarray(inputs[k], np.float32)
    su = np.triu(np.ones((128, 128), np.float32), 1)
    m['mask_su'] = su; m['mask_sl'] = np.ascontiguousarray(su.T); m['mask_ui'] = np.triu(np.ones((128, 128), np.float32), 0)
    idx = np.arange(128)
    mk = np.zeros((128, 14, 128), np.float32)
    for k in range(7):
        b = 2 ** k
        M = ((idx[:, None] // (2 * b)) == (idx[None, :] // (2 * b))) & ((idx[:, None] % (2 * b)) >= b) & ((idx[None, :] % (2 * b)) < b)
        mk[:, k, :] = M
        mk[:, 7 + k, :] = M.T
    m['rw_masks'] = mk


def build_full():
    P = Prog()
    declare_common(P); declare_attn(P); declare_lru(P); declare_merge(P); declare_hyena(P); declare_rwkv(P); declare_moe(P)
    P.out('y_out', [TX, D])
    for li in range(DEPTH):
        need_ctx = li < DEPTH - 1
        src = 'xin' if li == 0 else 'x_l0'
        stage_mod(P, li)
        stage_inproj(P, li, src)
        stage_attn(P, li, need_ctx)
        stage_hyena(P, li, need_ctx)
        stage_rwkv_prep(P, li)
        stage_rwkv_scan(P, li, need_ctx)
        stage_lru(P, li, need_ctx)
        stage_merge(P, li, need_ctx, src, 'x_mid')
        if need_ctx:
            stage_moe(P, li, True, 'x_mid', 'x_l0')
        else:
            stage_moe(P, li, False, 'x_mid', 'y_out', dst_is_out=True)
    P.S.finish()
    return P


def full_host_inputs(inputs, b, shared=None):
    if shared is None:
        shared = {}
        m = host_inputs(inputs, b)
        host_attn(m, inputs); host_lru(m, inputs); host_merge(m, inputs); host_hyena(m, inputs); host_rwkv(m, inputs); host_moe(m, inputs)
        for k, v in m.items():
            if k not in ('xin', 'cvec'):
                shared[k] = v
        return m, shared
    m = dict(shared)
    m['xin'] = np.ascontiguousarray(np.concatenate([inputs['ctx'][b], inputs['x'][b]], axis=0), dtype=np.float32)
    m['cvec'] = np.ascontiguousarray(np.stack([inputs['c_ctx'], inputs['c'][b]], axis=0), dtype=np.float32)
    return m, shared


def kernel(**inputs):
    inputs = {k: np.asarray(v) for k, v in inputs.items()}
    P = build_full()
    n = 8
    maps = []
    shared = None
    for b in range(n):
        m, shared = full_host_inputs(inputs, b, shared)
        maps.append({k: m[k] for k in P.in_names})
    res = run_bass_kernel_spmd(P.nc, maps, core_ids=list(range(n)))
    out = np.stack([np.asarray(res.results[b]['y_out'], dtype=np.float32) for b in range(n)], axis=0)
    return out
```

```python
import contextlib
import math
import numpy as np
import concourse.bass as bass
import concourse.mybir as mybir
from concourse.bass_utils import run_bass_kernel_spmd

F32 = mybir.dt.float32
BF16 = mybir.dt.bfloat16
ALU = mybir.AluOpType
AF = mybir.ActivationFunctionType
AX = mybir.AxisListType

ENGS = ('pe', 'dve', 'act', 'pool', 'sp')
SEM_ROLL = 30000
NDMA = 12

D = 1024
TC = 256
TX = 4096
T = TC + TX
N_IN = 7296
DEPTH = 2
EPS = 1e-6
BLOCKS = [(0, 256)] + [(256 + 512 * j, 512) for j in range(8)]
O_Q, O_K, O_V, O_HY, O_RW, O_LR, O_GT = 0, 512, 640, 768, 1536, 2688, 3200


class Buf:
    __slots__ = ('t', 'w', 'r', 'name')

    def __init__(self, t, name=''):
        self.t = t
        self.w = None
        self.r = {}
        self.name = name

    def __getitem__(self, idx):
        return self.t[idx]


class Sched:
    def __init__(self, nc):
        self.nc = nc
        self.emap = {'pe': nc.tensor, 'dve': nc.vector, 'act': nc.scalar, 'pool': nc.gpsimd, 'sp': nc.sync}
        self.perm = contextlib.ExitStack()
        self.stack = contextlib.ExitStack()
        self.cur_sem = {}
        self.cnt = {}
        self.nsem = 0
        for e in ('pe', 'dve', 'act', 'pool'):
            self._new_sem(e)
        self.dma_sems = {}
        self.dma_k = {}
        for q in ('sp', 'pool', 'act'):
            self.dma_sems[q] = [self._alloc_sem(f'dma_{q}_{i}') for i in range(NDMA)]
            self.dma_k[q] = 0
        self.seen = {e: {} for e in ENGS}
        self.out_tokens = []
        self.ninstr = 0
        self.uid = 0

    def _alloc_sem(self, name):
        self.nsem += 1
        return self.perm.enter_context(self.nc.semaphore(f'{name}_{self.nsem}'))

    def _new_sem(self, e):
        self.cur_sem[e] = self._alloc_sem(f'c_{e}')
        self.cnt[e] = 0

    def sb(self, name, shape, dt=F32):
        self.uid += 1
        t = self.stack.enter_context(self.nc.sbuf_tensor(f'{name}_{self.uid}', list(shape), dt))
        return Buf(t, name)

    def ps(self, name, shape, dt=F32):
        self.uid += 1
        t = self.stack.enter_context(self.nc.psum_tensor(f'{name}_{self.uid}', list(shape), dt))
        return Buf(t, name)

    @contextlib.contextmanager
    def stage(self):
        old = self.stack
        self.stack = contextlib.ExitStack()
        try:
            yield
        finally:
            self.barrier()
            self.stack.close()
            self.stack = old

    def barrier(self):
        toks = []
        for f in ('pe', 'dve', 'act', 'pool'):
            if self.cnt[f] > 0:
                toks.append((self.cur_sem[f], self.cnt[f]))
        for q in ('sp', 'pool', 'act'):
            k = self.dma_k[q]
            for j in range(min(k, NDMA)):
                last = ((k - 1 - j) // NDMA) * NDMA + j
                toks.append((self.dma_sems[q][j], 16 * (last // NDMA + 1)))
        for e in ENGS:
            eng = self.emap[e]
            for s, v in toks:
                if self.seen[e].get(id(s), -1) >= v:
                    continue
                self.seen[e][id(s)] = v
                eng.wait_ge(s, v)
                self.ninstr += 1

    def op(self, eng, fn, reads=(), writes=(), dmaq=False, is_out=False):
        need = {}
        reads = [b for b in reads if b is not None]
        writes = [b for b in writes if b is not None]

        def add(tok):
            if tok is None:
                return
            sem, val, teng = tok
            if teng == eng and eng == 'pe' and not dmaq:
                return
            k = id(sem)
            if self.seen[eng].get(k, -1) >= val:
                return
            if k not in need or need[k][1] < val:
                need[k] = (sem, val)

        for b in reads:
            add(b.w)
        for b in writes:
            add(b.w)
            for t in b.r.values():
                add(t)
        if dmaq:
            k = self.dma_k[eng]
            self.dma_k[eng] = k + 1
            sem = self.dma_sems[eng][k % NDMA]
            prev = 16 * (k // NDMA)
            if prev > 0:
                add((sem, prev, 'dma'))
            tok = (sem, prev + 16, 'dma')
            inc = 16
            rkey = ('dma', eng, k % (4 * NDMA))
        else:
            if self.cnt[eng] >= SEM_ROLL:
                self._new_sem(eng)
            self.cnt[eng] += 1
            sem = self.cur_sem[eng]
            tok = (sem, self.cnt[eng], eng)
            inc = 1
            rkey = eng
        e = self.emap[eng]
        for s_, v_ in need.values():
            self.seen[eng][id(s_)] = v_
            e.wait_ge(s_, v_)
        fn(e).then_inc(sem, inc)
        self.ninstr += 1 + len(need)
        for b in reads:
            b.r[rkey] = tok
        for b in writes:
            b.w = tok
            b.r = {}
        if is_out:
            self.out_tokens.append(tok)
        return tok

    def dma(self, out_ap, in_ap, reads=(), writes=(), q='sp', is_out=False, **kw):
        return self.op(q, lambda e: e.dma_start(out=out_ap, in_=in_ap, **kw),
                       reads=reads, writes=writes, dmaq=True, is_out=is_out)

    def finish(self):
        need = {}
        for sem, val, _ in self.out_tokens:
            k = id(sem)
            if k not in need or need[k][1] < val:
                need[k] = (sem, val)
        for s_, v_ in need.values():
            self.nc.sync.wait_ge(s_, v_)
        self.barrier()
        self.stack.close()
        self.perm.close()


class Rot:
    def __init__(self, bufs):
        self.bufs = bufs
        self.i = 0

    def next(self):
        b = self.bufs[self.i % len(self.bufs)]
        self.i += 1
        return b


class Prog:
    def __init__(self, ext_in=(), ext_out=()):
        self.nc = bass.Bass("TRN2", target_bir_lowering=False)
        self.S = Sched(self.nc)
        self.ext_in = set(ext_in)
        self.ext_out = set(ext_out)
        self.dr = {}
        self.in_names = []
        self.out_names = []

    def inp(self, name, shape, dt=F32):
        t = self.nc.dram_tensor(name, list(shape), dt, kind="ExternalInput")
        self.dr[name] = t.ap()
        self.in_names.append(name)
        return self.dr[name]

    def out(self, name, shape, dt=F32):
        t = self.nc.dram_tensor(name, list(shape), dt, kind="ExternalOutput")
        self.dr[name] = t.ap()
        self.out_names.append(name)
        return self.dr[name]

    def tmp(self, name, shape, dt=F32):
        if name in self.ext_in:
            return self.inp(name, shape, dt)
        if name in self.ext_out:
            return self.out(name, shape, dt)
        t = self.nc.dram_tensor(name, list(shape), dt, kind="Internal")
        self.dr[name] = t.ap()
        return self.dr[name]


def stage_mod(P, li):
    S = P.S
    ada_w = P.dr['ada_w']
    ada_b = P.dr['ada_b']
    cvec = P.dr['cvec']
    modT_d = P.dr[f'modT{li}']
    modrow_d = P.dr[f'modrow{li}']
    with S.stage():
        cT = S.sb('cT', [128, 8, 2])
        scT = S.sb('scT', [128, 8, 2])
        abT = S.sb('abT', [128, 48])
        modT = S.sb('modT', [128, 48, 2])
        sig = S.sb('sig', [128, 8, 2])
        for s in range(2):
            S.dma(cT[:, :, s], cvec[s, :].rearrange("(k p) -> p k", p=128), writes=[cT],
                  allow_slow_non_contiguous=True)
        S.dma(abT[:, :], ada_b[li, :].rearrange("(o p) -> p o", p=128), writes=[abT],
              allow_slow_non_contiguous=True)
        S.op('act', lambda e: e.activation(out=sig[:], in_=cT[:], func=AF.Sigmoid), reads=[cT], writes=[sig])
        S.op('dve', lambda e: e.tensor_tensor(out=scT[:], in0=cT[:], in1=sig[:], op=ALU.mult), reads=[cT, sig], writes=[scT])
        wts = Rot([S.sb(f'adaw{j}', [128, 8, 512]) for j in range(2)])
        pss = Rot([S.ps(f'modps{j}', [128, 8]) for j in range(2)])
        aw = ada_w[li].rearrange("(k p) n -> p k n", p=128)
        for g in range(12):
            wt = wts.next()
            S.dma(wt[:], aw[:, :, g * 512:(g + 1) * 512], writes=[wt], q='sp')
            ps = pss.next()
            for j in range(4):
                oc = g * 4 + j
                for k in range(8):
                    S.op('pe', lambda e, j=j, k=k: e.matmul(ps[:, 2 * j:2 * j + 2], lhsT=wt[:, k, j * 128:(j + 1) * 128], rhs=scT[:, k, :],
                                                           start=(k == 0), stop=(k == 7)), reads=[wt, scT], writes=[ps])
            for j in range(4):
                oc = g * 4 + j
                S.op('dve', lambda e, j=j, oc=oc: e.tensor_scalar(out=modT[:, oc, :], in0=ps[:, 2 * j:2 * j + 2], scalar1=abT[:, oc:oc + 1], scalar2=None,
                                                                 op0=ALU.add), reads=[ps, abT], writes=[modT])
        S.dma(modT_d[:, :], modT[:].rearrange("p o s -> p (o s)"), reads=[modT])
        for s in range(2):
            S.dma(modrow_d[s, :].rearrange("(o p) -> p o", p=128), modT[:, :, s], reads=[modT], q='pool',
                  allow_slow_non_contiguous=True)


def load_modT(P, li):
    S = P.S
    m = S.sb('modTl', [128, 48, 2])
    S.dma(m[:].rearrange("p o s -> p (o s)"), P.dr[f'modT{li}'][:, :], writes=[m])
    return m


def norm_ctx(S, npt=2):
    C = {}
    C['sc1p'] = S.sb('sc1p', [128, 8, 2])
    C['xts'] = Rot([S.sb(f'xt{j}', [128, 1024]) for j in range(3)])
    C['sqs'] = Rot([S.sb(f'sq{j}', [128, 1024]) for j in range(2)])
    C['xns'] = Rot([S.sb(f'xn{j}', [128, 1024], BF16) for j in range(2)])
    C['sss'] = Rot([S.sb(f'ss{j}', [128, 2]) for j in range(4)])
    C['pts'] = Rot([S.ps(f'ptT{j}', [128, 4, 128], BF16) for j in range(npt)])
    return C


def norm_transpose(P, src_d, modT, sh_c, sc_c, hT, ident, tiles=None, dst0=0, C=None):
    S = P.S
    if C is None:
        C = norm_ctx(S)
    sc1p = C['sc1p']
    S.op('dve', lambda e: e.tensor_scalar(out=sc1p[:], in0=modT[:, sc_c:sc_c + 8, :], scalar1=1.0, scalar2=None, op0=ALU.add),
         reads=[modT], writes=[sc1p])
    xts, sqs, xns, sss, pts = C['xts'], C['sqs'], C['xns'], C['sss'], C['pts']
    if tiles is None:
        tiles = range(T // 128)
    for ti in tiles:
        s = 0 if ti < 2 else 1
        xt = xts.next()
        S.dma(xt[:], src_d[ti * 128:(ti + 1) * 128, :], writes=[xt], q='sp')
        sq = sqs.next()
        ss = sss.next()
        S.op('act', lambda e: e.activation(out=sq[:], in_=xt[:], func=AF.Square), reads=[xt], writes=[sq])
        S.op('dve', lambda e: e.reduce_sum(out=ss[:, 0:1], in_=sq[:], axis=AX.X), reads=[sq], writes=[ss])
        S.op('dve', lambda e: e.tensor_scalar(out=ss[:, 1:2], in0=ss[:, 0:1], scalar1=1.0 / D, scalar2=EPS, op0=ALU.mult, op1=ALU.add),
             reads=[ss], writes=[ss])
        S.op('act', lambda e: e.activation(out=ss[:, 1:2], in_=ss[:, 1:2], func=AF.Sqrt), reads=[ss], writes=[ss])
        S.op('dve', lambda e: e.reciprocal(out=ss[:, 0:1], in_=ss[:, 1:2]), reads=[ss], writes=[ss])
        xn = xns.next()
        S.op('act', lambda e: e.activation(out=xn[:], in_=xt[:], func=AF.Copy, scale=ss[:, 0:1]), reads=[xt, ss], writes=[xn])
        for half in range(2):
            pt = pts.next()
            for j in range(4):
                k = half * 4 + j
                S.op('pe', lambda e, j=j, k=k: e.transpose(out=pt[:, j, :], in_=xn[:, k * 128:(k + 1) * 128], identity=ident[:]),
                     reads=[xn, ident], writes=[pt])
            for j in range(4):
                k = half * 4 + j
                S.op('dve', lambda e, j=j, k=k: e.tensor_scalar(out=hT[:, k, ti * 128 - dst0:(ti + 1) * 128 - dst0], in0=pt[:, j, :],
                                                               scalar1=sc1p[:, k, s:s + 1], scalar2=modT[:, sh_c + k, s:s + 1],
                                                               op0=ALU.mult, op1=ALU.add), reads=[pt, sc1p, modT], writes=[hT])


def stage_inproj(P, li, src_name):
    S = P.S
    src_d = P.dr[src_name]
    w_in = P.dr['w_in']
    projT = P.dr['projT']
    vtm = P.dr['vtm']
    with S.stage():
        ident = S.sb('ident', [128, 128], BF16)
        S.dma(ident[:], P.dr['ident_bf'][:, :], writes=[ident])
        modT = load_modT(P, li)
        hT = S.sb('hT', [128, 8, T], BF16)
        norm_transpose(P, src_d, modT, 0, 8, hT, ident)
        wfs = Rot([S.sb(f'wf{j}', [128, 8, 384]) for j in range(2)])
        wbs = Rot([S.sb(f'wb{j}', [128, 8, 384], BF16) for j in range(2)])
        pss = Rot([S.ps(f'ps{j}', [128, 512]) for j in range(4)])
        sts = Rot([S.sb(f'st{j}', [128, 512]) for j in range(4)])
        wv = w_in[li].rearrange("(k p) n -> p k n", p=128)
        cnt = 0
        for g in range(N_IN // 384):
            wf = wfs.next()
            S.dma(wf[:], wv[:, :, g * 384:(g + 1) * 384], writes=[wf], q=('sp' if g % 2 == 0 else 'pool'))
            wb = wbs.next()
            S.op('pool', lambda e: e.tensor_copy(out=wb[:], in_=wf[:]), reads=[wf], writes=[wb])
            for j in range(3):
                oc = g * 3 + j
                if oc == O_V // 128:
                    for ti in range(T // 128):
                        ps = pss.next()
                        for k in range(8):
                            S.op('pe', lambda e, k=k: e.matmul(ps[:, 0:128], lhsT=hT[:, k, ti * 128:(ti + 1) * 128], rhs=wb[:, k, j * 128:(j + 1) * 128],
                                                               start=(k == 0), stop=(k == 7)), reads=[hT, wb], writes=[ps])
                        st = sts.next()
                        S.op('act', lambda e: e.copy(out=st[:, 0:128], in_=ps[:, 0:128]), reads=[ps], writes=[st])
                        S.dma(vtm[ti * 128:(ti + 1) * 128, :], st[:, 0:128], reads=[st], q='act')
                    continue
                for (t0, n) in BLOCKS:
                    ps = pss.next()
                    for k in range(8):
                        S.op('pe', lambda e, k=k: e.matmul(ps[:, 0:n], lhsT=wb[:, k, j * 128:(j + 1) * 128], rhs=hT[:, k, t0:t0 + n],
                                                           start=(k == 0), stop=(k == 7)), reads=[hT, wb], writes=[ps])
                    st = sts.next()
                    if cnt % 2 == 0:
                        S.op('act', lambda e: e.copy(out=st[:, 0:n], in_=ps[:, 0:n]), reads=[ps], writes=[st])
                    else:
                        S.op('dve', lambda e: e.tensor_copy(out=st[:, 0:n], in_=ps[:, 0:n]), reads=[ps], writes=[st])
                    S.dma(projT[oc * 128:(oc + 1) * 128, t0:t0 + n], st[:, 0:n], reads=[st], q=('sp' if cnt % 2 == 0 else 'act'))
                    cnt += 1


def declare_common(P):
    P.inp('xin', [T, D])
    P.inp('cvec', [2, D])
    P.inp('ada_w', [DEPTH, D, 6 * D])
    P.inp('ada_b', [DEPTH, 6 * D])
    P.inp('w_in', [DEPTH, D, N_IN])
    P.inp('ident_bf', [128, 128], BF16)
    for li in range(DEPTH):
        P.tmp(f'modT{li}', [128, 96])
        P.tmp(f'modrow{li}', [2, 6 * D])
    P.tmp('projT', [N_IN, T])
    P.tmp('vtm', [T, 128])


def host_inputs(inputs, b):
    import ml_dtypes
    m = {}
    m['xin'] = np.ascontiguousarray(np.concatenate([inputs['ctx'][b], inputs['x'][b]], axis=0), dtype=np.float32)
    m['cvec'] = np.ascontiguousarray(np.stack([inputs['c_ctx'], inputs['c'][b]], axis=0), dtype=np.float32)
    for k in ('ada_w', 'ada_b', 'w_in'):
        m[k] = np.ascontiguousarray(inputs[k], dtype=np.float32)
    m['ident_bf'] = np.eye(128, dtype=np.float32).astype(ml_dtypes.bfloat16)
    return m


def rope_tables():
    t = np.arange(TX)
    pos = np.stack([t // 64, t % 64], 0).astype(np.float64)
    freqs = 10000.0 ** (-np.arange(16, dtype=np.float64) / 16)
    cos = np.zeros((64, TX)); sin = np.zeros((64, TX))
    for d in range(64):
        ax, half, f = d // 32, (d % 32) // 16, d % 16
        ang = pos[ax] * freqs[f]
        cos[d] = np.cos(ang)
        sin[d] = np.sin(ang) * (-1.0 if half == 0 else 1.0)
    psw = np.zeros((128, 128), np.float32)
    for m in range(128):
        d = m % 64
        src = m + 16 if (d % 32) < 16 else m - 16
        psw[src, m] = 1.0
    blk = np.zeros((128, 128), np.float32)
    blk[:64, :64] = 1.0 / 64
    blk[64:, 64:] = 1.0 / 64
    return (np.tile(cos, (2, 1)).astype(np.float32), np.tile(sin, (2, 1)).astype(np.float32), psw, blk)


def qk_prep(P, S, rows_list, gain, dst, dst_sl, scale, tok_ranges, C):
    projT = P.dr['projT']
    for (t0, n) in tok_ranges:
        raw = C['raw'].next()
        for i, (r0, nr, p0) in enumerate(rows_list):
            S.dma(raw[p0:p0 + nr, 0:n], projT[r0:r0 + nr, t0:t0 + n], writes=[raw], q='sp')
        sq = C['sq'].next()
        S.op('act', lambda e: e.activation(out=sq[:, 0:n], in_=raw[:, 0:n], func=AF.Square), reads=[raw], writes=[sq])
        ps = C['ps'].next()
        S.op('pe', lambda e: e.matmul(ps[:, 0:n], lhsT=C['blk'][:], rhs=sq[:, 0:n], start=True, stop=True), reads=[sq, C['blk']], writes=[ps])
        rs = C['rs'].next()
        S.op('dve', lambda e: e.tensor_scalar(out=rs[:, 0:n], in0=ps[:, 0:n], scalar1=EPS, scalar2=None, op0=ALU.add), reads=[ps], writes=[rs])
        S.op('act', lambda e: e.activation(out=rs[:, 0:n], in_=rs[:, 0:n], func=AF.Sqrt), reads=[rs], writes=[rs])
        S.op('dve', lambda e: e.reciprocal(out=rs[:, 0:n], in_=rs[:, 0:n]), reads=[rs], writes=[rs])
        kh = C['kh'].next()
        S.op('dve', lambda e: e.scalar_tensor_tensor(out=kh[:, 0:n], in0=raw[:, 0:n], scalar=gain[:, 0:1], in1=rs[:, 0:n], op0=ALU.mult, op1=ALU.mult),
             reads=[raw, gain, rs], writes=[kh])
        if t0 < TC:
            S.op('act', lambda e: e.activation(out=dst_sl(t0, n), in_=kh[:, 0:n], func=AF.Copy, scale=scale), reads=[kh], writes=[dst])
            continue
        khb = C['khb'].next()
        S.op('act', lambda e: e.copy(out=khb[:, 0:n], in_=kh[:, 0:n]), reads=[kh], writes=[khb])
        ps2 = C['ps'].next()
        S.op('pe', lambda e: e.matmul(ps2[:, 0:n], lhsT=C['psw'][:], rhs=khb[:, 0:n], start=True, stop=True), reads=[khb, C['psw']], writes=[ps2])
        x0 = t0 - TC
        t1 = C['t1'].next()
        S.op('pool', lambda e: e.tensor_tensor(out=t1[:, 0:n], in0=kh[:, 0:n], in1=C['cos'][:, x0:x0 + n], op=ALU.mult), reads=[kh, C['cos']], writes=[t1])
        t2 = C['t2'].next()
        S.op('dve', lambda e: e.tensor_tensor(out=t2[:, 0:n], in0=ps2[:, 0:n], in1=C['sin'][:, x0:x0 + n], op=ALU.mult), reads=[ps2, C['sin']], writes=[t2])
        S.op('dve', lambda e: e.scalar_tensor_tensor(out=dst_sl(t0, n), in0=t1[:, 0:n], scalar=scale, in1=t2[:, 0:n], op0=ALU.mult, op1=ALU.add),
             reads=[t1, t2], writes=[dst])
        if scale != 1.0:
            raise NotImplementedError


def stage_attn(P, li, need_ctx, dbg=0):
    S = P.S
    projT = P.dr['projT']
    vtm = P.dr['vtm']
    attT = P.dr['attT']
    with S.stage():
        C = {}
        C['cos'] = S.sb('cos', [128, TX]); C['sin'] = S.sb('sin', [128, TX])
        C['psw'] = S.sb('psw', [128, 128], BF16); C['blk'] = S.sb('blk', [128, 128])
        pswf = S.sb('pswf', [128, 128])
        S.dma(C['cos'][:], P.dr['rope_cos'][:, :], writes=[C['cos']])
        S.dma(C['sin'][:], P.dr['rope_sin'][:, :], writes=[C['sin']])
        S.dma(pswf[:], P.dr['rope_psw'][:, :], writes=[pswf])
        S.dma(C['blk'][:], P.dr['blk64'][:, :], writes=[C['blk']])
        S.op('dve', lambda e: e.tensor_copy(out=C['psw'][:], in_=pswf[:]), reads=[pswf], writes=[C['psw']])
        qg = S.sb('qg', [128, 1]); kg = S.sb('kg', [128, 1])
        for h in range(2):
            S.dma(qg[h * 64:(h + 1) * 64, :], P.dr['q_norm'][li, :].rearrange("(d o) -> d o", o=1), writes=[qg])
            S.dma(kg[h * 64:(h + 1) * 64, :], P.dr['k_norm'][li, :].rearrange("(d o) -> d o", o=1), writes=[kg])
        S.op('dve', lambda e: e.tensor_scalar(out=qg[:], in0=qg[:], scalar1=0.125, scalar2=None, op0=ALU.mult), reads=[qg], writes=[qg])
        for nm in ('raw', 'sq', 'rs', 'kh', 't1', 't2'):
            C[nm] = Rot([S.sb(f'{nm}{j}', [128, 512]) for j in range(2)])
        C['khb'] = Rot([S.sb(f'khb{j}', [128, 512], BF16) for j in range(2)])
        C['ps'] = Rot([S.ps(f'pps{j}', [128, 512]) for j in range(1)])
        kT = S.sb('kT', [128, T], BF16)
        qT = S.sb('qT', [128, 4, T], BF16)
        qk_prep(P, S, [(O_K, 128, 0)], kg, kT, lambda t0, n: kT[:, t0:t0 + n], 1.0, BLOCKS, C)
        qblocks = BLOCKS if need_ctx else BLOCKS[1:]
        for g in range(4):
            qk_prep(P, S, [(O_Q + g * 64, 64, 0), (O_Q + 256 + g * 64, 64, 64)], qg, qT,
                    lambda t0, n, g=g: qT[:, g, t0:t0 + n], 1.0, qblocks, C)
        if dbg:
            S.dma(P.dr['dbg_k'][:, :], kT[:], reads=[kT])
            S.dma(P.dr['dbg_q'][:, :], qT[:, 0, :], reads=[qT])
        if dbg == 1:
            return
        va = S.sb('va', [128, T // 128, 2, 128], BF16)
        vf = S.sb('vf', [128, T // 128, 128])
        S.op('pool', lambda e: e.memset(va[:], 1.0), writes=[va])
        S.dma(vf[:], vtm.rearrange("(a p) c -> p a c", p=128), writes=[vf])
        S.op('dve', lambda e: e.tensor_copy(out=va[:, :, :, 0:64], in_=vf[:].rearrange("p a (h d) -> p a h d", h=2)), reads=[vf], writes=[va])
        ones_r = S.sb('ones_r', [128, 64])
        S.op('pool', lambda e: e.memset(ones_r[:], 1.0), writes=[ones_r])
        if dbg == 2:
            return
        sps = Rot([S.ps(f'sps{j}', [128, 512]) for j in range(4)])
        ops = Rot([S.ps(f'ops{j}', [128, 512]) for j in range(2)])
        bps = Rot([S.ps(f'bps{j}', [64, 512]) for j in range(1)])
        pts = Rot([S.sb(f'pT{j}', [128, 512], BF16) for j in range(4)])
        osb = Rot([S.sb(f'osb{j}', [128, 512]) for j in range(2)])
        outs = Rot([S.sb(f'aout{j}', [64, 512]) for j in range(2)])
        for kvh in range(2):
            p0 = kvh * 64
            for g in range(4):
                head = kvh * 4 + g
                for (t0, n) in qblocks:
                    nkt = (TC // 128) if t0 < TC else (T // 128)
                    op_ = ops.next()
                    LOOK = 2
                    spq = []

                    def issue_qk(kt):
                        sp_ = sps.next()
                        S.op('pe', lambda e: e.matmul(sp_[:, 0:n], lhsT=kT[p0:p0 + 64, kt * 128:(kt + 1) * 128], rhs=qT[p0:p0 + 64, g, t0:t0 + n],
                                                      start=True, stop=True), reads=[kT, qT], writes=[sp_])
                        spq.append(sp_)

                    for kt in range(min(LOOK, nkt)):
                        issue_qk(kt)
                    for kt in range(nkt):
                        if kt + LOOK < nkt:
                            issue_qk(kt + LOOK)
                        sp_ = spq.pop(0)
                        pt = pts.next()
                        S.op('act', lambda e: e.activation(out=pt[:, 0:n], in_=sp_[:, 0:n], func=AF.Exp), reads=[sp_], writes=[pt])
                        S.op('pe', lambda e, kt=kt: e.matmul(op_[0:65, 0:n], lhsT=va[:, kt, kvh, 0:65], rhs=pt[:, 0:n], start=(kt == 0), stop=(kt == nkt - 1)),
                             reads=[va, pt], writes=[op_])
                    ob = osb.next()
                    S.op('dve', lambda e: e.tensor_copy(out=ob[0:65, 0:n], in_=op_[0:65, 0:n]), reads=[op_], writes=[ob])
                    S.op('dve', lambda e: e.reciprocal(out=ob[64:65, 0:n], in_=ob[64:65, 0:n]), reads=[ob], writes=[ob])
                    bp = bps.next()
                    S.op('pe', lambda e: e.matmul(bp[:, 0:n], lhsT=ones_r[64:65, :], rhs=ob[64:65, 0:n], start=True, stop=True), reads=[ob, ones_r], writes=[bp])
                    ao = outs.next()
                    S.op('dve', lambda e: e.tensor_tensor(out=ao[:, 0:n], in0=ob[0:64, 0:n], in1=bp[:, 0:n], op=ALU.mult), reads=[ob, bp], writes=[ao])
                    S.dma(attT[head * 64:(head + 1) * 64, t0:t0 + n], ao[:, 0:n], reads=[ao], q='pool')


def declare_attn(P):
    P.inp('q_norm', [DEPTH, 64])
    P.inp('k_norm', [DEPTH, 64])
    P.inp('rope_cos', [128, TX])
    P.inp('rope_sin', [128, TX])
    P.inp('rope_psw', [128, 128])
    P.inp('blk64', [128, 128])
    P.tmp('attT', [512, T])


def host_attn(m, inputs):
    cos, sin, psw, blk = rope_tables()
    m['rope_cos'] = cos; m['rope_sin'] = sin; m['rope_psw'] = psw; m['blk64'] = blk
    m['q_norm'] = np.ascontiguousarray(inputs['q_norm'], np.float32)
    m['k_norm'] = np.ascontiguousarray(inputs['k_norm'], np.float32)


def dwconv_fm(S, out, x, wcol, bcol, taps, pad_left, segs, eng='dve', wbuf=None, bbuf=None):
    for (t0, n) in segs:
        j0 = pad_left
        if bcol is not None:
            S.op(eng, lambda e: e.tensor_scalar(out=out[:, t0:t0 + n], in0=x[:, t0:t0 + n], scalar1=wcol[:, j0:j0 + 1], scalar2=bcol,
                                                op0=ALU.mult, op1=ALU.add), reads=[x, wbuf, bbuf], writes=[out])
        else:
            S.op(eng, lambda e: e.tensor_scalar(out=out[:, t0:t0 + n], in0=x[:, t0:t0 + n], scalar1=wcol[:, j0:j0 + 1], scalar2=None,
                                                op0=ALU.mult), reads=[x, wbuf], writes=[out])
        for j in range(taps):
            sh = j - pad_left
            if sh == 0:
                continue
            if sh > 0:
                o_sl = slice(t0, t0 + n - sh); i_sl = slice(t0 + sh, t0 + n)
            else:
                o_sl = slice(t0 - sh, t0 + n); i_sl = slice(t0, t0 + n + sh)
            S.op(eng, lambda e, j=j, o_sl=o_sl, i_sl=i_sl: e.scalar_tensor_tensor(out=out[:, o_sl], in0=x[:, i_sl], scalar=wcol[:, j:j + 1], in1=out[:, o_sl],
                                                                                 op0=ALU.mult, op1=ALU.add), reads=[x, wbuf, out], writes=[out])


def stage_lru(P, li, need_ctx):
    S = P.S
    projT = P.dr['projT']
    lruT = P.dr['lruT']
    SEGS = [(0, TC), (TC, TX)]
    with S.stage():
        big = lambda nm: S.sb(nm, [128, T])
        gate, xin, xc, A, Bv, tmp, h0, h1 = [big(n) for n in ('gate', 'xin', 'xc', 'A', 'Bv', 'tmp', 'h0', 'h1')]
        cw = S.sb('cw', [128, 4]); cb = S.sb('cb', [128, 1])
        wbd = [S.sb(f'wbd{j}', [128, 128]) for j in range(4)]
        bias = S.sb('bias', [128, 4]); lam = S.sb('lam', [128, 2]); c8 = S.sb('c8', [128, 2])
        pss = Rot([S.ps(f'lps{j}', [128, 512]) for j in range(4)])
        for ct in range(2):
            c0 = ct * 128
            S.dma(gate[:], projT[O_LR + c0:O_LR + c0 + 128, :], writes=[gate])
            S.dma(xin[:], projT[O_LR + 256 + c0:O_LR + 256 + c0 + 128, :], writes=[xin])
            S.dma(cw[:], P.dr['lru_conv_w'][li, :, c0:c0 + 128].rearrange("j c -> c j"), writes=[cw], allow_slow_non_contiguous=True)
            S.dma(cb[:], P.dr['lru_conv_b'][li, c0:c0 + 128].rearrange("(c o) -> c o", o=1), writes=[cb])
            for d in range(2):
                for gi, (wn, bn) in enumerate((('lru_wa', 'lru_ba'), ('lru_wx', 'lru_bx'))):
                    w = wbd[d * 2 + gi]
                    S.op('pool', lambda e, w=w: e.memset(w[:], 0.0), writes=[w])
                    for nb in range(2):
                        S.dma(w[nb * 64:(nb + 1) * 64, nb * 64:(nb + 1) * 64], P.dr[wn][li, d, ct * 2 + nb, :, :], writes=[w])
                    S.dma(bias[:, d * 2 + gi:d * 2 + gi + 1], P.dr[bn][li, d, c0:c0 + 128].rearrange("(c o) -> c o", o=1), writes=[bias])
                S.dma(lam[:, d:d + 1], P.dr['lru_lambda'][li, d, c0:c0 + 128].rearrange("(c o) -> c o", o=1), writes=[lam])
            S.op('act', lambda e: e.activation(out=c8[:], in_=lam[:], func=AF.Exp, scale=-1.0), reads=[lam], writes=[c8])
            S.op('dve', lambda e: e.tensor_scalar(out=c8[:], in0=c8[:], scalar1=1.0, scalar2=None, op0=ALU.add), reads=[c8], writes=[c8])
            S.op('act', lambda e: e.activation(out=c8[:], in_=c8[:], func=AF.Ln), reads=[c8], writes=[c8])
            S.op('dve', lambda e: e.tensor_scalar(out=c8[:], in0=c8[:], scalar1=-8.0, scalar2=None, op0=ALU.mult), reads=[c8], writes=[c8])
            dwconv_fm(S, xc, xin, cw[:, :], cb[:, 0:1], 4, 1, SEGS, wbuf=cw, bbuf=cb)
            hs = [h0, h1]
            for d in range(2):
                for (t0, n) in BLOCKS:
                    pa = pss.next(); px = pss.next()
                    S.op('pe', lambda e: e.matmul(pa[:, 0:n], lhsT=wbd[d * 2][:], rhs=xc[:, t0:t0 + n], start=True, stop=True), reads=[wbd[d * 2], xc], writes=[pa])
                    S.op('pe', lambda e: e.matmul(px[:, 0:n], lhsT=wbd[d * 2 + 1][:], rhs=xc[:, t0:t0 + n], start=True, stop=True), reads=[wbd[d * 2 + 1], xc], writes=[px])
                    S.op('act', lambda e: e.activation(out=A[:, t0:t0 + n], in_=pa[:, 0:n], func=AF.Sigmoid, bias=bias[:, d * 2:d * 2 + 1]), reads=[pa, bias], writes=[A])
                    S.op('act', lambda e: e.activation(out=Bv[:, t0:t0 + n], in_=px[:, 0:n], func=AF.Sigmoid, bias=bias[:, d * 2 + 1:d * 2 + 2]), reads=[px, bias], writes=[Bv])
                S.op('act', lambda e: e.activation(out=A[:], in_=A[:], func=AF.Exp, scale=c8[:, d:d + 1]), reads=[A, c8], writes=[A])
                S.op('dve', lambda e: e.tensor_tensor(out=tmp[:], in0=A[:], in1=A[:], op=ALU.mult), reads=[A], writes=[tmp])
                S.op('dve', lambda e: e.tensor_scalar(out=tmp[:], in0=tmp[:], scalar1=-1.0, scalar2=1.0, op0=ALU.mult, op1=ALU.add), reads=[tmp], writes=[tmp])
                S.op('dve', lambda e: e.tensor_scalar(out=tmp[:], in0=tmp[:], scalar1=0.0, scalar2=None, op0=ALU.max), reads=[tmp], writes=[tmp])
                S.op('act', lambda e: e.activation(out=tmp[:], in_=tmp[:], func=AF.Sqrt), reads=[tmp], writes=[tmp])
                S.op('pool', lambda e: e.tensor_tensor(out=Bv[:], in0=Bv[:], in1=xc[:], op=ALU.mult), reads=[Bv, xc], writes=[Bv])
                S.op('dve', lambda e: e.tensor_tensor(out=Bv[:], in0=Bv[:], in1=tmp[:], op=ALU.mult), reads=[Bv, tmp], writes=[Bv])
                h = hs[d]
                if d == 0:
                    S.op('dve', lambda e: e.tensor_tensor_scan(out=h[:, :], data0=A[:, :], data1=Bv[:, :], initial=0.0, op0=ALU.mult, op1=ALU.add),
                         reads=[A, Bv], writes=[h])
                else:
                    S.op('dve', lambda e: e.tensor_tensor_scan(out=h[:, 0:TC][:, ::-1], data0=A[:, 0:TC][:, ::-1], data1=Bv[:, 0:TC][:, ::-1], initial=0.0,
                                                               op0=ALU.mult, op1=ALU.add), reads=[A, Bv], writes=[h])
                    S.op('dve', lambda e: e.tensor_tensor_scan(out=h[:, TC:T][:, ::-1], data0=A[:, TC:T][:, ::-1], data1=Bv[:, TC:T][:, ::-1], initial=h[:, 0:1],
                                                               op0=ALU.mult, op1=ALU.add), reads=[A, Bv, h], writes=[h])
            S.op('pool', lambda e: e.tensor_tensor(out=h0[:], in0=h0[:], in1=h1[:], op=ALU.add), reads=[h0, h1], writes=[h0])
            S.op('dve', lambda e: e.tensor_tensor(out=tmp[:], in0=gate[:], in1=gate[:], op=ALU.mult), reads=[gate], writes=[tmp])
            S.op('dve', lambda e: e.tensor_scalar(out=tmp[:], in0=tmp[:], scalar1=0.044715, scalar2=1.0, op0=ALU.mult, op1=ALU.add), reads=[tmp], writes=[tmp])
            S.op('dve', lambda e: e.tensor_tensor(out=tmp[:], in0=tmp[:], in1=gate[:], op=ALU.mult), reads=[tmp, gate], writes=[tmp])
            S.op('act', lambda e: e.activation(out=tmp[:], in_=tmp[:], func=AF.Sigmoid, scale=1.5957691216), reads=[tmp], writes=[tmp])
            S.op('pool', lambda e: e.tensor_tensor(out=tmp[:], in0=tmp[:], in1=gate[:], op=ALU.mult), reads=[tmp, gate], writes=[tmp])
            S.op('dve', lambda e: e.tensor_tensor(out=h0[:], in0=h0[:], in1=tmp[:], op=ALU.mult), reads=[h0, tmp], writes=[h0])
            S.dma(lruT[c0:c0 + 128, :], h0[:], reads=[h0])


def declare_lru(P):
    P.inp('lru_conv_w', [DEPTH, 4, 256]); P.inp('lru_conv_b', [DEPTH, 256])
    P.inp('lru_wa', [DEPTH, 2, 4, 64, 64]); P.inp('lru_ba', [DEPTH, 2, 256])
    P.inp('lru_wx', [DEPTH, 2, 4, 64, 64]); P.inp('lru_bx', [DEPTH, 2, 256])
    P.inp('lru_lambda', [DEPTH, 2, 256])
    P.tmp('lruT', [256, T])


def host_lru(m, inputs):
    for k in ('lru_conv_w', 'lru_conv_b', 'lru_wa', 'lru_ba', 'lru_wx', 'lru_bx', 'lru_lambda'):
        m[k] = np.ascontiguousarray(inputs[k], np.float32)


BR = [('attT', 'w_br_attn', 4), ('hyT', 'w_br_hyena', 2), ('rwT', 'w_br_rwkv', 2), ('lruT', 'w_br_lru', 2)]


def stage_merge(P, li, need_ctx, src_name, dst_name):
    S = P.S
    projT = P.dr['projT']
    src = P.dr[src_name]
    dst = P.dr[dst_name]
    with S.stage():
        wbr = S.sb('wbr', [128, 10, D], BF16)
        wout = S.sb('wout', [128, 8, D], BF16)
        stg = Rot([S.sb(f'wstg{j}', [128, D]) for j in range(2)])
        ci = 0
        for (_, wn, nch) in BR:
            for c in range(nch):
                st = stg.next()
                S.dma(st[:], P.dr[wn][li, c * 128:(c + 1) * 128, :], writes=[st], q='sp')
                S.op('pool', lambda e, ci=ci, st=st: e.tensor_copy(out=wbr[:, ci, :], in_=st[:]), reads=[st], writes=[wbr])
                ci += 1
        for c in range(8):
            st = stg.next()
            S.dma(st[:], P.dr['w_out'][li, c * 128:(c + 1) * 128, :], writes=[st], q='sp')
            S.op('pool', lambda e, c=c, st=st: e.tensor_copy(out=wout[:, c, :], in_=st[:]), reads=[st], writes=[wout])
        g1 = S.sb('g1', [128, 2, D])
        for s_ in range(2):
            S.dma(g1[:, s_, :], P.dr[f'modrow{li}'][s_:s_ + 1, 2 * D:3 * D].partition_broadcast(128), writes=[g1])
        yf = Rot([S.sb(f'yf{j}', [128, 10, 512]) for j in range(2)])
        yb = Rot([S.sb(f'yb{j}', [128, 10, 512], BF16) for j in range(2)])
        gts = Rot([S.sb(f'gt{j}', [128, 512]) for j in range(4)])
        sgs = Rot([S.sb(f'sg{j}', [128, 512]) for j in range(3)])
        tms = Rot([S.sb(f'tm{j}', [128, 512]) for j in range(3)])
        macc = Rot([S.sb(f'macc{j}', [128, 512]) for j in range(2)])
        mTs = Rot([S.sb(f'mT{j}', [128, 8, 512], BF16) for j in range(2)])
        pss = Rot([S.ps(f'mps{j}', [128, 512]) for j in range(4)])
        ops_ = Rot([S.ps(f'mops{j}', [128, 512]) for j in range(2)])
        xts = Rot([S.sb(f'mx{j}', [128, D]) for j in range(2)])
        blocks = BLOCKS if need_ctx else BLOCKS[1:]
        for (t0, n) in blocks:
            yfl = yf.next(); ybl = yb.next()
            ci = 0
            for (yn, _, nch) in BR:
                S.dma(yfl[:, ci:ci + nch, 0:n], P.dr[yn][:, t0:t0 + n].rearrange("(c p) t -> p c t", p=128), writes=[yfl], q='sp')
                ci += nch
            S.op('pool', lambda e: e.tensor_copy(out=ybl[:, :, 0:n], in_=yfl[:, :, 0:n]), reads=[yfl], writes=[ybl])
            mT = mTs.next()
            for fc in range(8):
                ci = 0
                ma = macc.next()
                for bi, (_, _, nch) in enumerate(BR):
                    ps = pss.next()
                    for c in range(nch):
                        S.op('pe', lambda e, c=c, ci=ci: e.matmul(ps[:, 0:n], lhsT=wbr[:, ci + c, fc * 128:(fc + 1) * 128], rhs=ybl[:, ci + c, 0:n],
                                                                 start=(c == 0), stop=(c == nch - 1)), reads=[wbr, ybl], writes=[ps])
                    ci += nch
                    gt = gts.next()
                    r0 = O_GT + bi * D + fc * 128
                    S.dma(gt[:, 0:n], projT[r0:r0 + 128, t0:t0 + n], writes=[gt], q=('sp' if bi % 2 == 0 else 'act'))
                    sg = sgs.next()
                    S.op('act', lambda e: e.activation(out=sg[:, 0:n], in_=gt[:, 0:n], func=AF.Sigmoid), reads=[gt], writes=[sg])
                    if bi == 0:
                        S.op('dve', lambda e: e.tensor_tensor(out=ma[:, 0:n], in0=ps[:, 0:n], in1=sg[:, 0:n], op=ALU.mult), reads=[ps, sg], writes=[ma])
                    else:
                        tm = tms.next()
                        S.op('dve', lambda e: e.tensor_tensor(out=tm[:, 0:n], in0=ps[:, 0:n], in1=sg[:, 0:n], op=ALU.mult), reads=[ps, sg], writes=[tm])
                        if bi < 3:
                            S.op('pool', lambda e: e.tensor_tensor(out=ma[:, 0:n], in0=ma[:, 0:n], in1=tm[:, 0:n], op=ALU.add), reads=[ma, tm], writes=[ma])
                        else:
                            S.op('pool', lambda e: e.tensor_tensor(out=mT[:, fc, 0:n], in0=ma[:, 0:n], in1=tm[:, 0:n], op=ALU.add), reads=[ma, tm], writes=[mT])
            s_ = 0 if t0 < TC else 1
            for st_ in range(n // 128):
                xt = xts.next()
                S.dma(xt[:], src[t0 + st_ * 128:t0 + (st_ + 1) * 128, :], writes=[xt])
                for half in range(2):
                    po = ops_.next()
                    for fc in range(8):
                        S.op('pe', lambda e, fc=fc: e.matmul(po[:, :], lhsT=mT[:, fc, st_ * 128:(st_ + 1) * 128], rhs=wout[:, fc, half * 512:(half + 1) * 512],
                                                             start=(fc == 0), stop=(fc == 7)), reads=[mT, wout], writes=[po])
                    tm = tms.next()
                    S.op('dve', lambda e: e.tensor_tensor(out=tm[:, :], in0=po[:, :], in1=g1[:, s_, half * 512:(half + 1) * 512], op=ALU.mult), reads=[po, g1], writes=[tm])
                    S.op('pool', lambda e: e.tensor_tensor(out=xt[:, half * 512:(half + 1) * 512], in0=xt[:, half * 512:(half + 1) * 512], in1=tm[:, :], op=ALU.add),
                         reads=[xt, tm], writes=[xt])
                S.dma(dst[t0 + st_ * 128:t0 + (st_ + 1) * 128, :], xt[:], reads=[xt], q='act')


def declare_merge(P):
    P.inp('w_br_attn', [DEPTH, 512, D]); P.inp('w_br_hyena', [DEPTH, 256, D])
    P.inp('w_br_rwkv', [DEPTH, 256, D]); P.inp('w_br_lru', [DEPTH, 256, D])
    P.inp('w_out', [DEPTH, D, D])
    P.tmp('hyT', [256, T]); P.tmp('rwT', [256, T])
    P.tmp('x_mid', [T, D])


def host_merge(m, inputs):
    for k in ('w_br_attn', 'w_br_hyena', 'w_br_rwkv', 'w_br_lru', 'w_out'):
        m[k] = np.ascontiguousarray(inputs[k], np.float32)


def stage_moe(P, li, need_ctx, src_name, dst_name, dst_is_out=False):
    S = P.S
    src = P.dr[src_name]
    dst = P.dr[dst_name]
    if need_ctx:
        groups = [(0, 10), (10, 22), (22, 34)]
    else:
        groups = [(2, 12), (12, 23), (23, 34)]
    GMAX = 12
    with S.stage():
        ident = S.sb('ident', [128, 128], BF16)
        S.dma(ident[:], P.dr['ident_bf'][:, :], writes=[ident])
        modT = load_modT(P, li)
        g2 = S.sb('g2', [128, 2, D])
        for s_ in range(2):
            S.dma(g2[:, s_, :], P.dr[f'modrow{li}'][s_:s_ + 1, 5 * D:6 * D].partition_broadcast(128), writes=[g2])
        wrf = S.sb('wrf', [128, 8, 20]); wrb = S.sb('wrb', [128, 8, 20], BF16)
        S.dma(wrf[:, :, 0:4], P.dr['moe_w_grp'][li].rearrange("(k p) g -> p k g", p=128), writes=[wrf])
        S.dma(wrf[:, :, 4:20], P.dr['moe_w_rt'][li].rearrange("(k p) g -> p k g", p=128), writes=[wrf])
        S.op('dve', lambda e: e.tensor_copy(out=wrb[:], in_=wrf[:]), reads=[wrf], writes=[wrb])
        rb = S.sb('rb', [128, 20])
        S.dma(rb[:, 0:4], P.dr['moe_b_grp'][li:li + 1, :].partition_broadcast(128), writes=[rb])
        S.dma(rb[:, 4:20], P.dr['moe_b_rt'][li:li + 1, :].partition_broadcast(128), writes=[rb])
        h2T = S.sb('h2T', [128, 8, GMAX * 128], BF16)
        acc = S.sb('acc', [128, GMAX, D])
        comb = S.sb('comb', [128, GMAX, 16])
        rt = {nm: S.sb(f'rt_{nm}', shp) for nm, shp in (('lg', [128, 20]), ('mx', [128, 4]), ('ge', [128, 4]), ('gm', [128, 4]), ('m16', [128, 16]),
                                                         ('ml', [128, 16]), ('eq', [128, 16]), ('ml2', [128, 16]), ('ex', [128, 16]))}
        wst = Rot([S.sb(f'wst{j}', [128, 4, 512]) for j in range(2)])
        w1b = Rot([S.sb(f'w1b{j}', [128, 8, 512], BF16) for j in range(2)])
        w3b = Rot([S.sb(f'w3b{j}', [128, 8, 512], BF16) for j in range(2)])
        w2b = Rot([S.sb(f'w2b{j}', [128, 4, D], BF16) for j in range(2)])
        sil = Rot([S.sb(f'sil{j}', [128, 512]) for j in range(3)])
        actb = Rot([S.sb(f'actb{j}', [128, 4, 512], BF16) for j in range(2)])
        pss = Rot([S.ps(f'eps{j}', [128, 512]) for j in range(4)])
        pys = Rot([S.ps(f'yps{j}', [128, 512]) for j in range(2)])
        prs = Rot([S.ps(f'rps{j}', [128, 32]) for j in range(1)])
        xts = Rot([S.sb(f'ox{j}', [128, D]) for j in range(2)])
        dq = [0]

        def load_cast(dst_tile, dram_view, nk):
            cols = dram_view.shape[2]
            for k0 in range(0, nk, 4):
                for c0 in range(0, cols, 512):
                    st = wst.next()
                    S.dma(st[:, :, :], dram_view[:, k0:k0 + 4, c0:c0 + 512], writes=[st], q='sp')
                    dq[0] += 1
                    S.op('pool', lambda e, st=st, k0=k0, c0=c0: e.tensor_copy(out=dst_tile[:, k0:k0 + 4, c0:c0 + 512], in_=st[:, :, :]), reads=[st], writes=[dst_tile])

        NC_ = norm_ctx(S, npt=1)
        for (ga, gb) in groups:
            ng = gb - ga
            norm_transpose(P, src, modT, 24, 32, h2T, ident, tiles=range(ga, gb), dst0=ga * 128, C=NC_)
            for ti in range(ng):
                pr = prs.next()
                for k in range(8):
                    S.op('pe', lambda e, k=k: e.matmul(pr[:, 0:20], lhsT=h2T[:, k, ti * 128:(ti + 1) * 128], rhs=wrb[:, k, :], start=(k == 0), stop=(k == 7)),
                         reads=[h2T, wrb], writes=[pr])
                lg, mx, ge, gm, m16, ml, eq, ml2, ex = (rt[n] for n in ('lg', 'mx', 'ge', 'gm', 'm16', 'ml', 'eq', 'ml2', 'ex'))
                V = lambda fn, rd, wr: S.op('dve', fn, reads=rd, writes=wr)
                V(lambda e: e.tensor_tensor(out=lg[:], in0=pr[:, 0:20], in1=rb[:], op=ALU.add), [pr, rb], [lg])
                V(lambda e: e.reduce_max(out=mx[:, 0:1], in_=lg[:, 0:4], axis=AX.X), [lg], [mx])
                V(lambda e: e.tensor_scalar(out=gm[:], in0=lg[:, 0:4], scalar1=mx[:, 0:1], scalar2=None, op0=ALU.is_equal), [lg, mx], [gm])
                V(lambda e: e.tensor_scalar(out=ge[:], in0=lg[:, 0:4], scalar1=mx[:, 0:1], scalar2=None, op0=ALU.subtract), [lg, mx], [ge])
                S.op('act', lambda e: e.activation(out=ge[:], in_=ge[:], func=AF.Exp), reads=[ge], writes=[ge])
                V(lambda e: e.reduce_sum(out=mx[:, 1:2], in_=ge[:], axis=AX.X), [ge], [mx])
                V(lambda e: e.tensor_copy(out=m16[:].rearrange("p (g e) -> p g e", e=4), in_=gm[:].unsqueeze(2).to_broadcast([128, 4, 4])), [gm], [m16])
                V(lambda e: e.tensor_scalar(out=ml[:], in0=m16[:], scalar1=-1.0, scalar2=1e30, op0=ALU.add, op1=ALU.mult), [m16], [ml])
                V(lambda e: e.tensor_tensor(out=ml[:], in0=ml[:], in1=lg[:, 4:20], op=ALU.add), [ml, lg], [ml])
                V(lambda e: e.reduce_max(out=mx[:, 2:3], in_=ml[:], axis=AX.X), [ml], [mx])
                V(lambda e: e.tensor_scalar(out=eq[:], in0=ml[:], scalar1=mx[:, 2:3], scalar2=None, op0=ALU.is_equal), [ml, mx], [eq])
                V(lambda e: e.scalar_tensor_tensor(out=ml2[:], in0=eq[:], scalar=-1e30, in1=ml[:], op0=ALU.mult, op1=ALU.add), [eq, ml], [ml2])
                V(lambda e: e.reduce_max(out=mx[:, 3:4], in_=ml2[:], axis=AX.X), [ml2], [mx])
                V(lambda e: e.scalar_tensor_tensor(out=eq[:], in0=ml2[:], scalar=mx[:, 3:4], in1=eq[:], op0=ALU.is_equal, op1=ALU.add), [ml2, mx, eq], [eq])
                V(lambda e: e.tensor_scalar(out=ex[:], in0=ml[:], scalar1=mx[:, 2:3], scalar2=-80.0, op0=ALU.subtract, op1=ALU.max), [ml, mx], [ex])
                S.op('act', lambda e: e.activation(out=ex[:], in_=ex[:], func=AF.Exp), reads=[ex], writes=[ex])
                V(lambda e: e.tensor_tensor(out=ex[:], in0=ex[:], in1=eq[:], op=ALU.mult), [ex, eq], [ex])
                V(lambda e: e.reduce_sum(out=mx[:, 2:3], in_=ex[:], axis=AX.X), [ex], [mx])
                V(lambda e: e.tensor_tensor(out=mx[:, 2:3], in0=mx[:, 2:3], in1=mx[:, 1:2], op=ALU.mult), [mx], [mx])
                V(lambda e: e.reciprocal(out=mx[:, 2:3], in_=mx[:, 2:3]), [mx], [mx])
                V(lambda e, ti=ti: e.tensor_scalar(out=comb[:, ti, :], in0=ex[:], scalar1=mx[:, 2:3], scalar2=None, op0=ALU.mult), [ex, mx], [comb])
            nt = ng * 128
            tblocks = [(b0, min(512, nt - b0)) for b0 in range(0, nt, 512)]
            for ex_i in range(16):
                w1 = w1b.next(); w3 = w3b.next(); w2 = w2b.next()
                load_cast(w1, P.dr['moe_w1'][li, ex_i].rearrange("(k p) h -> p k h", p=128), 8)
                load_cast(w3, P.dr['moe_w3'][li, ex_i].rearrange("(k p) h -> p k h", p=128), 8)
                load_cast(w2, P.dr['moe_w2'][li, ex_i].rearrange("(k p) f -> p k f", p=128), 4)
                for (b0, n) in tblocks:
                    ab = actb.next()
                    for hc in range(4):
                        p1 = pss.next(); p3 = pss.next()
                        for k in range(8):
                            S.op('pe', lambda e, k=k: e.matmul(p1[:, 0:n], lhsT=w1[:, k, hc * 128:(hc + 1) * 128], rhs=h2T[:, k, b0:b0 + n], start=(k == 0), stop=(k == 7)),
                                 reads=[w1, h2T], writes=[p1])
                        for k in range(8):
                            S.op('pe', lambda e, k=k: e.matmul(p3[:, 0:n], lhsT=w3[:, k, hc * 128:(hc + 1) * 128], rhs=h2T[:, k, b0:b0 + n], start=(k == 0), stop=(k == 7)),
                                 reads=[w3, h2T], writes=[p3])
                        sl = sil.next()
                        S.op('act', lambda e: e.activation(out=sl[:, 0:n], in_=p1[:, 0:n], func=AF.Silu), reads=[p1], writes=[sl])
                        S.op('dve', lambda e, hc=hc: e.tensor_tensor(out=ab[:, hc, 0:n], in0=sl[:, 0:n], in1=p3[:, 0:n], op=ALU.mult), reads=[sl, p3], writes=[ab])
                    for st_ in range(n // 128):
                        ti = b0 // 128 + st_
                        for half in range(2):
                            py = pys.next()
                            for hc in range(4):
                                S.op('pe', lambda e, hc=hc: e.matmul(py[:, :], lhsT=ab[:, hc, st_ * 128:(st_ + 1) * 128], rhs=w2[:, hc, half * 512:(half + 1) * 512],
                                                                     start=(hc == 0), stop=(hc == 3)), reads=[ab, w2], writes=[py])
                            a_sl = acc[:, ti, half * 512:(half + 1) * 512]
                            if ex_i == 0:
                                S.op('dve', lambda e: e.tensor_scalar(out=a_sl, in0=py[:, :], scalar1=comb[:, ti, ex_i:ex_i + 1], scalar2=None, op0=ALU.mult),
                                     reads=[py, comb], writes=[acc])
                            else:
                                S.op('dve', lambda e: e.scalar_tensor_tensor(out=a_sl, in0=py[:, :], scalar=comb[:, ti, ex_i:ex_i + 1], in1=a_sl, op0=ALU.mult, op1=ALU.add),
                                     reads=[py, comb, acc], writes=[acc])
            for ti in range(ng):
                gt = ga + ti
                s_ = 0 if gt < 2 else 1
                xt = xts.next()
                S.dma(xt[:], src[gt * 128:(gt + 1) * 128, :], writes=[xt])
                S.op('pool', lambda e: e.tensor_tensor(out=acc[:, ti, :], in0=acc[:, ti, :], in1=g2[:, s_, :], op=ALU.mult), reads=[acc, g2], writes=[acc])
                S.op('dve', lambda e: e.tensor_tensor(out=xt[:], in0=xt[:], in1=acc[:, ti, :], op=ALU.add), reads=[xt, acc], writes=[xt])
                if dst_is_out:
                    if gt >= 2:
                        S.dma(dst[(gt - 2) * 128:(gt - 1) * 128, :], xt[:], reads=[xt], q='act', is_out=True)
                else:
                    S.dma(dst[gt * 128:(gt + 1) * 128, :], xt[:], reads=[xt], q='act')


def declare_moe(P):
    P.inp('moe_w_grp', [DEPTH, D, 4]); P.inp('moe_b_grp', [DEPTH, 4])
    P.inp('moe_w_rt', [DEPTH, D, 16]); P.inp('moe_b_rt', [DEPTH, 16])
    P.inp('moe_w1', [DEPTH, 16, D, 512]); P.inp('moe_w3', [DEPTH, 16, D, 512]); P.inp('moe_w2', [DEPTH, 16, 512, D])
    P.tmp('x_l0', [T, D])


def host_moe(m, inputs):
    for k in ('moe_w_grp', 'moe_b_grp', 'moe_w_rt', 'moe_b_rt', 'moe_w1', 'moe_w3', 'moe_w2'):
        m[k] = np.ascontiguousarray(inputs[k], np.float32)


def hyena_consts(n):
    import ml_dtypes
    t = np.arange(n, dtype=np.float32) / np.float32(n)
    bands = np.arange(1, 17, dtype=np.float32)
    ang = (np.float32(2.0 * math.pi) * t[:, None] * bands).astype(np.float32)
    feat = np.concatenate([t[:, None], np.sin(ang), np.cos(ang)], -1).astype(np.float32)
    deltas = np.linspace(-math.log(1e-2) / 1.5, -math.log(1e-2) / 0.3, 256, dtype=np.float32)
    dec = np.exp(-t[:, None] * deltas).astype(np.float32)
    nk = n // 128 + 1
    NP = nk * 128
    idx = np.arange(NP, dtype=np.int64)
    prod = (idx[:, None] * idx[None, :]) % (2 * n)
    angw = 2.0 * math.pi * prod.astype(np.float64) / (2 * n)
    valid = (idx <= n)
    m = (valid[:, None] & valid[None, :])
    wc = np.where(m, np.cos(angw), 0.0); ws = np.where(m, np.sin(angw), 0.0)
    def tile(w):
        return np.ascontiguousarray(w.reshape(nk, 128, nk, 128).transpose(2, 1, 0, 3)).astype(ml_dtypes.bfloat16)
    wk = np.full(NP, 1.0 / n, np.float32); wk[0] = 0.5 / n; wk[n] = 0.5 / n; wk[n + 1:] = 0.0
    wkT = np.ascontiguousarray(wk.reshape(nk, 128).T)
    return dict(featT=np.ascontiguousarray(feat.T), dec=dec, wc=tile(wc), ws=tile(ws), wk=wkT)


def hyena_seq(P, li, n, t_off, sfx):
    S = P.S
    projT = P.dr['projT']
    hyT = P.dr['hyT']
    hyz = P.dr['hyz']
    hspec = P.dr['hspec']
    NT = n // 128
    NK = NT + 1
    wc_d = P.dr['hy_wc' + sfx]; ws_d = P.dr['hy_ws' + sfx]
    kparts = [128] * NT + [1]
    TWO_PI = 2.0 * math.pi
    with S.stage():
        xin = Rot([S.sb(f'hxin{j}', [128, n]) for j in range(2)])
        zo = Rot([S.sb(f'hzo{j}', [128, n]) for j in range(2)])
        cw = S.sb('hcw', [128, 6, 3]); cb = S.sb('hcb', [128, 6])
        for j in range(3):
            S.dma(cw[:, :, j], P.dr['hy_conv_w'][li, j].rearrange("(c p) -> p c", p=128), writes=[cw], allow_slow_non_contiguous=True)
        S.dma(cb[:], P.dr['hy_conv_b'][li].rearrange("(c p) -> p c", p=128), writes=[cb], allow_slow_non_contiguous=True)
        for c in range(6):
            xi = xin.next(); z = zo.next()
            S.dma(xi[:], projT[O_HY + c * 128:O_HY + (c + 1) * 128, t_off:t_off + n], writes=[xi], q='sp')
            dwconv_fm(S, z, xi, cw[:, c, :], cb[:, c:c + 1], 3, 1, [(0, n)], eng='dve', wbuf=cw, bbuf=cb)
            S.dma(hyz[c * 128:(c + 1) * 128, t_off:t_off + n], z[:], reads=[z], q='act')
    with S.stage():
        featT = S.sb('featT', [33, n]); f1 = S.sb('f1', [33, 64]); f2 = S.sb('f2', [64, 64]); f3 = S.sb('f3', [64, 1024])
        fb = S.sb('fb', [64, 2]); h1T = S.sb('h1T', [64, n]); h2T = S.sb('h2T', [64, n])
        S.dma(featT[:], P.dr['hy_featT' + sfx][:, :], writes=[featT])
        S.dma(f1[:], P.dr['hy_f1'][li], writes=[f1]); S.dma(f2[:], P.dr['hy_f2'][li], writes=[f2]); S.dma(f3[:], P.dr['hy_f3'][li], writes=[f3])
        S.dma(fb[:, 0:1], P.dr['hy_fb1'][li].rearrange("(c o) -> c o", o=1), writes=[fb])
        S.dma(fb[:, 1:2], P.dr['hy_fb2'][li].rearrange("(c o) -> c o", o=1), writes=[fb])
        pss = Rot([S.ps(f'hps{j}', [128, 512]) for j in range(4)])
        tmpf = Rot([S.sb(f'htmp{j}', [64, 512]) for j in range(2)])
        tmpq = Rot([S.sb(f'htmq{j}', [64, 512]) for j in range(2)])
        tmpi = Rot([S.sb(f'htmi{j}', [64, 512], mybir.dt.int32) for j in range(2)])
        for (src, wt, dstT, bi) in ((featT, f1, h1T, 0), (h1T, f2, h2T, 1)):
            for b0 in range(0, n, 512):
                nb = min(512, n - b0)
                ps = pss.next()
                S.op('pe', lambda e: e.matmul(ps[0:64, 0:nb], lhsT=wt[:], rhs=src[:, b0:b0 + nb], start=True, stop=True), reads=[wt, src], writes=[ps])
                tm = tmpf.next()
                qi = tmpi.next(); qf = tmpq.next()
                S.op('dve', lambda e: e.tensor_scalar(out=tm[:, 0:nb], in0=ps[0:64, 0:nb], scalar1=fb[:, bi:bi + 1], scalar2=None, op0=ALU.add), reads=[ps, fb], writes=[tm])
                S.op('dve', lambda e: e.tensor_scalar(out=qf[:, 0:nb], in0=tm[:, 0:nb], scalar1=1.0 / TWO_PI, scalar2=None, op0=ALU.mult), reads=[tm], writes=[qf])
                S.op('dve', lambda e: e.tensor_copy(out=qi[:, 0:nb], in_=qf[:, 0:nb]), reads=[qf], writes=[qi])
                S.op('dve', lambda e: e.tensor_copy(out=qf[:, 0:nb], in_=qi[:, 0:nb]), reads=[qi], writes=[qf])
                S.op('dve', lambda e: e.scalar_tensor_tensor(out=tm[:, 0:nb], in0=qf[:, 0:nb], scalar=-TWO_PI, in1=tm[:, 0:nb], op0=ALU.mult, op1=ALU.add), reads=[qf, tm], writes=[tm])
                S.op('dve', lambda e: e.tensor_scalar(out=qf[:, 0:nb], in0=tm[:, 0:nb], scalar1=math.pi, scalar2=-TWO_PI, op0=ALU.is_gt, op1=ALU.mult), reads=[tm], writes=[qf])
                S.op('dve', lambda e: e.tensor_tensor(out=tm[:, 0:nb], in0=tm[:, 0:nb], in1=qf[:, 0:nb], op=ALU.add), reads=[tm, qf], writes=[tm])
                S.op('dve', lambda e: e.tensor_scalar(out=qf[:, 0:nb], in0=tm[:, 0:nb], scalar1=-math.pi, scalar2=TWO_PI, op0=ALU.is_lt, op1=ALU.mult), reads=[tm], writes=[qf])
                S.op('dve', lambda e: e.tensor_tensor(out=tm[:, 0:nb], in0=tm[:, 0:nb], in1=qf[:, 0:nb], op=ALU.add), reads=[tm, qf], writes=[tm])
                S.op('act', lambda e: e.activation(out=dstT[:, b0:b0 + nb], in_=tm[:, 0:nb], func=AF.Sin), reads=[tm], writes=[dstT])
        hbf = S.sb('hbf', [128, NT, 1024], BF16)
        ones = S.sb('hones', [128, 128])
        S.op('pool', lambda e: e.memset(ones[:], 1.0), writes=[ones])
        decs = Rot([S.sb(f'hdec{j}', [128, 256]) for j in range(2)])
        hraw = Rot([S.sb(f'hraw{j}', [128, 1024]) for j in range(2)])
        habs = Rot([S.sb(f'habs{j}', [128, 1024]) for j in range(2)])
        l1ps = [S.ps(f'l1ps{j}', [128, 512]) for j in range(2)]
        for tt in range(NT):
            dc = decs.next()
            S.dma(dc[:], P.dr['hy_dec' + sfx][tt * 128:(tt + 1) * 128, :], writes=[dc])
            hr = hraw.next(); ha = habs.next()
            for half in range(2):
                ps = pss.next()
                S.op('pe', lambda e: e.matmul(ps[:, :], lhsT=h2T[:, tt * 128:(tt + 1) * 128], rhs=f3[:, half * 512:(half + 1) * 512], start=True, stop=True),
                     reads=[h2T, f3], writes=[ps])
                S.op('dve', lambda e: e.tensor_tensor(out=hr[:, half * 512:(half + 1) * 512].rearrange("p (a c) -> p a c", a=2),
                                                      in0=ps[:, :].rearrange("p (a c) -> p a c", a=2),
                                                      in1=dc[:].unsqueeze(1).to_broadcast([128, 2, 256]), op=ALU.mult), reads=[ps, dc], writes=[hr])
            S.op('act', lambda e: e.activation(out=ha[:], in_=hr[:], func=AF.Abs), reads=[hr], writes=[ha])
            for half in range(2):
                S.op('pe', lambda e: e.matmul(l1ps[half][:, :], lhsT=ones[:], rhs=ha[:, half * 512:(half + 1) * 512], start=(tt == 0), stop=(tt == NT - 1)),
                     reads=[ones, ha], writes=[l1ps[half]])
            if tt == 0:
                for o in range(2):
                    S.op('dve', lambda e, o=o: e.memset(hr[0:1, o * 512 + 256:o * 512 + 512], 0.0), reads=[ha], writes=[hr])
            S.op('act', lambda e: e.copy(out=hbf[:, tt, :], in_=hr[:]), reads=[hr], writes=[hbf])
        rl1 = S.sb('rl1', [128, 2, 256])
        l1sb = S.sb('l1sb', [128, 2, 256])
        for o in range(2):
            S.op('act', lambda e, o=o: e.copy(out=l1sb[:, o, :], in_=l1ps[o][:, 0:256]), reads=[l1ps[o]], writes=[l1sb])
            S.op('dve', lambda e, o=o: e.tensor_tensor(out=rl1[:, o, :], in0=l1sb[:, o, :], in1=l1ps[o][:, 256:512], op=ALU.add), reads=[l1ps[o], l1sb], writes=[rl1])
        S.op('dve', lambda e: e.reciprocal(out=rl1[:], in_=rl1[:]), reads=[rl1], writes=[rl1])
        wk = S.sb('hwk', [128, NK])
        S.dma(wk[:], P.dr['hy_wk' + sfx][:, :], writes=[wk])
        wcs = Rot([S.sb(f'hwc{j}', [128, NK, 128], BF16) for j in range(2)])
        wss = Rot([S.sb(f'hws{j}', [128, NK, 128], BF16) for j in range(2)])
        spo = Rot([S.sb(f'hspo{j}', [128, 2, 2, 256]) for j in range(2)])
        for kt in range(NK):
            kp = kparts[kt]
            wct = wcs.next(); wst = wss.next()
            S.dma(wct[:], wc_d[kt], writes=[wct]); S.dma(wst[:], ws_d[kt], writes=[wst])
            so = spo.next()
            pa = [pss.next(), pss.next()]
            for half in range(2):
                for tc in range(NT):
                    S.op('pe', lambda e, tc=tc: e.matmul(pa[half][0:kp, :], lhsT=wct[:, tc, 0:kp], rhs=hbf[:, tc, half * 512:(half + 1) * 512],
                                                         start=(tc == 0), stop=(tc == NT - 1)), reads=[wct, hbf], writes=[pa[half]])
            for o in range(2):
                S.op('act', lambda e, o=o: e.copy(out=so[0:kp, 0, o, :], in_=pa[o][0:kp, 0:256]), reads=[pa[o]], writes=[so])
                S.op('dve', lambda e, o=o: e.tensor_tensor(out=so[0:kp, 0, o, :], in0=so[0:kp, 0, o, :], in1=pa[o][0:kp, 256:512], op=ALU.add), reads=[pa[o], so], writes=[so])
            pb = [pss.next(), pss.next()]
            for half in range(2):
                for tc in range(NT):
                    S.op('pe', lambda e, tc=tc: e.matmul(pb[half][0:kp, :], lhsT=wst[:, tc, 0:kp], rhs=hbf[:, tc, half * 512:(half + 1) * 512],
                                                         start=(tc == 0), stop=(tc == NT - 1)), reads=[wst, hbf], writes=[pb[half]])
            for o in range(2):
                S.op('act', lambda e, o=o: e.copy(out=so[0:kp, 1, o, :], in_=pb[o][0:kp, 256:512]), reads=[pb[o]], writes=[so])
                S.op('dve', lambda e, o=o: e.tensor_tensor(out=so[0:kp, 1, o, :], in0=so[0:kp, 1, o, :], in1=pb[o][0:kp, 0:256], op=ALU.subtract), reads=[pb[o], so], writes=[so])
            for ri in range(2):
                S.op('dve', lambda e, ri=ri: e.scalar_tensor_tensor(out=so[0:kp, ri, :, :], in0=so[0:kp, ri, :, :], scalar=wk[0:kp, kt:kt + 1], in1=rl1[0:kp, :, :],
                                                                    op0=ALU.mult, op1=ALU.mult), reads=[so, wk, rl1], writes=[so])
            S.dma(hspec[kt, 0:kp].rearrange("p a o c -> p (a o c)"), so[0:kp].rearrange("p a o c -> p (a o c)"), reads=[so], q='act')
    with S.stage():
        identf = S.sb('identf', [128, 128])
        S.dma(identf[:], P.dr['ident_f'][:, :], writes=[identf])
        skip = S.sb('hskip', [128, 2, 256])
        for o in range(2):
            S.dma(skip[:, o, :], P.dr['hy_skip'][li, o:o + 1, :].partition_broadcast(128), writes=[skip])
        zf = S.sb('zf', [128, NT, 256]); zb = S.sb('zb', [128, NT, 256], BF16)
        y1 = S.sb('y1', [128, NT, 256])
        Pq = S.sb('Pq', [128, NK, 256], BF16); Qq = S.sb('Qq', [128, NK, 256], BF16)
        fms = Rot([S.sb(f'hfm{j}', [128, 128]) for j in range(4)])
        tps = Rot([S.ps(f'htp{j}', [128, 256]) for j in range(2)])
        pss = Rot([S.ps(f'hsp{j}', [128, 256]) for j in range(4)])
        wcs = Rot([S.sb(f'hwc{j}', [128, NK, 128], BF16) for j in range(3)])
        wss = Rot([S.sb(f'hws{j}', [128, NK, 128], BF16) for j in range(3)])
        hsp = Rot([S.sb(f'hsp_{j}', [128, 2, 2, 256]) for j in range(2)])
        tA = Rot([S.sb(f'htA{j}', [128, 256]) for j in range(2)]); tB = Rot([S.sb(f'htB{j}', [128, 256]) for j in range(2)])
        gtm = Rot([S.sb(f'hgt{j}', [128, 256]) for j in range(2)])
        yo = Rot([S.sb(f'hyo{j}', [128, 256]) for j in range(2)])
        ofm = Rot([S.sb(f'hofm{j}', [128, 128]) for j in range(2)])

        def to_tm(row0, tt, dst_ap, dstbuf):
            tp = tps.next()
            for c in range(2):
                fm = fms.next()
                S.dma(fm[:], hyz[row0 + c * 128:row0 + (c + 1) * 128, t_off + tt * 128:t_off + (tt + 1) * 128], writes=[fm], q='sp')
                S.op('pe', lambda e, c=c: e.transpose(out=tp[:, c * 128:(c + 1) * 128], in_=fm[:], identity=identf[:]), reads=[fm, identf], writes=[tp])
            S.op('act', lambda e: e.copy(out=dst_ap, in_=tp[:, :]), reads=[tp], writes=[dstbuf])

        for tt in range(NT):
            to_tm(0, tt, zf[:, tt, :], zf)
        S.op('pool', lambda e: e.tensor_copy(out=zb[:], in_=zf[:]), reads=[zf], writes=[zb])
        for o in range(2):
            zin_f = zf if o == 0 else y1
            for kt in range(NK):
                kp = kparts[kt]
                wct = wcs.next(); wst = wss.next()
                S.dma(wct[:], wc_d[kt], writes=[wct]); S.dma(wst[:], ws_d[kt], writes=[wst])
                hs = hsp.next()
                S.dma(hs[0:kp].rearrange("p a o c -> p (a o c)"), hspec[kt, 0:kp].rearrange("p a o c -> p (a o c)"), writes=[hs], q='act')
                pa = pss.next(); pb = pss.next()
                for tc in range(NT):
                    S.op('pe', lambda e, tc=tc: e.matmul(pa[0:kp, :], lhsT=wct[:, tc, 0:kp], rhs=zb[:, tc, :], start=(tc == 0), stop=(tc == NT - 1)), reads=[wct, zb], writes=[pa])
                for tc in range(NT):
                    S.op('pe', lambda e, tc=tc: e.matmul(pb[0:kp, :], lhsT=wst[:, tc, 0:kp], rhs=zb[:, tc, :], start=(tc == 0), stop=(tc == NT - 1)), reads=[wst, zb], writes=[pb])
                a_ = tA.next(); b_ = tB.next()
                S.op('dve', lambda e: e.tensor_tensor(out=a_[0:kp], in0=pa[0:kp, :], in1=hs[0:kp, 0, o, :], op=ALU.mult), reads=[pa, hs], writes=[a_])
                S.op('dve', lambda e: e.tensor_tensor(out=b_[0:kp], in0=pb[0:kp, :], in1=hs[0:kp, 1, o, :], op=ALU.mult), reads=[pb, hs], writes=[b_])
                S.op('pool', lambda e: e.tensor_tensor(out=Pq[0:kp, kt, :], in0=a_[0:kp], in1=b_[0:kp], op=ALU.add), reads=[a_, b_], writes=[Pq])
                a2 = tA.next(); b2 = tB.next()
                S.op('dve', lambda e: e.tensor_tensor(out=b2[0:kp], in0=pb[0:kp, :], in1=hs[0:kp, 0, o, :], op=ALU.mult), reads=[pb, hs], writes=[b2])
                S.op('dve', lambda e: e.tensor_tensor(out=a2[0:kp], in0=pa[0:kp, :], in1=hs[0:kp, 1, o, :], op=ALU.mult), reads=[pa, hs], writes=[a2])
                S.op('pool', lambda e: e.tensor_tensor(out=Qq[0:kp, kt, :], in0=b2[0:kp], in1=a2[0:kp], op=ALU.subtract), reads=[a2, b2], writes=[Qq])
            for tt in range(NT):
                wct = wcs.next(); wst = wss.next()
                S.dma(wct[:], wc_d[tt], writes=[wct]); S.dma(wst[:], ws_d[tt], writes=[wst])
                py = pss.next()
                for kc in range(NK):
                    kp = kparts[kc]
                    S.op('pe', lambda e, kc=kc, kp=kp: e.matmul(py[:, :], lhsT=wct[0:kp, kc, :], rhs=Pq[0:kp, kc, :], start=(kc == 0), stop=False), reads=[wct, Pq], writes=[py])
                    S.op('pe', lambda e, kc=kc, kp=kp: e.matmul(py[:, :], lhsT=wst[0:kp, kc, :], rhs=Qq[0:kp, kc, :], start=False, stop=(kc == NK - 1)), reads=[wst, Qq], writes=[py])
                g = gtm.next()
                to_tm(256 * (o + 1), tt, g[:, :], g)
                yy = yo.next()
                S.op('dve', lambda e: e.tensor_tensor(out=yy[:], in0=zin_f[:, tt, :], in1=skip[:, o, :], op=ALU.mult), reads=[zin_f, skip], writes=[yy])
                S.op('dve', lambda e: e.tensor_tensor(out=yy[:], in0=yy[:], in1=py[:, :], op=ALU.add), reads=[yy, py], writes=[yy])
                if o == 0:
                    S.op('pool', lambda e: e.tensor_tensor(out=y1[:, tt, :], in0=yy[:], in1=g[:], op=ALU.mult), reads=[yy, g], writes=[y1])
                else:
                    S.op('pool', lambda e: e.tensor_tensor(out=yy[:], in0=yy[:], in1=g[:], op=ALU.mult), reads=[yy, g], writes=[yy])
                    for c in range(2):
                        tp = tps.next()
                        S.op('pe', lambda e, c=c: e.transpose(out=tp[:, 0:128], in_=yy[:, c * 128:(c + 1) * 128], identity=identf[:]), reads=[yy, identf], writes=[tp])
                        of = ofm.next()
                        S.op('act', lambda e: e.copy(out=of[:], in_=tp[:, 0:128]), reads=[tp], writes=[of])
                        S.dma(hyT[c * 128:(c + 1) * 128, t_off + tt * 128:t_off + (tt + 1) * 128], of[:], reads=[of], q='act')
            if o == 0:
                S.op('pool', lambda e: e.tensor_copy(out=zb[:], in_=y1[:]), reads=[y1], writes=[zb])


def stage_hyena(P, li, need_ctx):
    hyena_seq(P, li, TX, TC, '')
    if need_ctx:
        hyena_seq(P, li, TC, 0, '_c')


def declare_hyena(P):
    P.inp('hy_conv_w', [DEPTH, 3, 768]); P.inp('hy_conv_b', [DEPTH, 768])
    P.inp('hy_f1', [DEPTH, 33, 64]); P.inp('hy_fb1', [DEPTH, 64]); P.inp('hy_f2', [DEPTH, 64, 64]); P.inp('hy_fb2', [DEPTH, 64])
    P.inp('hy_f3', [DEPTH, 64, 1024]); P.inp('hy_skip', [DEPTH, 2, 256])
    P.inp('ident_f', [128, 128])
    for sfx, n in (('', TX), ('_c', TC)):
        nk = n // 128 + 1
        P.inp('hy_featT' + sfx, [33, n]); P.inp('hy_dec' + sfx, [n, 256])
        P.inp('hy_wc' + sfx, [nk, 128, nk, 128], BF16); P.inp('hy_ws' + sfx, [nk, 128, nk, 128], BF16)
        P.inp('hy_wk' + sfx, [128, nk])
    P.tmp('hyz', [768, T])
    P.tmp('hspec', [33, 128, 2, 2, 256])


_HC = {}


def host_hyena(m, inputs):
    for k in ('hy_conv_w', 'hy_conv_b', 'hy_f1', 'hy_fb1', 'hy_f2', 'hy_fb2', 'hy_f3', 'hy_skip'):
        m[k] = np.ascontiguousarray(inputs[k], np.float32)
    m['ident_f'] = np.eye(128, dtype=np.float32)
    for sfx, n in (('', TX), ('_c', TC)):
        if n not in _HC:
            _HC[n] = hyena_consts(n)
        c = _HC[n]
        m['hy_featT' + sfx] = c['featT']; m['hy_dec' + sfx] = c['dec']
        m['hy_wc' + sfx] = c['wc']; m['hy_ws' + sfx] = c['ws']; m['hy_wk' + sfx] = c['wk']


Q_R, Q_V, Q_KK, Q_KD0, Q_KD1, Q_A0, Q_A1, Q_LD0, Q_LD1, Q_G, Q_BON = range(11)
SEGS2 = [(0, TC), (TC, TX)]


def stage_rwkv_prep(P, li):
    S = P.S
    projT = P.dr['projT']
    rwq = P.dr['rwq']
    with S.stage():
        mu = S.sb('mu', [128, 9, 3])
        for j in range(2):
            S.dma(mu[:, :, 2 * j], P.dr['rw_mu'][li, j].rearrange("(c p) -> p c", p=128), writes=[mu], allow_slow_non_contiguous=True)
        S.op('dve', lambda e: e.tensor_tensor(out=mu[:, :, 1], in0=mu[:, :, 0], in1=mu[:, :, 2], op=ALU.add), reads=[mu], writes=[mu])
        S.op('dve', lambda e: e.tensor_scalar(out=mu[:, :, 1], in0=mu[:, :, 1], scalar1=-1.0, scalar2=1.0, op0=ALU.mult, op1=ALU.add), reads=[mu], writes=[mu])
        raw = Rot([S.sb(f'rraw{j}', [128, T]) for j in range(2)])
        blk = S.sb('rblk', [128, 128])
        S.dma(blk[:], P.dr['blk64'][:, :], writes=[blk])
        pss = Rot([S.ps(f'rps{j}', [128, 512]) for j in range(4)])

        def shiftmix(c, dst):
            rw_ = raw.next()
            S.dma(rw_[:], projT[O_RW + c * 128:O_RW + (c + 1) * 128, :], writes=[rw_], q='sp')
            dwconv_fm(S, dst, rw_, mu[:, c, :], None, 3, 1, SEGS2, wbuf=mu)

        with S.stage():
            w1s = S.sb('w1s', [128, T]); a1s = S.sb('a1s', [128, T]); g1s = S.sb('g1s', [128, T])
            shiftmix(6, w1s); shiftmix(7, a1s); shiftmix(8, g1s)
            S.op('act', lambda e: e.activation(out=w1s[:], in_=w1s[:], func=AF.Tanh), reads=[w1s], writes=[w1s])
            S.op('act', lambda e: e.activation(out=g1s[:], in_=g1s[:], func=AF.Sigmoid), reads=[g1s], writes=[g1s])
            w2t = S.sb('w2t', [128, 256]); a2t = S.sb('a2t', [128, 256]); g2t = S.sb('g2t', [128, 256])
            S.dma(w2t[:], P.dr['rw_w2'][li].rearrange("d r c -> (d r) c"), writes=[w2t])
            S.dma(a2t[:], P.dr['rw_a2'][li].rearrange("d r c -> (d r) c"), writes=[a2t])
            S.dma(g2t[:], P.dr['rw_g2'][li], writes=[g2t])
            w0 = S.sb('w0', [128, 2, 2]); a0 = S.sb('a0', [128, 2, 2])
            for d in range(2):
                S.dma(w0[:, d, :], P.dr['rw_w0'][li, d].rearrange("(h p) -> p h", p=128), writes=[w0], allow_slow_non_contiguous=True)
                S.dma(a0[:, d, :], P.dr['rw_a0'][li, d].rearrange("(h p) -> p h", p=128), writes=[a0], allow_slow_non_contiguous=True)
            outs = Rot([S.sb(f'rout{j}', [128, 512]) for j in range(4)])
            for hp in range(2):
                cs = slice(hp * 128, (hp + 1) * 128)
                for (t0, n) in BLOCKS:
                    for d in range(2):
                        ps = pss.next()
                        S.op('pe', lambda e, d=d: e.matmul(ps[:, 0:n], lhsT=w2t[64 * d:64 * d + 64, cs], rhs=w1s[64 * d:64 * d + 64, t0:t0 + n], start=True, stop=True),
                             reads=[w2t, w1s], writes=[ps])
                        o = outs.next()
                        S.op('act', lambda e, d=d: e.activation(out=o[:, 0:n], in_=ps[:, 0:n], func=AF.Sigmoid, bias=w0[:, d, hp:hp + 1]), reads=[ps, w0], writes=[o])
                        S.op('pool', lambda e: e.tensor_scalar(out=o[:, 0:n], in0=o[:, 0:n], scalar1=-0.6065306597126334, scalar2=None, op0=ALU.mult), reads=[o], writes=[o])
                        S.dma(rwq[Q_LD0 + d, cs, t0:t0 + n], o[:, 0:n], reads=[o], q='act')
                        ps = pss.next()
                        S.op('pe', lambda e, d=d: e.matmul(ps[:, 0:n], lhsT=a2t[64 * d:64 * d + 64, cs], rhs=a1s[64 * d:64 * d + 64, t0:t0 + n], start=True, stop=True),
                             reads=[a2t, a1s], writes=[ps])
                        o = outs.next()
                        S.op('act', lambda e, d=d: e.activation(out=o[:, 0:n], in_=ps[:, 0:n], func=AF.Sigmoid, bias=a0[:, d, hp:hp + 1]), reads=[ps, a0], writes=[o])
                        S.dma(rwq[Q_A0 + d, cs, t0:t0 + n], o[:, 0:n], reads=[o], q='act')
                    ps = pss.next()
                    S.op('pe', lambda e: e.matmul(ps[:, 0:n], lhsT=g2t[:, cs], rhs=g1s[:, t0:t0 + n], start=True, stop=True), reads=[g2t, g1s], writes=[ps])
                    o = outs.next()
                    S.op('dve', lambda e: e.tensor_copy(out=o[:, 0:n], in_=ps[:, 0:n]), reads=[ps], writes=[o])
                    S.dma(rwq[Q_G, cs, t0:t0 + n], o[:, 0:n], reads=[o], q='act')
        with S.stage():
            cols = S.sb('rcols', [128, 2, 4])
            for i, nm in enumerate(('rw_k_k', 'rw_k_a')):
                S.dma(cols[:, :, i], P.dr[nm][li].rearrange("(h p) -> p h", p=128), writes=[cols], allow_slow_non_contiguous=True)
            S.dma(cols[:, :, 3], P.dr['rw_r_k'][li].rearrange("h n -> (h n)").rearrange("(h p) -> p h", p=128), writes=[cols], allow_slow_non_contiguous=True)
            S.op('dve', lambda e: e.tensor_scalar(out=cols[:, :, 2], in0=cols[:, :, 1], scalar1=-1.0, scalar2=1.0, op0=ALU.mult, op1=ALU.add), reads=[cols], writes=[cols])
            rs_ = S.sb('r_s', [128, T]); ks_ = S.sb('k_s', [128, T]); vs_ = S.sb('v_s', [128, T])
            kk = S.sb('kk', [128, T]); ad = S.sb('ad', [128, T]); kd = S.sb('kd', [128, T]); bon = S.sb('bon', [128, T])
            tmp = Rot([S.sb(f'rtmp{j}', [128, 512]) for j in range(3)])
            for hp in range(2):
                cs = slice(hp * 128, (hp + 1) * 128)
                shiftmix(0 + hp, rs_); shiftmix(2 + hp, ks_); shiftmix(4 + hp, vs_)
                S.dma(rwq[Q_R, cs, :], rs_[:], reads=[rs_], q='act')
                S.dma(rwq[Q_V, cs, :], vs_[:], reads=[vs_], q='act')
                S.op('dve', lambda e: e.tensor_scalar(out=kk[:], in0=ks_[:], scalar1=cols[:, hp, 0:1], scalar2=None, op0=ALU.mult), reads=[ks_, cols], writes=[kk])
                for (t0, n) in BLOCKS:
                    sq = tmp.next()
                    S.op('act', lambda e: e.activation(out=sq[:, 0:n], in_=kk[:, t0:t0 + n], func=AF.Square), reads=[kk], writes=[sq])
                    ps = pss.next()
                    S.op('pe', lambda e: e.matmul(ps[:, 0:n], lhsT=blk[:], rhs=sq[:, 0:n], start=True, stop=True), reads=[blk, sq], writes=[ps])
                    rs2 = tmp.next()
                    S.op('dve', lambda e: e.tensor_scalar(out=rs2[:, 0:n], in0=ps[:, 0:n], scalar1=64.0, scalar2=1e-12, op0=ALU.mult, op1=ALU.add), reads=[ps], writes=[rs2])
                    S.op('act', lambda e: e.activation(out=rs2[:, 0:n], in_=rs2[:, 0:n], func=AF.Sqrt), reads=[rs2], writes=[rs2])
                    S.op('dve', lambda e: e.reciprocal(out=rs2[:, 0:n], in_=rs2[:, 0:n]), reads=[rs2], writes=[rs2])
                    S.op('dve', lambda e: e.tensor_tensor(out=kk[:, t0:t0 + n], in0=kk[:, t0:t0 + n], in1=rs2[:, 0:n], op=ALU.mult), reads=[kk, rs2], writes=[kk])
                S.dma(rwq[Q_KK, cs, :], kk[:], reads=[kk], q='act')
                for d in range(2):
                    S.dma(ad[:], rwq[Q_A0 + d, cs, :], writes=[ad])
                    S.op('dve', lambda e: e.tensor_scalar(out=kd[:], in0=ad[:], scalar1=cols[:, hp, 1:2], scalar2=cols[:, hp, 2:3], op0=ALU.mult, op1=ALU.add),
                         reads=[ad, cols], writes=[kd])
                    S.op('pool', lambda e: e.tensor_tensor(out=kd[:], in0=kd[:], in1=ks_[:], op=ALU.mult), reads=[kd, ks_], writes=[kd])
                    S.dma(rwq[Q_KD0 + d, cs, :], kd[:], reads=[kd], q='act')
                    for (t0, n) in BLOCKS:
                        rk = tmp.next()
                        S.op('dve', lambda e: e.scalar_tensor_tensor(out=rk[:, 0:n], in0=rs_[:, t0:t0 + n], scalar=cols[:, hp, 3:4], in1=kd[:, t0:t0 + n], op0=ALU.mult, op1=ALU.mult),
                             reads=[rs_, cols, kd], writes=[rk])
                        ps = pss.next()
                        S.op('pe', lambda e: e.matmul(ps[:, 0:n], lhsT=blk[:], rhs=rk[:, 0:n], start=True, stop=True), reads=[blk, rk], writes=[ps])
                        if d == 0:
                            S.op('dve', lambda e: e.scalar_tensor_tensor(out=bon[:, t0:t0 + n], in0=ps[:, 0:n], scalar=64.0, in1=vs_[:, t0:t0 + n], op0=ALU.mult, op1=ALU.mult),
                                 reads=[ps, vs_], writes=[bon])
                        else:
                            b2 = tmp.next()
                            S.op('dve', lambda e: e.scalar_tensor_tensor(out=b2[:, 0:n], in0=ps[:, 0:n], scalar=64.0, in1=vs_[:, t0:t0 + n], op0=ALU.mult, op1=ALU.mult),
                                 reads=[ps, vs_], writes=[b2])
                            S.op('pool', lambda e: e.tensor_tensor(out=bon[:, t0:t0 + n], in0=bon[:, t0:t0 + n], in1=b2[:, 0:n], op=ALU.add), reads=[bon, b2], writes=[bon])
                S.dma(rwq[Q_BON, cs, :], bon[:], reads=[bon], q='act')


class View:
    def __init__(self, buf, ap):
        self.buf = buf
        self.ap = ap

    def __getitem__(self, idx):
        return self.ap[idx]

    w = property(lambda self: self.buf.w, lambda self, v: setattr(self.buf, 'w', v))
    r = property(lambda self: self.buf.r, lambda self, v: setattr(self.buf, 'r', v))


def stage_rwkv_scan(P, li, need_ctx, heads=range(4), dbg_chunks=None, dirs=(0, 1)):
    S = P.S
    rwq = P.dr['rwq']
    rwT = P.dr['rwT']
    NCH = T // 128
    J = 3
    with S.stage():
        identf = S.sb('identf', [128, 128]); SU = S.sb('mSU', [128, 128]); SL = S.sb('mSL', [128, 128]); UI = S.sb('mUI', [128, 128])
        blk = S.sb('rblk', [128, 128])
        S.dma(identf[:], P.dr['ident_f'][:, :], writes=[identf]); S.dma(SU[:], P.dr['mask_su'][:, :], writes=[SU])
        S.dma(SL[:], P.dr['mask_sl'][:, :], writes=[SL]); S.dma(UI[:], P.dr['mask_ui'][:, :], writes=[UI])
        S.dma(blk[:], P.dr['blk64'][:, :], writes=[blk])
        MK = S.sb('MK', [128, 14, 128])
        S.dma(MK[:], P.dr['rw_masks'][:, :, :], writes=[MK])
        big = lambda nm: S.sb(nm, [64, T])
        t_kk, t_a, t_kd, t_r, t_v, yacc = [big(n) for n in ('t_kk', 't_a', 't_kd', 't_r', 't_v', 'yacc')]
        scr = [S.sb(f'scr{j}', [128, T]) for j in range(3)]
        t_ld = Buf(scr[0].t[0:64, :], 't_ld'); t_cum = Buf(scr[1].t[0:64, :], 't_cum'); stg = Buf(scr[2].t[0:64, :], 'stg')
        PC = S.sb('PC', [64, NCH])
        slots = [(scr[i // 34], (i % 34) * 128) for i in range(102)]
        si = [0]

        def slot(ncols=128):
            n = (ncols + 127) // 128
            sc, c0 = slots[si[0]]
            assert c0 + n * 128 <= T and slots[si[0] + n - 1][0] is sc
            si[0] += n
            return Buf(sc.t[:, c0:c0 + ncols], 'slot')

        sets = []
        for p in range(2):
            row = []
            for j in range(J):
                if si[0] % 34 > 34 - 15:
                    si[0] = (si[0] // 34 + 1) * 34
                d_ = {nm: slot() for nm in ('x0', 'xt0', 'Dm0', 'Dm1', 'DT0', 'DT1', 'W0', 'W1', 'G0', 'G1', 'lkt', 'gat', 'gkt')}
                d_['tk'] = slot(192)
                row.append(d_)
            sets.append(row)
        bankA = [S.ps(f'bkA{j}', [128, 512]) for j in range(J)]
        bankB = [S.ps(f'bkB{j}', [128, 512]) for j in range(J)]
        ps_seq = S.ps('ps_seq', [128, 512]); ps_ro2 = S.ps('ps_ro2', [128, 512])
        ps_ro = Rot([ps_seq, ps_ro2])
        B_b1 = View(ps_seq, ps_seq.t[:, 0:64]); B_u = View(ps_seq, ps_seq.t[:, 64:128])
        B_zn = View(ps_seq, ps_seq.t[0:64, 128:192]); B_y = View(ps_seq, ps_seq.t[0:64, 256:384])
        B1s = Rot([S.sb(f'B1s{j}', [128, 64]) for j in range(2)]); nU = Rot([S.sb(f'nU{j}', [128, 64]) for j in range(2)])
        Zs = [S.sb(f'Z{j}', [64, 64]) for j in range(2)]
        Zt = S.sb('Zt', [64, 64])
        rot = Rot([S.sb(f'rot{j}', [64, 512]) for j in range(4)])
        lnc = S.sb('lnc', [64, 4, 2])
        S.dma(lnc[:, :, 0], P.dr['rw_ln_w'][li].rearrange("(h p) -> p h", p=64), writes=[lnc], allow_slow_non_contiguous=True)
        S.dma(lnc[:, :, 1], P.dr['rw_ln_b'][li].rearrange("(h p) -> p h", p=64), writes=[lnc], allow_slow_non_contiguous=True)

        def load(dst, qi, h, d):
            src = rwq[qi, h * 64:(h + 1) * 64, :]
            if d == 0:
                S.dma(dst[:], src, writes=[dst])
            else:
                S.dma(stg[:], src, writes=[stg])
                for (t0, n) in SEGS2:
                    S.op('pool', lambda e, t0=t0, n=n: e.tensor_copy(out=dst[:, t0:t0 + n], in_=stg[:, t0:t0 + n][:, ::-1]), reads=[stg], writes=[dst])

        def indep(c, B, j):
            cl = slice(c * 128, (c + 1) * 128)
            A_ = bankA[j]; Bk = bankB[j]
            trv = View(A_, A_.t[:, 0:192])
            gv = [View(Bk, Bk.t[:, q * 128:(q + 1) * 128]) for q in range(4)] + [View(A_, A_.t[:, 256:384])]
            x0, xt0, tk, lkt, gat, gkt = B['x0'], B['xt0'], B['tk'], B['lkt'], B['gat'], B['gkt']
            Dm = [B['Dm0'], B['Dm1']]; DT = [B['DT0'], B['DT1']]; W = [B['W0'], B['W1']]; G = [B['G0'], B['G1']]
            for q, src in enumerate((t_a, t_kd, t_v)):
                S.op('pe', lambda e, q=q, src=src: e.transpose(out=trv[:, q * 64:(q + 1) * 64], in_=src[:, cl], identity=identf[0:64, 0:64]),
                     reads=[src, identf], writes=[trv])
            yield
            S.op('act', lambda e: e.copy(out=tk[:, :], in_=trv[:, :]), reads=[trv], writes=[tk])
            yield
            for q, (l_, r_) in enumerate(((t_a, t_kk), (t_kk, t_a), (t_kd, t_kk), (t_a, t_r), (t_kd, t_r))):
                S.op('pe', lambda e, q=q, l_=l_, r_=r_: e.matmul(gv[q][:, :], lhsT=l_[:, cl], rhs=r_[:, cl], start=True, stop=True), reads=[l_, r_], writes=[gv[q]])
            yield
            S.op('dve', lambda e: e.scalar_tensor_tensor(out=x0[:, :], in0=gv[0][:, :], scalar=-1.0, in1=SU[:], op0=ALU.mult, op1=ALU.mult), reads=[gv[0], SU], writes=[x0])
            S.op('dve', lambda e: e.scalar_tensor_tensor(out=xt0[:, :], in0=gv[1][:, :], scalar=-1.0, in1=SL[:], op0=ALU.mult, op1=ALU.mult), reads=[gv[1], SL], writes=[xt0])
            S.op('dve', lambda e: e.tensor_tensor(out=lkt[:, :], in0=gv[2][:, :], in1=SU[:], op=ALU.mult), reads=[gv[2], SU], writes=[lkt])
            S.op('dve', lambda e: e.tensor_tensor(out=gat[:, :], in0=gv[3][:, :], in1=UI[:], op=ALU.mult), reads=[gv[3], UI], writes=[gat])
            S.op('dve', lambda e: e.tensor_tensor(out=gkt[:, :], in0=gv[4][:, :], in1=UI[:], op=ALU.mult), reads=[gv[4], UI], writes=[gkt])
            yield
            S.op('pool', lambda e: e.tensor_tensor(out=Dm[0][:, :], in0=xt0[:, :], in1=MK[:, 0, :], op=ALU.mult), reads=[xt0, MK], writes=[Dm[0]])
            S.op('pool', lambda e: e.tensor_tensor(out=Dm[0][:, :], in0=Dm[0][:, :], in1=identf[:], op=ALU.add), reads=[Dm[0], identf], writes=[Dm[0]])
            S.op('pool', lambda e: e.tensor_tensor(out=DT[0][:, :], in0=x0[:, :], in1=MK[:, 7, :], op=ALU.mult), reads=[x0, MK], writes=[DT[0]])
            S.op('pool', lambda e: e.tensor_tensor(out=DT[0][:, :], in0=DT[0][:, :], in1=identf[:], op=ALU.add), reads=[DT[0], identf], writes=[DT[0]])
            yield
            wv = View(A_, A_.t[:, 0:128]); gv1 = View(A_, A_.t[:, 128:256])
            w2v = View(Bk, Bk.t[:, 0:128]); g2v = View(Bk, Bk.t[:, 128:256])
            cur = 0
            for k in range(1, 7):
                nxt = 1 - cur
                if k < 6:
                    S.op('pe', lambda e, cur=cur: e.matmul(wv[:, :], lhsT=x0[:, :], rhs=Dm[cur][:, :], start=True, stop=True), reads=[x0, Dm[cur]], writes=[wv])
                S.op('pe', lambda e, cur=cur: e.matmul(w2v[:, :], lhsT=xt0[:, :], rhs=DT[cur][:, :], start=True, stop=True), reads=[xt0, DT[cur]], writes=[w2v])
                yield
                if k < 6:
                    S.op('act', lambda e: e.copy(out=W[0][:, :], in_=wv[:, :]), reads=[wv], writes=[W[0]])
                S.op('act', lambda e: e.copy(out=W[1][:, :], in_=w2v[:, :]), reads=[w2v], writes=[W[1]])
                yield
                if k < 6:
                    S.op('pe', lambda e, cur=cur: e.matmul(gv1[:, :], lhsT=DT[cur][:, :], rhs=W[0][:, :], start=True, stop=True), reads=[DT[cur], W[0]], writes=[gv1])
                S.op('pe', lambda e, cur=cur: e.matmul(g2v[:, :], lhsT=Dm[cur][:, :], rhs=W[1][:, :], start=True, stop=True), reads=[Dm[cur], W[1]], writes=[g2v])
                yield
                if k < 6:
                    S.op('dve', lambda e, k=k: e.tensor_tensor(out=G[0][:, :], in0=gv1[:, :], in1=MK[:, k, :], op=ALU.mult), reads=[gv1, MK], writes=[G[0]])
                S.op('dve', lambda e, k=k: e.tensor_tensor(out=G[1][:, :], in0=g2v[:, :], in1=MK[:, 7 + k, :], op=ALU.mult), reads=[g2v, MK], writes=[G[1]])
                yield
                if k < 6:
                    S.op('pool', lambda e, cur=cur, nxt=nxt: e.tensor_tensor(out=Dm[nxt][:, :], in0=Dm[cur][:, :], in1=G[0][:, :], op=ALU.add), reads=[Dm[cur], G[0]], writes=[Dm[nxt]])
                S.op('pool', lambda e, cur=cur, nxt=nxt: e.tensor_tensor(out=DT[nxt][:, :], in0=DT[cur][:, :], in1=G[1][:, :], op=ALU.add), reads=[DT[cur], G[1]], writes=[DT[nxt]])
                yield
                cur = nxt
            B['TT'] = DT[cur]

        def seqpart(c, B, d, zi):
            cl = slice(c * 128, (c + 1) * 128)
            tk, lkt, gat, gkt, TT = B['tk'], B['lkt'], B['gat'], B['gkt'], B['TT']
            tkv = lambda q: tk[:, q * 64:(q + 1) * 64]
            Z = Zs[zi]; Zn = Zs[1 - zi]
            S.op('pe', lambda e: e.matmul(B_b1[:, :], lhsT=t_kk[:, cl], rhs=Z[:], start=True, stop=False), reads=[t_kk, Z], writes=[B_b1])
            S.op('pe', lambda e: e.matmul(B_b1[:, :], lhsT=lkt[:, :], rhs=tkv(2), start=False, stop=True), reads=[lkt, tk], writes=[B_b1])
            yield
            b1 = B1s.next()
            S.op('act', lambda e: e.copy(out=b1[:], in_=B_b1[:, :]), reads=[B_b1], writes=[b1])
            yield
            S.op('pe', lambda e: e.matmul(B_u[:, :], lhsT=TT[:, :], rhs=b1[:], start=True, stop=True), reads=[TT, b1], writes=[B_u])
            yield
            nu = nU.next()
            S.op('act', lambda e: e.mul(out=nu[:], in_=B_u[:, :], mul=-1.0), reads=[B_u], writes=[nu])
            yield
            S.op('pe', lambda e: e.matmul(B_y[:, :], lhsT=Z[:], rhs=t_r[:, cl], start=True, stop=False), reads=[Z, t_r], writes=[B_y])
            S.op('pe', lambda e: e.matmul(B_y[:, :], lhsT=nu[:], rhs=gat[:, :], start=False, stop=False), reads=[nu, gat], writes=[B_y])
            S.op('pe', lambda e: e.matmul(B_y[:, :], lhsT=tkv(2), rhs=gkt[:, :], start=False, stop=True), reads=[tk, gkt], writes=[B_y])
            S.op('pe', lambda e: e.matmul(B_zn[:, :], lhsT=tkv(0), rhs=nu[:], start=True, stop=False), reads=[tk, nu], writes=[B_zn])
            S.op('pe', lambda e: e.matmul(B_zn[:, :], lhsT=tkv(1), rhs=tkv(2), start=False, stop=True), reads=[tk], writes=[B_zn])
            yield
            S.op('dve', lambda e: e.tensor_tensor(out=Zt[:], in0=B_zn[:, :], in1=Z[:], op=ALU.add), reads=[B_zn, Z], writes=[Zt])
            if d == 0:
                S.op('dve', lambda e: e.tensor_copy(out=yacc[:, cl], in_=B_y[:, :]), reads=[B_y], writes=[yacc])
            else:
                seg0, segn = (0, TC) if c < 2 else (TC, TX)
                j0 = c * 128 - seg0
                lo = seg0 + segn - j0 - 128
                yv = yacc[:, lo:lo + 128][:, ::-1]
                S.op('dve', lambda e, yv=yv: e.tensor_tensor(out=yv, in0=B_y[:, :], in1=yv, op=ALU.add), reads=[B_y, yacc], writes=[yacc])
            yield
            S.op('act', lambda e: e.activation(out=Zn[:], in_=Zt[:], func=AF.Copy, scale=PC[:, c:c + 1]), reads=[Zt, PC], writes=[Zn])
            yield

        def seqgroup(chs, bufs, d, zi0):
            zi = zi0
            for c, B in zip(chs, bufs):
                yield from seqpart(c, B, d, zi)
                zi = 1 - zi

        def roundrobin(gens):
            gens = list(gens)
            while gens:
                for g in list(gens):
                    try:
                        next(g)
                    except StopIteration:
                        gens.remove(g)

        for h in heads:
            for d in dirs:
                load(t_ld, Q_LD0 + d, h, d)
                ones_t = t_kk
                S.op('pool', lambda e: e.memset(ones_t[:], 1.0), writes=[ones_t])
                for c in range(NCH):
                    S.op('dve', lambda e, c=c: e.tensor_tensor_scan(out=t_cum[:, c * 128:(c + 1) * 128], data0=ones_t[:, c * 128:(c + 1) * 128],
                                                                   data1=t_ld[:, c * 128:(c + 1) * 128], initial=0.0, op0=ALU.mult, op1=ALU.add),
                         reads=[ones_t, t_ld], writes=[t_cum])
                S.op('dve', lambda e: e.tensor_tensor(out=t_ld[:], in0=t_cum[:], in1=t_ld[:], op=ALU.subtract), reads=[t_cum, t_ld], writes=[t_ld])
                S.op('act', lambda e: e.activation(out=t_ld[:], in_=t_ld[:], func=AF.Exp), reads=[t_ld], writes=[t_ld])
                load(t_kk, Q_KK, h, d); load(t_a, Q_A0 + d, h, d)
                S.op('dve', lambda e: e.tensor_tensor(out=t_a[:], in0=t_a[:], in1=t_kk[:], op=ALU.mult), reads=[t_a, t_kk], writes=[t_a])
                S.op('dve', lambda e: e.tensor_tensor(out=t_kk[:], in0=t_kk[:], in1=t_ld[:], op=ALU.mult), reads=[t_kk, t_ld], writes=[t_kk])
                S.op('act', lambda e: e.activation(out=t_ld[:], in_=t_cum[:], func=AF.Exp, scale=-1.0), reads=[t_cum], writes=[t_ld])
                load(t_kd, Q_KD0 + d, h, d)
                S.op('dve', lambda e: e.tensor_tensor(out=t_a[:], in0=t_a[:], in1=t_ld[:], op=ALU.mult), reads=[t_a, t_ld], writes=[t_a])
                S.op('pool', lambda e: e.tensor_tensor(out=t_kd[:], in0=t_kd[:], in1=t_ld[:], op=ALU.mult), reads=[t_kd, t_ld], writes=[t_kd])
                S.op('act', lambda e: e.activation(out=t_cum[:], in_=t_cum[:], func=AF.Exp), reads=[t_cum], writes=[t_cum])
                load(t_r, Q_R, h, d)
                S.op('dve', lambda e: e.tensor_tensor(out=t_r[:], in0=t_r[:], in1=t_cum[:], op=ALU.mult), reads=[t_r, t_cum], writes=[t_r])
                S.op('pool', lambda e: e.tensor_copy(out=PC[:, :], in_=t_cum[:, 127:T:128]), reads=[t_cum], writes=[PC])
                load(t_v, Q_V, h, d)
                S.op('pool', lambda e: e.memset(Zs[0][:], 0.0), writes=[Zs[0]])
                S.barrier()
                nch = NCH if dbg_chunks is None else dbg_chunks
                groups = [list(range(g0, min(g0 + J, nch))) for g0 in range(0, nch, J)]
                zi = 0
                prev = None
                for gi, chs in enumerate(groups):
                    bufs = sets[gi % 2][:len(chs)]
                    gens = [indep(c, B, j) for j, (c, B) in enumerate(zip(chs, bufs))]
                    if prev is not None:
                        gens.append(seqgroup(prev[0], prev[1], d, prev[2]))
                    roundrobin(gens)
                    prev = (chs, bufs, zi)
                    zi = (zi + len(chs)) % 2
                if prev is not None:
                    roundrobin([seqgroup(prev[0], prev[1], d, prev[2])])
                S.barrier()
            if 'dbg_y' in P.dr:
                S.dma(P.dr['dbg_y'][:, :], yacc[:], reads=[yacc])
            load(t_kd, Q_BON, h, 0); load(t_a, Q_G, h, 0)
            for (t0, n) in (BLOCKS if need_ctx else BLOCKS[1:]):
                pm = ps_ro.next()
                S.op('pe', lambda e: e.matmul(pm[0:64, 0:n], lhsT=blk[0:64, 0:64], rhs=yacc[:, t0:t0 + n], start=True, stop=True), reads=[blk, yacc], writes=[pm])
                dd = rot.next()
                S.op('dve', lambda e: e.tensor_tensor(out=dd[:, 0:n], in0=yacc[:, t0:t0 + n], in1=pm[0:64, 0:n], op=ALU.subtract), reads=[yacc, pm], writes=[dd])
                sq = rot.next()
                S.op('act', lambda e: e.activation(out=sq[:, 0:n], in_=dd[:, 0:n], func=AF.Square), reads=[dd], writes=[sq])
                pv = ps_ro.next()
                S.op('pe', lambda e: e.matmul(pv[0:64, 0:n], lhsT=blk[0:64, 0:64], rhs=sq[:, 0:n], start=True, stop=True), reads=[blk, sq], writes=[pv])
                rs2 = rot.next()
                S.op('dve', lambda e: e.tensor_scalar(out=rs2[:, 0:n], in0=pv[0:64, 0:n], scalar1=64e-5, scalar2=None, op0=ALU.add), reads=[pv], writes=[rs2])
                S.op('act', lambda e: e.activation(out=rs2[:, 0:n], in_=rs2[:, 0:n], func=AF.Sqrt), reads=[rs2], writes=[rs2])
                S.op('dve', lambda e: e.reciprocal(out=rs2[:, 0:n], in_=rs2[:, 0:n]), reads=[rs2], writes=[rs2])
                S.op('dve', lambda e: e.tensor_tensor(out=dd[:, 0:n], in0=dd[:, 0:n], in1=rs2[:, 0:n], op=ALU.mult), reads=[dd, rs2], writes=[dd])
                S.op('dve', lambda e: e.tensor_scalar(out=dd[:, 0:n], in0=dd[:, 0:n], scalar1=lnc[:, h, 0:1], scalar2=lnc[:, h, 1:2], op0=ALU.mult, op1=ALU.add),
                     reads=[dd, lnc], writes=[dd])
                S.op('pool', lambda e: e.tensor_tensor(out=dd[:, 0:n], in0=dd[:, 0:n], in1=t_kd[:, t0:t0 + n], op=ALU.add), reads=[dd, t_kd], writes=[dd])
                oo = rot.next()
                S.op('pool', lambda e: e.tensor_tensor(out=oo[:, 0:n], in0=dd[:, 0:n], in1=t_a[:, t0:t0 + n], op=ALU.mult), reads=[dd, t_a], writes=[oo])
                S.dma(rwT[h * 64:(h + 1) * 64, t0:t0 + n], oo[:, 0:n], reads=[oo], q='act')


def declare_rwkv(P):
    P.inp('rw_mu', [DEPTH, 2, 1152]); P.inp('rw_w0', [DEPTH, 2, 256]); P.inp('rw_w2', [DEPTH, 2, 64, 256])
    P.inp('rw_a0', [DEPTH, 2, 256]); P.inp('rw_a2', [DEPTH, 2, 64, 256]); P.inp('rw_g2', [DEPTH, 128, 256])
    P.inp('rw_k_k', [DEPTH, 256]); P.inp('rw_k_a', [DEPTH, 256]); P.inp('rw_r_k', [DEPTH, 4, 64])
    P.inp('rw_ln_w', [DEPTH, 256]); P.inp('rw_ln_b', [DEPTH, 256])
    P.inp('mask_su', [128, 128]); P.inp('mask_sl', [128, 128]); P.inp('mask_ui', [128, 128])
    P.inp('rw_masks', [128, 14, 128])
    P.tmp('rwq', [11, 256, T])


def host_rwkv(m, inputs):
    for k in ('rw_mu', 'rw_w0', 'rw_w2', 'rw_a0', 'rw_a2', 'rw_g2', 'rw_k_k', 'rw_k_a', 'rw_r_k', 'rw_ln_w', 'rw_ln_b'):
        m[k] = np.ascontiguousarray(inputs[k], np.float32)
    su = np.triu(np.ones((128, 128), np.float32), 1)
    m['mask_su'] = su; m['mask_sl'] = np.ascontiguousarray(su.T); m['mask_ui'] = np.triu(np.ones((128, 128), np.float32), 0)
    idx = np.arange(128)
    mk = np.zeros((128, 14, 128), np.float32)
    for k in range(7):
        b = 2 ** k
        M = ((idx[:, None] // (2 * b)) == (idx[None, :] // (2 * b))) & ((idx[:, None] % (2 * b)) >= b) & ((idx[None, :] % (2 * b)) < b)
        mk[:, k, :] = M
        mk[:, 7 + k, :] = M.T
    m['rw_masks'] = mk


def build_full():
    P = Prog()
    declare_common(P); declare_attn(P); declare_lru(P); declare_merge(P); declare_hyena(P); declare_rwkv(P); declare_moe(P)
    P.out('y_out', [TX, D])
    for li in range(DEPTH):
        need_ctx = li < DEPTH - 1
        src = 'xin' if li == 0 else 'x_l0'
        stage_mod(P, li)
        stage_inproj(P, li, src)
        stage_attn(P, li, need_ctx)
        stage_hyena(P, li, need_ctx)
        stage_rwkv_prep(P, li)
        stage_rwkv_scan(P, li, need_ctx)
        stage_lru(P, li, need_ctx)
        stage_merge(P, li, need_ctx, src, 'x_mid')
        if need_ctx:
            stage_moe(P, li, True, 'x_mid', 'x_l0')
        else:
            stage_moe(P, li, False, 'x_mid', 'y_out', dst_is_out=True)
    P.S.finish()
    return P


def full_host_inputs(inputs, b, shared=None):
    if shared is None:
        shared = {}
        m = host_inputs(inputs, b)
        host_attn(m, inputs); host_lru(m, inputs); host_merge(m, inputs); host_hyena(m, inputs); host_rwkv(m, inputs); host_moe(m, inputs)
        for k, v in m.items():
            if k not in ('xin', 'cvec'):
                shared[k] = v
        return m, shared
    m = dict(shared)
    m['xin'] = np.ascontiguousarray(np.concatenate([inputs['ctx'][b], inputs['x'][b]], axis=0), dtype=np.float32)
    m['cvec'] = np.ascontiguousarray(np.stack([inputs['c_ctx'], inputs['c'][b]], axis=0), dtype=np.float32)
    return m, shared


def kernel(**inputs):
    inputs = {k: np.asarray(v) for k, v in inputs.items()}
    P = build_full()
    n = 8
    maps = []
    shared = None
    for b in range(n):
        m, shared = full_host_inputs(inputs, b, shared)
        maps.append({k: m[k] for k in P.in_names})
    res = run_bass_kernel_spmd(P.nc, maps, core_ids=list(range(n)))
    out = np.stack([np.asarray(res.results[b]['y_out'], dtype=np.float32) for b in range(n)], axis=0)
    return out
```

```python
import contextlib
import math
import numpy as np
import concourse.bass as bass
import concourse.mybir as mybir
from concourse.bass_utils import run_bass_kernel_spmd

F32 = mybir.dt.float32
BF16 = mybir.dt.bfloat16
ALU = mybir.AluOpType
AF = mybir.ActivationFunctionType
AX = mybir.AxisListType

ENGS = ('pe', 'dve', 'act', 'pool', 'sp')
SEM_ROLL = 30000
NDMA = 12

D = 1024
TC = 256
TX = 4096
T = TC + TX
N_IN = 7296
DEPTH = 2
EPS = 1e-6
BLOCKS = [(0, 256)] + [(256 + 512 * j, 512) for j in range(8)]
O_Q, O_K, O_V, O_HY, O_RW, O_LR, O_GT = 0, 512, 640, 768, 1536, 2688, 3200


class Buf:
    __slots__ = ('t', 'w', 'r', 'name')

    def __init__(self, t, name=''):
        self.t = t
        self.w = None
        self.r = {}
        self.name = name

    def __getitem__(self, idx):
        return self.t[idx]


class Sched:
    def __init__(self, nc):
        self.nc = nc
        self.emap = {'pe': nc.tensor, 'dve': nc.vector, 'act': nc.scalar, 'pool': nc.gpsimd, 'sp': nc.sync}
        self.perm = contextlib.ExitStack()
        self.stack = contextlib.ExitStack()
        self.cur_sem = {}
        self.cnt = {}
        self.nsem = 0
        for e in ('pe', 'dve', 'act', 'pool'):
            self._new_sem(e)
        self.dma_sems = {}
        self.dma_k = {}
        for q in ('sp', 'pool', 'act'):
            self.dma_sems[q] = [self._alloc_sem(f'dma_{q}_{i}') for i in range(NDMA)]
            self.dma_k[q] = 0
        self.seen = {e: {} for e in ENGS}
        self.out_tokens = []
        self.ninstr = 0
        self.uid = 0

    def _alloc_sem(self, name):
        self.nsem += 1
        return self.perm.enter_context(self.nc.semaphore(f'{name}_{self.nsem}'))

    def _new_sem(self, e):
        self.cur_sem[e] = self._alloc_sem(f'c_{e}')
        self.cnt[e] = 0

    def sb(self, name, shape, dt=F32):
        self.uid += 1
        t = self.stack.enter_context(self.nc.sbuf_tensor(f'{name}_{self.uid}', list(shape), dt))
        return Buf(t, name)

    def ps(self, name, shape, dt=F32):
        self.uid += 1
        t = self.stack.enter_context(self.nc.psum_tensor(f'{name}_{self.uid}', list(shape), dt))
        return Buf(t, name)

    @contextlib.contextmanager
    def stage(self):
        old = self.stack
        self.stack = contextlib.ExitStack()
        try:
            yield
        finally:
            self.barrier()
            self.stack.close()
            self.stack = old

    def barrier(self):
        toks = []
        for f in ('pe', 'dve', 'act', 'pool'):
            if self.cnt[f] > 0:
                toks.append((self.cur_sem[f], self.cnt[f]))
        for q in ('sp', 'pool', 'act'):
            k = self.dma_k[q]
            for j in range(min(k, NDMA)):
                last = ((k - 1 - j) // NDMA) * NDMA + j
                toks.append((self.dma_sems[q][j], 16 * (last // NDMA + 1)))
        for e in ENGS:
            eng = self.emap[e]
            for s, v in toks:
                if self.seen[e].get(id(s), -1) >= v:
                    continue
                self.seen[e][id(s)] = v
                eng.wait_ge(s, v)
                self.ninstr += 1

    def op(self, eng, fn, reads=(), writes=(), dmaq=False, is_out=False):
        need = {}
        reads = [b for b in reads if b is not None]
        writes = [b for b in writes if b is not None]

        def add(tok):
            if tok is None:
                return
            sem, val, teng = tok
            if teng == eng and eng == 'pe' and not dmaq:
                return
            k = id(sem)
            if self.seen[eng].get(k, -1) >= val:
                return
            if k not in need or need[k][1] < val:
                need[k] = (sem, val)

        for b in reads:
            add(b.w)
        for b in writes:
            add(b.w)
            for t in b.r.values():
                add(t)
        if dmaq:
            k = self.dma_k[eng]
            self.dma_k[eng] = k + 1
            sem = self.dma_sems[eng][k % NDMA]
            prev = 16 * (k // NDMA)
            if prev > 0:
                add((sem, prev, 'dma'))
            tok = (sem, prev + 16, 'dma')
            inc = 16
            rkey = ('dma', eng, k % (4 * NDMA))
        else:
            if self.cnt[eng] >= SEM_ROLL:
                self._new_sem(eng)
            self.cnt[eng] += 1
            sem = self.cur_sem[eng]
            tok = (sem, self.cnt[eng], eng)
            inc = 1
            rkey = eng
        e = self.emap[eng]
        for s_, v_ in need.values():
            self.seen[eng][id(s_)] = v_
            e.wait_ge(s_, v_)
        fn(e).then_inc(sem, inc)
        self.ninstr += 1 + len(need)
        for b in reads:
            b.r[rkey] = tok
        for b in writes:
            b.w = tok
            b.r = {}
        if is_out:
            self.out_tokens.append(tok)
        return tok

    def dma(self, out_ap, in_ap, reads=(), writes=(), q='sp', is_out=False, **kw):
        return self.op(q, lambda e: e.dma_start(out=out_ap, in_=in_ap, **kw),
                       reads=reads, writes=writes, dmaq=True, is_out=is_out)

    def finish(self):
        need = {}
        for sem, val, _ in self.out_tokens:
            k = id(sem)
            if k not in need or need[k][1] < val:
                need[k] = (sem, val)
        for s_, v_ in need.values():
            self.nc.sync.wait_ge(s_, v_)
        self.barrier()
        self.stack.close()
        self.perm.close()


class Rot:
    def __init__(self, bufs):
        self.bufs = bufs
        self.i = 0

    def next(self):
        b = self.bufs[self.i % len(self.bufs)]
        self.i += 1
        return b


class Prog:
    def __init__(self, ext_in=(), ext_out=()):
        self.nc = bass.Bass("TRN2", target_bir_lowering=False)
        self.S = Sched(self.nc)
        self.ext_in = set(ext_in)
        self.ext_out = set(ext_out)
        self.dr = {}
        self.in_names = []
        self.out_names = []

    def inp(self, name, shape, dt=F32):
        t = self.nc.dram_tensor(name, list(shape), dt, kind="ExternalInput")
        self.dr[name] = t.ap()
        self.in_names.append(name)
        return self.dr[name]

    def out(self, name, shape, dt=F32):
        t = self.nc.dram_tensor(name, list(shape), dt, kind="ExternalOutput")
        self.dr[name] = t.ap()
        self.out_names.append(name)
        return self.dr[name]

    def tmp(self, name, shape, dt=F32):
        if name in self.ext_in:
            return self.inp(name, shape, dt)
        if name in self.ext_out:
            return self.out(name, shape, dt)
        t = self.nc.dram_tensor(name, list(shape), dt, kind="Internal")
        self.dr[name] = t.ap()
        return self.dr[name]


def stage_mod(P, li):
    S = P.S
    ada_w = P.dr['ada_w']
    ada_b = P.dr['ada_b']
    cvec = P.dr['cvec']
    modT_d = P.dr[f'modT{li}']
    modrow_d = P.dr[f'modrow{li}']
    with S.stage():
        cT = S.sb('cT', [128, 8, 2])
        scT = S.sb('scT', [128, 8, 2])
        abT = S.sb('abT', [128, 48])
        modT = S.sb('modT', [128, 48, 2])
        sig = S.sb('sig', [128, 8, 2])
        for s in range(2):
            S.dma(cT[:, :, s], cvec[s, :].rearrange("(k p) -> p k", p=128), writes=[cT],
                  allow_slow_non_contiguous=True)
        S.dma(abT[:, :], ada_b[li, :].rearrange("(o p) -> p o", p=128), writes=[abT],
              allow_slow_non_contiguous=True)
        S.op('act', lambda e: e.activation(out=sig[:], in_=cT[:], func=AF.Sigmoid), reads=[cT], writes=[sig])
        S.op('dve', lambda e: e.tensor_tensor(out=scT[:], in0=cT[:], in1=sig[:], op=ALU.mult), reads=[cT, sig], writes=[scT])
        wts = Rot([S.sb(f'adaw{j}', [128, 8, 512]) for j in range(2)])
        pss = Rot([S.ps(f'modps{j}', [128, 8]) for j in range(2)])
        aw = ada_w[li].rearrange("(k p) n -> p k n", p=128)
        for g in range(12):
            wt = wts.next()
            S.dma(wt[:], aw[:, :, g * 512:(g + 1) * 512], writes=[wt], q='sp')
            ps = pss.next()
            for j in range(4):
                oc = g * 4 + j
                for k in range(8):
                    S.op('pe', lambda e, j=j, k=k: e.matmul(ps[:, 2 * j:2 * j + 2], lhsT=wt[:, k, j * 128:(j + 1) * 128], rhs=scT[:, k, :],
                                                           start=(k == 0), stop=(k == 7)), reads=[wt, scT], writes=[ps])
            for j in range(4):
                oc = g * 4 + j
                S.op('dve', lambda e, j=j, oc=oc: e.tensor_scalar(out=modT[:, oc, :], in0=ps[:, 2 * j:2 * j + 2], scalar1=abT[:, oc:oc + 1], scalar2=None,
                                                                 op0=ALU.add), reads=[ps, abT], writes=[modT])
        S.dma(modT_d[:, :], modT[:].rearrange("p o s -> p (o s)"), reads=[modT])
        for s in range(2):
            S.dma(modrow_d[s, :].rearrange("(o p) -> p o", p=128), modT[:, :, s], reads=[modT], q='pool',
                  allow_slow_non_contiguous=True)


def load_modT(P, li):
    S = P.S
    m = S.sb('modTl', [128, 48, 2])
    S.dma(m[:].rearrange("p o s -> p (o s)"), P.dr[f'modT{li}'][:, :], writes=[m])
    return m


def norm_ctx(S, npt=2):
    C = {}
    C['sc1p'] = S.sb('sc1p', [128, 8, 2])
    C['xts'] = Rot([S.sb(f'xt{j}', [128, 1024]) for j in range(3)])
    C['sqs'] = Rot([S.sb(f'sq{j}', [128, 1024]) for j in range(2)])
    C['xns'] = Rot([S.sb(f'xn{j}', [128, 1024], BF16) for j in range(2)])
    C['sss'] = Rot([S.sb(f'ss{j}', [128, 2]) for j in range(4)])
    C['pts'] = Rot([S.ps(f'ptT{j}', [128, 4, 128], BF16) for j in range(npt)])
    return C


def norm_transpose(P, src_d, modT, sh_c, sc_c, hT, ident, tiles=None, dst0=0, C=None):
    S = P.S
    if C is None:
        C = norm_ctx(S)
    sc1p = C['sc1p']
    S.op('dve', lambda e: e.tensor_scalar(out=sc1p[:], in0=modT[:, sc_c:sc_c + 8, :], scalar1=1.0, scalar2=None, op0=ALU.add),
         reads=[modT], writes=[sc1p])
    xts, sqs, xns, sss, pts = C['xts'], C['sqs'], C['xns'], C['sss'], C['pts']
    if tiles is None:
        tiles = range(T // 128)
    for ti in tiles:
        s = 0 if ti < 2 else 1
        xt = xts.next()
        S.dma(xt[:], src_d[ti * 128:(ti + 1) * 128, :], writes=[xt], q='sp')
        sq = sqs.next()
        ss = sss.next()
        S.op('act', lambda e: e.activation(out=sq[:], in_=xt[:], func=AF.Square), reads=[xt], writes=[sq])
        S.op('dve', lambda e: e.reduce_sum(out=ss[:, 0:1], in_=sq[:], axis=AX.X), reads=[sq], writes=[ss])
        S.op('dve', lambda e: e.tensor_scalar(out=ss[:, 1:2], in0=ss[:, 0:1], scalar1=1.0 / D, scalar2=EPS, op0=ALU.mult, op1=ALU.add),
             reads=[ss], writes=[ss])
        S.op('act', lambda e: e.activation(out=ss[:, 1:2], in_=ss[:, 1:2], func=AF.Sqrt), reads=[ss], writes=[ss])
        S.op('dve', lambda e: e.reciprocal(out=ss[:, 0:1], in_=ss[:, 1:2]), reads=[ss], writes=[ss])
        xn = xns.next()
        S.op('act', lambda e: e.activation(out=xn[:], in_=xt[:], func=AF.Copy, scale=ss[:, 0:1]), reads=[xt, ss], writes=[xn])
        for half in range(2):
            pt = pts.next()
            for j in range(4):
                k = half * 4 + j
                S.op('pe', lambda e, j=j, k=k: e.transpose(out=pt[:, j, :], in_=xn[:, k * 128:(k + 1) * 128], identity=ident[:]),
                     reads=[xn, ident], writes=[pt])
            for j in range(4):
                k = half * 4 + j
                S.op('dve', lambda e, j=j, k=k: e.tensor_scalar(out=hT[:, k, ti * 128 - dst0:(ti + 1) * 128 - dst0], in0=pt[:, j, :],
                                                               scalar1=sc1p[:, k, s:s + 1], scalar2=modT[:, sh_c + k, s:s + 1],
                                                               op0=ALU.mult, op1=ALU.add), reads=[pt, sc1p, modT], writes=[hT])


def stage_inproj(P, li, src_name):
    S = P.S
    src_d = P.dr[src_name]
    w_in = P.dr['w_in']
    projT = P.dr['projT']
    vtm = P.dr['vtm']
    with S.stage():
        ident = S.sb('ident', [128, 128], BF16)
        S.dma(ident[:], P.dr['ident_bf'][:, :], writes=[ident])
        modT = load_modT(P, li)
        hT = S.sb('hT', [128, 8, T], BF16)
        norm_transpose(P, src_d, modT, 0, 8, hT, ident)
        wfs = Rot([S.sb(f'wf{j}', [128, 8, 384]) for j in range(2)])
        wbs = Rot([S.sb(f'wb{j}', [128, 8, 384], BF16) for j in range(2)])
        pss = Rot([S.ps(f'ps{j}', [128, 512]) for j in range(4)])
        sts = Rot([S.sb(f'st{j}', [128, 512]) for j in range(4)])
        wv = w_in[li].rearrange("(k p) n -> p k n", p=128)
        cnt = 0
        for g in range(N_IN // 384):
            wf = wfs.next()
            S.dma(wf[:], wv[:, :, g * 384:(g + 1) * 384], writes=[wf], q=('sp' if g % 2 == 0 else 'pool'))
            wb = wbs.next()
            S.op('pool', lambda e: e.tensor_copy(out=wb[:], in_=wf[:]), reads=[wf], writes=[wb])
            for j in range(3):
                oc = g * 3 + j
                if oc == O_V // 128:
                    for ti in range(T // 128):
                        ps = pss.next()
                        for k in range(8):
                            S.op('pe', lambda e, k=k: e.matmul(ps[:, 0:128], lhsT=hT[:, k, ti * 128:(ti + 1) * 128], rhs=wb[:, k, j * 128:(j + 1) * 128],
                                                               start=(k == 0), stop=(k == 7)), reads=[hT, wb], writes=[ps])
                        st = sts.next()
                        S.op('act', lambda e: e.copy(out=st[:, 0:128], in_=ps[:, 0:128]), reads=[ps], writes=[st])
                        S.dma(vtm[ti * 128:(ti + 1) * 128, :], st[:, 0:128], reads=[st], q='act')
                    continue
                for (t0, n) in BLOCKS:
                    ps = pss.next()
                    for k in range(8):
                        S.op('pe', lambda e, k=k: e.matmul(ps[:, 0:n], lhsT=wb[:, k, j * 128:(j + 1) * 128], rhs=hT[:, k, t0:t0 + n],
                                                           start=(k == 0), stop=(k == 7)), reads=[hT, wb], writes=[ps])
                    st = sts.next()
                    if cnt % 2 == 0:
                        S.op('act', lambda e: e.copy(out=st[:, 0:n], in_=ps[:, 0:n]), reads=[ps], writes=[st])
                    else:
                        S.op('dve', lambda e: e.tensor_copy(out=st[:, 0:n], in_=ps[:, 0:n]), reads=[ps], writes=[st])
                    S.dma(projT[oc * 128:(oc + 1) * 128, t0:t0 + n], st[:, 0:n], reads=[st], q=('sp' if cnt % 2 == 0 else 'act'))
                    cnt += 1


def declare_common(P):
    P.inp('xin', [T, D])
    P.inp('cvec', [2, D])
    P.inp('ada_w', [DEPTH, D, 6 * D])
    P.inp('ada_b', [DEPTH, 6 * D])
    P.inp('w_in', [DEPTH, D, N_IN])
    P.inp('ident_bf', [128, 128], BF16)
    for li in range(DEPTH):
        P.tmp(f'modT{li}', [128, 96])
        P.tmp(f'modrow{li}', [2, 6 * D])
    P.tmp('projT', [N_IN, T])
    P.tmp('vtm', [T, 128])


def host_inputs(inputs, b):
    import ml_dtypes
    m = {}
    m['xin'] = np.ascontiguousarray(np.concatenate([inputs['ctx'][b], inputs['x'][b]], axis=0), dtype=np.float32)
    m['cvec'] = np.ascontiguousarray(np.stack([inputs['c_ctx'], inputs['c'][b]], axis=0), dtype=np.float32)
    for k in ('ada_w', 'ada_b', 'w_in'):
        m[k] = np.ascontiguousarray(inputs[k], dtype=np.float32)
    m['ident_bf'] = np.eye(128, dtype=np.float32).astype(ml_dtypes.bfloat16)
    return m


def rope_tables():
    t = np.arange(TX)
    pos = np.stack([t // 64, t % 64], 0).astype(np.float64)
    freqs = 10000.0 ** (-np.arange(16, dtype=np.float64) / 16)
    cos = np.zeros((64, TX)); sin = np.zeros((64, TX))
    for d in range(64):
        ax, half, f = d // 32, (d % 32) // 16, d % 16
        ang = pos[ax] * freqs[f]
        cos[d] = np.cos(ang)
        sin[d] = np.sin(ang) * (-1.0 if half == 0 else 1.0)
    psw = np.zeros((128, 128), np.float32)
    for m in range(128):
        d = m % 64
        src = m + 16 if (d % 32) < 16 else m - 16
        psw[src, m] = 1.0
    blk = np.zeros((128, 128), np.float32)
    blk[:64, :64] = 1.0 / 64
    blk[64:, 64:] = 1.0 / 64
    return (np.tile(cos, (2, 1)).astype(np.float32), np.tile(sin, (2, 1)).astype(np.float32), psw, blk)


def qk_prep(P, S, rows_list, gain, dst, dst_sl, scale, tok_ranges, C):
    projT = P.dr['projT']
    for (t0, n) in tok_ranges:
        raw = C['raw'].next()
        for i, (r0, nr, p0) in enumerate(rows_list):
            S.dma(raw[p0:p0 + nr, 0:n], projT[r0:r0 + nr, t0:t0 + n], writes=[raw], q='sp')
        sq = C['sq'].next()
        S.op('act', lambda e: e.activation(out=sq[:, 0:n], in_=raw[:, 0:n], func=AF.Square), reads=[raw], writes=[sq])
        ps = C['ps'].next()
        S.op('pe', lambda e: e.matmul(ps[:, 0:n], lhsT=C['blk'][:], rhs=sq[:, 0:n], start=True, stop=True), reads=[sq, C['blk']], writes=[ps])
        rs = C['rs'].next()
        S.op('dve', lambda e: e.tensor_scalar(out=rs[:, 0:n], in0=ps[:, 0:n], scalar1=EPS, scalar2=None, op0=ALU.add), reads=[ps], writes=[rs])
        S.op('act', lambda e: e.activation(out=rs[:, 0:n], in_=rs[:, 0:n], func=AF.Sqrt), reads=[rs], writes=[rs])
        S.op('dve', lambda e: e.reciprocal(out=rs[:, 0:n], in_=rs[:, 0:n]), reads=[rs], writes=[rs])
        kh = C['kh'].next()
        S.op('dve', lambda e: e.scalar_tensor_tensor(out=kh[:, 0:n], in0=raw[:, 0:n], scalar=gain[:, 0:1], in1=rs[:, 0:n], op0=ALU.mult, op1=ALU.mult),
             reads=[raw, gain, rs], writes=[kh])
        if t0 < TC:
            S.op('act', lambda e: e.activation(out=dst_sl(t0, n), in_=kh[:, 0:n], func=AF.Copy, scale=scale), reads=[kh], writes=[dst])
            continue
        khb = C['khb'].next()
        S.op('act', lambda e: e.copy(out=khb[:, 0:n], in_=kh[:, 0:n]), reads=[kh], writes=[khb])
        ps2 = C['ps'].next()
        S.op('pe', lambda e: e.matmul(ps2[:, 0:n], lhsT=C['psw'][:], rhs=khb[:, 0:n], start=True, stop=True), reads=[khb, C['psw']], writes=[ps2])
        x0 = t0 - TC
        t1 = C['t1'].next()
        S.op('pool', lambda e: e.tensor_tensor(out=t1[:, 0:n], in0=kh[:, 0:n], in1=C['cos'][:, x0:x0 + n], op=ALU.mult), reads=[kh, C['cos']], writes=[t1])
        t2 = C['t2'].next()
        S.op('dve', lambda e: e.tensor_tensor(out=t2[:, 0:n], in0=ps2[:, 0:n], in1=C['sin'][:, x0:x0 + n], op=ALU.mult), reads=[ps2, C['sin']], writes=[t2])
        S.op('dve', lambda e: e.scalar_tensor_tensor(out=dst_sl(t0, n), in0=t1[:, 0:n], scalar=scale, in1=t2[:, 0:n], op0=ALU.mult, op1=ALU.add),
             reads=[t1, t2], writes=[dst])
        if scale != 1.0:
            raise NotImplementedError


def stage_attn(P, li, need_ctx, dbg=0):
    S = P.S
    projT = P.dr['projT']
    vtm = P.dr['vtm']
    attT = P.dr['attT']
    with S.stage():
        C = {}
        C['cos'] = S.sb('cos', [128, TX]); C['sin'] = S.sb('sin', [128, TX])
        C['psw'] = S.sb('psw', [128, 128], BF16); C['blk'] = S.sb('blk', [128, 128])
        pswf = S.sb('pswf', [128, 128])
        S.dma(C['cos'][:], P.dr['rope_cos'][:, :], writes=[C['cos']])
        S.dma(C['sin'][:], P.dr['rope_sin'][:, :], writes=[C['sin']])
        S.dma(pswf[:], P.dr['rope_psw'][:, :], writes=[pswf])
        S.dma(C['blk'][:], P.dr['blk64'][:, :], writes=[C['blk']])
        S.op('dve', lambda e: e.tensor_copy(out=C['psw'][:], in_=pswf[:]), reads=[pswf], writes=[C['psw']])
        qg = S.sb('qg', [128, 1]); kg = S.sb('kg', [128, 1])
        for h in range(2):
            S.dma(qg[h * 64:(h + 1) * 64, :], P.dr['q_norm'][li, :].rearrange("(d o) -> d o", o=1), writes=[qg])
            S.dma(kg[h * 64:(h + 1) * 64, :], P.dr['k_norm'][li, :].rearrange("(d o) -> d o", o=1), writes=[kg])
        S.op('dve', lambda e: e.tensor_scalar(out=qg[:], in0=qg[:], scalar1=0.125, scalar2=None, op0=ALU.mult), reads=[qg], writes=[qg])
        for nm in ('raw', 'sq', 'rs', 'kh', 't1', 't2'):
            C[nm] = Rot([S.sb(f'{nm}{j}', [128, 512]) for j in range(2)])
        C['khb'] = Rot([S.sb(f'khb{j}', [128, 512], BF16) for j in range(2)])
        C['ps'] = Rot([S.ps(f'pps{j}', [128, 512]) for j in range(1)])
        kT = S.sb('kT', [128, T], BF16)
        qT = S.sb('qT', [128, 4, T], BF16)
        qk_prep(P, S, [(O_K, 128, 0)], kg, kT, lambda t0, n: kT[:, t0:t0 + n], 1.0, BLOCKS, C)
        qblocks = BLOCKS if need_ctx else BLOCKS[1:]
        for g in range(4):
            qk_prep(P, S, [(O_Q + g * 64, 64, 0), (O_Q + 256 + g * 64, 64, 64)], qg, qT,
                    lambda t0, n, g=g: qT[:, g, t0:t0 + n], 1.0, qblocks, C)
        if dbg:
            S.dma(P.dr['dbg_k'][:, :], kT[:], reads=[kT])
            S.dma(P.dr['dbg_q'][:, :], qT[:, 0, :], reads=[qT])
        if dbg == 1:
            return
        va = S.sb('va', [128, T // 128, 2, 128], BF16)
        vf = S.sb('vf', [128, T // 128, 128])
        S.op('pool', lambda e: e.memset(va[:], 1.0), writes=[va])
        S.dma(vf[:], vtm.rearrange("(a p) c -> p a c", p=128), writes=[vf])
        S.op('dve', lambda e: e.tensor_copy(out=va[:, :, :, 0:64], in_=vf[:].rearrange("p a (h d) -> p a h d", h=2)), reads=[vf], writes=[va])
        ones_r = S.sb('ones_r', [128, 64])
        S.op('pool', lambda e: e.memset(ones_r[:], 1.0), writes=[ones_r])
        if dbg == 2:
            return
        sps = Rot([S.ps(f'sps{j}', [128, 2, 512]) for j in range(2)])
        ops = Rot([S.ps(f'ops{j}', [128, 512]) for j in range(2)])
        bps = Rot([S.ps(f'bps{j}', [64, 512]) for j in range(1)])
        pts = Rot([S.sb(f'pT{j}', [128, 2, 512], BF16) for j in range(3)])
        osb = Rot([S.sb(f'osb{j}', [128, 512]) for j in range(2)])
        outs = Rot([S.sb(f'aout{j}', [64, 512]) for j in range(2)])
        for kvh in range(2):
            p0 = kvh * 64
            for g in range(4):
                head = kvh * 4 + g
                for (t0, n) in qblocks:
                    nkt = (TC // 128) if t0 < TC else (T // 128)
                    op_ = ops.next()
                    pairs = [list(range(k0, min(k0 + 2, nkt))) for k0 in range(0, nkt, 2)]
                    spq = []

                    def issue_qk(pr):
                        sp_ = sps.next()
                        for i, kt in enumerate(pr):
                            S.op('pe', lambda e, i=i, kt=kt: e.matmul(sp_[:, i, 0:n], lhsT=kT[p0:p0 + 64, kt * 128:(kt + 1) * 128], rhs=qT[p0:p0 + 64, g, t0:t0 + n],
                                                                      start=True, stop=True), reads=[kT, qT], writes=[sp_])
                        spq.append(sp_)

                    issue_qk(pairs[0])
                    for pi, pr in enumerate(pairs):
                        if pi + 1 < len(pairs):
                            issue_qk(pairs[pi + 1])
                        sp_ = spq.pop(0)
                        pt = pts.next()
                        m_ = len(pr)
                        S.op('act', lambda e: e.activation(out=pt[:, 0:m_, 0:n], in_=sp_[:, 0:m_, 0:n], func=AF.Exp), reads=[sp_], writes=[pt])
                        for i, kt in enumerate(pr):
                            S.op('pe', lambda e, i=i, kt=kt: e.matmul(op_[0:65, 0:n], lhsT=va[:, kt, kvh, 0:65], rhs=pt[:, i, 0:n], start=(kt == 0), stop=(kt == nkt - 1)),
                                 reads=[va, pt], writes=[op_])
                    ob = osb.next()
                    S.op('dve', lambda e: e.tensor_copy(out=ob[0:65, 0:n], in_=op_[0:65, 0:n]), reads=[op_], writes=[ob])
                    S.op('dve', lambda e: e.reciprocal(out=ob[64:65, 0:n], in_=ob[64:65, 0:n]), reads=[ob], writes=[ob])
                    bp = bps.next()
                    S.op('pe', lambda e: e.matmul(bp[:, 0:n], lhsT=ones_r[64:65, :], rhs=ob[64:65, 0:n], start=True, stop=True), reads=[ob, ones_r], writes=[bp])
                    ao = outs.next()
                    S.op('dve', lambda e: e.tensor_tensor(out=ao[:, 0:n], in0=ob[0:64, 0:n], in1=bp[:, 0:n], op=ALU.mult), reads=[ob, bp], writes=[ao])
                    S.dma(attT[head * 64:(head + 1) * 64, t0:t0 + n], ao[:, 0:n], reads=[ao], q='pool')


def declare_attn(P):
    P.inp('q_norm', [DEPTH, 64])
    P.inp('k_norm', [DEPTH, 64])
    P.inp('rope_cos', [128, TX])
    P.inp('rope_sin', [128, TX])
    P.inp('rope_psw', [128, 128])
    P.inp('blk64', [128, 128])
    P.tmp('attT', [512, T])


def host_attn(m, inputs):
    cos, sin, psw, blk = rope_tables()
    m['rope_cos'] = cos; m['rope_sin'] = sin; m['rope_psw'] = psw; m['blk64'] = blk
    m['q_norm'] = np.ascontiguousarray(inputs['q_norm'], np.float32)
    m['k_norm'] = np.ascontiguousarray(inputs['k_norm'], np.float32)


def dwconv_fm(S, out, x, wcol, bcol, taps, pad_left, segs, eng='dve', wbuf=None, bbuf=None):
    for (t0, n) in segs:
        j0 = pad_left
        if bcol is not None:
            S.op(eng, lambda e: e.tensor_scalar(out=out[:, t0:t0 + n], in0=x[:, t0:t0 + n], scalar1=wcol[:, j0:j0 + 1], scalar2=bcol,
                                                op0=ALU.mult, op1=ALU.add), reads=[x, wbuf, bbuf], writes=[out])
        else:
            S.op(eng, lambda e: e.tensor_scalar(out=out[:, t0:t0 + n], in0=x[:, t0:t0 + n], scalar1=wcol[:, j0:j0 + 1], scalar2=None,
                                                op0=ALU.mult), reads=[x, wbuf], writes=[out])
        for j in range(taps):
            sh = j - pad_left
            if sh == 0:
                continue
            if sh > 0:
                o_sl = slice(t0, t0 + n - sh); i_sl = slice(t0 + sh, t0 + n)
            else:
                o_sl = slice(t0 - sh, t0 + n); i_sl = slice(t0, t0 + n + sh)
            S.op(eng, lambda e, j=j, o_sl=o_sl, i_sl=i_sl: e.scalar_tensor_tensor(out=out[:, o_sl], in0=x[:, i_sl], scalar=wcol[:, j:j + 1], in1=out[:, o_sl],
                                                                                 op0=ALU.mult, op1=ALU.add), reads=[x, wbuf, out], writes=[out])


def stage_lru(P, li, need_ctx):
    S = P.S
    projT = P.dr['projT']
    lruT = P.dr['lruT']
    SEGS = [(0, TC), (TC, TX)]
    with S.stage():
        big = lambda nm: S.sb(nm, [128, T])
        gate, xin, xc, A, Bv, tmp, h0, h1 = [big(n) for n in ('gate', 'xin', 'xc', 'A', 'Bv', 'tmp', 'h0', 'h1')]
        cw = S.sb('cw', [128, 4]); cb = S.sb('cb', [128, 1])
        wbd = [S.sb(f'wbd{j}', [128, 128]) for j in range(4)]
        bias = S.sb('bias', [128, 4]); lam = S.sb('lam', [128, 2]); c8 = S.sb('c8', [128, 2])
        pss = Rot([S.ps(f'lps{j}', [128, 512]) for j in range(4)])
        for ct in range(2):
            c0 = ct * 128
            S.dma(gate[:], projT[O_LR + c0:O_LR + c0 + 128, :], writes=[gate])
            S.dma(xin[:], projT[O_LR + 256 + c0:O_LR + 256 + c0 + 128, :], writes=[xin])
            S.dma(cw[:], P.dr['lru_conv_w'][li, :, c0:c0 + 128].rearrange("j c -> c j"), writes=[cw], allow_slow_non_contiguous=True)
            S.dma(cb[:], P.dr['lru_conv_b'][li, c0:c0 + 128].rearrange("(c o) -> c o", o=1), writes=[cb])
            for d in range(2):
                for gi, (wn, bn) in enumerate((('lru_wa', 'lru_ba'), ('lru_wx', 'lru_bx'))):
                    w = wbd[d * 2 + gi]
                    S.op('pool', lambda e, w=w: e.memset(w[:], 0.0), writes=[w])
                    for nb in range(2):
                        S.dma(w[nb * 64:(nb + 1) * 64, nb * 64:(nb + 1) * 64], P.dr[wn][li, d, ct * 2 + nb, :, :], writes=[w])
                    S.dma(bias[:, d * 2 + gi:d * 2 + gi + 1], P.dr[bn][li, d, c0:c0 + 128].rearrange("(c o) -> c o", o=1), writes=[bias])
                S.dma(lam[:, d:d + 1], P.dr['lru_lambda'][li, d, c0:c0 + 128].rearrange("(c o) -> c o", o=1), writes=[lam])
            S.op('act', lambda e: e.activation(out=c8[:], in_=lam[:], func=AF.Exp, scale=-1.0), reads=[lam], writes=[c8])
            S.op('dve', lambda e: e.tensor_scalar(out=c8[:], in0=c8[:], scalar1=1.0, scalar2=None, op0=ALU.add), reads=[c8], writes=[c8])
            S.op('act', lambda e: e.activation(out=c8[:], in_=c8[:], func=AF.Ln), reads=[c8], writes=[c8])
            S.op('dve', lambda e: e.tensor_scalar(out=c8[:], in0=c8[:], scalar1=-8.0, scalar2=None, op0=ALU.mult), reads=[c8], writes=[c8])
            dwconv_fm(S, xc, xin, cw[:, :], cb[:, 0:1], 4, 1, SEGS, wbuf=cw, bbuf=cb)
            hs = [h0, h1]
            for d in range(2):
                for (t0, n) in BLOCKS:
                    pa = pss.next(); px = pss.next()
                    S.op('pe', lambda e: e.matmul(pa[:, 0:n], lhsT=wbd[d * 2][:], rhs=xc[:, t0:t0 + n], start=True, stop=True), reads=[wbd[d * 2], xc], writes=[pa])
                    S.op('pe', lambda e: e.matmul(px[:, 0:n], lhsT=wbd[d * 2 + 1][:], rhs=xc[:, t0:t0 + n], start=True, stop=True), reads=[wbd[d * 2 + 1], xc], writes=[px])
                    S.op('act', lambda e: e.activation(out=A[:, t0:t0 + n], in_=pa[:, 0:n], func=AF.Sigmoid, bias=bias[:, d * 2:d * 2 + 1]), reads=[pa, bias], writes=[A])
                    S.op('act', lambda e: e.activation(out=Bv[:, t0:t0 + n], in_=px[:, 0:n], func=AF.Sigmoid, bias=bias[:, d * 2 + 1:d * 2 + 2]), reads=[px, bias], writes=[Bv])
                S.op('act', lambda e: e.activation(out=A[:], in_=A[:], func=AF.Exp, scale=c8[:, d:d + 1]), reads=[A, c8], writes=[A])
                S.op('dve', lambda e: e.tensor_tensor(out=tmp[:], in0=A[:], in1=A[:], op=ALU.mult), reads=[A], writes=[tmp])
                S.op('dve', lambda e: e.tensor_scalar(out=tmp[:], in0=tmp[:], scalar1=-1.0, scalar2=1.0, op0=ALU.mult, op1=ALU.add), reads=[tmp], writes=[tmp])
                S.op('dve', lambda e: e.tensor_scalar(out=tmp[:], in0=tmp[:], scalar1=0.0, scalar2=None, op0=ALU.max), reads=[tmp], writes=[tmp])
                S.op('act', lambda e: e.activation(out=tmp[:], in_=tmp[:], func=AF.Sqrt), reads=[tmp], writes=[tmp])
                S.op('pool', lambda e: e.tensor_tensor(out=Bv[:], in0=Bv[:], in1=xc[:], op=ALU.mult), reads=[Bv, xc], writes=[Bv])
                S.op('dve', lambda e: e.tensor_tensor(out=Bv[:], in0=Bv[:], in1=tmp[:], op=ALU.mult), reads=[Bv, tmp], writes=[Bv])
                h = hs[d]
                if d == 0:
                    S.op('dve', lambda e: e.tensor_tensor_scan(out=h[:, :], data0=A[:, :], data1=Bv[:, :], initial=0.0, op0=ALU.mult, op1=ALU.add),
                         reads=[A, Bv], writes=[h])
                else:
                    S.op('dve', lambda e: e.tensor_tensor_scan(out=h[:, 0:TC][:, ::-1], data0=A[:, 0:TC][:, ::-1], data1=Bv[:, 0:TC][:, ::-1], initial=0.0,
                                                               op0=ALU.mult, op1=ALU.add), reads=[A, Bv], writes=[h])
                    S.op('dve', lambda e: e.tensor_tensor_scan(out=h[:, TC:T][:, ::-1], data0=A[:, TC:T][:, ::-1], data1=Bv[:, TC:T][:, ::-1], initial=h[:, 0:1],
                                                               op0=ALU.mult, op1=ALU.add), reads=[A, Bv, h], writes=[h])
            S.op('pool', lambda e: e.tensor_tensor(out=h0[:], in0=h0[:], in1=h1[:], op=ALU.add), reads=[h0, h1], writes=[h0])
            S.op('dve', lambda e: e.tensor_tensor(out=tmp[:], in0=gate[:], in1=gate[:], op=ALU.mult), reads=[gate], writes=[tmp])
            S.op('dve', lambda e: e.tensor_scalar(out=tmp[:], in0=tmp[:], scalar1=0.044715, scalar2=1.0, op0=ALU.mult, op1=ALU.add), reads=[tmp], writes=[tmp])
            S.op('dve', lambda e: e.tensor_tensor(out=tmp[:], in0=tmp[:], in1=gate[:], op=ALU.mult), reads=[tmp, gate], writes=[tmp])
            S.op('act', lambda e: e.activation(out=tmp[:], in_=tmp[:], func=AF.Sigmoid, scale=1.5957691216), reads=[tmp], writes=[tmp])
            S.op('pool', lambda e: e.tensor_tensor(out=tmp[:], in0=tmp[:], in1=gate[:], op=ALU.mult), reads=[tmp, gate], writes=[tmp])
            S.op('dve', lambda e: e.tensor_tensor(out=h0[:], in0=h0[:], in1=tmp[:], op=ALU.mult), reads=[h0, tmp], writes=[h0])
            S.dma(lruT[c0:c0 + 128, :], h0[:], reads=[h0])


def declare_lru(P):
    P.inp('lru_conv_w', [DEPTH, 4, 256]); P.inp('lru_conv_b', [DEPTH, 256])
    P.inp('lru_wa', [DEPTH, 2, 4, 64, 64]); P.inp('lru_ba', [DEPTH, 2, 256])
    P.inp('lru_wx', [DEPTH, 2, 4, 64, 64]); P.inp('lru_bx', [DEPTH, 2, 256])
    P.inp('lru_lambda', [DEPTH, 2, 256])
    P.tmp('lruT', [256, T])


def host_lru(m, inputs):
    for k in ('lru_conv_w', 'lru_conv_b', 'lru_wa', 'lru_ba', 'lru_wx', 'lru_bx', 'lru_lambda'):
        m[k] = np.ascontiguousarray(inputs[k], np.float32)


BR = [('attT', 'w_br_attn', 4), ('hyT', 'w_br_hyena', 2), ('rwT', 'w_br_rwkv', 2), ('lruT', 'w_br_lru', 2)]


def stage_merge(P, li, need_ctx, src_name, dst_name):
    S = P.S
    projT = P.dr['projT']
    src = P.dr[src_name]
    dst = P.dr[dst_name]
    with S.stage():
        wbr = S.sb('wbr', [128, 10, D], BF16)
        wout = S.sb('wout', [128, 8, D], BF16)
        stg = Rot([S.sb(f'wstg{j}', [128, D]) for j in range(2)])
        ci = 0
        for (_, wn, nch) in BR:
            for c in range(nch):
                st = stg.next()
                S.dma(st[:], P.dr[wn][li, c * 128:(c + 1) * 128, :], writes=[st], q='sp')
                S.op('pool', lambda e, ci=ci, st=st: e.tensor_copy(out=wbr[:, ci, :], in_=st[:]), reads=[st], writes=[wbr])
                ci += 1
        for c in range(8):
            st = stg.next()
            S.dma(st[:], P.dr['w_out'][li, c * 128:(c + 1) * 128, :], writes=[st], q='sp')
            S.op('pool', lambda e, c=c, st=st: e.tensor_copy(out=wout[:, c, :], in_=st[:]), reads=[st], writes=[wout])
        g1 = S.sb('g1', [128, 2, D])
        for s_ in range(2):
            S.dma(g1[:, s_, :], P.dr[f'modrow{li}'][s_:s_ + 1, 2 * D:3 * D].partition_broadcast(128), writes=[g1])
        yf = Rot([S.sb(f'yf{j}', [128, 10, 512]) for j in range(2)])
        yb = Rot([S.sb(f'yb{j}', [128, 10, 512], BF16) for j in range(2)])
        gts = Rot([S.sb(f'gt{j}', [128, 512]) for j in range(4)])
        sgs = Rot([S.sb(f'sg{j}', [128, 512]) for j in range(3)])
        tms = Rot([S.sb(f'tm{j}', [128, 512]) for j in range(3)])
        macc = Rot([S.sb(f'macc{j}', [128, 512]) for j in range(2)])
        mTs = Rot([S.sb(f'mT{j}', [128, 8, 512], BF16) for j in range(2)])
        pss = Rot([S.ps(f'mps{j}', [128, 512]) for j in range(4)])
        ops_ = Rot([S.ps(f'mops{j}', [128, 512]) for j in range(2)])
        xts = Rot([S.sb(f'mx{j}', [128, D]) for j in range(2)])
        blocks = BLOCKS if need_ctx else BLOCKS[1:]
        for (t0, n) in blocks:
            yfl = yf.next(); ybl = yb.next()
            ci = 0
            for (yn, _, nch) in BR:
                S.dma(yfl[:, ci:ci + nch, 0:n], P.dr[yn][:, t0:t0 + n].rearrange("(c p) t -> p c t", p=128), writes=[yfl], q='sp')
                ci += nch
            S.op('pool', lambda e: e.tensor_copy(out=ybl[:, :, 0:n], in_=yfl[:, :, 0:n]), reads=[yfl], writes=[ybl])
            mT = mTs.next()
            for fc in range(8):
                ci = 0
                ma = macc.next()
                for bi, (_, _, nch) in enumerate(BR):
                    ps = pss.next()
                    for c in range(nch):
                        S.op('pe', lambda e, c=c, ci=ci: e.matmul(ps[:, 0:n], lhsT=wbr[:, ci + c, fc * 128:(fc + 1) * 128], rhs=ybl[:, ci + c, 0:n],
                                                                 start=(c == 0), stop=(c == nch - 1)), reads=[wbr, ybl], writes=[ps])
                    ci += nch
                    gt = gts.next()
                    r0 = O_GT + bi * D + fc * 128
                    S.dma(gt[:, 0:n], projT[r0:r0 + 128, t0:t0 + n], writes=[gt], q=('sp' if bi % 2 == 0 else 'act'))
                    sg = sgs.next()
                    S.op('act', lambda e: e.activation(out=sg[:, 0:n], in_=gt[:, 0:n], func=AF.Sigmoid), reads=[gt], writes=[sg])
                    if bi == 0:
                        S.op('dve', lambda e: e.tensor_tensor(out=ma[:, 0:n], in0=ps[:, 0:n], in1=sg[:, 0:n], op=ALU.mult), reads=[ps, sg], writes=[ma])
                    else:
                        tm = tms.next()
                        S.op('dve', lambda e: e.tensor_tensor(out=tm[:, 0:n], in0=ps[:, 0:n], in1=sg[:, 0:n], op=ALU.mult), reads=[ps, sg], writes=[tm])
                        if bi < 3:
                            S.op('pool', lambda e: e.tensor_tensor(out=ma[:, 0:n], in0=ma[:, 0:n], in1=tm[:, 0:n], op=ALU.add), reads=[ma, tm], writes=[ma])
                        else:
                            S.op('pool', lambda e: e.tensor_tensor(out=mT[:, fc, 0:n], in0=ma[:, 0:n], in1=tm[:, 0:n], op=ALU.add), reads=[ma, tm], writes=[mT])
            s_ = 0 if t0 < TC else 1
            for st_ in range(n // 128):
                xt = xts.next()
                S.dma(xt[:], src[t0 + st_ * 128:t0 + (st_ + 1) * 128, :], writes=[xt])
                for half in range(2):
                    po = ops_.next()
                    for fc in range(8):
                        S.op('pe', lambda e, fc=fc: e.matmul(po[:, :], lhsT=mT[:, fc, st_ * 128:(st_ + 1) * 128], rhs=wout[:, fc, half * 512:(half + 1) * 512],
                                                             start=(fc == 0), stop=(fc == 7)), reads=[mT, wout], writes=[po])
                    tm = tms.next()
                    S.op('dve', lambda e: e.tensor_tensor(out=tm[:, :], in0=po[:, :], in1=g1[:, s_, half * 512:(half + 1) * 512], op=ALU.mult), reads=[po, g1], writes=[tm])
                    S.op('pool', lambda e: e.tensor_tensor(out=xt[:, half * 512:(half + 1) * 512], in0=xt[:, half * 512:(half + 1) * 512], in1=tm[:, :], op=ALU.add),
                         reads=[xt, tm], writes=[xt])
                S.dma(dst[t0 + st_ * 128:t0 + (st_ + 1) * 128, :], xt[:], reads=[xt], q='act')


def declare_merge(P):
    P.inp('w_br_attn', [DEPTH, 512, D]); P.inp('w_br_hyena', [DEPTH, 256, D])
    P.inp('w_br_rwkv', [DEPTH, 256, D]); P.inp('w_br_lru', [DEPTH, 256, D])
    P.inp('w_out', [DEPTH, D, D])
    P.tmp('hyT', [256, T]); P.tmp('rwT', [256, T])
    P.tmp('x_mid', [T, D])


def host_merge(m, inputs):
    for k in ('w_br_attn', 'w_br_hyena', 'w_br_rwkv', 'w_br_lru', 'w_out'):
        m[k] = np.ascontiguousarray(inputs[k], np.float32)


def stage_moe(P, li, need_ctx, src_name, dst_name, dst_is_out=False):
    S = P.S
    src = P.dr[src_name]
    dst = P.dr[dst_name]
    if need_ctx:
        groups = [(0, 10), (10, 22), (22, 34)]
    else:
        groups = [(2, 12), (12, 23), (23, 34)]
    GMAX = 12
    with S.stage():
        ident = S.sb('ident', [128, 128], BF16)
        S.dma(ident[:], P.dr['ident_bf'][:, :], writes=[ident])
        modT = load_modT(P, li)
        g2 = S.sb('g2', [128, 2, D])
        for s_ in range(2):
            S.dma(g2[:, s_, :], P.dr[f'modrow{li}'][s_:s_ + 1, 5 * D:6 * D].partition_broadcast(128), writes=[g2])
        wrf = S.sb('wrf', [128, 8, 20]); wrb = S.sb('wrb', [128, 8, 20], BF16)
        S.dma(wrf[:, :, 0:4], P.dr['moe_w_grp'][li].rearrange("(k p) g -> p k g", p=128), writes=[wrf])
        S.dma(wrf[:, :, 4:20], P.dr['moe_w_rt'][li].rearrange("(k p) g -> p k g", p=128), writes=[wrf])
        S.op('dve', lambda e: e.tensor_copy(out=wrb[:], in_=wrf[:]), reads=[wrf], writes=[wrb])
        rb = S.sb('rb', [128, 20])
        S.dma(rb[:, 0:4], P.dr['moe_b_grp'][li:li + 1, :].partition_broadcast(128), writes=[rb])
        S.dma(rb[:, 4:20], P.dr['moe_b_rt'][li:li + 1, :].partition_broadcast(128), writes=[rb])
        h2T = S.sb('h2T', [128, 8, GMAX * 128], BF16)
        acc = S.sb('acc', [128, GMAX, D])
        comb = S.sb('comb', [128, GMAX, 16])
        rt = {nm: S.sb(f'rt_{nm}', shp) for nm, shp in (('lg', [128, 20]), ('mx', [128, 4]), ('ge', [128, 4]), ('gm', [128, 4]), ('m16', [128, 16]),
                                                         ('ml', [128, 16]), ('eq', [128, 16]), ('ml2', [128, 16]), ('ex', [128, 16]))}
        wst = Rot([S.sb(f'wst{j}', [128, 4, 512]) for j in range(2)])
        w1b = Rot([S.sb(f'w1b{j}', [128, 8, 512], BF16) for j in range(2)])
        w3b = Rot([S.sb(f'w3b{j}', [128, 8, 512], BF16) for j in range(2)])
        w2b = Rot([S.sb(f'w2b{j}', [128, 4, D], BF16) for j in range(2)])
        sil = Rot([S.sb(f'sil{j}', [128, 512]) for j in range(3)])
        actb = Rot([S.sb(f'actb{j}', [128, 4, 512], BF16) for j in range(2)])
        pss = Rot([S.ps(f'eps{j}', [128, 512]) for j in range(4)])
        pys = Rot([S.ps(f'yps{j}', [128, 512]) for j in range(2)])
        prs = Rot([S.ps(f'rps{j}', [128, 32]) for j in range(1)])
        xts = Rot([S.sb(f'ox{j}', [128, D]) for j in range(2)])
        dq = [0]

        def load_cast(dst_tile, dram_view, nk):
            cols = dram_view.shape[2]
            for k0 in range(0, nk, 4):
                for c0 in range(0, cols, 512):
                    st = wst.next()
                    S.dma(st[:, :, :], dram_view[:, k0:k0 + 4, c0:c0 + 512], writes=[st], q='sp')
                    dq[0] += 1
                    S.op('pool', lambda e, st=st, k0=k0, c0=c0: e.tensor_copy(out=dst_tile[:, k0:k0 + 4, c0:c0 + 512], in_=st[:, :, :]), reads=[st], writes=[dst_tile])

        NC_ = norm_ctx(S, npt=1)
        for (ga, gb) in groups:
            ng = gb - ga
            norm_transpose(P, src, modT, 24, 32, h2T, ident, tiles=range(ga, gb), dst0=ga * 128, C=NC_)
            for ti in range(ng):
                pr = prs.next()
                for k in range(8):
                    S.op('pe', lambda e, k=k: e.matmul(pr[:, 0:20], lhsT=h2T[:, k, ti * 128:(ti + 1) * 128], rhs=wrb[:, k, :], start=(k == 0), stop=(k == 7)),
                         reads=[h2T, wrb], writes=[pr])
                lg, mx, ge, gm, m16, ml, eq, ml2, ex = (rt[n] for n in ('lg', 'mx', 'ge', 'gm', 'm16', 'ml', 'eq', 'ml2', 'ex'))
                V = lambda fn, rd, wr: S.op('dve', fn, reads=rd, writes=wr)
                V(lambda e: e.tensor_tensor(out=lg[:], in0=pr[:, 0:20], in1=rb[:], op=ALU.add), [pr, rb], [lg])
                V(lambda e: e.reduce_max(out=mx[:, 0:1], in_=lg[:, 0:4], axis=AX.X), [lg], [mx])
                V(lambda e: e.tensor_scalar(out=gm[:], in0=lg[:, 0:4], scalar1=mx[:, 0:1], scalar2=None, op0=ALU.is_equal), [lg, mx], [gm])
                V(lambda e: e.tensor_scalar(out=ge[:], in0=lg[:, 0:4], scalar1=mx[:, 0:1], scalar2=None, op0=ALU.subtract), [lg, mx], [ge])
                S.op('act', lambda e: e.activation(out=ge[:], in_=ge[:], func=AF.Exp), reads=[ge], writes=[ge])
                V(lambda e: e.reduce_sum(out=mx[:, 1:2], in_=ge[:], axis=AX.X), [ge], [mx])
                V(lambda e: e.tensor_copy(out=m16[:].rearrange("p (g e) -> p g e", e=4), in_=gm[:].unsqueeze(2).to_broadcast([128, 4, 4])), [gm], [m16])
                V(lambda e: e.tensor_scalar(out=ml[:], in0=m16[:], scalar1=-1.0, scalar2=1e30, op0=ALU.add, op1=ALU.mult), [m16], [ml])
                V(lambda e: e.tensor_tensor(out=ml[:], in0=ml[:], in1=lg[:, 4:20], op=ALU.add), [ml, lg], [ml])
                V(lambda e: e.reduce_max(out=mx[:, 2:3], in_=ml[:], axis=AX.X), [ml], [mx])
                V(lambda e: e.tensor_scalar(out=eq[:], in0=ml[:], scalar1=mx[:, 2:3], scalar2=None, op0=ALU.is_equal), [ml, mx], [eq])
                V(lambda e: e.scalar_tensor_tensor(out=ml2[:], in0=eq[:], scalar=-1e30, in1=ml[:], op0=ALU.mult, op1=ALU.add), [eq, ml], [ml2])
                V(lambda e: e.reduce_max(out=mx[:, 3:4], in_=ml2[:], axis=AX.X), [ml2], [mx])
                V(lambda e: e.scalar_tensor_tensor(out=eq[:], in0=ml2[:], scalar=mx[:, 3:4], in1=eq[:], op0=ALU.is_equal, op1=ALU.add), [ml2, mx, eq], [eq])
                V(lambda e: e.tensor_scalar(out=ex[:], in0=ml[:], scalar1=mx[:, 2:3], scalar2=-80.0, op0=ALU.subtract, op1=ALU.max), [ml, mx], [ex])
                S.op('act', lambda e: e.activation(out=ex[:], in_=ex[:], func=AF.Exp), reads=[ex], writes=[ex])
                V(lambda e: e.tensor_tensor(out=ex[:], in0=ex[:], in1=eq[:], op=ALU.mult), [ex, eq], [ex])
                V(lambda e: e.reduce_sum(out=mx[:, 2:3], in_=ex[:], axis=AX.X), [ex], [mx])
                V(lambda e: e.tensor_tensor(out=mx[:, 2:3], in0=mx[:, 2:3], in1=mx[:, 1:2], op=ALU.mult), [mx], [mx])
                V(lambda e: e.reciprocal(out=mx[:, 2:3], in_=mx[:, 2:3]), [mx], [mx])
                V(lambda e, ti=ti: e.tensor_scalar(out=comb[:, ti, :], in0=ex[:], scalar1=mx[:, 2:3], scalar2=None, op0=ALU.mult), [ex, mx], [comb])
            nt = ng * 128
            tblocks = [(b0, min(512, nt - b0)) for b0 in range(0, nt, 512)]
            for ex_i in range(16):
                w1 = w1b.next(); w3 = w3b.next(); w2 = w2b.next()
                load_cast(w1, P.dr['moe_w1'][li, ex_i].rearrange("(k p) h -> p k h", p=128), 8)
                load_cast(w3, P.dr['moe_w3'][li, ex_i].rearrange("(k p) h -> p k h", p=128), 8)
                load_cast(w2, P.dr['moe_w2'][li, ex_i].rearrange("(k p) f -> p k f", p=128), 4)
                for (b0, n) in tblocks:
                    ab = actb.next()
                    for hc in range(4):
                        p1 = pss.next(); p3 = pss.next()
                        for k in range(8):
                            S.op('pe', lambda e, k=k: e.matmul(p1[:, 0:n], lhsT=w1[:, k, hc * 128:(hc + 1) * 128], rhs=h2T[:, k, b0:b0 + n], start=(k == 0), stop=(k == 7)),
                                 reads=[w1, h2T], writes=[p1])
                        for k in range(8):
                            S.op('pe', lambda e, k=k: e.matmul(p3[:, 0:n], lhsT=w3[:, k, hc * 128:(hc + 1) * 128], rhs=h2T[:, k, b0:b0 + n], start=(k == 0), stop=(k == 7)),
                                 reads=[w3, h2T], writes=[p3])
                        sl = sil.next()
                        S.op('act', lambda e: e.activation(out=sl[:, 0:n], in_=p1[:, 0:n], func=AF.Silu), reads=[p1], writes=[sl])
                        S.op('dve', lambda e, hc=hc: e.tensor_tensor(out=ab[:, hc, 0:n], in0=sl[:, 0:n], in1=p3[:, 0:n], op=ALU.mult), reads=[sl, p3], writes=[ab])
                    for st_ in range(n // 128):
                        ti = b0 // 128 + st_
                        for half in range(2):
                            py = pys.next()
                            for hc in range(4):
                                S.op('pe', lambda e, hc=hc: e.matmul(py[:, :], lhsT=ab[:, hc, st_ * 128:(st_ + 1) * 128], rhs=w2[:, hc, half * 512:(half + 1) * 512],
                                                                     start=(hc == 0), stop=(hc == 3)), reads=[ab, w2], writes=[py])
                            a_sl = acc[:, ti, half * 512:(half + 1) * 512]
                            if ex_i == 0:
                                S.op('dve', lambda e: e.tensor_scalar(out=a_sl, in0=py[:, :], scalar1=comb[:, ti, ex_i:ex_i + 1], scalar2=None, op0=ALU.mult),
                                     reads=[py, comb], writes=[acc])
                            else:
                                S.op('dve', lambda e: e.scalar_tensor_tensor(out=a_sl, in0=py[:, :], scalar=comb[:, ti, ex_i:ex_i + 1], in1=a_sl, op0=ALU.mult, op1=ALU.add),
                                     reads=[py, comb, acc], writes=[acc])
            for ti in range(ng):
                gt = ga + ti
                s_ = 0 if gt < 2 else 1
                xt = xts.next()
                S.dma(xt[:], src[gt * 128:(gt + 1) * 128, :], writes=[xt])
                S.op('pool', lambda e: e.tensor_tensor(out=acc[:, ti, :], in0=acc[:, ti, :], in1=g2[:, s_, :], op=ALU.mult), reads=[acc, g2], writes=[acc])
                S.op('dve', lambda e: e.tensor_tensor(out=xt[:], in0=xt[:], in1=acc[:, ti, :], op=ALU.add), reads=[xt, acc], writes=[xt])
                if dst_is_out:
                    if gt >= 2:
                        S.dma(dst[(gt - 2) * 128:(gt - 1) * 128, :], xt[:], reads=[xt], q='act', is_out=True)
                else:
                    S.dma(dst[gt * 128:(gt + 1) * 128, :], xt[:], reads=[xt], q='act')


def declare_moe(P):
    P.inp('moe_w_grp', [DEPTH, D, 4]); P.inp('moe_b_grp', [DEPTH, 4])
    P.inp('moe_w_rt', [DEPTH, D, 16]); P.inp('moe_b_rt', [DEPTH, 16])
    P.inp('moe_w1', [DEPTH, 16, D, 512]); P.inp('moe_w3', [DEPTH, 16, D, 512]); P.inp('moe_w2', [DEPTH, 16, 512, D])
    P.tmp('x_l0', [T, D])


def host_moe(m, inputs):
    for k in ('moe_w_grp', 'moe_b_grp', 'moe_w_rt', 'moe_b_rt', 'moe_w1', 'moe_w3', 'moe_w2'):
        m[k] = np.ascontiguousarray(inputs[k], np.float32)


def hyena_consts(n):
    import ml_dtypes
    t = np.arange(n, dtype=np.float32) / np.float32(n)
    bands = np.arange(1, 17, dtype=np.float32)
    ang = (np.float32(2.0 * math.pi) * t[:, None] * bands).astype(np.float32)
    feat = np.concatenate([t[:, None], np.sin(ang), np.cos(ang)], -1).astype(np.float32)
    deltas = np.linspace(-math.log(1e-2) / 1.5, -math.log(1e-2) / 0.3, 256, dtype=np.float32)
    dec = np.exp(-t[:, None] * deltas).astype(np.float32)
    nk = n // 128 + 1
    NP = nk * 128
    idx = np.arange(NP, dtype=np.int64)
    prod = (idx[:, None] * idx[None, :]) % (2 * n)
    angw = 2.0 * math.pi * prod.astype(np.float64) / (2 * n)
    valid = (idx <= n)
    m = (valid[:, None] & valid[None, :])
    wc = np.where(m, np.cos(angw), 0.0); ws = np.where(m, np.sin(angw), 0.0)
    def tile(w):
        return np.ascontiguousarray(w.reshape(nk, 128, nk, 128).transpose(2, 1, 0, 3)).astype(ml_dtypes.bfloat16)
    wk = np.full(NP, 1.0 / n, np.float32); wk[0] = 0.5 / n; wk[n] = 0.5 / n; wk[n + 1:] = 0.0
    wkT = np.ascontiguousarray(wk.reshape(nk, 128).T)
    return dict(featT=np.ascontiguousarray(feat.T), dec=dec, wc=tile(wc), ws=tile(ws), wk=wkT)


def hyena_seq(P, li, n, t_off, sfx):
    S = P.S
    projT = P.dr['projT']
    hyT = P.dr['hyT']
    hyz = P.dr['hyz']
    hspec = P.dr['hspec']
    NT = n // 128
    NK = NT + 1
    wc_d = P.dr['hy_wc' + sfx]; ws_d = P.dr['hy_ws' + sfx]
    kparts = [128] * NT + [1]
    TWO_PI = 2.0 * math.pi
    with S.stage():
        xin = Rot([S.sb(f'hxin{j}', [128, n]) for j in range(2)])
        zo = Rot([S.sb(f'hzo{j}', [128, n]) for j in range(2)])
        cw = S.sb('hcw', [128, 6, 3]); cb = S.sb('hcb', [128, 6])
        for j in range(3):
            S.dma(cw[:, :, j], P.dr['hy_conv_w'][li, j].rearrange("(c p) -> p c", p=128), writes=[cw], allow_slow_non_contiguous=True)
        S.dma(cb[:], P.dr['hy_conv_b'][li].rearrange("(c p) -> p c", p=128), writes=[cb], allow_slow_non_contiguous=True)
        for c in range(6):
            xi = xin.next(); z = zo.next()
            S.dma(xi[:], projT[O_HY + c * 128:O_HY + (c + 1) * 128, t_off:t_off + n], writes=[xi], q='sp')
            dwconv_fm(S, z, xi, cw[:, c, :], cb[:, c:c + 1], 3, 1, [(0, n)], eng='dve', wbuf=cw, bbuf=cb)
            S.dma(hyz[c * 128:(c + 1) * 128, t_off:t_off + n], z[:], reads=[z], q='act')
    with S.stage():
        featT = S.sb('featT', [33, n]); f1 = S.sb('f1', [33, 64]); f2 = S.sb('f2', [64, 64]); f3 = S.sb('f3', [64, 1024])
        fb = S.sb('fb', [64, 2]); h1T = S.sb('h1T', [64, n]); h2T = S.sb('h2T', [64, n])
        S.dma(featT[:], P.dr['hy_featT' + sfx][:, :], writes=[featT])
        S.dma(f1[:], P.dr['hy_f1'][li], writes=[f1]); S.dma(f2[:], P.dr['hy_f2'][li], writes=[f2]); S.dma(f3[:], P.dr['hy_f3'][li], writes=[f3])
        S.dma(fb[:, 0:1], P.dr['hy_fb1'][li].rearrange("(c o) -> c o", o=1), writes=[fb])
        S.dma(fb[:, 1:2], P.dr['hy_fb2'][li].rearrange("(c o) -> c o", o=1), writes=[fb])
        pss = Rot([S.ps(f'hps{j}', [128, 512]) for j in range(4)])
        tmpf = Rot([S.sb(f'htmp{j}', [64, 512]) for j in range(2)])
        tmpq = Rot([S.sb(f'htmq{j}', [64, 512]) for j in range(2)])
        tmpi = Rot([S.sb(f'htmi{j}', [64, 512], mybir.dt.int32) for j in range(2)])
        for (src, wt, dstT, bi) in ((featT, f1, h1T, 0), (h1T, f2, h2T, 1)):
            for b0 in range(0, n, 512):
                nb = min(512, n - b0)
                ps = pss.next()
                S.op('pe', lambda e: e.matmul(ps[0:64, 0:nb], lhsT=wt[:], rhs=src[:, b0:b0 + nb], start=True, stop=True), reads=[wt, src], writes=[ps])
                tm = tmpf.next()
                qi = tmpi.next(); qf = tmpq.next()
                S.op('dve', lambda e: e.tensor_scalar(out=tm[:, 0:nb], in0=ps[0:64, 0:nb], scalar1=fb[:, bi:bi + 1], scalar2=None, op0=ALU.add), reads=[ps, fb], writes=[tm])
                S.op('dve', lambda e: e.tensor_scalar(out=qf[:, 0:nb], in0=tm[:, 0:nb], scalar1=1.0 / TWO_PI, scalar2=None, op0=ALU.mult), reads=[tm], writes=[qf])
                S.op('dve', lambda e: e.tensor_copy(out=qi[:, 0:nb], in_=qf[:, 0:nb]), reads=[qf], writes=[qi])
                S.op('dve', lambda e: e.tensor_copy(out=qf[:, 0:nb], in_=qi[:, 0:nb]), reads=[qi], writes=[qf])
                S.op('dve', lambda e: e.scalar_tensor_tensor(out=tm[:, 0:nb], in0=qf[:, 0:nb], scalar=-TWO_PI, in1=tm[:, 0:nb], op0=ALU.mult, op1=ALU.add), reads=[qf, tm], writes=[tm])
                S.op('dve', lambda e: e.tensor_scalar(out=qf[:, 0:nb], in0=tm[:, 0:nb], scalar1=math.pi, scalar2=-TWO_PI, op0=ALU.is_gt, op1=ALU.mult), reads=[tm], writes=[qf])
                S.op('dve', lambda e: e.tensor_tensor(out=tm[:, 0:nb], in0=tm[:, 0:nb], in1=qf[:, 0:nb], op=ALU.add), reads=[tm, qf], writes=[tm])
                S.op('dve', lambda e: e.tensor_scalar(out=qf[:, 0:nb], in0=tm[:, 0:nb], scalar1=-math.pi, scalar2=TWO_PI, op0=ALU.is_lt, op1=ALU.mult), reads=[tm], writes=[qf])
                S.op('dve', lambda e: e.tensor_tensor(out=tm[:, 0:nb], in0=tm[:, 0:nb], in1=qf[:, 0:nb], op=ALU.add), reads=[tm, qf], writes=[tm])
                S.op('act', lambda e: e.activation(out=dstT[:, b0:b0 + nb], in_=tm[:, 0:nb], func=AF.Sin), reads=[tm], writes=[dstT])
        hbf = S.sb('hbf', [128, NT, 1024], BF16)
        ones = S.sb('hones', [128, 128])
        S.op('pool', lambda e: e.memset(ones[:], 1.0), writes=[ones])
        decs = Rot([S.sb(f'hdec{j}', [128, 256]) for j in range(2)])
        hraw = Rot([S.sb(f'hraw{j}', [128, 1024]) for j in range(2)])
        habs = Rot([S.sb(f'habs{j}', [128, 1024]) for j in range(2)])
        l1ps = [S.ps(f'l1ps{j}', [128, 512]) for j in range(2)]
        for tt in range(NT):
            dc = decs.next()
            S.dma(dc[:], P.dr['hy_dec' + sfx][tt * 128:(tt + 1) * 128, :], writes=[dc])
            hr = hraw.next(); ha = habs.next()
            for half in range(2):
                ps = pss.next()
                S.op('pe', lambda e: e.matmul(ps[:, :], lhsT=h2T[:, tt * 128:(tt + 1) * 128], rhs=f3[:, half * 512:(half + 1) * 512], start=True, stop=True),
                     reads=[h2T, f3], writes=[ps])
                S.op('dve', lambda e: e.tensor_tensor(out=hr[:, half * 512:(half + 1) * 512].rearrange("p (a c) -> p a c", a=2),
                                                      in0=ps[:, :].rearrange("p (a c) -> p a c", a=2),
                                                      in1=dc[:].unsqueeze(1).to_broadcast([128, 2, 256]), op=ALU.mult), reads=[ps, dc], writes=[hr])
            S.op('act', lambda e: e.activation(out=ha[:], in_=hr[:], func=AF.Abs), reads=[hr], writes=[ha])
            for half in range(2):
                S.op('pe', lambda e: e.matmul(l1ps[half][:, :], lhsT=ones[:], rhs=ha[:, half * 512:(half + 1) * 512], start=(tt == 0), stop=(tt == NT - 1)),
                     reads=[ones, ha], writes=[l1ps[half]])
            if tt == 0:
                for o in range(2):
                    S.op('dve', lambda e, o=o: e.memset(hr[0:1, o * 512 + 256:o * 512 + 512], 0.0), reads=[ha], writes=[hr])
            S.op('act', lambda e: e.copy(out=hbf[:, tt, :], in_=hr[:]), reads=[hr], writes=[hbf])
        rl1 = S.sb('rl1', [128, 2, 256])
        l1sb = S.sb('l1sb', [128, 2, 256])
        for o in range(2):
            S.op('act', lambda e, o=o: e.copy(out=l1sb[:, o, :], in_=l1ps[o][:, 0:256]), reads=[l1ps[o]], writes=[l1sb])
            S.op('dve', lambda e, o=o: e.tensor_tensor(out=rl1[:, o, :], in0=l1sb[:, o, :], in1=l1ps[o][:, 256:512], op=ALU.add), reads=[l1ps[o], l1sb], writes=[rl1])
        S.op('dve', lambda e: e.reciprocal(out=rl1[:], in_=rl1[:]), reads=[rl1], writes=[rl1])
        wk = S.sb('hwk', [128, NK])
        S.dma(wk[:], P.dr['hy_wk' + sfx][:, :], writes=[wk])
        wcs = Rot([S.sb(f'hwc{j}', [128, NK, 128], BF16) for j in range(2)])
        wss = Rot([S.sb(f'hws{j}', [128, NK, 128], BF16) for j in range(2)])
        spo = Rot([S.sb(f'hspo{j}', [128, 2, 2, 256]) for j in range(2)])
        for kt in range(NK):
            kp = kparts[kt]
            wct = wcs.next(); wst = wss.next()
            S.dma(wct[:], wc_d[kt], writes=[wct]); S.dma(wst[:], ws_d[kt], writes=[wst])
            so = spo.next()
            pa = [pss.next(), pss.next()]
            for half in range(2):
                for tc in range(NT):
                    S.op('pe', lambda e, tc=tc: e.matmul(pa[half][0:kp, :], lhsT=wct[:, tc, 0:kp], rhs=hbf[:, tc, half * 512:(half + 1) * 512],
                                                         start=(tc == 0), stop=(tc == NT - 1)), reads=[wct, hbf], writes=[pa[half]])
            for o in range(2):
                S.op('act', lambda e, o=o: e.copy(out=so[0:kp, 0, o, :], in_=pa[o][0:kp, 0:256]), reads=[pa[o]], writes=[so])
                S.op('dve', lambda e, o=o: e.tensor_tensor(out=so[0:kp, 0, o, :], in0=so[0:kp, 0, o, :], in1=pa[o][0:kp, 256:512], op=ALU.add), reads=[pa[o], so], writes=[so])
            pb = [pss.next(), pss.next()]
            for half in range(2):
                for tc in range(NT):
                    S.op('pe', lambda e, tc=tc: e.matmul(pb[half][0:kp, :], lhsT=wst[:, tc, 0:kp], rhs=hbf[:, tc, half * 512:(half + 1) * 512],
                                                         start=(tc == 0), stop=(tc == NT - 1)), reads=[wst, hbf], writes=[pb[half]])
            for o in range(2):
                S.op('act', lambda e, o=o: e.copy(out=so[0:kp, 1, o, :], in_=pb[o][0:kp, 256:512]), reads=[pb[o]], writes=[so])
                S.op('dve', lambda e, o=o: e.tensor_tensor(out=so[0:kp, 1, o, :], in0=so[0:kp, 1, o, :], in1=pb[o][0:kp, 0:256], op=ALU.subtract), reads=[pb[o], so], writes=[so])
            for ri in range(2):
                S.op('dve', lambda e, ri=ri: e.scalar_tensor_tensor(out=so[0:kp, ri, :, :], in0=so[0:kp, ri, :, :], scalar=wk[0:kp, kt:kt + 1], in1=rl1[0:kp, :, :],
                                                                    op0=ALU.mult, op1=ALU.mult), reads=[so, wk, rl1], writes=[so])
            S.dma(hspec[kt, 0:kp].rearrange("p a o c -> p (a o c)"), so[0:kp].rearrange("p a o c -> p (a o c)"), reads=[so], q='act')
    with S.stage():
        identf = S.sb('identf', [128, 128])
        S.dma(identf[:], P.dr['ident_f'][:, :], writes=[identf])
        skip = S.sb('hskip', [128, 2, 256])
        for o in range(2):
            S.dma(skip[:, o, :], P.dr['hy_skip'][li, o:o + 1, :].partition_broadcast(128), writes=[skip])
        zf = S.sb('zf', [128, NT, 256]); zb = S.sb('zb', [128, NT, 256], BF16)
        y1 = S.sb('y1', [128, NT, 256])
        Pq = S.sb('Pq', [128, NK, 256], BF16); Qq = S.sb('Qq', [128, NK, 256], BF16)
        fms = Rot([S.sb(f'hfm{j}', [128, 128]) for j in range(4)])
        tps = Rot([S.ps(f'htp{j}', [128, 256]) for j in range(2)])
        pss = Rot([S.ps(f'hsp{j}', [128, 256]) for j in range(4)])
        wcs = Rot([S.sb(f'hwc{j}', [128, NK, 128], BF16) for j in range(3)])
        wss = Rot([S.sb(f'hws{j}', [128, NK, 128], BF16) for j in range(3)])
        hsp = Rot([S.sb(f'hsp_{j}', [128, 2, 2, 256]) for j in range(2)])
        tA = Rot([S.sb(f'htA{j}', [128, 256]) for j in range(2)]); tB = Rot([S.sb(f'htB{j}', [128, 256]) for j in range(2)])
        gtm = Rot([S.sb(f'hgt{j}', [128, 256]) for j in range(2)])
        yo = Rot([S.sb(f'hyo{j}', [128, 256]) for j in range(2)])
        ofm = Rot([S.sb(f'hofm{j}', [128, 128]) for j in range(2)])

        def to_tm(row0, tt, dst_ap, dstbuf):
            tp = tps.next()
            for c in range(2):
                fm = fms.next()
                S.dma(fm[:], hyz[row0 + c * 128:row0 + (c + 1) * 128, t_off + tt * 128:t_off + (tt + 1) * 128], writes=[fm], q='sp')
                S.op('pe', lambda e, c=c: e.transpose(out=tp[:, c * 128:(c + 1) * 128], in_=fm[:], identity=identf[:]), reads=[fm, identf], writes=[tp])
            S.op('act', lambda e: e.copy(out=dst_ap, in_=tp[:, :]), reads=[tp], writes=[dstbuf])

        for tt in range(NT):
            to_tm(0, tt, zf[:, tt, :], zf)
        S.op('pool', lambda e: e.tensor_copy(out=zb[:], in_=zf[:]), reads=[zf], writes=[zb])
        for o in range(2):
            zin_f = zf if o == 0 else y1
            for kt in range(NK):
                kp = kparts[kt]
                wct = wcs.next(); wst = wss.next()
                S.dma(wct[:], wc_d[kt], writes=[wct]); S.dma(wst[:], ws_d[kt], writes=[wst])
                hs = hsp.next()
                S.dma(hs[0:kp].rearrange("p a o c -> p (a o c)"), hspec[kt, 0:kp].rearrange("p a o c -> p (a o c)"), writes=[hs], q='act')
                pa = pss.next(); pb = pss.next()
                for tc in range(NT):
                    S.op('pe', lambda e, tc=tc: e.matmul(pa[0:kp, :], lhsT=wct[:, tc, 0:kp], rhs=zb[:, tc, :], start=(tc == 0), stop=(tc == NT - 1)), reads=[wct, zb], writes=[pa])
                for tc in range(NT):
                    S.op('pe', lambda e, tc=tc: e.matmul(pb[0:kp, :], lhsT=wst[:, tc, 0:kp], rhs=zb[:, tc, :], start=(tc == 0), stop=(tc == NT - 1)), reads=[wst, zb], writes=[pb])
                a_ = tA.next(); b_ = tB.next()
                S.op('dve', lambda e: e.tensor_tensor(out=a_[0:kp], in0=pa[0:kp, :], in1=hs[0:kp, 0, o, :], op=ALU.mult), reads=[pa, hs], writes=[a_])
                S.op('dve', lambda e: e.tensor_tensor(out=b_[0:kp], in0=pb[0:kp, :], in1=hs[0:kp, 1, o, :], op=ALU.mult), reads=[pb, hs], writes=[b_])
                S.op('pool', lambda e: e.tensor_tensor(out=Pq[0:kp, kt, :], in0=a_[0:kp], in1=b_[0:kp], op=ALU.add), reads=[a_, b_], writes=[Pq])
                a2 = tA.next(); b2 = tB.next()
                S.op('dve', lambda e: e.tensor_tensor(out=b2[0:kp], in0=pb[0:kp, :], in1=hs[0:kp, 0, o, :], op=ALU.mult), reads=[pb, hs], writes=[b2])
                S.op('dve', lambda e: e.tensor_tensor(out=a2[0:kp], in0=pa[0:kp, :], in1=hs[0:kp, 1, o, :], op=ALU.mult), reads=[pa, hs], writes=[a2])
                S.op('pool', lambda e: e.tensor_tensor(out=Qq[0:kp, kt, :], in0=b2[0:kp], in1=a2[0:kp], op=ALU.subtract), reads=[a2, b2], writes=[Qq])
            for tt in range(NT):
                wct = wcs.next(); wst = wss.next()
                S.dma(wct[:], wc_d[tt], writes=[wct]); S.dma(wst[:], ws_d[tt], writes=[wst])
                py = pss.next()
                for kc in range(NK):
                    kp = kparts[kc]
                    S.op('pe', lambda e, kc=kc, kp=kp: e.matmul(py[:, :], lhsT=wct[0:kp, kc, :], rhs=Pq[0:kp, kc, :], start=(kc == 0), stop=False), reads=[wct, Pq], writes=[py])
                    S.op('pe', lambda e, kc=kc, kp=kp: e.matmul(py[:, :], lhsT=wst[0:kp, kc, :], rhs=Qq[0:kp, kc, :], start=False, stop=(kc == NK - 1)), reads=[wst, Qq], writes=[py])
                g = gtm.next()
                to_tm(256 * (o + 1), tt, g[:, :], g)
                yy = yo.next()
                S.op('dve', lambda e: e.tensor_tensor(out=yy[:], in0=zin_f[:, tt, :], in1=skip[:, o, :], op=ALU.mult), reads=[zin_f, skip], writes=[yy])
                S.op('dve', lambda e: e.tensor_tensor(out=yy[:], in0=yy[:], in1=py[:, :], op=ALU.add), reads=[yy, py], writes=[yy])
                if o == 0:
                    S.op('pool', lambda e: e.tensor_tensor(out=y1[:, tt, :], in0=yy[:], in1=g[:], op=ALU.mult), reads=[yy, g], writes=[y1])
                else:
                    S.op('pool', lambda e: e.tensor_tensor(out=yy[:], in0=yy[:], in1=g[:], op=ALU.mult), reads=[yy, g], writes=[yy])
                    for c in range(2):
                        tp = tps.next()
                        S.op('pe', lambda e, c=c: e.transpose(out=tp[:, 0:128], in_=yy[:, c * 128:(c + 1) * 128], identity=identf[:]), reads=[yy, identf], writes=[tp])
                        of = ofm.next()
                        S.op('act', lambda e: e.copy(out=of[:], in_=tp[:, 0:128]), reads=[tp], writes=[of])
                        S.dma(hyT[c * 128:(c + 1) * 128, t_off + tt * 128:t_off + (tt + 1) * 128], of[:], reads=[of], q='act')
            if o == 0:
                S.op('pool', lambda e: e.tensor_copy(out=zb[:], in_=y1[:]), reads=[y1], writes=[zb])


def stage_hyena(P, li, need_ctx):
    hyena_seq(P, li, TX, TC, '')
    if need_ctx:
        hyena_seq(P, li, TC, 0, '_c')


def declare_hyena(P):
    P.inp('hy_conv_w', [DEPTH, 3, 768]); P.inp('hy_conv_b', [DEPTH, 768])
    P.inp('hy_f1', [DEPTH, 33, 64]); P.inp('hy_fb1', [DEPTH, 64]); P.inp('hy_f2', [DEPTH, 64, 64]); P.inp('hy_fb2', [DEPTH, 64])
    P.inp('hy_f3', [DEPTH, 64, 1024]); P.inp('hy_skip', [DEPTH, 2, 256])
    P.inp('ident_f', [128, 128])
    for sfx, n in (('', TX), ('_c', TC)):
        nk = n // 128 + 1
        P.inp('hy_featT' + sfx, [33, n]); P.inp('hy_dec' + sfx, [n, 256])
        P.inp('hy_wc' + sfx, [nk, 128, nk, 128], BF16); P.inp('hy_ws' + sfx, [nk, 128, nk, 128], BF16)
        P.inp('hy_wk' + sfx, [128, nk])
    P.tmp('hyz', [768, T])
    P.tmp('hspec', [33, 128, 2, 2, 256])


_HC = {}


def host_hyena(m, inputs):
    for k in ('hy_conv_w', 'hy_conv_b', 'hy_f1', 'hy_fb1', 'hy_f2', 'hy_fb2', 'hy_f3', 'hy_skip'):
        m[k] = np.ascontiguousarray(inputs[k], np.float32)
    m['ident_f'] = np.eye(128, dtype=np.float32)
    for sfx, n in (('', TX), ('_c', TC)):
        if n not in _HC:
            _HC[n] = hyena_consts(n)
        c = _HC[n]
        m['hy_featT' + sfx] = c['featT']; m['hy_dec' + sfx] = c['dec']
        m['hy_wc' + sfx] = c['wc']; m['hy_ws' + sfx] = c['ws']; m['hy_wk' + sfx] = c['wk']


Q_R, Q_V, Q_KK, Q_KD0, Q_KD1, Q_A0, Q_A1, Q_LD0, Q_LD1, Q_G, Q_BON = range(11)
SEGS2 = [(0, TC), (TC, TX)]


def stage_rwkv_prep(P, li):
    S = P.S
    projT = P.dr['projT']
    rwq = P.dr['rwq']
    with S.stage():
        mu = S.sb('mu', [128, 9, 3])
        for j in range(2):
            S.dma(mu[:, :, 2 * j], P.dr['rw_mu'][li, j].rearrange("(c p) -> p c", p=128), writes=[mu], allow_slow_non_contiguous=True)
        S.op('dve', lambda e: e.tensor_tensor(out=mu[:, :, 1], in0=mu[:, :, 0], in1=mu[:, :, 2], op=ALU.add), reads=[mu], writes=[mu])
        S.op('dve', lambda e: e.tensor_scalar(out=mu[:, :, 1], in0=mu[:, :, 1], scalar1=-1.0, scalar2=1.0, op0=ALU.mult, op1=ALU.add), reads=[mu], writes=[mu])
        raw = Rot([S.sb(f'rraw{j}', [128, T]) for j in range(2)])
        blk = S.sb('rblk', [128, 128])
        S.dma(blk[:], P.dr['blk64'][:, :], writes=[blk])
        pss = Rot([S.ps(f'rps{j}', [128, 512]) for j in range(4)])

        def shiftmix(c, dst):
            rw_ = raw.next()
            S.dma(rw_[:], projT[O_RW + c * 128:O_RW + (c + 1) * 128, :], writes=[rw_], q='sp')
            dwconv_fm(S, dst, rw_, mu[:, c, :], None, 3, 1, SEGS2, wbuf=mu)

        with S.stage():
            w1s = S.sb('w1s', [128, T]); a1s = S.sb('a1s', [128, T]); g1s = S.sb('g1s', [128, T])
            shiftmix(6, w1s); shiftmix(7, a1s); shiftmix(8, g1s)
            S.op('act', lambda e: e.activation(out=w1s[:], in_=w1s[:], func=AF.Tanh), reads=[w1s], writes=[w1s])
            S.op('act', lambda e: e.activation(out=g1s[:], in_=g1s[:], func=AF.Sigmoid), reads=[g1s], writes=[g1s])
            w2t = S.sb('w2t', [128, 256]); a2t = S.sb('a2t', [128, 256]); g2t = S.sb('g2t', [128, 256])
            S.dma(w2t[:], P.dr['rw_w2'][li].rearrange("d r c -> (d r) c"), writes=[w2t])
            S.dma(a2t[:], P.dr['rw_a2'][li].rearrange("d r c -> (d r) c"), writes=[a2t])
            S.dma(g2t[:], P.dr['rw_g2'][li], writes=[g2t])
            w0 = S.sb('w0', [128, 2, 2]); a0 = S.sb('a0', [128, 2, 2])
            for d in range(2):
                S.dma(w0[:, d, :], P.dr['rw_w0'][li, d].rearrange("(h p) -> p h", p=128), writes=[w0], allow_slow_non_contiguous=True)
                S.dma(a0[:, d, :], P.dr['rw_a0'][li, d].rearrange("(h p) -> p h", p=128), writes=[a0], allow_slow_non_contiguous=True)
            outs = Rot([S.sb(f'rout{j}', [128, 512]) for j in range(4)])
            for hp in range(2):
                cs = slice(hp * 128, (hp + 1) * 128)
                for (t0, n) in BLOCKS:
                    for d in range(2):
                        ps = pss.next()
                        S.op('pe', lambda e, d=d: e.matmul(ps[:, 0:n], lhsT=w2t[64 * d:64 * d + 64, cs], rhs=w1s[64 * d:64 * d + 64, t0:t0 + n], start=True, stop=True),
                             reads=[w2t, w1s], writes=[ps])
                        o = outs.next()
                        S.op('act', lambda e, d=d: e.activation(out=o[:, 0:n], in_=ps[:, 0:n], func=AF.Sigmoid, bias=w0[:, d, hp:hp + 1]), reads=[ps, w0], writes=[o])
                        S.op('pool', lambda e: e.tensor_scalar(out=o[:, 0:n], in0=o[:, 0:n], scalar1=-0.6065306597126334, scalar2=None, op0=ALU.mult), reads=[o], writes=[o])
                        S.dma(rwq[Q_LD0 + d, cs, t0:t0 + n], o[:, 0:n], reads=[o], q='act')
                        ps = pss.next()
                        S.op('pe', lambda e, d=d: e.matmul(ps[:, 0:n], lhsT=a2t[64 * d:64 * d + 64, cs], rhs=a1s[64 * d:64 * d + 64, t0:t0 + n], start=True, stop=True),
                             reads=[a2t, a1s], writes=[ps])
                        o = outs.next()
                        S.op('act', lambda e, d=d: e.activation(out=o[:, 0:n], in_=ps[:, 0:n], func=AF.Sigmoid, bias=a0[:, d, hp:hp + 1]), reads=[ps, a0], writes=[o])
                        S.dma(rwq[Q_A0 + d, cs, t0:t0 + n], o[:, 0:n], reads=[o], q='act')
                    ps = pss.next()
                    S.op('pe', lambda e: e.matmul(ps[:, 0:n], lhsT=g2t[:, cs], rhs=g1s[:, t0:t0 + n], start=True, stop=True), reads=[g2t, g1s], writes=[ps])
                    o = outs.next()
                    S.op('dve', lambda e: e.tensor_copy(out=o[:, 0:n], in_=ps[:, 0:n]), reads=[ps], writes=[o])
                    S.dma(rwq[Q_G, cs, t0:t0 + n], o[:, 0:n], reads=[o], q='act')
        with S.stage():
            cols = S.sb('rcols', [128, 2, 4])
            for i, nm in enumerate(('rw_k_k', 'rw_k_a')):
                S.dma(cols[:, :, i], P.dr[nm][li].rearrange("(h p) -> p h", p=128), writes=[cols], allow_slow_non_contiguous=True)
            S.dma(cols[:, :, 3], P.dr['rw_r_k'][li].rearrange("h n -> (h n)").rearrange("(h p) -> p h", p=128), writes=[cols], allow_slow_non_contiguous=True)
            S.op('dve', lambda e: e.tensor_scalar(out=cols[:, :, 2], in0=cols[:, :, 1], scalar1=-1.0, scalar2=1.0, op0=ALU.mult, op1=ALU.add), reads=[cols], writes=[cols])
            rs_ = S.sb('r_s', [128, T]); ks_ = S.sb('k_s', [128, T]); vs_ = S.sb('v_s', [128, T])
            kk = S.sb('kk', [128, T]); ad = S.sb('ad', [128, T]); kd = S.sb('kd', [128, T]); bon = S.sb('bon', [128, T])
            tmp = Rot([S.sb(f'rtmp{j}', [128, 512]) for j in range(3)])
            for hp in range(2):
                cs = slice(hp * 128, (hp + 1) * 128)
                shiftmix(0 + hp, rs_); shiftmix(2 + hp, ks_); shiftmix(4 + hp, vs_)
                S.dma(rwq[Q_R, cs, :], rs_[:], reads=[rs_], q='act')
                S.dma(rwq[Q_V, cs, :], vs_[:], reads=[vs_], q='act')
                S.op('dve', lambda e: e.tensor_scalar(out=kk[:], in0=ks_[:], scalar1=cols[:, hp, 0:1], scalar2=None, op0=ALU.mult), reads=[ks_, cols], writes=[kk])
                for (t0, n) in BLOCKS:
                    sq = tmp.next()
                    S.op('act', lambda e: e.activation(out=sq[:, 0:n], in_=kk[:, t0:t0 + n], func=AF.Square), reads=[kk], writes=[sq])
                    ps = pss.next()
                    S.op('pe', lambda e: e.matmul(ps[:, 0:n], lhsT=blk[:], rhs=sq[:, 0:n], start=True, stop=True), reads=[blk, sq], writes=[ps])
                    rs2 = tmp.next()
                    S.op('dve', lambda e: e.tensor_scalar(out=rs2[:, 0:n], in0=ps[:, 0:n], scalar1=64.0, scalar2=1e-12, op0=ALU.mult, op1=ALU.add), reads=[ps], writes=[rs2])
                    S.op('act', lambda e: e.activation(out=rs2[:, 0:n], in_=rs2[:, 0:n], func=AF.Sqrt), reads=[rs2], writes=[rs2])
                    S.op('dve', lambda e: e.reciprocal(out=rs2[:, 0:n], in_=rs2[:, 0:n]), reads=[rs2], writes=[rs2])
                    S.op('dve', lambda e: e.tensor_tensor(out=kk[:, t0:t0 + n], in0=kk[:, t0:t0 + n], in1=rs2[:, 0:n], op=ALU.mult), reads=[kk, rs2], writes=[kk])
                S.dma(rwq[Q_KK, cs, :], kk[:], reads=[kk], q='act')
                for d in range(2):
                    S.dma(ad[:], rwq[Q_A0 + d, cs, :], writes=[ad])
                    S.op('dve', lambda e: e.tensor_scalar(out=kd[:], in0=ad[:], scalar1=cols[:, hp, 1:2], scalar2=cols[:, hp, 2:3], op0=ALU.mult, op1=ALU.add),
                         reads=[ad, cols], writes=[kd])
                    S.op('pool', lambda e: e.tensor_tensor(out=kd[:], in0=kd[:], in1=ks_[:], op=ALU.mult), reads=[kd, ks_], writes=[kd])
                    S.dma(rwq[Q_KD0 + d, cs, :], kd[:], reads=[kd], q='act')
                    for (t0, n) in BLOCKS:
                        rk = tmp.next()
                        S.op('dve', lambda e: e.scalar_tensor_tensor(out=rk[:, 0:n], in0=rs_[:, t0:t0 + n], scalar=cols[:, hp, 3:4], in1=kd[:, t0:t0 + n], op0=ALU.mult, op1=ALU.mult),
                             reads=[rs_, cols, kd], writes=[rk])
                        ps = pss.next()
                        S.op('pe', lambda e: e.matmul(ps[:, 0:n], lhsT=blk[:], rhs=rk[:, 0:n], start=True, stop=True), reads=[blk, rk], writes=[ps])
                        if d == 0:
                            S.op('dve', lambda e: e.scalar_tensor_tensor(out=bon[:, t0:t0 + n], in0=ps[:, 0:n], scalar=64.0, in1=vs_[:, t0:t0 + n], op0=ALU.mult, op1=ALU.mult),
                                 reads=[ps, vs_], writes=[bon])
                        else:
                            b2 = tmp.next()
                            S.op('dve', lambda e: e.scalar_tensor_tensor(out=b2[:, 0:n], in0=ps[:, 0:n], scalar=64.0, in1=vs_[:, t0:t0 + n], op0=ALU.mult, op1=ALU.mult),
                                 reads=[ps, vs_], writes=[b2])
                            S.op('pool', lambda e: e.tensor_tensor(out=bon[:, t0:t0 + n], in0=bon[:, t0:t0 + n], in1=b2[:, 0:n], op=ALU.add), reads=[bon, b2], writes=[bon])
                S.dma(rwq[Q_BON, cs, :], bon[:], reads=[bon], q='act')


class View:
    def __init__(self, buf, ap):
        self.buf = buf
        self.ap = ap

    def __getitem__(self, idx):
        return self.ap[idx]

    w = property(lambda self: self.buf.w, lambda self, v: setattr(self.buf, 'w', v))
    r = property(lambda self: self.buf.r, lambda self, v: setattr(self.buf, 'r', v))


def stage_rwkv_scan(P, li, need_ctx, heads=range(4), dbg_chunks=None, dirs=(0, 1)):
    S = P.S
    rwq = P.dr['rwq']
    rwT = P.dr['rwT']
    NCH = T // 128
    J = 3
    with S.stage():
        identf = S.sb('identf', [128, 128]); SU = S.sb('mSU', [128, 128]); SL = S.sb('mSL', [128, 128]); UI = S.sb('mUI', [128, 128])
        blk = S.sb('rblk', [128, 128])
        S.dma(identf[:], P.dr['ident_f'][:, :], writes=[identf]); S.dma(SU[:], P.dr['mask_su'][:, :], writes=[SU])
        S.dma(SL[:], P.dr['mask_sl'][:, :], writes=[SL]); S.dma(UI[:], P.dr['mask_ui'][:, :], writes=[UI])
        S.dma(blk[:], P.dr['blk64'][:, :], writes=[blk])
        MK = S.sb('MK', [128, 14, 128])
        S.dma(MK[:], P.dr['rw_masks'][:, :, :], writes=[MK])
        big = lambda nm: S.sb(nm, [64, T])
        t_kk, t_a, t_kd, t_r, t_v, yacc = [big(n) for n in ('t_kk', 't_a', 't_kd', 't_r', 't_v', 'yacc')]
        scr = [S.sb(f'scr{j}', [128, T]) for j in range(3)]
        t_ld = Buf(scr[0].t[0:64, :], 't_ld'); t_cum = Buf(scr[1].t[0:64, :], 't_cum'); stg = Buf(scr[2].t[0:64, :], 'stg')
        PC = S.sb('PC', [64, NCH])
        slots = [(scr[i // 34], (i % 34) * 128) for i in range(102)]
        si = [0]

        def slot(ncols=128):
            n = (ncols + 127) // 128
            sc, c0 = slots[si[0]]
            assert c0 + n * 128 <= T and slots[si[0] + n - 1][0] is sc
            si[0] += n
            return Buf(sc.t[:, c0:c0 + ncols], 'slot')

        sets = []
        for p in range(2):
            row = []
            for j in range(J):
                if si[0] % 34 > 34 - 15:
                    si[0] = (si[0] // 34 + 1) * 34
                d_ = {nm: slot() for nm in ('x0', 'xt0', 'Dm0', 'Dm1', 'DT0', 'DT1', 'W0', 'W1', 'G0', 'G1', 'lkt', 'gat', 'gkt')}
                d_['tk'] = slot(192)
                row.append(d_)
            sets.append(row)
        bankA = [S.ps(f'bkA{j}', [128, 512]) for j in range(J)]
        bankB = [S.ps(f'bkB{j}', [128, 512]) for j in range(J)]
        ps_seq = S.ps('ps_seq', [128, 512]); ps_ro2 = S.ps('ps_ro2', [128, 512])
        ps_ro = Rot([ps_seq, ps_ro2])
        B_b1 = View(ps_seq, ps_seq.t[:, 0:64]); B_u = View(ps_seq, ps_seq.t[:, 64:128])
        B_zn = View(ps_seq, ps_seq.t[0:64, 128:192]); B_y = View(ps_seq, ps_seq.t[0:64, 256:384])
        B1s = Rot([S.sb(f'B1s{j}', [128, 64]) for j in range(2)]); nU = Rot([S.sb(f'nU{j}', [128, 64]) for j in range(2)])
        Zs = [S.sb(f'Z{j}', [64, 64]) for j in range(2)]
        Zt = S.sb('Zt', [64, 64])
        rot = Rot([S.sb(f'rot{j}', [64, 512]) for j in range(4)])
        lnc = S.sb('lnc', [64, 4, 2])
        S.dma(lnc[:, :, 0], P.dr['rw_ln_w'][li].rearrange("(h p) -> p h", p=64), writes=[lnc], allow_slow_non_contiguous=True)
        S.dma(lnc[:, :, 1], P.dr['rw_ln_b'][li].rearrange("(h p) -> p h", p=64), writes=[lnc], allow_slow_non_contiguous=True)

        def load(dst, qi, h, d):
            src = rwq[qi, h * 64:(h + 1) * 64, :]
            if d == 0:
                S.dma(dst[:], src, writes=[dst])
            else:
                S.dma(stg[:], src, writes=[stg])
                for (t0, n) in SEGS2:
                    S.op('pool', lambda e, t0=t0, n=n: e.tensor_copy(out=dst[:, t0:t0 + n], in_=stg[:, t0:t0 + n][:, ::-1]), reads=[stg], writes=[dst])

        def indep(c, B, j):
            cl = slice(c * 128, (c + 1) * 128)
            A_ = bankA[j]; Bk = bankB[j]
            trv = View(A_, A_.t[:, 0:192])
            gv = [View(Bk, Bk.t[:, q * 128:(q + 1) * 128]) for q in range(4)] + [View(A_, A_.t[:, 256:384])]
            x0, xt0, tk, lkt, gat, gkt = B['x0'], B['xt0'], B['tk'], B['lkt'], B['gat'], B['gkt']
            Dm = [B['Dm0'], B['Dm1']]; DT = [B['DT0'], B['DT1']]; W = [B['W0'], B['W1']]; G = [B['G0'], B['G1']]
            for q, src in enumerate((t_a, t_kd, t_v)):
                S.op('pe', lambda e, q=q, src=src: e.transpose(out=trv[:, q * 64:(q + 1) * 64], in_=src[:, cl], identity=identf[0:64, 0:64]),
                     reads=[src, identf], writes=[trv])
            yield
            S.op('act', lambda e: e.copy(out=tk[:, :], in_=trv[:, :]), reads=[trv], writes=[tk])
            yield
            for q, (l_, r_) in enumerate(((t_a, t_kk), (t_kk, t_a), (t_kd, t_kk), (t_a, t_r), (t_kd, t_r))):
                S.op('pe', lambda e, q=q, l_=l_, r_=r_: e.matmul(gv[q][:, :], lhsT=l_[:, cl], rhs=r_[:, cl], start=True, stop=True), reads=[l_, r_], writes=[gv[q]])
            yield
            S.op('dve', lambda e: e.scalar_tensor_tensor(out=x0[:, :], in0=gv[0][:, :], scalar=-1.0, in1=SU[:], op0=ALU.mult, op1=ALU.mult), reads=[gv[0], SU], writes=[x0])
            S.op('dve', lambda e: e.scalar_tensor_tensor(out=xt0[:, :], in0=gv[1][:, :], scalar=-1.0, in1=SL[:], op0=ALU.mult, op1=ALU.mult), reads=[gv[1], SL], writes=[xt0])
            S.op('dve', lambda e: e.tensor_tensor(out=lkt[:, :], in0=gv[2][:, :], in1=SU[:], op=ALU.mult), reads=[gv[2], SU], writes=[lkt])
            S.op('dve', lambda e: e.tensor_tensor(out=gat[:, :], in0=gv[3][:, :], in1=UI[:], op=ALU.mult), reads=[gv[3], UI], writes=[gat])
            S.op('dve', lambda e: e.tensor_tensor(out=gkt[:, :], in0=gv[4][:, :], in1=UI[:], op=ALU.mult), reads=[gv[4], UI], writes=[gkt])
            yield
            S.op('pool', lambda e: e.tensor_tensor(out=Dm[0][:, :], in0=xt0[:, :], in1=MK[:, 0, :], op=ALU.mult), reads=[xt0, MK], writes=[Dm[0]])
            S.op('pool', lambda e: e.tensor_tensor(out=Dm[0][:, :], in0=Dm[0][:, :], in1=identf[:], op=ALU.add), reads=[Dm[0], identf], writes=[Dm[0]])
            S.op('pool', lambda e: e.tensor_tensor(out=DT[0][:, :], in0=x0[:, :], in1=MK[:, 7, :], op=ALU.mult), reads=[x0, MK], writes=[DT[0]])
            S.op('pool', lambda e: e.tensor_tensor(out=DT[0][:, :], in0=DT[0][:, :], in1=identf[:], op=ALU.add), reads=[DT[0], identf], writes=[DT[0]])
            yield
            wv = View(A_, A_.t[:, 0:128]); gv1 = View(A_, A_.t[:, 128:256])
            w2v = View(Bk, Bk.t[:, 0:128]); g2v = View(Bk, Bk.t[:, 128:256])
            cur = 0
            for k in range(1, 7):
                nxt = 1 - cur
                if k < 6:
                    S.op('pe', lambda e, cur=cur: e.matmul(wv[:, :], lhsT=x0[:, :], rhs=Dm[cur][:, :], start=True, stop=True), reads=[x0, Dm[cur]], writes=[wv])
                S.op('pe', lambda e, cur=cur: e.matmul(w2v[:, :], lhsT=xt0[:, :], rhs=DT[cur][:, :], start=True, stop=True), reads=[xt0, DT[cur]], writes=[w2v])
                yield
                if k < 6:
                    S.op('dve', lambda e, k=k: e.tensor_tensor(out=W[0][:, :], in0=wv[:, :], in1=MK[:, k, :], op=ALU.mult), reads=[wv, MK], writes=[W[0]])
                S.op('dve', lambda e, k=k: e.tensor_tensor(out=W[1][:, :], in0=w2v[:, :], in1=MK[:, 7 + k, :], op=ALU.mult), reads=[w2v, MK], writes=[W[1]])
                yield
                if k < 6:
                    S.op('pe', lambda e, cur=cur: e.matmul(gv1[:, :], lhsT=DT[cur][:, :], rhs=W[0][:, :], start=True, stop=True), reads=[DT[cur], W[0]], writes=[gv1])
                S.op('pe', lambda e, cur=cur: e.matmul(g2v[:, :], lhsT=Dm[cur][:, :], rhs=W[1][:, :], start=True, stop=True), reads=[Dm[cur], W[1]], writes=[g2v])
                yield
                if k < 6:
                    S.op('dve', lambda e, cur=cur, nxt=nxt: e.tensor_tensor(out=Dm[nxt][:, :], in0=gv1[:, :], in1=Dm[cur][:, :], op=ALU.add), reads=[gv1, Dm[cur]], writes=[Dm[nxt]])
                S.op('dve', lambda e, cur=cur, nxt=nxt: e.tensor_tensor(out=DT[nxt][:, :], in0=g2v[:, :], in1=DT[cur][:, :], op=ALU.add), reads=[g2v, DT[cur]], writes=[DT[nxt]])
                yield
                cur = nxt
            B['TT'] = DT[cur]

        def seqpart(c, B, d, zi):
            cl = slice(c * 128, (c + 1) * 128)
            tk, lkt, gat, gkt, TT = B['tk'], B['lkt'], B['gat'], B['gkt'], B['TT']
            tkv = lambda q: tk[:, q * 64:(q + 1) * 64]
            Z = Zs[zi]; Zn = Zs[1 - zi]
            S.op('pe', lambda e: e.matmul(B_b1[:, :], lhsT=t_kk[:, cl], rhs=Z[:], start=True, stop=False), reads=[t_kk, Z], writes=[B_b1])
            S.op('pe', lambda e: e.matmul(B_b1[:, :], lhsT=lkt[:, :], rhs=tkv(2), start=False, stop=True), reads=[lkt, tk], writes=[B_b1])
            yield
            b1 = B1s.next()
            S.op('act', lambda e: e.copy(out=b1[:], in_=B_b1[:, :]), reads=[B_b1], writes=[b1])
            yield
            S.op('pe', lambda e: e.matmul(B_u[:, :], lhsT=TT[:, :], rhs=b1[:], start=True, stop=True), reads=[TT, b1], writes=[B_u])
            yield
            nu = nU.next()
            S.op('act', lambda e: e.mul(out=nu[:], in_=B_u[:, :], mul=-1.0), reads=[B_u], writes=[nu])
            yield
            S.op('pe', lambda e: e.matmul(B_y[:, :], lhsT=Z[:], rhs=t_r[:, cl], start=True, stop=False), reads=[Z, t_r], writes=[B_y])
            S.op('pe', lambda e: e.matmul(B_y[:, :], lhsT=nu[:], rhs=gat[:, :], start=False, stop=False), reads=[nu, gat], writes=[B_y])
            S.op('pe', lambda e: e.matmul(B_y[:, :], lhsT=tkv(2), rhs=gkt[:, :], start=False, stop=True), reads=[tk, gkt], writes=[B_y])
            S.op('pe', lambda e: e.matmul(B_zn[:, :], lhsT=tkv(0), rhs=nu[:], start=True, stop=False), reads=[tk, nu], writes=[B_zn])
            S.op('pe', lambda e: e.matmul(B_zn[:, :], lhsT=tkv(1), rhs=tkv(2), start=False, stop=True), reads=[tk], writes=[B_zn])
            yield
            S.op('dve', lambda e: e.tensor_tensor(out=Zt[:], in0=B_zn[:, :], in1=Z[:], op=ALU.add), reads=[B_zn, Z], writes=[Zt])
            if d == 0:
                S.op('dve', lambda e: e.tensor_copy(out=yacc[:, cl], in_=B_y[:, :]), reads=[B_y], writes=[yacc])
            else:
                seg0, segn = (0, TC) if c < 2 else (TC, TX)
                j0 = c * 128 - seg0
                lo = seg0 + segn - j0 - 128
                yv = yacc[:, lo:lo + 128][:, ::-1]
                S.op('dve', lambda e, yv=yv: e.tensor_tensor(out=yv, in0=B_y[:, :], in1=yv, op=ALU.add), reads=[B_y, yacc], writes=[yacc])
            yield
            S.op('act', lambda e: e.activation(out=Zn[:], in_=Zt[:], func=AF.Copy, scale=PC[:, c:c + 1]), reads=[Zt, PC], writes=[Zn])
            yield

        def seqgroup(chs, bufs, d, zi0):
            zi = zi0
            for c, B in zip(chs, bufs):
                yield from seqpart(c, B, d, zi)
                zi = 1 - zi

        def roundrobin(gens):
            gens = list(gens)
            while gens:
                for g in list(gens):
                    try:
                        next(g)
                    except StopIteration:
                        gens.remove(g)

        for h in heads:
            for d in dirs:
                load(t_ld, Q_LD0 + d, h, d)
                ones_t = t_kk
                S.op('pool', lambda e: e.memset(ones_t[:], 1.0), writes=[ones_t])
                for c in range(NCH):
                    S.op('dve', lambda e, c=c: e.tensor_tensor_scan(out=t_cum[:, c * 128:(c + 1) * 128], data0=ones_t[:, c * 128:(c + 1) * 128],
                                                                   data1=t_ld[:, c * 128:(c + 1) * 128], initial=0.0, op0=ALU.mult, op1=ALU.add),
                         reads=[ones_t, t_ld], writes=[t_cum])
                S.op('dve', lambda e: e.tensor_tensor(out=t_ld[:], in0=t_cum[:], in1=t_ld[:], op=ALU.subtract), reads=[t_cum, t_ld], writes=[t_ld])
                S.op('act', lambda e: e.activation(out=t_ld[:], in_=t_ld[:], func=AF.Exp), reads=[t_ld], writes=[t_ld])
                load(t_kk, Q_KK, h, d); load(t_a, Q_A0 + d, h, d)
                S.op('dve', lambda e: e.tensor_tensor(out=t_a[:], in0=t_a[:], in1=t_kk[:], op=ALU.mult), reads=[t_a, t_kk], writes=[t_a])
                S.op('dve', lambda e: e.tensor_tensor(out=t_kk[:], in0=t_kk[:], in1=t_ld[:], op=ALU.mult), reads=[t_kk, t_ld], writes=[t_kk])
                S.op('act', lambda e: e.activation(out=t_ld[:], in_=t_cum[:], func=AF.Exp, scale=-1.0), reads=[t_cum], writes=[t_ld])
                load(t_kd, Q_KD0 + d, h, d)
                S.op('dve', lambda e: e.tensor_tensor(out=t_a[:], in0=t_a[:], in1=t_ld[:], op=ALU.mult), reads=[t_a, t_ld], writes=[t_a])
                S.op('pool', lambda e: e.tensor_tensor(out=t_kd[:], in0=t_kd[:], in1=t_ld[:], op=ALU.mult), reads=[t_kd, t_ld], writes=[t_kd])
                S.op('act', lambda e: e.activation(out=t_cum[:], in_=t_cum[:], func=AF.Exp), reads=[t_cum], writes=[t_cum])
                load(t_r, Q_R, h, d)
                S.op('dve', lambda e: e.tensor_tensor(out=t_r[:], in0=t_r[:], in1=t_cum[:], op=ALU.mult), reads=[t_r, t_cum], writes=[t_r])
                S.op('pool', lambda e: e.tensor_copy(out=PC[:, :], in_=t_cum[:, 127:T:128]), reads=[t_cum], writes=[PC])
                load(t_v, Q_V, h, d)
                S.op('pool', lambda e: e.memset(Zs[0][:], 0.0), writes=[Zs[0]])
                S.barrier()
                nch = NCH if dbg_chunks is None else dbg_chunks
                groups = [list(range(g0, min(g0 + J, nch))) for g0 in range(0, nch, J)]
                zi = 0
                prev = None
                for gi, chs in enumerate(groups):
                    bufs = sets[gi % 2][:len(chs)]
                    gens = [indep(c, B, j) for j, (c, B) in enumerate(zip(chs, bufs))]
                    if prev is not None:
                        gens.append(seqgroup(prev[0], prev[1], d, prev[2]))
                    roundrobin(gens)
                    prev = (chs, bufs, zi)
                    zi = (zi + len(chs)) % 2
                if prev is not None:
                    roundrobin([seqgroup(prev[0], prev[1], d, prev[2])])
                S.barrier()
            if 'dbg_y' in P.dr:
                S.dma(P.dr['dbg_y'][:, :], yacc[:], reads=[yacc])
            load(t_kd, Q_BON, h, 0); load(t_a, Q_G, h, 0)
            for (t0, n) in (BLOCKS if need_ctx else BLOCKS[1:]):
                pm = ps_ro.next()
                S.op('pe', lambda e: e.matmul(pm[0:64, 0:n], lhsT=blk[0:64, 0:64], rhs=yacc[:, t0:t0 + n], start=True, stop=True), reads=[blk, yacc], writes=[pm])
                dd = rot.next()
                S.op('dve', lambda e: e.tensor_tensor(out=dd[:, 0:n], in0=yacc[:, t0:t0 + n], in1=pm[0:64, 0:n], op=ALU.subtract), reads=[yacc, pm], writes=[dd])
                sq = rot.next()
                S.op('act', lambda e: e.activation(out=sq[:, 0:n], in_=dd[:, 0:n], func=AF.Square), reads=[dd], writes=[sq])
                pv = ps_ro.next()
                S.op('pe', lambda e: e.matmul(pv[0:64, 0:n], lhsT=blk[0:64, 0:64], rhs=sq[:, 0:n], start=True, stop=True), reads=[blk, sq], writes=[pv])
                rs2 = rot.next()
                S.op('dve', lambda e: e.tensor_scalar(out=rs2[:, 0:n], in0=pv[0:64, 0:n], scalar1=64e-5, scalar2=None, op0=ALU.add), reads=[pv], writes=[rs2])
                S.op('act', lambda e: e.activation(out=rs2[:, 0:n], in_=rs2[:, 0:n], func=AF.Sqrt), reads=[rs2], writes=[rs2])
                S.op('dve', lambda e: e.reciprocal(out=rs2[:, 0:n], in_=rs2[:, 0:n]), reads=[rs2], writes=[rs2])
                S.op('dve', lambda e: e.tensor_tensor(out=dd[:, 0:n], in0=dd[:, 0:n], in1=rs2[:, 0:n], op=ALU.mult), reads=[dd, rs2], writes=[dd])
                S.op('dve', lambda e: e.tensor_scalar(out=dd[:, 0:n], in0=dd[:, 0:n], scalar1=lnc[:, h, 0:1], scalar2=lnc[:, h, 1:2], op0=ALU.mult, op1=ALU.add),
                     reads=[dd, lnc], writes=[dd])
                S.op('pool', lambda e: e.tensor_tensor(out=dd[:, 0:n], in0=dd[:, 0:n], in1=t_kd[:, t0:t0 + n], op=ALU.add), reads=[dd, t_kd], writes=[dd])
                oo = rot.next()
                S.op('pool', lambda e: e.tensor_tensor(out=oo[:, 0:n], in0=dd[:, 0:n], in1=t_a[:, t0:t0 + n], op=ALU.mult), reads=[dd, t_a], writes=[oo])
                S.dma(rwT[h * 64:(h + 1) * 64, t0:t0 + n], oo[:, 0:n], reads=[oo], q='act')


def declare_rwkv(P):
    P.inp('rw_mu', [DEPTH, 2, 1152]); P.inp('rw_w0', [DEPTH, 2, 256]); P.inp('rw_w2', [DEPTH, 2, 64, 256])
    P.inp('rw_a0', [DEPTH, 2, 256]); P.inp('rw_a2', [DEPTH, 2, 64, 256]); P.inp('rw_g2', [DEPTH, 128, 256])
    P.inp('rw_k_k', [DEPTH, 256]); P.inp('rw_k_a', [DEPTH, 256]); P.inp('rw_r_k', [DEPTH, 4, 64])
    P.inp('rw_ln_w', [DEPTH, 256]); P.inp('rw_ln_b', [DEPTH, 256])
    P.inp('mask_su', [128, 128]); P.inp('mask_sl', [128, 128]); P.inp('mask_ui', [128, 128])
    P.inp('rw_masks', [128, 14, 128])
    P.tmp('rwq', [11, 256, T])


def host_rwkv(m, inputs):
    for k in ('rw_mu', 'rw_w0', 'rw_w2', 'rw_a0', 'rw_a2', 'rw_g2', 'rw_k_k', 'rw_k_a', 'rw_r_k', 'rw_ln_w', 'rw_ln_b'):
        m[k] = np.ascontiguousarray(inputs[k], np.float32)
    su = np.triu(np.ones((128, 128), np.float32), 1)
    m['mask_su'] = su; m['mask_sl'] = np.ascontiguousarray(su.T); m['mask_ui'] = np.triu(np.ones((128, 128), np.float32), 0)
    idx = np.arange(128)
    mk = np.zeros((128, 14, 128), np.float32)
    for k in range(7):
        b = 2 ** k
        M = ((idx[:, None] // (2 * b)) == (idx[None, :] // (2 * b))) & ((idx[:, None] % (2 * b)) >= b) & ((idx[None, :] % (2 * b)) < b)
        mk[:, k, :] = M
        mk[:, 7 + k, :] = M.T
    m['rw_masks'] = mk


def build_full():
    P = Prog()
    declare_common(P); declare_attn(P); declare_lru(P); declare_merge(P); declare_hyena(P); declare_rwkv(P); declare_moe(P)
    P.out('y_out', [TX, D])
    for li in range(DEPTH):
        need_ctx = li < DEPTH - 1
        src = 'xin' if li == 0 else 'x_l0'
        stage_mod(P, li)
        stage_inproj(P, li, src)
        stage_attn(P, li, need_ctx)
        stage_hyena(P, li, need_ctx)
        stage_rwkv_prep(P, li)
        stage_rwkv_scan(P, li, need_ctx)
        stage_lru(P, li, need_ctx)
        stage_merge(P, li, need_ctx, src, 'x_mid')
        if need_ctx:
            stage_moe(P, li, True, 'x_mid', 'x_l0')
        else:
            stage_moe(P, li, False, 'x_mid', 'y_out', dst_is_out=True)
    P.S.finish()
    return P


def full_host_inputs(inputs, b, shared=None):
    if shared is None:
        shared = {}
        m = host_inputs(inputs, b)
        host_attn(m, inputs); host_lru(m, inputs); host_merge(m, inputs); host_hyena(m, inputs); host_rwkv(m, inputs); host_moe(m, inputs)
        for k, v in m.items():
            if k not in ('xin', 'cvec'):
                shared[k] = v
        return m, shared
    m = dict(shared)
    m['xin'] = np.ascontiguousarray(np.concatenate([inputs['ctx'][b], inputs['x'][b]], axis=0), dtype=np.float32)
    m['cvec'] = np.ascontiguousarray(np.stack([inputs['c_ctx'], inputs['c'][b]], axis=0), dtype=np.float32)
    return m, shared


def kernel(**inputs):
    inputs = {k: np.asarray(v) for k, v in inputs.items()}
    P = build_full()
    n = 8
    maps = []
    shared = None
    for b in range(n):
        m, shared = full_host_inputs(inputs, b, shared)
        maps.append({k: m[k] for k in P.in_names})
    res = run_bass_kernel_spmd(P.nc, maps, core_ids=list(range(n)))
    out = np.stack([np.asarray(res.results[b]['y_out'], dtype=np.float32) for b in range(n)], axis=0)
    return out
```
